# Optimizing a Trainium2 kernel written in Bass

```python
import math
import jax, jax.numpy as jnp
from jax import lax
import numpy as np

D_MODEL = 1024
BATCH = 8
SEQ = 2048
DEPTH = 2

ATTN_HEADS = 4
ATTN_HEAD_DIM = 64
ATTN_WIDTH = ATTN_HEADS * ATTN_HEAD_DIM
DILATED_CONFIGS = ((128, 1), (512, 4), (2048, 16))
ATTN_BLOCK = 128
REL_BUCKETS = 32
REL_MAX_DIST = 1024

RWKV_HEADS = 6
RWKV_HEAD_DIM = 64
RWKV_WIDTH = RWKV_HEADS * RWKV_HEAD_DIM
RWKV_DECAY_RANK = 64
RWKV_ICL_RANK = 64
RWKV_GATE_RANK = 128
RWKV_GN_EPS = 64e-5
RWKV_DIR_WIDTH = 3 * RWKV_WIDTH + RWKV_DECAY_RANK + RWKV_ICL_RANK

MLSTM_HEADS = 4
MLSTM_HEAD_DIM = 96
MLSTM_WIDTH = MLSTM_HEADS * MLSTM_HEAD_DIM
MLSTM_CONV = 5
MLSTM_CHUNK = 64
MLSTM_GN_EPS = 1e-6
MLSTM_M_INIT = -1e30

MIX_WIDTH = ATTN_WIDTH + RWKV_WIDTH + MLSTM_WIDTH

IN_SPLITS = (3 * ATTN_WIDTH,
             3 * RWKV_WIDTH,
             RWKV_DECAY_RANK, RWKV_DECAY_RANK,
             RWKV_ICL_RANK, RWKV_ICL_RANK,
             RWKV_GATE_RANK,
             2 * MLSTM_WIDTH,
             MLSTM_WIDTH,
             MLSTM_WIDTH,
             4 * MLSTM_HEADS)
IN_WIDTH = sum(IN_SPLITS)

N_EXPERTS = 16
EC_FACTOR = 2
D_FF_EXPERT = 2816
RMS_EPS = 1e-6

kernel_name = "hybrid_dilated_rwkv7_mlstm_ec_moe_encoder"


def split_cols(z, sizes):
    offs = np.cumsum(sizes)[:-1].tolist()
    return jnp.split(z, offs, axis=-1)


def rmsnorm(x, g):
    xf = x.astype(jnp.float32)
    y = xf * lax.rsqrt(jnp.mean(xf * xf, axis=-1, keepdims=True) + RMS_EPS) * g.astype(jnp.float32)
    return y.astype(x.dtype)


def head_norm(y, eps):
    mu = jnp.mean(y, axis=-1, keepdims=True)
    var = jnp.mean(jnp.square(y - mu), axis=-1, keepdims=True)
    return (y - mu) * lax.rsqrt(var + eps)


def t5_bucket(rel):
    nb = REL_BUCKETS // 2
    ret = jnp.where(rel > 0, nb, 0)
    n = jnp.abs(rel)
    max_exact = nb // 2
    nf = jnp.maximum(n, 1).astype(jnp.float32)
    large = max_exact + (jnp.log(nf / max_exact) / math.log(REL_MAX_DIST / max_exact)
                         * (nb - max_exact)).astype(jnp.int32)
    large = jnp.minimum(large, nb - 1)
    return ret + jnp.where(n < max_exact, n, large)


def dilated_branch(q, k, v, rel_bias, radius, dil):
    Bsz, S, H, hd = q.shape
    L = S // dil
    bq = math.gcd(L, ATTN_BLOCK)
    nb = L // bq
    kw = bq + 2 * radius

    def to_sub(t):
        return t.reshape(Bsz, L, dil, H, hd).transpose(0, 2, 3, 1, 4)

    qs, ks, vs = to_sub(q), to_sub(k), to_sub(v)
    pad = ((0, 0), (0, 0), (0, 0), (radius, radius), (0, 0))
    kp, vp = jnp.pad(ks, pad), jnp.pad(vs, pad)
    idx = jnp.arange(nb)[:, None] * bq + jnp.arange(kw)[None, :]
    kb = jnp.take(kp, idx, axis=3)
    vb = jnp.take(vp, idx, axis=3).astype(jnp.float32)
    qb = qs.reshape(Bsz, dil, H, nb, bq, hd)
    s = jnp.einsum('brhnqc,brhnkc->brhnqk', qb, kb).astype(jnp.float32) * (hd ** -0.5)
    rel = jnp.arange(kw)[None, :] - radius - jnp.arange(bq)[:, None]
    kpos = idx[:, None, :] - radius
    valid = (jnp.abs(rel) <= radius)[None] & (kpos >= 0) & (kpos < L)
    bias = rel_bias[t5_bucket(rel * dil)].astype(jnp.float32).transpose(2, 0, 1)
    s = jnp.where(valid, s + bias[:, None], -jnp.inf)
    lse = jax.nn.logsumexp(s, axis=-1)
    p = jnp.exp(s - lse[..., None])
    o = jnp.einsum('brhnqk,brhnkc->brhnqc', p, vb)
    o = o.reshape(Bsz, dil, H, L, hd).transpose(0, 3, 1, 2, 4).reshape(Bsz, S, H, hd)
    lse = lse.reshape(Bsz, dil, H, L).transpose(0, 3, 1, 2).reshape(Bsz, S, H)
    return o, lse


def dilated_attention(za, rel_bias):
    Bsz, S, _ = za.shape
    q, k, v = [t.reshape(Bsz, S, ATTN_HEADS, ATTN_HEAD_DIM) for t in jnp.split(za, 3, axis=-1)]
    outs, lses = [], []
    for window, dil in DILATED_CONFIGS:
        o, l = dilated_branch(q, k, v, rel_bias, window // (2 * dil), dil)
        outs.append(o)
        lses.append(l)
    wts = jax.nn.softmax(jnp.stack(lses), axis=0)
    out = jnp.sum(wts[..., None] * jnp.stack(outs), axis=0)
    return out.reshape(Bsz, S, ATTN_WIDTH)


def token_shift(x):
    return jnp.pad(x, ((0, 0), (1, 0), (0, 0)))[:, :-1]


def rwkv7_scan(r, w, k, v, kk, a):
    Bsz, _, H, N = r.shape

    def step(state, inp):
        r_t, w_t, k_t, v_t, kk_t, a_t = inp
        sa = jnp.einsum('bhvk,bhk->bhv', state, -kk_t)
        state = (state * w_t[:, :, None, :] + sa[..., None] * (kk_t * a_t)[:, :, None, :]
                 + v_t[..., None] * k_t[:, :, None, :])
        y = jnp.einsum('bhvk,bhk->bhv', state, r_t)
        return state, y

    xs = tuple(jnp.moveaxis(t, 1, 0) for t in (r, w, k, v, kk, a))
    s0 = jnp.zeros((Bsz, H, N, N), jnp.float32)
    _, ys = lax.scan(step, s0, xs)
    return jnp.moveaxis(ys, 0, 1)


def rwkv7_direction(xd, mu, w0, w2, a0, a2, k_k, k_a, r_k):
    Bsz, S, _ = xd.shape
    xd = xd + mu * (token_shift(xd) - xd)
    r, k, v, wd, ad = split_cols(xd, (RWKV_WIDTH, RWKV_WIDTH, RWKV_WIDTH, RWKV_DECAY_RANK, RWKV_ICL_RANK))
    w_log = -jax.nn.softplus(-(w0 + jnp.tanh(wd) @ w2)) - 0.5
    decay = jnp.exp(-jnp.exp(w_log))
    a = jax.nn.sigmoid(a0 + ad @ a2)
    kk = k * k_k
    k = k * (1.0 + (a - 1.0) * k_a)
    heads = lambda t: t.reshape(Bsz, S, RWKV_HEADS, RWKV_HEAD_DIM)
    r, k, v, decay, a, kk = map(heads, (r, k, v, decay, a, kk))
    kk = kk / jnp.maximum(jnp.sqrt(jnp.sum(kk * kk, axis=-1, keepdims=True)), 1e-12)
    y = rwkv7_scan(r, decay, k, v, kk, a)
    bonus = jnp.sum(r * k * r_k, axis=-1, keepdims=True) * v
    return y, bonus


def rwkv7_mixer(rkv, wd_f, wd_b, ad_f, ad_b, gd, mu, w0, w2, a0, a2, k_k, k_a, r_k, g2, ln_g, ln_b):
    f32 = lambda t: t.astype(jnp.float32)
    Bsz, S, _ = rkv.shape
    rkv, wd_f, wd_b, ad_f, ad_b, gd = map(f32, (rkv, wd_f, wd_b, ad_f, ad_b, gd))
    mu, w0, w2, a0, a2, k_k, k_a, r_k, g2, ln_g, ln_b = map(f32, (mu, w0, w2, a0, a2, k_k, k_a, r_k, g2, ln_g, ln_b))
    x_f = jnp.concatenate([rkv, wd_f, ad_f], axis=-1)
    x_b = jnp.concatenate([rkv, wd_b, ad_b], axis=-1)[:, ::-1]
    y_f, bon_f = rwkv7_direction(x_f, mu[0], w0[0], w2[0], a0[0], a2[0], k_k, k_a, r_k)
    y_b, bon_b = rwkv7_direction(x_b, mu[1], w0[1], w2[1], a0[1], a2[1], k_k, k_a, r_k)
    y_b, bon_b = y_b[:, ::-1], bon_b[:, ::-1]
    y = head_norm(y_f + y_b, RWKV_GN_EPS).reshape(Bsz, S, RWKV_WIDTH) * ln_g + ln_b
    y = y + (bon_f + bon_b).reshape(Bsz, S, RWKV_WIDTH)
    g = jax.nn.sigmoid(gd) @ g2
    return y * g


def depthwise_conv(x, w, b):
    K, C = w.shape
    y = lax.conv_general_dilated(x, w[:, None, :].astype(x.dtype), window_strides=(1,),
                                 padding=[(K // 2, K // 2)],
                                 dimension_numbers=('NWC', 'WIO', 'NWC'),
                                 feature_group_count=C)
    return y + b.astype(x.dtype)


def mlstm_chunkwise(q, k, v, ig, lf):
    Bsz, H, S, D = q.shape
    L = MLSTM_CHUNK
    nc = S // L
    chunk = lambda t: jnp.moveaxis(t.reshape(Bsz, H, nc, L, *t.shape[3:]), 2, 0)
    xs = tuple(chunk(t) for t in (q, k, v, ig, lf))
    tril = jnp.tril(jnp.ones((L, L), dtype=bool))

    def step(carry, inp):
        C, n, m = carry
        qc, kc, vc, ic, fc = inp
        b = jnp.cumsum(fc, axis=-1)
        dmat = jnp.where(tril, b[..., :, None] - b[..., None, :] + ic[..., None, :], -jnp.inf)
        m_inter = b + m[..., None]
        m_t = jnp.maximum(jnp.max(dmat, axis=-1), m_inter)
        w = jnp.exp(dmat - m_t[..., None]) * jnp.einsum('bhtd,bhsd->bhts', qc, kc)
        inter = jnp.exp(m_inter - m_t)
        num = jnp.einsum('bhts,bhsd->bhtd', w, vc) + inter[..., None] * jnp.einsum('bhtk,bhkv->bhtv', qc, C)
        den = jnp.sum(w, axis=-1) + inter * jnp.einsum('bhtk,bhk->bht', qc, n)
        h = num / jnp.maximum(jnp.abs(den), jnp.exp(-m_t))[..., None]
        b_end = b[..., -1]
        g_end = b_end[..., None] - b + ic
        m_new = jnp.maximum(b_end + m, jnp.max(g_end, axis=-1))
        sk = jnp.exp(g_end - m_new[..., None])
        carry_decay = jnp.exp(b_end + m - m_new)
        C = carry_decay[..., None, None] * C + jnp.einsum('bhs,bhsk,bhsv->bhkv', sk, kc, vc)
        n = carry_decay[..., None] * n + jnp.einsum('bhs,bhsk->bhk', sk, kc)
        return (C, n, m_new), h

    init = (jnp.zeros((Bsz, H, D, D), jnp.float32), jnp.zeros((Bsz, H, D), jnp.float32),
            jnp.full((Bsz, H), MLSTM_M_INIT, jnp.float32))
    _, hs = lax.scan(step, init, xs)
    return jnp.moveaxis(hs, 0, 2).reshape(Bsz, H, S, D)


def mlstm_mixer(qk, v, o, gates, conv_w, conv_b, ib, fb, ln_g):
    Bsz, S, _ = v.shape
    qk = jax.nn.silu(depthwise_conv(qk.astype(jnp.float32), conv_w.astype(jnp.float32), conv_b))
    q, k = jnp.split(qk, 2, axis=-1)
    k = k * (MLSTM_HEAD_DIM ** -0.5)
    heads = lambda t: t.astype(jnp.float32).reshape(Bsz, S, MLSTM_HEADS, MLSTM_HEAD_DIM).transpose(0, 2, 1, 3)
    q, k, v = heads(q), heads(k), heads(v)
    g = gates.astype(jnp.float32).reshape(Bsz, S, 4, MLSTM_HEADS).transpose(2, 0, 3, 1)
    ib = ib.astype(jnp.float32)
    fb = fb.astype(jnp.float32)
    ig_f = g[0] + ib[0][:, None]
    lf_f = jax.nn.log_sigmoid(g[1] + fb[0][:, None])
    ig_b = g[2] + ib[1][:, None]
    lf_b = jax.nn.log_sigmoid(g[3] + fb[1][:, None])
    flip = lambda t: jnp.flip(t, axis=2)
    h_f = mlstm_chunkwise(q, k, v, ig_f, lf_f)
    h_b = flip(mlstm_chunkwise(flip(q), flip(k), flip(v), flip(ig_b), flip(lf_b)))
    h = (h_f + h_b).transpose(0, 2, 1, 3)
    h = head_norm(h, MLSTM_GN_EPS).reshape(Bsz, S, MLSTM_WIDTH) * ln_g.astype(jnp.float32)
    return h * jax.nn.sigmoid(o.astype(jnp.float32))


def expert_choice_ffn(h, router, w1, w3, w2):
    Bsz, S, D = h.shape
    cap = EC_FACTOR * S // N_EXPERTS
    aff = jax.nn.softmax(jnp.einsum('bsd,de->bse', h, router).astype(jnp.float32), axis=-1)
    gate, idx = lax.top_k(jnp.swapaxes(aff, 1, 2), cap)
    flat = idx + (jnp.arange(Bsz) * S)[:, None, None]
    xg = h.reshape(Bsz * S, D)[flat]
    a1 = jnp.einsum('becd,edf->becf', xg, w1)
    a3 = jnp.einsum('becd,edf->becf', xg, w3)
    y = jnp.einsum('becf,efd->becd', jax.nn.silu(a1) * a3, w2)
    y = (y.astype(jnp.float32) * gate[..., None]).astype(h.dtype)
    out = jax.ops.segment_sum(y.reshape(-1, D), flat.reshape(-1), num_segments=Bsz * S)
    return out.reshape(Bsz, S, D)


def setup_inputs(seed: int = 0) -> dict:
    key = jax.random.key(seed)
    ks = iter(jax.random.split(key, 40))
    f32 = jnp.float32
    nrm = lambda shape, scale: scale * jax.random.normal(next(ks), shape, f32)
    L = DEPTH
    inp = {}
    inp["x"] = nrm((BATCH, SEQ, D_MODEL), 1.0)
    inp["rel_bias"] = nrm((REL_BUCKETS, ATTN_HEADS), 0.5)
    inp["ln1_g"] = 1.0 + nrm((L, D_MODEL), 0.02)
    inp["w_in"] = nrm((L, D_MODEL, IN_WIDTH), D_MODEL ** -0.5)
    inp["w_out"] = nrm((L, MIX_WIDTH, D_MODEL), MIX_WIDTH ** -0.5)
    inp["rk_mu"] = jax.random.uniform(next(ks), (L, 2, RWKV_DIR_WIDTH), f32, 0.2, 0.8)
    inp["rk_w0"] = jax.random.uniform(next(ks), (L, 2, RWKV_WIDTH), f32, -5.0, -0.5)
    inp["rk_w2"] = nrm((L, 2, RWKV_DECAY_RANK, RWKV_WIDTH), 0.1)
    inp["rk_a0"] = nrm((L, 2, RWKV_WIDTH), 0.1)
    inp["rk_a2"] = nrm((L, 2, RWKV_ICL_RANK, RWKV_WIDTH), 0.1)
    inp["rk_kk"] = 0.85 + nrm((L, RWKV_WIDTH), 0.05)
    inp["rk_ka"] = 1.0 + nrm((L, RWKV_WIDTH), 0.05)
    inp["rk_rk"] = nrm((L, RWKV_HEADS, RWKV_HEAD_DIM), 0.1)
    inp["rk_g2"] = nrm((L, RWKV_GATE_RANK, RWKV_WIDTH), RWKV_GATE_RANK ** -0.5)
    inp["rk_ln_g"] = 1.0 + nrm((L, RWKV_WIDTH), 0.02)
    inp["rk_ln_b"] = nrm((L, RWKV_WIDTH), 0.02)
    inp["ml_conv_w"] = nrm((L, MLSTM_CONV, 2 * MLSTM_WIDTH), MLSTM_CONV ** -0.5)
    inp["ml_conv_b"] = nrm((L, 2 * MLSTM_WIDTH), 0.02)
    inp["ml_ib"] = nrm((L, 2, MLSTM_HEADS), 0.1)
    inp["ml_fb"] = jnp.linspace(3.0, 6.0, MLSTM_HEADS, dtype=f32) + nrm((L, 2, MLSTM_HEADS), 0.1)
    inp["ml_ln_g"] = 1.0 + nrm((L, MLSTM_WIDTH), 0.02)
    inp["ln2_g"] = 1.0 + nrm((L, D_MODEL), 0.02)
    inp["router"] = nrm((L, D_MODEL, N_EXPERTS), D_MODEL ** -0.5)
    inp["e_w1"] = nrm((L, N_EXPERTS, D_MODEL, D_FF_EXPERT), D_MODEL ** -0.5)
    inp["e_w3"] = nrm((L, N_EXPERTS, D_MODEL, D_FF_EXPERT), D_MODEL ** -0.5)
    inp["e_w2"] = nrm((L, N_EXPERTS, D_FF_EXPERT, D_MODEL), D_FF_EXPERT ** -0.5)
    inp["final_g"] = 1.0 + nrm((D_MODEL,), 0.02)
    return inp


def reference(x, rel_bias, ln1_g, w_in, w_out, rk_mu, rk_w0, rk_w2, rk_a0, rk_a2, rk_kk, rk_ka,
              rk_rk, rk_g2, rk_ln_g, rk_ln_b, ml_conv_w, ml_conv_b, ml_ib, ml_fb, ml_ln_g,
              ln2_g, router, e_w1, e_w3, e_w2, final_g):
    for l in range(DEPTH):
        h = rmsnorm(x, ln1_g[l])
        z = h @ w_in[l]
        (za, rkv, wd_f, wd_b, ad_f, ad_b, gd, mqk, mv, mo, mg) = split_cols(z, IN_SPLITS)
        ya = dilated_attention(za, rel_bias)
        yb = rwkv7_mixer(rkv, wd_f, wd_b, ad_f, ad_b, gd, rk_mu[l], rk_w0[l], rk_w2[l], rk_a0[l],
                         rk_a2[l], rk_kk[l], rk_ka[l], rk_rk[l], rk_g2[l], rk_ln_g[l], rk_ln_b[l])
        yc = mlstm_mixer(mqk, mv, mo, mg, ml_conv_w[l], ml_conv_b[l], ml_ib[l], ml_fb[l], ml_ln_g[l])
        y = jnp.concatenate([ya.astype(x.dtype), yb.astype(x.dtype), yc.astype(x.dtype)], axis=-1)
        x = x + y @ w_out[l]
        x = x + expert_choice_ffn(rmsnorm(x, ln2_g[l]), router[l], e_w1[l], e_w3[l], e_w2[l])
    return rmsnorm(x, final_g)
```

```python
from contextlib import ExitStack
import numpy as np
import concourse.bass as bass
import concourse.mybir as mybir

F32 = mybir.dt.float32
BF16 = mybir.dt.bfloat16
I32 = mybir.dt.int32
ALU = mybir.AluOpType
AF = mybir.ActivationFunctionType
AX = mybir.AxisListType

ENGS = ("pe", "act", "dve", "pool", "sp")


class Prog:
    def __init__(self, nc, strict_same_engine=False):
        self.nc = nc
        self.same_dist = 3
        self.ops = {e: [] for e in ENGS}
        self.keys = {}
        self.dma_cnt = {}
        self.es = ExitStack()
        self.n_ops = 0

    def _deps(self, reads, writes):
        deps = []
        for k in reads:
            deps.extend((ev, True) for ev in self._st(k)["w"])
        for k in writes:
            st = self._st(k)
            deps.extend((ev, True) for ev in st["w"])
            deps.extend((ev, False) for ev in st["r"].values())
        return deps

    def _st(self, k):
        st = self.keys.get(k)
        if st is None:
            inh = getattr(self, "inherit", {}).get(k[0], []) if isinstance(k, tuple) else []
            st = self.keys[k] = {"w": list(inh), "r": {}}
        return st

    def _record(self, ev, reads, writes):
        for k in reads:
            st = self._st(k)
            st["r"][(ev[0], ev[1])] = ev
        for k in writes:
            st = self._st(k)
            st["w"] = [ev]
            st["r"] = {}

    @staticmethod
    def _norm(reads, writes):
        r2, w2 = [], []
        for k in writes:
            if isinstance(k, str) and k.startswith("ps"):
                k = k.split("q")[0]
            if k not in w2:
                w2.append(k)
        for k in reads:
            if isinstance(k, str) and k.startswith("ps"):
                k = k.split("q")[0]
                if k not in w2:
                    w2.append(k)
            elif k not in r2:
                r2.append(k)
        return r2, w2

    def op(self, eng, fn, reads=(), writes=(), serial=False):
        reads, writes = self._norm(reads, writes)
        deps = self._deps(reads, writes)
        idx = len(self.ops[eng])
        if serial and idx > 0:
            deps.append((("eng", eng, idx - 1), "force"))
        ev = ("eng", eng, idx)
        self.ops[eng].append(dict(fn=fn, deps=deps, kind="c"))
        self._record(ev, reads, writes)
        self.n_ops += 1
        return ev

    def dma(self, q, out, in_, reads=(), writes=(), semkey=None, **kw):
        assert semkey is not None
        reads, writes = self._norm(reads, writes)
        deps = self._deps(reads, writes)
        c = self.dma_cnt.get(semkey, 0) + 1
        self.dma_cnt[semkey] = c
        ev = ("dma", semkey, c)
        fn = lambda e, out=out, in_=in_, kw=kw: e.dma_start(out=out, in_=in_, **kw)
        self.ops[q].append(dict(fn=fn, deps=deps, kind="d", semkey=semkey))
        self._record(ev, reads, writes)
        self.n_ops += 1
        return ev

    def emit(self, final_wait_events=()):
        nc = self.nc
        signal = {e: set() for e in ENGS}
        for e in ENGS:
            for i, o in enumerate(self.ops[e]):
                nd = []
                for (d, is_w) in o["deps"]:
                    if d[0] == "eng" and d[1] == e and is_w != "force":
                        if e == "pe" or not is_w or (i - d[2]) > self.same_dist:
                            continue
                    nd.append(d)
                    if d[0] == "eng":
                        signal[d[1]].add(d[2])
                o["deps"] = nd
        for d in final_wait_events:
            if d[0] == "eng":
                signal[d[1]].add(d[2])
        rank = {}
        for e in ENGS:
            r = 0
            for i in range(len(self.ops[e])):
                if i in signal[e]:
                    r += 1
                    rank[(e, i)] = r
        self.max_rank = {e: max([v for (ee, i), v in rank.items() if ee == e] + [0]) for e in ENGS}
        es = self.es
        sem_e = {e: es.enter_context(nc.semaphore("s_" + e)) for e in ENGS}
        sem_d = {k: es.enter_context(nc.semaphore("d_%d" % i)) for i, k in enumerate(self.dma_cnt)}
        self.n_sems = len(sem_e) + len(sem_d)

        def lower(ev):
            if ev[0] == "eng":
                return ("e_" + ev[1], sem_e[ev[1]], rank[(ev[1], ev[2])])
            return ("d_" + str(ev[1]), sem_d[ev[1]], 16 * ev[2])

        block = es.enter_context(nc.Block())
        engobj = {"pe": block.tensor, "act": block.scalar, "dve": block.vector,
                  "pool": block.gpsimd, "sp": block.sync}
        fw = self

        def make(e):
            def body(eng):
                known = {}
                for i, o in enumerate(fw.ops[e]):
                    need = {}
                    for d in o["deps"]:
                        nm, s, v = lower(d)
                        if known.get(nm, 0) >= v:
                            continue
                        if nm not in need or need[nm][1] < v:
                            need[nm] = (s, v)
                    for nm, (s, v) in need.items():
                        eng.wait_ge(s, v)
                        known[nm] = v
                    ins = o["fn"](eng)
                    if o["kind"] == "d":
                        ins.then_inc(sem_d[o["semkey"]], 16)
                    elif (e, i) in rank:
                        ins.then_inc(sem_e[e], 1)
                if e == "sp":
                    for d in final_wait_events:
                        nm, s, v = lower(d)
                        if known.get(nm, 0) < v:
                            eng.wait_ge(s, v)
                            known[nm] = v
            return body

        for e in ENGS:
            if self.ops[e] or e == "sp":
                engobj[e](make(e))
        es.close()


class Region:
    def __init__(self, uid, ap, lo, hi):
        self.uid, self.ap, self.lo, self.hi = uid, ap, lo, hi

    def k(self, *i):
        return (self.uid,) + tuple(i)

    def __getitem__(self, idx):
        return self.ap[idx]


class Arena:
    def __init__(self, P, tensor, ncols_f32):
        self.P, self.t, self.n = P, tensor, ncols_f32
        self.top = 0
        self.gen = 0
        self.pending = []
        self.live = []
        P.inherit = {}
        P._arena = self

    def alloc(self, name, free_shape, dtype=F32):
        nel = int(np.prod(free_shape))
        bpe = {F32: 4, BF16: 2, I32: 4}[dtype]
        ncol = (nel * bpe + 3) // 4
        lo, hi = self.top, self.top + ncol
        assert hi <= self.n, f"arena overflow allocating {name}: need {hi} cols of {self.n}"
        self.top = hi
        self.gen += 1
        uid = f"{name}#{self.gen}"
        ap = self.t[:, lo:hi]
        if dtype != F32:
            ap = ap.bitcast(dtype)
        ap = ap[:, 0:nel]
        if len(free_shape) > 1:
            names = " ".join(f"a{i}" for i in range(len(free_shape)))
            kw = {f"a{i}": int(s) for i, s in enumerate(free_shape)}
            ap = ap.rearrange(f"p ({names}) -> p {names}", **kw)
        evs = []
        keep = []
        for (plo, phi, pe) in self.pending:
            if plo < hi and lo < phi:
                evs.extend(pe)
            keep.append((plo, phi, pe))
        self.P.inherit[uid] = evs
        r = Region(uid, ap, lo, hi)
        self.live.append(r)
        return r

    def mark(self):
        return (self.top, len(self.live))

    def release(self, mark):
        top, nlive = mark
        P = self.P
        for r in self.live[nlive:]:
            evs = []
            for k in [k for k in P.keys if k[0] == r.uid]:
                st = P.keys.pop(k)
                evs.extend(st["w"])
                evs.extend(st["r"].values())
            evs.extend(P.inherit.get(r.uid, []))
            best = {}
            for ev in evs:
                kk = (ev[0], ev[1])
                if kk not in best or best[kk][2] < ev[2]:
                    best[kk] = ev
            self.pending.append((r.lo, r.hi, list(best.values())))
        del self.live[nlive:]
        self.top = top
from concourse.bass_utils import run_bass_kernel_spmd
S = 2048; D = 1024; NT = 16; INW = 3856; DEPTH = 2
ND = 3072; EC = 1535
NE = 16; CAP = 256; DFF = 2816


def t5_bucket_np(rel):
    nb = 16
    ret = np.where(rel > 0, nb, 0)
    n = np.abs(rel)
    max_exact = 8
    nf = np.maximum(n, 1).astype(np.float32)
    large = max_exact + (np.log(nf / np.float32(max_exact)) / np.float32(np.log(1024 / max_exact))
                         * np.float32(nb - max_exact)).astype(np.int32)
    large = np.minimum(large, nb - 1)
    return ret + np.where(n < max_exact, n, large)


def host_consts():
    d = np.arange(ND) - EC
    ad = np.abs(d)
    cnt = ((ad <= 64).astype(np.float32) + ((d % 4 == 0) & (ad <= 256)).astype(np.float32)
           + ((d % 16 == 0) & (ad <= 1024)).astype(np.float32))
    bk = t5_bucket_np(d)
    oh = np.zeros((32, ND), np.float32)
    oh[bk, np.arange(ND)] = 1.0
    c = {}
    c["c_oh"] = oh
    c["c_cnt"] = np.tile(cnt[None], (4, 1)).astype(np.float32)
    c["c_ident"] = np.eye(128, dtype=np.float32)
    c["c_jmat"] = np.eye(128, dtype=np.float32)[::-1].copy()
    c["c_triu"] = np.triu(np.ones((128, 128), np.float32))
    c["c_tril"] = np.tril(np.ones((128, 128), np.float32))
    ob = np.zeros((128, 128), np.float32); ob[:64, :64] = 1; ob[64:, 64:] = 1
    c["c_onesblk"] = ob
    tus = np.triu(np.ones((128, 128), np.float32), 1); tui = np.triu(np.ones((128, 128), np.float32), 0)
    c["c_mask4"] = np.concatenate([tus, tui, tus, tui], axis=1)
    c["c_trils"] = np.tril(np.ones((128, 128), np.float32), -1)
    rm = np.ones((128, 256), np.float32); rm[:, 0] = 0; rm[:, 128] = 0
    c["c_rmask"] = rm
    ohb = np.zeros((16, 2048), np.float32)
    for e_ in range(16):
        ohb[e_, e_ * 128:(e_ + 1) * 128] = 1.0
    c["c_ohb"] = ohb
    return c


class K:
    pass


def build(dbg=None, nlayers=DEPTH, stages=("attn", "mlstm", "rwkv", "moe")):
    nc = bass.Bass("TRN2", target_bir_lowering=False)
    k = K()
    k.nc = nc
    SH = dict(x=[S, D], rel_bias=[32, 4], ln1_g=[DEPTH, D], w_in=[DEPTH, D, INW], w_out=[DEPTH, D, D], rk_mu=[DEPTH, 2, 1280],
              rk_w0=[DEPTH, 2, 384], rk_w2=[DEPTH, 2, 64, 384], rk_a0=[DEPTH, 2, 384], rk_a2=[DEPTH, 2, 64, 384],
              rk_kk=[DEPTH, 384], rk_ka=[DEPTH, 384], rk_rk=[DEPTH, 6, 64], rk_g2=[DEPTH, 128, 384], rk_ln_g=[DEPTH, 384],
              rk_ln_b=[DEPTH, 384], ml_conv_w=[DEPTH, 5, 768], ml_conv_b=[DEPTH, 768], ml_ib=[DEPTH, 2, 4], ml_fb=[DEPTH, 2, 4],
              ml_ln_g=[DEPTH, 384], ln2_g=[DEPTH, D], router=[DEPTH, D, NE], e_w1=[DEPTH, NE, D, DFF], e_w3=[DEPTH, NE, D, DFF],
              e_w2=[DEPTH, NE, DFF, D], final_g=[D], c_oh=[32, ND], c_cnt=[4, ND], c_ident=[128, 128], c_jmat=[128, 128],
              c_triu=[128, 128], c_tril=[128, 128], c_onesblk=[128, 128], c_mask4=[128, 512], c_trils=[128, 128],
              c_rmask=[128, 256], c_ohb=[16, 2048], c_pc=[DEPTH, 128, NPC], c_wx=[DEPTH, D, 384])

    class _IN(dict):
        def __missing__(self, name):
            t = nc.dram_tensor(name, list(SH[name]), F32, kind="ExternalInput")
            self[name] = t
            return t
    IN = _IN()
    k.IN = IN
    out_t = nc.dram_tensor("out", [S, D], F32, kind="ExternalOutput")
    k.mscr = nc.dram_tensor("mscr", [4, ND], F32, kind="Internal")
    k.mtab_d = nc.dram_tensor("mtab_d", [4, 128, 23 * 128], F32, kind="Internal")
    k.xspill = nc.dram_tensor("xspill", [S, D], F32, kind="Internal")
    k.dbg = dbg
    k.dbg_out = {}
    with ExitStack() as es:
        ACOLS = 53000
        arena_t = es.enter_context(nc.sbuf_tensor("arena", [128, ACOLS], F32))
        k.ps = [es.enter_context(nc.psum_tensor(f"ps{i}", [128, 512], F32)) for i in range(8)]
        P = Prog(nc)
        A = Arena(P, arena_t, ACOLS)
        k.P, k.A = P, A
        k.final_events = []
        setup_consts(k)
        k.xres = A.alloc("xres", (NT, D))
        xin = IN["x"].ap().rearrange("(i p) d -> p i d", p=128)
        for i in range(NT):
            P.dma("sp" if i % 2 == 0 else "act", k.xres.ap[:, i, :], xin[:, i, :], writes=[k.xres.k(i)], semkey=("xld", i % 4))
        build_mask_table(k)
        for l in range(nlayers):
            m0 = A.mark()
            norm_to_hT(k, IN["ln1_g"].ap()[l], "hT")
            if "attn" in stages:
                attn_stage(k, l)
            if "mlstm" in stages:
                mlstm_stage(k, l)
            if "rwkv" in stages:
                rwkv_stage(k, l)
            A.release(m0)
            if "moe" in stages:
                moe_stage(k, l)
        final_stage(k, out_t)
        P.emit(final_wait_events=k.final_events)
    return nc, k


def dbg_dump(k, name, region_ap, shape, dt, reads):
    if not k.dbg or name not in k.dbg:
        return None
    t = k.nc.dram_tensor("dbg_" + name, list(shape), dt, kind="ExternalOutput")
    k.dbg_out[name] = t
    return t


def setup_consts(k):
    P, A, IN = k.P, k.A, k.IN
    k.ident = A.alloc("ident", (128,))
    k.jmat = A.alloc("jmat", (128,))
    k.ident16 = A.alloc("ident16", (128,), BF16)
    k.triu = A.alloc("triu", (128,))
    k.tril = A.alloc("tril", (128,))
    P.dma("sp", k.ident.ap, IN["c_ident"].ap(), writes=[k.ident.k()], semkey="c0")
    P.dma("sp", k.jmat.ap, IN["c_jmat"].ap(), writes=[k.jmat.k()], semkey="c1")
    P.dma("sp", k.triu.ap, IN["c_triu"].ap(), writes=[k.triu.k()], semkey="c2")
    P.dma("sp", k.tril.ap, IN["c_tril"].ap(), writes=[k.tril.k()], semkey="c3")
    P.op("dve", lambda e: e.tensor_copy(k.ident16.ap, k.ident.ap), reads=[k.ident.k()], writes=[k.ident16.k()])
    k.one = A.alloc("one", (1,))
    P.op("pool", lambda e: e.memset(k.one.ap, 1.0), writes=[k.one.k()])
    k.ones32 = A.alloc("ones32", (128,))
    P.op("pool", lambda e: e.memset(k.ones32.ap, 1.0), writes=[k.ones32.k()])
    k.negm = [A.alloc("negm0", (128,)), A.alloc("negm1", (128,))]
    P.op("dve", lambda e: e.tensor_scalar(k.negm[0].ap, k.triu.ap, -1.0, 30000.0, op0=ALU.add, op1=ALU.mult), reads=[k.triu.k()], writes=[k.negm[0].k()])
    P.op("dve", lambda e: e.tensor_scalar(k.negm[1].ap, k.tril.ap, -1.0, 30000.0, op0=ALU.add, op1=ALU.mult), reads=[k.tril.k()], writes=[k.negm[1].k()])
    k.epsgn = A.alloc("epsgn", (1,))
    P.op("pool", lambda e: e.memset(k.epsgn.ap, 64e-5), writes=[k.epsgn.k()])
    k.onesblk = A.alloc("onesblk", (128,)); k.onesblk64 = A.alloc("onesblk64", (128,))
    k.trils = A.alloc("trils", (128,))
    P.dma("act", k.onesblk.ap, IN["c_onesblk"].ap(), writes=[k.onesblk.k()], semkey="c4")
    P.dma("act", k.trils.ap, IN["c_trils"].ap(), writes=[k.trils.k()], semkey="c6")
    P.op("dve", lambda e: e.tensor_scalar(k.onesblk64.ap, k.onesblk.ap, 1.0 / 64, None, op0=ALU.mult), reads=[k.onesblk.k()], writes=[k.onesblk64.k()])
    k.iota256 = A.alloc("iota256", (256,)); k.slotidx = A.alloc("slotidx", (2,))
    k.ones16 = A.alloc("ones16", (128,), BF16); k.trius16 = A.alloc("trius16", (128,), BF16)
    P.op("pool", lambda e: e.iota(k.iota256.ap, [[1, 256]], base=0, channel_multiplier=0, allow_small_or_imprecise_dtypes=True), writes=[k.iota256.k()])
    P.op("pool", lambda e: e.iota(k.slotidx.ap, [[128, 2]], base=0, channel_multiplier=1, allow_small_or_imprecise_dtypes=True), writes=[k.slotidx.k()])
    P.op("dve", lambda e: e.tensor_copy(k.ones16.ap, k.ones32.ap), reads=[k.ones32.k()], writes=[k.ones16.k()])
    P.op("dve", lambda e: e.tensor_tensor(k.trius16.ap, k.triu.ap, k.ident.ap, op=ALU.subtract), reads=[k.triu.k(), k.ident.k()], writes=[k.trius16.k()])
    k.eps6 = A.alloc("eps6", (1,))
    P.op("pool", lambda e: e.memset(k.eps6.ap, 1e-6), writes=[k.eps6.k()])


def build_mask_table(k):
    P, A, IN, ps = k.P, k.A, k.IN, k.ps
    m0 = A.mark()
    rb = A.alloc("rb", (4,)); oh = A.alloc("oh", (ND,)); cnt = A.alloc("cnt", (ND,)); mm = A.alloc("mm", (ND,))
    P.dma("sp", rb.ap[0:32, :], IN["rel_bias"].ap(), writes=[rb.k()], semkey="mt0")
    P.dma("sp", oh.ap[0:32, :], IN["c_oh"].ap(), writes=[oh.k()], semkey="mt1")
    P.dma("act", cnt.ap[0:4, :], IN["c_cnt"].ap(), writes=[cnt.k()], semkey="mt2")
    for c in range(ND // 512):
        b = ps[c % 2]
        P.op("pe", lambda e, c=c, b=b: e.matmul(b[0:4, :], rb.ap[0:32, :], oh.ap[0:32, c * 512:(c + 1) * 512], start=True, stop=True),
             reads=[rb.k(), oh.k()], writes=[f"ps{c%2}"])
        P.op("act", lambda e, c=c, b=b: e.activation(mm.ap[0:4, c * 512:(c + 1) * 512], b[0:4, :], AF.Exp),
             reads=[f"ps{c%2}"], writes=[mm.k(c)])
        P.op("dve", lambda e, c=c: e.tensor_tensor(mm.ap[0:4, c * 512:(c + 1) * 512], mm.ap[0:4, c * 512:(c + 1) * 512],
                                                  cnt.ap[0:4, c * 512:(c + 1) * 512], op=ALU.mult),
             reads=[mm.k(c), cnt.k()], writes=[mm.k(c)])
    P.dma("sp", k.mscr.ap(), mm.ap[0:4, :], reads=[mm.k(c) for c in range(ND // 512)], writes=["mscr"], semkey="mt3")
    hanks = [A.alloc(f"hank{i}", (23, 128)) for i in range(2)]
    mts = [A.alloc(f"mt{i}", (23, 128)) for i in range(2)]
    for h in range(4):
        hank = hanks[h % 2]
        mt = mts[h % 2]
        src = bass.AP(k.mscr, h * ND, [[1, 128], [128, 23], [1, 128]])
        P.dma("sp" if h % 2 == 0 else "act", hank.ap, src, reads=["mscr"], writes=[hank.k()], semkey=("mt4", h))
        for j in range(23):
            jj = 22 - j
            b = 2 + (j // 4) % 2
            P.op("pe", lambda e, j=j, b=b, hank=hank: e.matmul(ps[b][:, (j % 4) * 128:(j % 4 + 1) * 128], hank.ap[:, j, :], k.jmat.ap, start=True, stop=True),
                 reads=[hank.k(), k.jmat.k()], writes=[f"ps{b}"])
            P.op("act" if j % 2 == 0 else "dve",
                 (lambda e, jj=jj, j=j, b=b, mt=mt: e.copy(mt.ap[:, jj, :], ps[b][:, (j % 4) * 128:(j % 4 + 1) * 128])) if j % 2 == 0 else
                 (lambda e, jj=jj, j=j, b=b, mt=mt: e.tensor_copy(mt.ap[:, jj, :], ps[b][:, (j % 4) * 128:(j % 4 + 1) * 128])),
                 reads=[f"ps{b}"], writes=[mt.k()])
        P.dma("sp", k.mtab_d.ap()[h].rearrange("p (j q) -> p j q", j=23), mt.ap, reads=[mt.k()], writes=[("mtab_d", h)], semkey=("mt5", h))
    A.release(m0)


def norm_to_hT(k, g_ap, name, want_f32T=False):
    P, A, ps = k.P, k.A, k.ps
    k.hT = A.alloc(name, (8, S), BF16)
    m0 = A.mark()
    gb = A.alloc("gb", (D,)); junk = A.alloc("junk", (D,)); ss = A.alloc("ss", (NT,)); rstd = A.alloc("rstd", (NT,))
    hb = [A.alloc(f"hb{i}", (D,), BF16) for i in range(2)]
    P.dma("sp", gb.ap, g_ap.partition_broadcast(128), writes=[gb.k()], semkey="gb")
    P.op("pool", lambda e: e.memset(ss.ap, 0.0), writes=[ss.k(i) for i in range(NT)])
    for i in range(NT):
        P.op("act", lambda e, i=i: e.activation(junk.ap, k.xres.ap[:, i, :], AF.Square, accum_out=ss.ap[:, i:i + 1]),
             reads=[k.xres.k(i)], writes=[junk.k(), ss.k(i)])
    P.op("act", lambda e: e.activation(rstd.ap, ss.ap, AF.Sqrt, scale=1.0 / D, bias=k.eps6.ap[:, 0:1]),
         reads=[ss.k(i) for i in range(NT)] + [k.eps6.k()], writes=[rstd.k()])
    P.op("dve", lambda e: e.reciprocal(rstd.ap, rstd.ap), reads=[rstd.k()], writes=[rstd.k()])
    for i in range(NT):
        h_ = hb[i % 2]
        P.op("dve", lambda e, i=i, h_=h_: e.scalar_tensor_tensor(h_.ap, k.xres.ap[:, i, :], rstd.ap[:, i:i + 1], gb.ap, op0=ALU.mult, op1=ALU.mult),
             reads=[k.xres.k(i), rstd.k(), gb.k()], writes=[h_.k()])
        b = 4 + i % 2
        pb = ps[b][:, :].bitcast(BF16)
        for c in range(8):
            P.op("pe", lambda e, c=c, pb=pb, h_=h_: e.transpose(pb[:, c * 128:(c + 1) * 128], h_.ap[:, c * 128:(c + 1) * 128], k.ident16.ap),
                 reads=[h_.k(), k.ident16.k()], writes=[f"ps{b}"])
        P.op("act", lambda e, i=i, pb=pb: e.copy(k.hT.ap[:, :, i * 128:(i + 1) * 128], pb.rearrange("p (c t) -> p c t", c=8)),
             reads=[f"ps{b}"], writes=[k.hT.k(i)])
    A.release(m0)


def load_w_bf16(k, name, src_ap, nchunks, ncols, semkey, split=None):
    P, A = k.P, k.A
    w = A.alloc(name, (nchunks, ncols), BF16)
    v = src_ap.rearrange("(c p) n -> p c n", p=128)
    for c in range(nchunks):
        P.dma("pool", w.ap[:, c, :], v[:, c, :], writes=[w.k(c)], semkey=(semkey, c % 4))
    return w


def outproj_partial(k, l, yT, nch, row0, tag):
    P, A, ps, IN = k.P, k.A, k.ps, k.IN
    wo = load_w_bf16(k, "wo_" + tag, IN["w_out"].ap()[l][row0:row0 + nch * 128, :], nch, D, "wo_" + tag)
    n = 0
    for i in range(NT):
        for half in range(2):
            b = 6 + n % 2
            n += 1
            for c in range(nch):
                P.op("pe", lambda e, i=i, half=half, c=c, b=b: e.matmul(ps[b][:, :], yT.ap[:, c, i * 128:(i + 1) * 128],
                                                                     wo.ap[:, c, half * 512:(half + 1) * 512], start=(c == 0), stop=(c == nch - 1)),
                     reads=[yT.k(i), wo.k(c)], writes=[f"ps{b}"])
            P.op("dve", lambda e, i=i, half=half, b=b: e.tensor_tensor(k.xres.ap[:, i, half * 512:(half + 1) * 512],
                                                                      k.xres.ap[:, i, half * 512:(half + 1) * 512], ps[b][:, :], op=ALU.add),
                 reads=[f"ps{b}", k.xres.k(i)], writes=[k.xres.k(i)])


def transpose_to_T(k, y, nch, yT):
    P, ps = k.P, k.ps
    for i in range(NT):
        b = 4 + i % 2
        pb = ps[b][:, :].bitcast(BF16)
        for c in range(nch):
            P.op("pe", lambda e, i=i, c=c, pb=pb: e.transpose(pb[:, c * 128:(c + 1) * 128], y.ap[:, i, c * 128:(c + 1) * 128], k.ident16.ap),
                 reads=[y.k(i), k.ident16.k()], writes=[f"ps{b}"])
        P.op("act", lambda e, i=i, pb=pb: e.copy(yT.ap[:, :, i * 128:(i + 1) * 128], pb[:, 0:nch * 128].rearrange("p (c t) -> p c t", c=nch)),
             reads=[f"ps{b}"], writes=[yT.k(i)])


def attn_stage(k, l):
    P, A, ps, IN = k.P, k.A, k.ps, k.IN
    m0 = A.mark()
    ya = A.alloc("ya", (NT, 256), BF16)
    m1 = A.mark()
    wa = load_w_bf16(k, "wa", IN["w_in"].ap()[l][:, 0:768], 8, 768, "wa")
    qT = A.alloc("qT", (2, S), BF16); kT = A.alloc("kT", (2, S), BF16)
    vp = A.alloc("vp", (NT, 4, 65), BF16)
    P.op("pool", lambda e: e.memset(vp.ap, 1.0), writes=[vp.k(i) for i in range(NT)])
    n = 0
    for cc in range(4):
        dst = qT if cc < 2 else kT
        for tg in range(4):
            b = n % 2; n += 1
            for c in range(8):
                P.op("pe", lambda e, c=c, cc=cc, tg=tg, b=b: e.matmul(ps[b][:, :], wa.ap[:, c, cc * 128:(cc + 1) * 128], k.hT.ap[:, c, tg * 512:(tg + 1) * 512],
                                                                  start=(c == 0), stop=(c == 7)),
                     reads=[wa.k(c)] + [k.hT.k(4 * tg + j) for j in range(4)], writes=[f"ps{b}"])
            P.op("act", lambda e, cc=cc, tg=tg, b=b, dst=dst: e.copy(dst.ap[:, cc % 2, tg * 512:(tg + 1) * 512], ps[b][:, :]),
                 reads=[f"ps{b}"], writes=[dst.k(tg)])
    for i in range(NT):
        b = 2 + i % 2
        for c in range(8):
            P.op("pe", lambda e, c=c, i=i, b=b: e.matmul(ps[b][:, 0:256], k.hT.ap[:, c, i * 128:(i + 1) * 128], wa.ap[:, c, 512:768], start=(c == 0), stop=(c == 7)),
                 reads=[wa.k(c), k.hT.k(i)], writes=[f"ps{b}"])
        P.op("dve", lambda e, i=i, b=b: e.tensor_copy(vp.ap[:, i, :, 0:64], ps[b][:, 0:256].rearrange("p (h c) -> p h c", h=4)),
             reads=[f"ps{b}"], writes=[vp.k(i)])
    mtab = [A.alloc(f"mtab{i}", (23, 128)) for i in range(2)]
    e32 = [A.alloc(f"e32_{i}", (512,)) for i in range(2)]
    p16 = [A.alloc(f"p16_{i}", (512,), BF16) for i in range(2)]
    rc = A.alloc("rc", (8,))
    it = 0
    for h in range(4):
        mt = mtab[h % 2]
        P.dma("sp", mt.ap, k.mtab_d.ap()[h].rearrange("p (j q) -> p j q", j=23), reads=[("mtab_d", h)], writes=[mt.k()], semkey=("mtl", h % 2))
        hp, hb_ = h // 2, (h % 2) * 64
        for g in range(4):
            kts = [kt for kt in range(NT) if -8 <= kt - 4 * g <= 11]
            for kt in kts:
                sb = it % 2; it += 1
                jj0 = 11 - kt + 4 * g
                P.op("pe", lambda e, kt=kt, g=g, sb=sb, hp=hp, hb_=hb_: e.matmul(ps[sb][:, :], kT.ap[hb_:hb_ + 64, hp, kt * 128:(kt + 1) * 128],
                                                                           qT.ap[hb_:hb_ + 64, hp, g * 512:(g + 1) * 512], start=True, stop=True),
                     reads=[kT.k(kt // 4), qT.k(g)], writes=[f"ps{sb}"])
                P.op("act", lambda e, sb=sb: e.activation(e32[sb].ap, ps[sb][:, :], AF.Exp, scale=0.125),
                     reads=[f"ps{sb}"], writes=[e32[sb].k()])
                P.op("dve", lambda e, sb=sb, jj0=jj0, mt=mt: e.tensor_tensor(p16[sb].ap, e32[sb].ap, mt.ap[:, jj0:jj0 + 4, :].rearrange("p j q -> p (j q)"), op=ALU.mult),
                     reads=[e32[sb].k(), mt.k()], writes=[p16[sb].k()])
                for i in range(4):
                    P.op("pe", lambda e, i=i, sb=sb, kt=kt, h=h, kts=kts: e.matmul(ps[2 + i][:, 0:65], p16[sb].ap[:, i * 128:(i + 1) * 128], vp.ap[:, kt, h, :],
                                                                             start=(kt == kts[0]), stop=(kt == kts[-1])),
                         reads=[p16[sb].k(), vp.k(kt)], writes=[f"ps{2+i}"])
            for i in range(4):
                P.op("dve", lambda e, i=i: e.reciprocal(rc.ap[:, i:i + 1], ps[2 + i][:, 64:65]), reads=[f"ps{2+i}"], writes=[rc.k(i)])
                P.op("dve", lambda e, i=i, g=g, h=h: e.tensor_scalar(ya.ap[:, 4 * g + i, h * 64:(h + 1) * 64], ps[2 + i][:, 0:64], rc.ap[:, i:i + 1], None, op0=ALU.mult),
                     reads=[f"ps{2+i}", rc.k(i)], writes=[ya.k(4 * g + i)])
    A.release(m1)
    if k.dbg and "ya" in k.dbg:
        t = k.nc.dram_tensor("dbg_ya", [S, 256], BF16, kind="ExternalOutput")
        k.final_events.append(P.dma("sp", t.ap().rearrange("(i p) c -> p i c", p=128), ya.ap, reads=[ya.k(i) for i in range(NT)], semkey="dbg"))
    yaT = A.alloc("yaT", (2, S), BF16)
    transpose_to_T(k, ya, 2, yaT)
    outproj_partial(k, l, yaT, 2, 0, "a")
    A.release(m0)


def final_stage(k, out_t):
    P, A = k.P, k.A
    m0 = A.mark()
    gb = A.alloc("gbf", (D,)); junk = A.alloc("junkf", (D,)); ss = A.alloc("ssf", (NT,)); rstd = A.alloc("rstdf", (NT,))
    ob = [A.alloc(f"ob{i}", (D,)) for i in range(2)]
    P.dma("sp", gb.ap, k.IN["final_g"].ap().partition_broadcast(128), writes=[gb.k()], semkey="gbf")
    P.op("pool", lambda e: e.memset(ss.ap, 0.0), writes=[ss.k(i) for i in range(NT)])
    for i in range(NT):
        P.op("act", lambda e, i=i: e.activation(junk.ap, k.xres.ap[:, i, :], AF.Square, accum_out=ss.ap[:, i:i + 1]),
             reads=[k.xres.k(i)], writes=[junk.k(), ss.k(i)])
    P.op("act", lambda e: e.activation(rstd.ap, ss.ap, AF.Sqrt, scale=1.0 / D, bias=k.eps6.ap[:, 0:1]),
         reads=[ss.k(i) for i in range(NT)] + [k.eps6.k()], writes=[rstd.k()])
    P.op("dve", lambda e: e.reciprocal(rstd.ap, rstd.ap), reads=[rstd.k()], writes=[rstd.k()])
    ov = out_t.ap().rearrange("(i p) d -> p i d", p=128)
    for i in range(NT):
        o = ob[i % 2]
        P.op("dve", lambda e, i=i, o=o: e.scalar_tensor_tensor(o.ap, k.xres.ap[:, i, :], rstd.ap[:, i:i + 1], gb.ap, op0=ALU.mult, op1=ALU.mult),
             reads=[k.xres.k(i), rstd.k(), gb.k()], writes=[o.k()])
        k.final_events.append(P.dma("sp" if i % 2 == 0 else "act", ov[:, i, :], o.ap, reads=[o.k()], semkey=("out", i % 2)))
    A.release(m0)


OFF_MQK = 2304; OFF_MV = 3072; OFF_MO = 3456; OFF_MG = 3840


def mlstm_stage(k, l):
    P, A, ps, IN = k.P, k.A, k.ps, k.IN
    m0 = A.mark()
    hsum = A.alloc("hsum", (NT, 4, 96))
    m1 = A.mark()
    qkT = A.alloc("qkT", (8, S), BF16)
    vp = A.alloc("vpm", (NT, 4, 97), BF16)
    G = A.alloc("G", (NT, 16))
    BL = [A.alloc(f"BL{d}", (NT, 4)) for d in range(2)]
    EBL = [A.alloc(f"EBL{d}", (NT, 4)) for d in range(2)]
    IMB = [A.alloc(f"IMB{d}", (NT, 4)) for d in range(2)]
    m2 = A.mark()
    wm = load_w_bf16(k, "wm", IN["w_in"].ap()[l][:, OFF_MQK:OFF_MV], 8, OFF_MV - OFF_MQK, "wm")
    pre = A.alloc("pre", (S + 4,)); cacc = A.alloc("cacc", (S,))
    cw = A.alloc("cw", (8, 6))
    for g_ in range(8):
        P.dma("sp", cw.ap[0:96, g_, 0:5], IN["ml_conv_w"].ap()[l][:, g_ * 96:(g_ + 1) * 96].rearrange("j c -> c j"), writes=[cw.k()], semkey=("cw0", g_ % 4), allow_slow_non_contiguous=True)
    P.dma("sp", cw.ap[0:96, :, 5:6], IN["ml_conv_b"].ap()[l].rearrange("(g c o) -> c g o", c=96, o=1), writes=[cw.k()], semkey="cw1", allow_slow_non_contiguous=True)
    P.op("pool", lambda e: e.memset(pre.ap, 0.0), writes=[pre.k()])
    P.op("pool", lambda e: e.memset(vp.ap, 1.0), writes=[vp.k(i) for i in range(NT)])
    n = 0
    for j in range(8):
        for tg in range(4):
            b = n % 2; n += 1
            for c in range(8):
                P.op("pe", lambda e, c=c, j=j, tg=tg, b=b: e.matmul(ps[b][0:96, :], wm.ap[:, c, j * 96:(j + 1) * 96], k.hT.ap[:, c, tg * 512:(tg + 1) * 512],
                                                                 start=(c == 0), stop=(c == 7)),
                     reads=[wm.k(c)] + [k.hT.k(4 * tg + q) for q in range(4)], writes=[f"ps{b}"])
            P.op("act", lambda e, tg=tg, b=b: e.copy(pre.ap[0:96, 2 + tg * 512:2 + (tg + 1) * 512], ps[b][0:96, :]),
                 reads=[f"ps{b}"], writes=[pre.k()])
        P.op("dve", lambda e, j=j: e.tensor_scalar(cacc.ap[0:96, :], pre.ap[0:96, 0:S], cw.ap[0:96, j, 0:1], None, op0=ALU.mult),
             reads=[pre.k(), cw.k()], writes=[cacc.k()])
        for t in range(1, 5):
            P.op("dve", lambda e, j=j, t=t: e.scalar_tensor_tensor(cacc.ap[0:96, :], pre.ap[0:96, t:t + S], cw.ap[0:96, j, t:t + 1], cacc.ap[0:96, :],
                                                                 op0=ALU.mult, op1=ALU.add),
                 reads=[pre.k(), cw.k(), cacc.k()], writes=[cacc.k()])
        if j < 4:
            P.op("act", lambda e, j=j: e.activation(qkT.ap[0:96, j, :], cacc.ap[0:96, :], AF.Silu, bias=cw.ap[0:96, j, 5:6]),
                 reads=[cacc.k(), cw.k()], writes=[qkT.k(j)])
        else:
            P.op("act", lambda e, j=j: e.activation(cacc.ap[0:96, :], cacc.ap[0:96, :], AF.Silu, bias=cw.ap[0:96, j, 5:6]),
                 reads=[cacc.k(), cw.k()], writes=[cacc.k()])
            P.op("dve", lambda e, j=j: e.tensor_scalar(qkT.ap[0:96, j, :], cacc.ap[0:96, :], 96.0 ** -0.5, None, op0=ALU.mult),
                 reads=[cacc.k()], writes=[qkT.k(j)])
    A.release(m2)
    wv = load_w_bf16(k, "wv", IN["w_in"].ap()[l][:, OFF_MV:OFF_MO], 8, 384, "wv")
    wg = load_w_bf16(k, "wg", IN["w_in"].ap()[l][:, OFF_MG:INW], 8, 16, "wg")
    gbias = A.alloc("gbias", (16,))
    for d in range(2):
        P.dma("act", gbias.ap[:, 8 * d:8 * d + 4], IN["ml_ib"].ap()[l][d].partition_broadcast(128), writes=[gbias.k()], semkey=("gbi", d))
        P.dma("act", gbias.ap[:, 8 * d + 4:8 * d + 8], IN["ml_fb"].ap()[l][d].partition_broadcast(128), writes=[gbias.k()], semkey=("gbf_", d))
    for i in range(NT):
        b = 2 + i % 2
        for c in range(8):
            P.op("pe", lambda e, c=c, i=i, b=b: e.matmul(ps[b][:, 0:384], k.hT.ap[:, c, i * 128:(i + 1) * 128], wv.ap[:, c, :],
                                                     start=(c == 0), stop=(c == 7)),
                 reads=[wv.k(c), k.hT.k(i)], writes=[f"ps{b}"])
        P.op("dve", lambda e, i=i, b=b: e.tensor_copy(vp.ap[:, i, :, 0:96], ps[b][:, 0:384].rearrange("p (h c) -> p h c", h=4)),
             reads=[f"ps{b}"], writes=[vp.k(i)])
        b2 = 4 + i % 2
        for c in range(8):
            P.op("pe", lambda e, c=c, i=i, b2=b2: e.matmul(ps[b2][:, 0:16], k.hT.ap[:, c, i * 128:(i + 1) * 128], wg.ap[:, c, :],
                                                       start=(c == 0), stop=(c == 7)),
                 reads=[wg.k(c), k.hT.k(i)], writes=[f"ps{b2}"])
        P.op("dve", lambda e, i=i, b2=b2: e.tensor_tensor(G.ap[:, i, :], ps[b2][:, 0:16], gbias.ap, op=ALU.add),
             reads=[f"ps{b2}", gbias.k()], writes=[G.k()])
    A.release(m2)
    for d in range(2):
        v_ = G.ap[:, :, 8 * d + 4:8 * d + 8]
        P.op("act", lambda e, v_=v_: e.activation(v_, v_, AF.Exp, scale=-1.0), reads=[G.k()], writes=[G.k()])
        P.op("act", lambda e, v_=v_: e.activation(v_, v_, AF.Ln, bias=k.one.ap[:, 0:1]), reads=[G.k(), k.one.k()], writes=[G.k()])
        P.op("dve", lambda e, v_=v_: e.tensor_scalar(v_, v_, -1.0, None, op0=ALU.mult), reads=[G.k()], writes=[G.k()])
    for d in range(2):
        tri = k.triu if d == 0 else k.tril
        P.op("pe", lambda e, d=d, tri=tri: e.matmul(ps[7][:, 0:64].rearrange("p (i h) -> p i h", h=4), tri.ap, G.ap[:, :, 8 * d + 4:8 * d + 8], start=True, stop=True),
             reads=[tri.k(), G.k()], writes=["ps7"])
        P.op("dve", lambda e, d=d: e.tensor_copy(BL[d].ap, ps[7][:, 0:64].rearrange("p (i h) -> p i h", h=4)), reads=["ps7"], writes=[BL[d].k()])
        P.op("act", lambda e, d=d: e.activation(EBL[d].ap, BL[d].ap, AF.Exp), reads=[BL[d].k()], writes=[EBL[d].k()])
        P.op("dve", lambda e, d=d: e.tensor_tensor(IMB[d].ap, G.ap[:, :, 8 * d:8 * d + 4], BL[d].ap, op=ALU.subtract), reads=[G.k(), BL[d].k()], writes=[IMB[d].k()])
    C32 = A.alloc("C32", (4, 97)); Cb = A.alloc("Cb", (4, 97), BF16)
    dg = [A.alloc(f"dg{i}", (128,)) for i in range(2)]
    Dm = [A.alloc(f"Dm{i}", (128,)) for i in range(2)]
    W16 = [A.alloc(f"W16{i}", (128,), BF16) for i in range(2)]
    nis = [A.alloc(f"nis{i}", (97,)) for i in range(2)]
    nsb = [A.alloc(f"nsb{i}", (97,)) for i in range(2)]
    kh = [A.alloc(f"kh{i}", (96,), BF16) for i in range(2)]
    sm = A.alloc("sm", (8,))
    it = 0
    for d in range(2):
        P.op("pool", lambda e: e.memset(C32.ap, 0.0), writes=[C32.k(h) for h in range(4)])
        P.op("pool", lambda e: e.memset(Cb.ap, 0.0), writes=[Cb.k(h) for h in range(4)])
        nm = k.negm[d]
        endc = 127 if d == 0 else 0
        order = range(NT) if d == 0 else range(NT - 1, -1, -1)
        for i in order:
            for h in range(4):
                r = it % 2; it += 1
                tsl = slice(i * 128, (i + 1) * 128)
                P.op("pe", lambda e, r=r, h=h, tsl=tsl: e.matmul(ps[r][:, 0:128], qkT.ap[0:96, 4 + h, tsl], qkT.ap[0:96, h, tsl], start=True, stop=True),
                     reads=[qkT.k(4 + h), qkT.k(h)], writes=[f"ps{r}"])
                P.op("dve", lambda e, r=r, d=d, i=i, h=h: e.tensor_scalar(dg[r].ap, k.ident.ap, BL[d].ap[:, i, h:h + 1], None, op0=ALU.mult),
                     reads=[k.ident.k(), BL[d].k()], writes=[dg[r].k()])
                P.op("pe", lambda e, r=r: e.matmul(ps[2 + r][:, 0:128], k.ones32.ap, dg[r].ap, start=True, stop=False),
                     reads=[k.ones32.k(), dg[r].k()], writes=[f"ps{2+r}"])
                P.op("pe", lambda e, r=r, nm=nm: e.matmul(ps[2 + r][:, 0:128], k.ident.ap, nm.ap, start=False, stop=True),
                     reads=[k.ident.k(), nm.k()], writes=[f"ps{2+r}"])
                P.op("act", lambda e, r=r, d=d, i=i, h=h: e.activation(Dm[r].ap, ps[2 + r][:, 0:128], AF.Exp, bias=IMB[d].ap[:, i, h:h + 1]),
                     reads=[f"ps{2+r}", IMB[d].k()], writes=[Dm[r].k()])
                P.op("act", lambda e, r=r, endc=endc: e.activation(sm.ap[:, 4 + r:5 + r], ps[2 + r][:, endc:endc + 1], AF.Exp),
                     reads=[f"ps{2+r}"], writes=[sm.k(4 + r)])
                P.op("dve", lambda e, r=r: e.tensor_tensor(W16[r].ap, ps[r][:, 0:128], Dm[r].ap, op=ALU.mult),
                     reads=[f"ps{r}", Dm[r].k()], writes=[W16[r].k()])
                P.op("pe", lambda e, r=r, i=i, h=h: e.matmul(ps[4 + r][:, 0:97], W16[r].ap, vp.ap[:, i, h, :], start=True, stop=True),
                     reads=[W16[r].k(), vp.k(i)], writes=[f"ps{4+r}"])
                P.op("pe", lambda e, r=r, h=h, tsl=tsl: e.matmul(ps[4 + r][:, 128:225], qkT.ap[0:96, h, tsl], Cb.ap[0:96, h, :], start=True, stop=True),
                     reads=[qkT.k(h), Cb.k(h)], writes=[f"ps{4+r}"])
                P.op("act", lambda e, r=r: e.copy(nis[r].ap, ps[4 + r][:, 0:97]), reads=[f"ps{4+r}"], writes=[nis[r].k()])
                P.op("dve", lambda e, r=r, d=d, i=i, h=h: e.scalar_tensor_tensor(nsb[r].ap, ps[4 + r][:, 128:225], EBL[d].ap[:, i, h:h + 1], nis[r].ap,
                                                                              op0=ALU.mult, op1=ALU.add),
                     reads=[f"ps{4+r}", EBL[d].k(), nis[r].k()], writes=[nsb[r].k()])
                P.op("dve", lambda e, r=r: e.tensor_scalar(sm.ap[:, r:r + 1], nsb[r].ap[:, 96:97], -1.0, None, op0=ALU.mult),
                     reads=[nsb[r].k()], writes=[sm.k(r)])
                P.op("dve", lambda e, r=r: e.scalar_tensor_tensor(sm.ap[:, r:r + 1], nsb[r].ap[:, 96:97], 1.0, sm.ap[:, r:r + 1], op0=ALU.max, op1=ALU.max),
                     reads=[nsb[r].k(), sm.k(r)], writes=[sm.k(r)])
                P.op("dve", lambda e, r=r: e.reciprocal(sm.ap[:, r:r + 1], sm.ap[:, r:r + 1]), reads=[sm.k(r)], writes=[sm.k(r)])
                if d == 0:
                    P.op("dve", lambda e, r=r, i=i, h=h: e.tensor_scalar(hsum.ap[:, i, h, :], nsb[r].ap[:, 0:96], sm.ap[:, r:r + 1], None, op0=ALU.mult),
                         reads=[nsb[r].k(), sm.k(r)], writes=[hsum.k(i)])
                else:
                    P.op("dve", lambda e, r=r, i=i, h=h: e.scalar_tensor_tensor(hsum.ap[:, i, h, :], nsb[r].ap[:, 0:96], sm.ap[:, r:r + 1], hsum.ap[:, i, h, :],
                                                                             op0=ALU.mult, op1=ALU.add),
                         reads=[nsb[r].k(), sm.k(r), hsum.k(i)], writes=[hsum.k(i)])
                pb = ps[6][:, :].bitcast(BF16)
                P.op("pe", lambda e, r=r, h=h, tsl=tsl, pb=pb: e.transpose(pb[:, r * 128:r * 128 + 96], qkT.ap[0:96, 4 + h, tsl], k.ident16.ap[0:96, 0:96]),
                     reads=[qkT.k(4 + h), k.ident16.k()], writes=["ps6"])
                P.op("dve", lambda e, r=r, pb=pb, endc=endc: e.tensor_scalar(kh[r].ap, pb[:, r * 128:r * 128 + 96], Dm[r].ap[:, endc:endc + 1], None, op0=ALU.mult),
                     reads=["ps6", Dm[r].k()], writes=[kh[r].k()])
                P.op("pe", lambda e, r=r, i=i, h=h: e.matmul(ps[7][0:96, r * 128:r * 128 + 97], kh[r].ap, vp.ap[:, i, h, :], start=True, stop=True),
                     reads=[kh[r].k(), vp.k(i)], writes=["ps7"])
                P.op("dve", lambda e, r=r, h=h: e.scalar_tensor_tensor(C32.ap[0:96, h, :], C32.ap[0:96, h, :], sm.ap[0:96, 4 + r:5 + r], ps[7][0:96, r * 128:r * 128 + 97],
                                                                     op0=ALU.mult, op1=ALU.add),
                     reads=[C32.k(h), sm.k(4 + r), "ps7"], writes=[C32.k(h)])
                P.op("act", lambda e, h=h: e.copy(Cb.ap[0:96, h, :], C32.ap[0:96, h, :]), reads=[C32.k(h)], writes=[Cb.k(h)])
    A.release(m1)
    yc = A.alloc("yc", (NT, 384), BF16)
    m3 = A.mark()
    wmo = load_w_bf16(k, "wmo", IN["w_in"].ap()[l][:, OFF_MO:OFF_MG], 8, 384, "wmo")
    lng = A.alloc("lng", (384,))
    P.dma("sp", lng.ap, IN["ml_ln_g"].ap()[l].partition_broadcast(128), writes=[lng.k()], semkey="lng")
    st = A.alloc("st", (16,)); cen = [A.alloc(f"cen{i}", (4, 96)) for i in range(2)]; junk = A.alloc("junkm", (96,))
    og = [A.alloc(f"og{i}", (384,)) for i in range(2)]
    for i in range(NT):
        r = i % 2
        b = 2 + r
        for c in range(8):
            P.op("pe", lambda e, c=c, i=i, b=b: e.matmul(ps[b][:, 0:384], k.hT.ap[:, c, i * 128:(i + 1) * 128], wmo.ap[:, c, :], start=(c == 0), stop=(c == 7)),
                 reads=[wmo.k(c), k.hT.k(i)], writes=[f"ps{b}"])
        P.op("act", lambda e, r=r, b=b: e.activation(og[r].ap, ps[b][:, 0:384], AF.Sigmoid), reads=[f"ps{b}"], writes=[og[r].k()])
        sk = st.k()
        P.op("dve", lambda e, i=i: e.tensor_reduce(st.ap[:, 0:4], hsum.ap[:, i, :, :], axis=AX.X, op=ALU.add), reads=[hsum.k(i)], writes=[sk])
        P.op("dve", lambda e: e.tensor_scalar(st.ap[:, 0:4], st.ap[:, 0:4], 1.0 / 96, None, op0=ALU.mult), reads=[sk], writes=[sk])
        P.op("pool", lambda e: e.memset(st.ap[:, 4:8], 0.0), writes=[sk])
        for h in range(4):
            P.op("dve", lambda e, i=i, h=h, r=r: e.tensor_scalar(cen[r].ap[:, h, :], hsum.ap[:, i, h, :], st.ap[:, h:h + 1], None, op0=ALU.subtract),
                 reads=[hsum.k(i), sk], writes=[cen[r].k(h)])
            P.op("act", lambda e, h=h, r=r: e.activation(junk.ap, cen[r].ap[:, h, :], AF.Square, accum_out=st.ap[:, 4 + h:5 + h]),
                 reads=[cen[r].k(h)], writes=[junk.k(), sk])
        P.op("act", lambda e: e.activation(st.ap[:, 8:12], st.ap[:, 4:8], AF.Sqrt, scale=1.0 / 96, bias=k.eps6.ap[:, 0:1]), reads=[sk, k.eps6.k()], writes=[sk])
        P.op("dve", lambda e: e.reciprocal(st.ap[:, 8:12], st.ap[:, 8:12]), reads=[sk], writes=[sk])
        for h in range(4):
            P.op("dve", lambda e, h=h, r=r: e.scalar_tensor_tensor(cen[r].ap[:, h, :], cen[r].ap[:, h, :], st.ap[:, 8 + h:9 + h], lng.ap[:, h * 96:(h + 1) * 96],
                                                                 op0=ALU.mult, op1=ALU.mult),
                 reads=[cen[r].k(h), sk, lng.k()], writes=[cen[r].k(h)])
        P.op("dve", lambda e, i=i, r=r: e.tensor_tensor(yc.ap[:, i, :], cen[r].ap.rearrange("p h c -> p (h c)"), og[r].ap, op=ALU.mult),
             reads=[cen[r].k(h) for h in range(4)] + [og[r].k()], writes=[yc.k(i)])
    A.release(m3)
    if k.dbg and "yc" in k.dbg:
        t = k.nc.dram_tensor("dbg_yc", [S, 384], BF16, kind="ExternalOutput")
        k.final_events.append(P.dma("sp", t.ap().rearrange("(i p) c -> p i c", p=128), yc.ap, reads=[yc.k(i) for i in range(NT)], semkey="dbg"))
    ycT = A.alloc("ycT", (3, S), BF16)
    transpose_to_T(k, yc, 3, ycT)
    outproj_partial(k, l, ycT, 3, 640, "c")
    A.release(m0)


OFF_RKV = 768
BLK = 256
PC_MU, PC_MUX, PC_W0, PC_A0, PC_KK, PC_KA, PC_RK, PC_LNG, PC_LNB, NPC = 0, 18, 20, 26, 32, 35, 38, 41, 44, 48


def host_rwkv_layout(inputs):
    out = {}
    pc = np.zeros((DEPTH, 128, NPC), np.float32)
    for l in range(DEPTH):
        mu = np.asarray(inputs["rk_mu"][l], np.float32)
        for d in range(2):
            for j in range(3):
                for p in range(3):
                    pc[l, :, PC_MU + d * 9 + j * 3 + p] = mu[d, j * 384 + p * 128: j * 384 + (p + 1) * 128]
            pc[l, 0:64, PC_MUX + d] = mu[d, 1152:1216]
            pc[l, 64:128, PC_MUX + d] = mu[d, 1216:1280]
            for p in range(3):
                pc[l, :, PC_W0 + d * 3 + p] = inputs["rk_w0"][l][d][p * 128:(p + 1) * 128]
                pc[l, :, PC_A0 + d * 3 + p] = inputs["rk_a0"][l][d][p * 128:(p + 1) * 128]
        for p in range(3):
            sl = slice(p * 128, (p + 1) * 128)
            pc[l, :, PC_KK + p] = inputs["rk_kk"][l][sl]
            pc[l, :, PC_KA + p] = inputs["rk_ka"][l][sl]
            pc[l, :, PC_RK + p] = np.asarray(inputs["rk_rk"][l]).reshape(384)[sl]
            pc[l, :, PC_LNG + p] = inputs["rk_ln_g"][l][sl]
            pc[l, :, PC_LNB + p] = inputs["rk_ln_b"][l][sl]
    out["c_pc"] = pc
    w_in = np.asarray(inputs["w_in"], np.float32)
    out["c_wx"] = np.ascontiguousarray(np.concatenate(
        [w_in[:, :, 1920:1984], w_in[:, :, 2048:2112], w_in[:, :, 1984:2048], w_in[:, :, 2112:2176], w_in[:, :, 2176:2304]], axis=2))
    return out


def rev_ap(ap, n):
    a = ap.ap
    return bass.AP(ap.tensor, ap.offset + (n - 1) * a[-1][0], [list(a[0]), [-a[-1][0], n]])


def psk(b, c0, c1):
    return [f"ps{b}q{q}" for q in range(c0 // 128, (c1 + 127) // 128)]


def rwkv_stage(k, l):
    P, A, ps, IN = k.P, k.A, k.ps, k.IN
    m0 = A.mark()
    k.mask4 = A.alloc("mask4", (512,)); k.rmask = A.alloc("rmask", (256,))
    P.dma("act", k.mask4.ap, IN["c_mask4"].ap(), writes=[k.mask4.k()], semkey="c5")
    P.dma("act", k.rmask.ap, IN["c_rmask"].ap(), writes=[k.rmask.k()], semkey="c7")
    ybT = A.alloc("ybT", (3, S), BF16)
    P.op("pool", lambda e: e.memset(ybT.ap, 0.0), writes=[ybT.k(i) for i in range(NT)])
    xsp = k.xspill.ap().rearrange("(i p) d -> p i d", p=128)
    for i in range(NT):
        P.dma("sp" if i % 2 == 0 else "act", xsp[:, i, :], k.xres.ap[:, i, :], reads=[k.xres.k(i)], writes=[("xsp", i)], semkey=("xso", i % 4))
    evs = []
    for kk_ in [q for q in P.keys if isinstance(q, tuple) and q[0] == k.xres.uid]:
        st = P.keys.pop(kk_)
        evs.extend(st["w"]); evs.extend(st["r"].values())
    AX = Arena.__new__(Arena)
    AX.P, AX.t, AX.n, AX.top, AX.gen, AX.live = P, A.t, k.xres.hi, k.xres.lo, 100000 + 1000 * l, []
    AX.pending = [(k.xres.lo, k.xres.hi, evs)]
    import os as _os
    LVL = int(_os.environ.get("RWKV_SETUP", "9"))
    pc = A.alloc("pc", (NPC + 4,))
    P.dma("sp", pc.ap[:, 0:NPC], IN["c_pc"].ap()[l], writes=[pc.k()], semkey="pc")
    PCO = NPC
    P.op("dve", lambda e: e.tensor_scalar(pc.ap[:, PCO:PCO + 3], pc.ap[:, PC_KA:PC_KA + 3], -1.0, 1.0, op0=ALU.mult, op1=ALU.add), reads=[pc.k()], writes=[pc.k()])
    wl = [A.alloc(f"wl{d}", (384,)) for d in range(2)]
    for d in range(2):
        P.dma("sp", wl[d].ap[0:64, :], IN["rk_w2"].ap()[l][d], writes=[wl[d].k()], semkey=("wl", d))
        P.dma("act", wl[d].ap[64:128, :], IN["rk_a2"].ap()[l][d], writes=[wl[d].k()], semkey=("wl2", d))
    g2b = A.alloc("g2b", (384,), BF16)
    P.dma("pool", g2b.ap, IN["rk_g2"].ap()[l], writes=[g2b.k()], semkey="g2b")
    siggd = A.alloc("siggd", (S,), BF16)
    wdad = [(A if int(_os.environ.get("WDAD_MAIN", "0")) else AX).alloc(f"wdad{d}", (S,)) for d in range(2)]
    mA = A.mark()
    wx = load_w_bf16(k, "wx", IN["c_wx"].ap()[l], 8, 384, "wx")
    zx = A.alloc("zx", (S + 2,)); tmp = A.alloc("tmpx", (S,))
    P.op("pool", lambda e: e.memset(zx.ap[:, 0:1], 0.0), writes=[zx.k()])
    P.op("pool", lambda e: e.memset(zx.ap[:, S + 1:S + 2], 0.0), writes=[zx.k()])
    n = 0
    for d in range(2 if LVL >= 2 else 0):
        for tg in range(4):
            b = n % 2; n += 1
            for c in range(8):
                P.op("pe", lambda e, c=c, d=d, tg=tg, b=b: e.matmul(ps[b][:, :], wx.ap[:, c, d * 128:(d + 1) * 128], k.hT.ap[:, c, tg * 512:(tg + 1) * 512], start=(c == 0), stop=(c == 7)),
                     reads=[wx.k(c)] + [k.hT.k(4 * tg + q) for q in range(4)], writes=[f"ps{b}"])
            P.op("act", lambda e, tg=tg, b=b: e.copy(zx.ap[:, 1 + tg * 512:1 + (tg + 1) * 512], ps[b][:, :]), reads=[f"ps{b}"], writes=[zx.k()])
        if LVL < 3:
            continue
        if d == 0:
            cur, prv = zx.ap[:, 1:S + 1], zx.ap[:, 0:S]
        else:
            cur, prv = rev_ap(zx.ap[:, 1:S + 1], S), rev_ap(zx.ap[:, 2:S + 2], S)
        P.op("dve", lambda e, cur=cur, prv=prv: e.tensor_tensor(tmp.ap, prv, cur, op=ALU.subtract), reads=[zx.k()], writes=[tmp.k()])
        P.op("dve", lambda e, cur=cur, d=d: e.scalar_tensor_tensor(wdad[d].ap, tmp.ap, pc.ap[:, PC_MUX + d:PC_MUX + d + 1], cur, op0=ALU.mult, op1=ALU.add),
             reads=[tmp.k(), pc.k(), zx.k()], writes=[wdad[d].k()])
        P.op("act", lambda e, d=d: e.activation(wdad[d].ap[0:64, :], wdad[d].ap[0:64, :], AF.Tanh), reads=[wdad[d].k()], writes=[wdad[d].k()])
    for tg in range(4 if LVL >= 4 else 0):
        b = n % 2; n += 1
        for c in range(8):
            P.op("pe", lambda e, c=c, tg=tg, b=b: e.matmul(ps[b][:, :], wx.ap[:, c, 256:384], k.hT.ap[:, c, tg * 512:(tg + 1) * 512], start=(c == 0), stop=(c == 7)),
                 reads=[wx.k(c)] + [k.hT.k(4 * tg + q) for q in range(4)], writes=[f"ps{b}"])
        P.op("act", lambda e, tg=tg, b=b: e.activation(siggd.ap[:, tg * 512:(tg + 1) * 512], ps[b][:, :], AF.Sigmoid), reads=[f"ps{b}"], writes=[siggd.k()])
    A.release(mA)
    NB = ["XR", "XK", "XV", "T1", "LW", "AA", "KK", "SQ", "CUM"]
    NB16 = ["XRb", "XKb", "KKb", "AAb", "XVb", "BH", "KH"]
    SB = []
    for s_ in range(2):
        sb = {nm: A.alloc(f"{nm}{s_}", (BLK,)) for nm in NB}
        sb.update({nm: A.alloc(f"{nm}{s_}", (BLK,), BF16) for nm in NB16})
        sb["PCc"] = A.alloc(f"PCc{s_}", (BLK // 128,))
        sb["FT"] = A.alloc(f"FT{s_}", (512,))
        sb["AM"] = [A.alloc(f"AM{s_}{x}", (512,), BF16) for x in range(2)]
        sb["M"] = [[A.alloc(f"M{s_}{x}{q}", (128,), BF16) for q in range(2)] for x in range(2)]
        sb["MT"] = [[A.alloc(f"MT{s_}{x}{q}", (128,), BF16) for q in range(2)] for x in range(2)]
        sb["MM"] = [[A.alloc(f"MM{s_}{x}{q}", (256,), BF16) for q in range(2)] for x in range(2)]
        sb["Q"] = [A.alloc(f"Q{s_}{x}", (128,)) for x in range(2)]
        sb["Q16"] = [A.alloc(f"Qb{s_}{x}", (128,), BF16) for x in range(2)]
        sb["RHS"] = A.alloc(f"RHS{s_}", (128,), BF16)
        sb["SAz"] = [A.alloc(f"SAz{s_}{x}", (128,), BF16) for x in range(2)]
        sb["Vz"] = [A.alloc(f"Vz{s_}{x}", (128,), BF16) for x in range(2)]
        sb["BHt"] = A.alloc(f"BHt{s_}", (128,), BF16); sb["KHt"] = A.alloc(f"KHt{s_}", (128,), BF16)
        sb["T"] = A.alloc(f"Tst{s_}", (128,)); sb["T16"] = A.alloc(f"Tsb{s_}", (128,), BF16)
        sb["pb"] = 4 * s_
        SB.append(sb)
    import os as _os
    for p in range(int(_os.environ.get('RWKV_PAIRS', '3'))):
        mX = AX.mark()
        wp = AX.alloc("wp", (8, 3, 128), BF16)
        wv_ = IN["w_in"].ap()[l].rearrange("(c q) n -> q c n", q=128)
        for j in range(3):
            c0 = OFF_RKV + j * 384 + p * 128
            for c in range(8):
                P.dma("pool", wp.ap[:, c, j, :], wv_[:, c, c0:c0 + 128], writes=[wp.k(j)], semkey=("wp", j))
        zp = AX.alloc("zp", (3, S + 2))
        yacc = AX.alloc("yacc", (S,)); bonacc = AX.alloc("bonacc", (S,))
        P.op("pool", lambda e: e.memset(yacc.ap, 0.0), writes=[yacc.k(c) for c in range(NT)])
        P.op("pool", lambda e: e.memset(bonacc.ap, 0.0), writes=[bonacc.k(c) for c in range(NT)])
        for j in range(3):
            P.op("pool", lambda e, j=j: e.memset(zp.ap[:, j, 0:1], 0.0), writes=[zp.k(j)])
            P.op("pool", lambda e, j=j: e.memset(zp.ap[:, j, S + 1:S + 2], 0.0), writes=[zp.k(j)])
            for tg in range(4):
                b = n % 2; n += 1
                for c in range(8):
                    P.op("pe", lambda e, c=c, j=j, tg=tg, b=b: e.matmul(ps[b][:, :], wp.ap[:, c, j, :], k.hT.ap[:, c, tg * 512:(tg + 1) * 512], start=(c == 0), stop=(c == 7)),
                         reads=[wp.k(j)] + [k.hT.k(4 * tg + q) for q in range(4)], writes=[f"ps{b}"] + psk(b, 0, 512))
                P.op("act", lambda e, j=j, tg=tg, b=b: e.copy(zp.ap[:, j, 1 + tg * 512:1 + (tg + 1) * 512], ps[b][:, :]), reads=[f"ps{b}"] + psk(b, 0, 512), writes=[zp.k(j)])
        gens = [rwkv_stream(k, l, p, d, SB[d], pc, wl[d], wdad[d], zp, yacc, bonacc, PCO) for d in range(int(_os.environ.get("RWKV_NDIR", "2")))]
        while gens:
            for g in list(gens):
                try:
                    next(g)
                except StopIteration:
                    gens.remove(g)
        T1 = SB[0]["FT"]; T2 = SB[1]["FT"]
        for tg in range(4 if int(_os.environ.get("RWKV_FIN", "1")) else 0):
            cs = slice(tg * 512, (tg + 1) * 512)
            yk = [yacc.k(c) for c in range(4 * tg, 4 * tg + 4)]
            P.op("pe", lambda e, cs=cs: e.matmul(ps[0][:, :], k.onesblk64.ap, yacc.ap[:, cs], start=True, stop=True), reads=[k.onesblk64.k()] + yk, writes=["ps0"] + psk(0, 0, 512))
            P.op("dve", lambda e, cs=cs: e.tensor_tensor(yacc.ap[:, cs], yacc.ap[:, cs], ps[0][:, :], op=ALU.subtract), reads=["ps0"] + psk(0, 0, 512) + yk, writes=yk)
            P.op("act", lambda e, cs=cs: e.activation(T1.ap, yacc.ap[:, cs], AF.Square), reads=yk, writes=[T1.k()])
            P.op("pe", lambda e: e.matmul(ps[1][:, :], k.onesblk64.ap, T1.ap, start=True, stop=True), reads=[k.onesblk64.k(), T1.k()], writes=["ps1"] + psk(1, 0, 512))
            P.op("act", lambda e: e.activation(T2.ap, ps[1][:, :], AF.Sqrt, bias=k.epsgn.ap[:, 0:1]), reads=["ps1", k.epsgn.k()] + psk(1, 0, 512), writes=[T2.k()])
            P.op("dve", lambda e: e.reciprocal(T2.ap, T2.ap), reads=[T2.k()], writes=[T2.k()])
            P.op("dve", lambda e, cs=cs: e.tensor_tensor(yacc.ap[:, cs], yacc.ap[:, cs], T2.ap, op=ALU.mult), reads=yk + [T2.k()], writes=yk)
            P.op("dve", lambda e, cs=cs, p=p: e.tensor_scalar(yacc.ap[:, cs], yacc.ap[:, cs], pc.ap[:, PC_LNG + p:PC_LNG + p + 1], pc.ap[:, PC_LNB + p:PC_LNB + p + 1], op0=ALU.mult, op1=ALU.add),
                 reads=yk + [pc.k()], writes=yk)
            P.op("dve", lambda e, cs=cs: e.tensor_tensor(yacc.ap[:, cs], yacc.ap[:, cs], bonacc.ap[:, cs], op=ALU.add), reads=yk + [bonacc.k(c) for c in range(4 * tg, 4 * tg + 4)], writes=yk)
            P.op("pe", lambda e, cs=cs, p=p: e.matmul(ps[2][:, :], g2b.ap[:, p * 128:(p + 1) * 128], siggd.ap[:, cs], start=True, stop=True), reads=[g2b.k(), siggd.k()], writes=["ps2"] + psk(2, 0, 512))
            P.op("dve", lambda e, cs=cs, p=p: e.tensor_tensor(ybT.ap[:, p, cs], yacc.ap[:, cs], ps[2][:, :], op=ALU.mult), reads=yk + ["ps2"] + psk(2, 0, 512), writes=[ybT.k(c) for c in range(4 * tg, 4 * tg + 4)])
        AX.release(mX)
    AX.release((k.xres.lo, 0))
    evs = []
    for (_, _, e_) in AX.pending:
        evs.extend(e_)
    P.inherit[k.xres.uid] = evs
    for i in range(NT):
        P.dma("sp" if i % 2 == 0 else "act", k.xres.ap[:, i, :], xsp[:, i, :], reads=[("xsp", i)], writes=[k.xres.k(i)], semkey=("xsi", i % 4))
    if k.dbg and "yb" in k.dbg:
        t = k.nc.dram_tensor("dbg_yb", [384, S], BF16, kind="ExternalOutput")
        k.final_events.append(P.dma("sp", t.ap().rearrange("(c p) s -> p c s", p=128), ybT.ap, reads=[ybT.k(i) for i in range(NT)], semkey="dbg"))
    if LVL >= 5:
        outproj_partial(k, l, ybT, 3, 256, "b")
    A.release(m0)


def rwkv_stream(k, l, p, d, sb, pc, wl, wdad, zp, yacc, bonacc, PCO):
    P, ps = k.P, k.ps
    pb = sb["pb"]
    B0, B1, B2, B3 = pb, pb + 1, pb + 2, pb + 3
    XR, XK, XV, T1, LW, AA, KK, SQ, CUM, BH, KH, PCc = (sb[n_] for n_ in ["XR", "XK", "XV", "T1", "LW", "AA", "KK", "SQ", "CUM", "BH", "KH", "PCc"])
    XRb, XKb, KKb, AAb, XVb = (sb[n_] for n_ in ["XRb", "XKb", "KKb", "AAb", "XVb"])
    T = sb["T"]; T16 = sb["T16"]
    P.op("pool", lambda e: e.memset(T16.ap, 0.0), writes=[T16.k()])
    col = lambda c: pc.ap[:, c:c + 1]
    P.op("pool", lambda e: e.memset(T.ap, 0.0), writes=[T.k()])
    for x in range(2):
        P.op("pool", lambda e, x=x: e.memset(sb["SAz"][x].ap, 0.0), writes=[sb["SAz"][x].k()])
        P.op("pool", lambda e, x=x: e.memset(sb["Vz"][x].ap, 0.0), writes=[sb["Vz"][x].k()])
    import os as _os
    NBLK = int(_os.environ.get("RWKV_NBLK", str(S // BLK))); PH = int(_os.environ.get("RWKV_PHASE", "9"))
    def _blk(bi):
        t0 = bi * BLK
        if d == 0:
            cur = lambda j: zp.ap[:, j, 1 + t0:1 + t0 + BLK]
            prv = lambda j: zp.ap[:, j, t0:t0 + BLK]
            nat = lambda buf: buf.ap[:, t0:t0 + BLK]
            nchunks = list(range(t0 // 128, (t0 + BLK) // 128))
        else:
            a_ = S - t0 - BLK
            cur = lambda j: rev_ap(zp.ap[:, j, 1 + a_:1 + a_ + BLK], BLK)
            prv = lambda j: rev_ap(zp.ap[:, j, 2 + a_:2 + a_ + BLK], BLK)
            nat = lambda buf: rev_ap(buf.ap[:, a_:a_ + BLK], BLK)
            nchunks = list(range(a_ // 128, (a_ + BLK) // 128))
        scols = slice(t0, t0 + BLK)
        P0 = int(_os.environ.get("RWKV_P0", "9"))
        for j, X in enumerate((XR, XK, XV)):
            if P0 < 2:
                break
            P.op("dve", lambda e, j=j, prv=prv, cur=cur: e.tensor_tensor(T1.ap, prv(j), cur(j), op=ALU.subtract), reads=[zp.k(j)], writes=[T1.k()])
            P.op("dve", lambda e, j=j, X=X, cur=cur: e.scalar_tensor_tensor(X.ap, T1.ap, col(PC_MU + d * 9 + j * 3 + p), cur(j), op0=ALU.mult, op1=ALU.add),
                 reads=[T1.k(), zp.k(j), pc.k()], writes=[X.k()])
        if P0 >= 3:
            P.op("pe", lambda e, scols=scols: e.matmul(ps[B3][:, 0:BLK], wl.ap[0:64, p * 128:(p + 1) * 128], wdad.ap[0:64, scols], start=True, stop=True),
                 reads=[wl.k(), wdad.k()], writes=psk(B3, 0, BLK), serial=True)
        if P0 >= 4:
            P.op("pe", lambda e, scols=scols: e.matmul(ps[B3][:, 256:256 + BLK], wl.ap[64:128, p * 128:(p + 1) * 128], wdad.ap[64:128, scols], start=True, stop=True),
                 reads=[wl.k(), wdad.k()], writes=psk(B3, 256, 256 + BLK), serial=True)
        if P0 >= 5:
            P.op("act", lambda e: e.activation(LW.ap, ps[B3][:, 0:BLK], AF.Sigmoid, bias=col(PC_W0 + d * 3 + p)), reads=psk(B3, 0, BLK) + [pc.k()], writes=[LW.k()])
            P.op("act", lambda e: e.activation(AA.ap, ps[B3][:, 256:256 + BLK], AF.Sigmoid, bias=col(PC_A0 + d * 3 + p)), reads=psk(B3, 256, 256 + BLK) + [pc.k()], writes=[AA.k()])
        if P0 >= 6:
            P.op("dve", lambda e: e.tensor_scalar(LW.ap, LW.ap, -0.6065306597126334, None, op0=ALU.mult), reads=[LW.k()], writes=[LW.k()])
        yield
        if PH <= 1:
            return
        P.op("dve", lambda e: e.tensor_scalar(KK.ap, XK.ap, col(PC_KK + p), None, op0=ALU.mult), reads=[XK.k(), pc.k()], writes=[KK.k()])
        P.op("act", lambda e: e.activation(SQ.ap, KK.ap, AF.Square), reads=[KK.k()], writes=[SQ.k()])
        P.op("pe", lambda e: e.matmul(ps[B2][:, 0:BLK], k.onesblk.ap, SQ.ap, start=True, stop=True), reads=[k.onesblk.k(), SQ.k()], writes=psk(B2, 0, BLK))
        P.op("act", lambda e: e.activation(SQ.ap, ps[B2][:, 0:BLK], AF.Sqrt), reads=psk(B2, 0, BLK), writes=[SQ.k()])
        P.op("dve", lambda e: e.tensor_scalar(SQ.ap, SQ.ap, 1e-12, None, op0=ALU.max), reads=[SQ.k()], writes=[SQ.k()])
        P.op("dve", lambda e: e.reciprocal(SQ.ap, SQ.ap), reads=[SQ.k()], writes=[SQ.k()])
        P.op("dve", lambda e: e.tensor_tensor(KK.ap, KK.ap, SQ.ap, op=ALU.mult), reads=[KK.k(), SQ.k()], writes=[KK.k()])
        P.op("dve", lambda e: e.tensor_scalar(T1.ap, AA.ap, col(PC_KA + p), col(PCO + p), op0=ALU.mult, op1=ALU.add), reads=[AA.k(), pc.k()], writes=[T1.k()])
        P.op("dve", lambda e: e.tensor_tensor(XK.ap, XK.ap, T1.ap, op=ALU.mult), reads=[XK.k(), T1.k()], writes=[XK.k()])
        P.op("dve", lambda e: e.scalar_tensor_tensor(T1.ap, XR.ap, col(PC_RK + p), XK.ap, op0=ALU.mult, op1=ALU.mult), reads=[XR.k(), XK.k(), pc.k()], writes=[T1.k()])
        P.op("pe", lambda e: e.matmul(ps[B2][:, 256:256 + BLK], k.onesblk.ap, T1.ap, start=True, stop=True), reads=[k.onesblk.k(), T1.k()], writes=psk(B2, 256, 256 + BLK))
        P.op("dve", lambda e: e.tensor_tensor(T1.ap, ps[B2][:, 256:256 + BLK], XV.ap, op=ALU.mult), reads=psk(B2, 256, 256 + BLK) + [XV.k()], writes=[T1.k()])
        bk = [bonacc.k(c) for c in nchunks]
        P.op("dve", lambda e: e.tensor_tensor(nat(bonacc), nat(bonacc), T1.ap, op=ALU.add), reads=bk + [T1.k()], writes=bk)
        P.op("dve", lambda e: e.tensor_tensor(AA.ap, AA.ap, KK.ap, op=ALU.mult), reads=[AA.k(), KK.k()], writes=[AA.k()])
        yield
        if PH <= 2:
            return
        P.op("dve", lambda e: e.tensor_tensor_scan(CUM.ap, k.rmask.ap[:, 0:BLK], LW.ap, 0.0, op0=ALU.mult, op1=ALU.add), reads=[k.rmask.k(), LW.k()], writes=[CUM.k()])
        P.op("dve", lambda e: e.tensor_tensor(LW.ap, CUM.ap, LW.ap, op=ALU.subtract), reads=[CUM.k(), LW.k()], writes=[LW.k()])
        P.op("act", lambda e: e.activation(T1.ap, CUM.ap, AF.Exp), reads=[CUM.k()], writes=[T1.k()])
        P.op("dve", lambda e: e.tensor_tensor(XR.ap, XR.ap, T1.ap, op=ALU.mult), reads=[XR.k(), T1.k()], writes=[XR.k()])
        P.op("act", lambda e: e.activation(SQ.ap, CUM.ap, AF.Exp, scale=-1.0), reads=[CUM.k()], writes=[SQ.k()])
        P.op("dve", lambda e: e.tensor_tensor(AA.ap, AA.ap, SQ.ap, op=ALU.mult), reads=[AA.k(), SQ.k()], writes=[AA.k()])
        P.op("dve", lambda e: e.tensor_tensor(XK.ap, XK.ap, SQ.ap, op=ALU.mult), reads=[XK.k(), SQ.k()], writes=[XK.k()])
        P.op("act", lambda e: e.activation(T1.ap, LW.ap, AF.Exp), reads=[LW.k()], writes=[T1.k()])
        P.op("dve", lambda e: e.scalar_tensor_tensor(KK.ap, KK.ap, -1.0, T1.ap, op0=ALU.mult, op1=ALU.mult), reads=[KK.k(), T1.k()], writes=[KK.k()])
        for src_, dst_ in ((XR, XRb), (XK, XKb), (KK, KKb), (AA, AAb), (XV, XVb)):
            P.op("act", lambda e, src_=src_, dst_=dst_: e.copy(dst_.ap, src_.ap), reads=[src_.k()], writes=[dst_.k()])
        P.op("act", lambda e: e.activation(PCc.ap, CUM.ap.rearrange("p (c t) -> p c t", t=128)[:, :, 127], AF.Exp), reads=[CUM.k()], writes=[PCc.k()])
        for ch in range(BLK // 128):
            cs = slice(ch * 128, (ch + 1) * 128)
            P.op("dve", lambda e, cs=cs, ch=ch: e.tensor_scalar(BH.ap[:, cs], AA.ap[:, cs], PCc.ap[:, ch:ch + 1], None, op0=ALU.mult), reads=[AA.k(), PCc.k()], writes=[BH.k()])
            P.op("dve", lambda e, cs=cs, ch=ch: e.tensor_scalar(KH.ap[:, cs], XK.ap[:, cs], PCc.ap[:, ch:ch + 1], None, op0=ALU.mult), reads=[XK.k(), PCc.k()], writes=[KH.k()])
        yield
        if PH <= 3:
            return
        def _chunk(ch):
            cs = slice(ch * 128, (ch + 1) * 128)
            AM, Mb, MTb, Q, RHS, SAz, Vz, BHt, KHt = sb["AM"], sb["M"], sb["MT"], sb["Q"], sb["RHS"], sb["SAz"], sb["Vz"], sb["BHt"], sb["KHt"]
            for x in range(2):
                hs = slice(64 * x, 64 * x + 64)
                bx = B0 + x
                for q, (lh, rh) in enumerate(((AAb, KKb), (AAb, XRb), (XKb, KKb), (XKb, XRb))):
                    P.op("pe", lambda e, lh=lh, rh=rh, q=q, hs=hs, bx=bx: e.matmul(ps[bx][:, q * 128:(q + 1) * 128], lh.ap[hs, cs], rh.ap[hs, cs], start=True, stop=True),
                         reads=[lh.k(), rh.k()], writes=psk(bx, q * 128, (q + 1) * 128), serial=True)
                P.op("pe", lambda e, hs=hs, x=x: e.matmul(ps[B2][:, x * 128:(x + 1) * 128], KKb.ap[hs, cs], AAb.ap[hs, cs], start=True, stop=True),
                     reads=[KKb.k(), AAb.k()], writes=psk(B2, x * 128, (x + 1) * 128), serial=True)
                P.op("dve", lambda e, x=x, bx=bx: e.tensor_tensor(AM[x].ap, ps[bx][:, :], k.mask4.ap, op=ALU.mult), reads=psk(bx, 0, 512) + [k.mask4.k()], writes=[AM[x].k()])
                P.op("dve", lambda e, x=x: e.tensor_tensor(MTb[x][0].ap, ps[B2][:, x * 128:(x + 1) * 128], k.trils.ap, op=ALU.mult), reads=psk(B2, x * 128, (x + 1) * 128) + [k.trils.k()], writes=[MTb[x][0].k()])
                P.op("dve", lambda e, x=x: e.tensor_tensor(Q[x].ap, AM[x].ap[:, 0:128], k.ident.ap, op=ALU.add), reads=[AM[x].k(), k.ident.k()], writes=[Q[x].k()])
                P.op("act", lambda e, x=x: e.copy(sb["Q16"][x].ap, Q[x].ap), reads=[Q[x].k()], writes=[sb["Q16"][x].k()])
            yield
            if PH <= 4:
                return
            pbt = ps[B3][:, 0:192].bitcast(BF16)
            for q, src in enumerate((XVb, BH, KH)):
                P.op("pe", lambda e, q=q, src=src: e.transpose(pbt[:, q * 128:(q + 1) * 128], src.ap[:, cs], k.ident16.ap), reads=[src.k(), k.ident16.k()], writes=psk(B3, 0, 192))
            P.op("act", lambda e: e.copy(Vz[0].ap[:, 0:64], pbt[:, 0:64]), reads=psk(B3, 0, 192), writes=[Vz[0].k()])
            P.op("act", lambda e: e.copy(Vz[1].ap[:, 64:128], pbt[:, 64:128]), reads=psk(B3, 0, 192), writes=[Vz[1].k()])
            P.op("dve", lambda e: e.tensor_copy(BHt.ap, pbt[:, 128:256]), reads=psk(B3, 0, 192), writes=[BHt.k()])
            P.op("dve", lambda e: e.tensor_copy(KHt.ap, pbt[:, 256:384]), reads=psk(B3, 0, 192), writes=[KHt.k()])
            MM = sb["MM"]
            for lev in range(6):
                po = lev % 2
                for x in range(2):
                    bx = B0 + x
                    if lev == 0:
                        Mi = (AM[x].ap[:, 0:128], AM[x].k()); MTi = (MTb[x][0].ap, MTb[x][0].k())
                    else:
                        Mi = (MM[x][1 - po].ap[:, 0:128], MM[x][1 - po].k()); MTi = (MM[x][1 - po].ap[:, 128:256], MM[x][1 - po].k())
                    if lev < 5:
                        P.op("pe", lambda e, bx=bx, Mi=Mi, MTi=MTi: e.matmul(ps[bx][:, 0:128], MTi[0], Mi[0], start=True, stop=True), reads=[Mi[1], MTi[1]], writes=psk(bx, 0, 128))
                    P.op("pe", lambda e, bx=bx, Mi=Mi, MTi=MTi: e.matmul(ps[bx][:, 128:256], Mi[0], MTi[0], start=True, stop=True), reads=[Mi[1], MTi[1]], writes=psk(bx, 128, 256))
                    lo_ = 0 if lev < 5 else 128
                    P.op("act", lambda e, bx=bx, x=x, po=po, lo_=lo_: e.copy(MM[x][po].ap[:, lo_:256], ps[bx][:, lo_:256]), reads=psk(bx, 0, 256), writes=[MM[x][po].k()])
                for x in range(2):
                    P.op("pe", lambda e, x=x, po=po: e.matmul(ps[B2][:, x * 128:(x + 1) * 128], MM[x][po].ap[:, 128:256], sb["Q16"][x].ap, start=True, stop=True),
                         reads=[MM[x][po].k(), sb["Q16"][x].k()], writes=psk(B2, 0, 256))
                    P.op("dve", lambda e, x=x: e.tensor_tensor(sb["Q16"][x].ap, Q[x].ap, ps[B2][:, x * 128:(x + 1) * 128], op=ALU.add), reads=psk(B2, 0, 256) + [Q[x].k()], writes=[sb["Q16"][x].k()])
                    if lev < 5:
                        P.op("dve", lambda e, x=x: e.tensor_tensor(Q[x].ap, Q[x].ap, ps[B2][:, x * 128:(x + 1) * 128], op=ALU.add), reads=psk(B2, 0, 256) + [Q[x].k()], writes=[Q[x].k()])
                yield
            for x in range(2):
                hs = slice(64 * x, 64 * x + 64)
                P.op("pe", lambda e, hs=hs: e.matmul(ps[B2][:, 256 + hs.start:256 + hs.stop], KKb.ap[hs, cs], T16.ap[hs, hs], start=True, stop=False), reads=[KKb.k(), T16.k()], writes=psk(B2, 256, 384), serial=True)
                P.op("pe", lambda e, hs=hs, x=x: e.matmul(ps[B2][:, 256 + hs.start:256 + hs.stop], AM[x].ap[:, 256:384], Vz[x].ap[:, hs], start=False, stop=True), reads=[AM[x].k(), Vz[x].k()], writes=psk(B2, 256, 384))
            P.op("act", lambda e: e.copy(RHS.ap, ps[B2][:, 256:384]), reads=psk(B2, 256, 384), writes=[RHS.k()])
            for x in range(2):
                hs = slice(64 * x, 64 * x + 64)
                P.op("pe", lambda e, hs=hs, x=x: e.matmul(ps[B2][:, 384 + hs.start:384 + hs.stop], sb["Q16"][x].ap, RHS.ap[:, hs], start=True, stop=True), reads=[sb["Q16"][x].k(), RHS.k()], writes=psk(B2, 384, 512))
                P.op("act" if x == 0 else "dve", (lambda e, hs=hs, x=x: e.copy(SAz[x].ap[:, hs], ps[B2][:, 384 + hs.start:384 + hs.stop])) if x == 0 else
                     (lambda e, hs=hs, x=x: e.tensor_copy(SAz[x].ap[:, hs], ps[B2][:, 384 + hs.start:384 + hs.stop])), reads=psk(B2, 384, 512), writes=[SAz[x].k()])
            yield
            if PH <= 6:
                return
            ops_ = []
            for x in range(2):
                hs = slice(64 * x, 64 * x + 64)
                ops_.append((T16.ap[hs, :], XRb.ap[hs, cs], [T16.k(), XRb.k()]))
                ops_.append((SAz[x].ap, AM[x].ap[:, 128:256], [SAz[x].k(), AM[x].k()]))
                ops_.append((Vz[x].ap, AM[x].ap[:, 384:512], [Vz[x].k(), AM[x].k()]))
            for q, (lh, rh, rd) in enumerate(ops_):
                P.op("pe", lambda e, lh=lh, rh=rh, q=q: e.matmul(ps[B3][:, 384:512], lh, rh, start=(q == 0), stop=(q == len(ops_) - 1)), reads=rd, writes=psk(B3, 384, 512), serial=(q % 3 == 0))
            cn = nchunks[ch] if d == 0 else nchunks[len(nchunks) - 1 - ch]
            if d == 0:
                ydst = yacc.ap[:, cn * 128:(cn + 1) * 128]
            else:
                ydst = rev_ap(yacc.ap[:, cn * 128:(cn + 1) * 128], 128)
            P.op("dve", lambda e, ydst=ydst: e.tensor_tensor(ydst, ydst, ps[B3][:, 384:512], op=ALU.add), reads=psk(B3, 384, 512) + [yacc.k(cn)], writes=[yacc.k(cn)])
            for x in range(2):
                hs = slice(64 * x, 64 * x + 64)
                P.op("pe", lambda e, hs=hs, x=x: e.matmul(ps[B2][:, hs], BHt.ap, SAz[x].ap[:, hs], start=True, stop=False), reads=[BHt.k(), SAz[x].k()], writes=psk(B2, 0, 128))
                P.op("pe", lambda e, hs=hs, x=x: e.matmul(ps[B2][:, hs], KHt.ap, Vz[x].ap[:, hs], start=False, stop=True), reads=[KHt.k(), Vz[x].k()], writes=psk(B2, 0, 128))
            for x in range(2):
                hs = slice(64 * x, 64 * x + 64)
                P.op("dve", lambda e, hs=hs, ch=ch: e.scalar_tensor_tensor(T.ap[hs, hs], T.ap[hs, hs], PCc.ap[hs, ch:ch + 1], ps[B2][hs, hs], op0=ALU.mult, op1=ALU.add),
                     reads=[T.k(), PCc.k()] + psk(B2, 0, 128), writes=[T.k()])
            P.op("act", lambda e: e.copy(T16.ap, T.ap), reads=[T.k()], writes=[T16.k()])
            yield
            if PH <= 7:
                return
        for ch in range(BLK // 128):
            yield from _chunk(ch)

    for bi in range(NBLK):
        yield from _blk(bi)


FB = 256
NFB = DFF // FB
NFC = DFF // 128
W2G = 2


def moe_stage(k, l):
    P, A, ps, IN = k.P, k.A, k.ps, k.IN
    m0 = A.mark()
    k.ohb = A.alloc("ohb", (2048,))
    P.dma("act", k.ohb.ap[0:16, :], IN["c_ohb"].ap(), writes=[k.ohb.k()], semkey="c4")
    x2b = A.alloc("x2b", (NT, D), BF16)
    aff = A.alloc("aff", (NT, NE))
    pm = A.alloc("pm", (NT, NE))
    pmT = A.alloc("pmT", (S,))
    m1 = A.mark()
    gb = A.alloc("gb2", (D,)); junk = A.alloc("junk2", (D,)); ss = A.alloc("ss2", (NT,)); rstd = A.alloc("rstd2", (NT,))
    x2f = [A.alloc(f"x2f{i}", (D,)) for i in range(2)]
    x2T = [A.alloc(f"x2T{i}", (8, 128)) for i in range(2)]
    rsb = A.alloc("rsb", (8, NE)); sm = A.alloc("smx", (NT, 4))
    affT = A.alloc("affT", (S,))
    P.dma("sp", gb.ap, IN["ln2_g"].ap()[l].partition_broadcast(128), writes=[gb.k()], semkey="gb")
    P.dma("act", rsb.ap, IN["router"].ap()[l].rearrange("(c p) e -> p c e", p=128), writes=[rsb.k()], semkey="rsb")
    P.op("pool", lambda e: e.memset(ss.ap, 0.0), writes=[ss.k(i) for i in range(NT)])
    P.op("pool", lambda e: e.memset(sm.ap, 0.0), writes=[sm.k()])
    for i in range(NT):
        P.op("act", lambda e, i=i: e.activation(junk.ap, k.xres.ap[:, i, :], AF.Square, accum_out=ss.ap[:, i:i + 1]),
             reads=[k.xres.k(i)], writes=[junk.k(), ss.k(i)])
    P.op("act", lambda e: e.activation(rstd.ap, ss.ap, AF.Sqrt, scale=1.0 / D, bias=k.eps6.ap[:, 0:1]),
         reads=[ss.k(i) for i in range(NT)] + [k.eps6.k()], writes=[rstd.k()])
    P.op("dve", lambda e: e.reciprocal(rstd.ap, rstd.ap), reads=[rstd.k()], writes=[rstd.k()])
    for i in range(NT):
        xf = x2f[i % 2]; xt = x2T[i % 2]
        P.op("dve", lambda e, i=i, xf=xf: e.scalar_tensor_tensor(xf.ap, k.xres.ap[:, i, :], rstd.ap[:, i:i + 1], gb.ap, op0=ALU.mult, op1=ALU.mult),
             reads=[k.xres.k(i), rstd.k(), gb.k()], writes=[xf.k()])
        P.op("act", lambda e, i=i, xf=xf: e.copy(x2b.ap[:, i, :], xf.ap), reads=[xf.k()], writes=[x2b.k(i)])
        for hb_ in range(2):
            b = 2 * (i % 2) + hb_
            for c in range(4):
                cc = hb_ * 4 + c
                P.op("pe", lambda e, b=b, c=c, cc=cc, xf=xf: e.transpose(ps[b][:, c * 128:(c + 1) * 128], xf.ap[:, cc * 128:(cc + 1) * 128], k.ident.ap),
                     reads=[xf.k(), k.ident.k()], writes=[f"ps{b}"])
            P.op("act" if hb_ == 0 else "dve",
                 (lambda e, b=b, hb_=hb_, xt=xt: e.copy(xt.ap[:, hb_ * 4:hb_ * 4 + 4, :], ps[b][:, :].rearrange("p (c t) -> p c t", c=4))) if hb_ == 0 else
                 (lambda e, b=b, hb_=hb_, xt=xt: e.tensor_copy(xt.ap[:, hb_ * 4:hb_ * 4 + 4, :], ps[b][:, :].rearrange("p (c t) -> p c t", c=4))),
                 reads=[f"ps{b}"], writes=[xt.k(hb_)])
        lb = 4 + i % 2
        for c in range(8):
            P.op("pe", lambda e, c=c, lb=lb, xt=xt: e.matmul(ps[lb][:, 0:NE], xt.ap[:, c, :], rsb.ap[:, c, :], start=(c == 0), stop=(c == 7)),
                 reads=[xt.k(0), xt.k(1), rsb.k()], writes=[f"ps{lb}"])
        P.op("dve", lambda e, i=i, lb=lb: e.tensor_reduce(sm.ap[:, i, 0:1], ps[lb][:, 0:NE], axis=AX.X, op=ALU.max), reads=[f"ps{lb}"], writes=[sm.k()])
        P.op("dve", lambda e, i=i: e.tensor_scalar(sm.ap[:, i, 0:1], sm.ap[:, i, 0:1], -1.0, None, op0=ALU.mult), reads=[sm.k()], writes=[sm.k()])
        P.op("act", lambda e, i=i, lb=lb: e.activation(aff.ap[:, i, :], ps[lb][:, 0:NE], AF.Exp, bias=sm.ap[:, i, 0:1], accum_out=sm.ap[:, i, 1:2]),
             reads=[f"ps{lb}", sm.k()], writes=[aff.k(), sm.k()])
        P.op("dve", lambda e, i=i: e.reciprocal(sm.ap[:, i, 2:3], sm.ap[:, i, 1:2]), reads=[sm.k()], writes=[sm.k()])
        P.op("dve", lambda e, i=i: e.tensor_scalar(aff.ap[:, i, :], aff.ap[:, i, :], sm.ap[:, i, 2:3], None, op0=ALU.mult), reads=[aff.k(), sm.k()], writes=[aff.k()])
        tb = 6 + (i // 4) % 2
        P.op("pe", lambda e, i=i, tb=tb: e.transpose(ps[tb][0:NE, (i % 4) * 128:(i % 4 + 1) * 128], aff.ap[:, i, :], k.ident.ap), reads=[aff.k(), k.ident.k()], writes=[f"ps{tb}"])
        if i % 4 == 3:
            P.op("act", lambda e, i=i, tb=tb: e.copy(affT.ap[0:NE, (i - 3) * 128:(i + 1) * 128], ps[tb][0:NE, :]), reads=[f"ps{tb}"], writes=[affT.k()])
    bs = A.alloc("bs", (8,)); bjunk = A.alloc("bjunk", (S,))
    LO, HI, MID, CNT, GE, D1 = (bs.ap[0:NE, j:j + 1] for j in range(6))
    P.op("pool", lambda e: e.memset(bs.ap, 0.0), writes=[bs.k()])
    P.op("pool", lambda e: e.memset(bs.ap[:, 1:2], 1.0), writes=[bs.k()])
    for it in range(30):
        P.op("dve", lambda e: e.tensor_tensor(MID, LO, HI, op=ALU.add), reads=[bs.k()], writes=[bs.k()])
        P.op("dve", lambda e: e.tensor_scalar(MID, MID, 0.5, None, op0=ALU.mult), reads=[bs.k()], writes=[bs.k()])
        P.op("dve", lambda e: e.tensor_scalar(bjunk.ap[0:NE, :], affT.ap[0:NE, :], MID, None, op0=ALU.is_gt, op1=ALU.add, accum_out=CNT),
             reads=[bs.k(), affT.k()], writes=[bs.k(), bjunk.k()])
        P.op("dve", lambda e: e.tensor_scalar(GE, CNT, float(CAP) - 0.5, None, op0=ALU.is_gt), reads=[bs.k()], writes=[bs.k()])
        P.op("dve", lambda e: e.tensor_tensor(D1, MID, LO, op=ALU.subtract), reads=[bs.k()], writes=[bs.k()])
        P.op("dve", lambda e: e.scalar_tensor_tensor(LO, D1, GE, LO, op0=ALU.mult, op1=ALU.add), reads=[bs.k()], writes=[bs.k()])
        P.op("dve", lambda e: e.tensor_tensor(D1, HI, MID, op=ALU.subtract), reads=[bs.k()], writes=[bs.k()])
        P.op("dve", lambda e: e.scalar_tensor_tensor(HI, D1, GE, MID, op0=ALU.mult, op1=ALU.add), reads=[bs.k()], writes=[bs.k()])
    dgt = A.alloc("dgt", (NE,)); thrb = A.alloc("thrb", (NE,))
    P.op("dve", lambda e: e.tensor_scalar(dgt.ap[0:NE, :], k.ident.ap[0:NE, 0:NE], LO, None, op0=ALU.mult), reads=[bs.k(), k.ident.k()], writes=[dgt.k()])
    P.op("pe", lambda e: e.matmul(ps[0][:, 0:NE], k.ones32.ap[0:NE, :], dgt.ap[0:NE, :], start=True, stop=True), reads=[k.ones32.k(), dgt.k()], writes=["ps0"])
    P.op("act", lambda e: e.copy(thrb.ap, ps[0][:, 0:NE]), reads=["ps0"], writes=[thrb.k()])
    mk16 = A.alloc("mk16", (NT, NE), BF16); mk32 = A.alloc("mk32", (NT, NE)); base = A.alloc("basec", (NE,))
    P.op("pool", lambda e: e.memset(base.ap, 0.0), writes=[base.k()])
    for i in range(NT):
        P.op("dve", lambda e, i=i: e.tensor_tensor(mk32.ap[:, i, :], aff.ap[:, i, :], thrb.ap, op=ALU.is_gt), reads=[aff.k(), thrb.k()], writes=[mk32.k(i)])
        P.op("act", lambda e, i=i: e.copy(mk16.ap[:, i, :], mk32.ap[:, i, :]), reads=[mk32.k(i)], writes=[mk16.k(i)])
        b = i % 2
        P.op("pe", lambda e, i=i, b=b: e.matmul(ps[b][:, 0:NE], k.trius16.ap, mk16.ap[:, i, :], start=True, stop=True), reads=[k.trius16.k(), mk16.k(i)], writes=[f"ps{b}"])
        P.op("pe", lambda e, i=i, b=b: e.matmul(ps[b][:, 128:128 + NE], k.ones16.ap, mk16.ap[:, i, :], start=True, stop=True), reads=[k.ones16.k(), mk16.k(i)], writes=[f"ps{b}"])
        P.op("dve", lambda e, i=i, b=b: e.scalar_tensor_tensor(pm.ap[:, i, :], ps[b][:, 0:NE], 1.0, base.ap, op0=ALU.add, op1=ALU.add), reads=[f"ps{b}", base.k()], writes=[pm.k(i)])
        P.op("dve", lambda e, i=i: e.tensor_tensor(pm.ap[:, i, :], pm.ap[:, i, :], mk32.ap[:, i, :], op=ALU.mult), reads=[pm.k(i), mk32.k(i)], writes=[pm.k(i)])
        P.op("dve", lambda e, i=i: e.tensor_scalar(pm.ap[:, i, :], pm.ap[:, i, :], -1.0, None, op0=ALU.add), reads=[pm.k(i)], writes=[pm.k(i)])
        P.op("dve", lambda e, b=b: e.tensor_tensor(base.ap, base.ap, ps[b][:, 128:128 + NE], op=ALU.add), reads=[f"ps{b}", base.k()], writes=[base.k()])
        tb = 6 + (i // 4) % 2
        P.op("pe", lambda e, i=i, tb=tb: e.transpose(ps[tb][0:NE, (i % 4) * 128:(i % 4 + 1) * 128], pm.ap[:, i, :], k.ident.ap), reads=[pm.k(i), k.ident.k()], writes=[f"ps{tb}"])
        if i % 4 == 3:
            P.op("act", lambda e, i=i, tb=tb: e.copy(pmT.ap[0:NE, (i - 3) * 128:(i + 1) * 128], ps[tb][0:NE, :]), reads=[f"ps{tb}"], writes=[pmT.k()])
    A.release(m1)
    SelE = A.alloc("SelE", (NT, CAP), BF16); SelT = A.alloc("SelT", (2, S), BF16)
    xgT = A.alloc("xgT", (8, CAP), BF16); actT = A.alloc("actT", (NFC, CAP), BF16)
    s1 = [A.alloc(f"s1_{i}", (CAP,)) for i in range(2)]
    ysb = A.alloc("ysb", (2, D), BF16)
    w1b = [A.alloc(f"w1b{i}", (8, FB), BF16) for i in range(2)]
    w3b = [A.alloc(f"w3b{i}", (8, FB), BF16) for i in range(2)]
    w2b = [A.alloc(f"w2b{i}", (W2G, D), BF16) for i in range(2)]
    wn = 0
    for ex in range(NE):
        w1v = IN["e_w1"].ap()[l][ex].rearrange("(c p) f -> p c f", p=128)
        w3v = IN["e_w3"].ap()[l][ex].rearrange("(c p) f -> p c f", p=128)
        w2v = IN["e_w2"].ap()[l][ex].rearrange("(g p) d -> p g d", p=128)
        for i in range(NT):
            P.op("dve", lambda e, i=i, ex=ex: e.tensor_scalar(SelE.ap[:, i, :], k.iota256.ap, pm.ap[:, i, ex:ex + 1], None, op0=ALU.is_equal),
                 reads=[k.iota256.k(), pm.k(i)], writes=[SelE.k(i)])
        for st in range(2):
            for tg in range(4):
                P.op("pe", lambda e, ex=ex, tg=tg: e.matmul(ps[6][:, :], k.ohb.ap[0:NE, ex * 128:(ex + 1) * 128], pmT.ap[0:NE, tg * 512:(tg + 1) * 512], start=True, stop=True),
                     reads=[k.ohb.k(), pmT.k()], writes=["ps6"])
                P.op("dve", lambda e, st=st, tg=tg: e.tensor_scalar(SelT.ap[:, st, tg * 512:(tg + 1) * 512], ps[6][:, :], k.slotidx.ap[:, st:st + 1], None, op0=ALU.is_equal),
                     reads=["ps6", k.slotidx.k()], writes=[SelT.k(st, tg)])
        for c in range(8):
            b = 6 + c % 2
            for i in range(NT):
                P.op("pe", lambda e, c=c, i=i, b=b: e.matmul(ps[b][:, 0:CAP], x2b.ap[:, i, c * 128:(c + 1) * 128], SelE.ap[:, i, :], start=(i == 0), stop=(i == NT - 1)),
                     reads=[x2b.k(i), SelE.k(i)], writes=[f"ps{b}"])
            P.op("act", lambda e, c=c, b=b: e.copy(xgT.ap[:, c, :], ps[b][:, 0:CAP]), reads=[f"ps{b}"], writes=[xgT.k(c)])
        for fb in range(NFB):
            r = wn % 2; wn += 1
            P.dma("pool", w1b[r].ap, w1v[:, :, fb * FB:(fb + 1) * FB], writes=[w1b[r].k()], semkey=("w1", r))
            P.dma("pool", w3b[r].ap, w3v[:, :, fb * FB:(fb + 1) * FB], writes=[w3b[r].k()], semkey=("w3", r))
            P.dma("pool", w2b[r].ap, w2v[:, fb * W2G:(fb + 1) * W2G, :], writes=[w2b[r].k()], semkey=("w2", r))
            for q in range(FB // 128):
                fc = fb * (FB // 128) + q
                b = fc % 2
                for c in range(8):
                    P.op("pe", lambda e, c=c, q=q, b=b, r=r: e.matmul(ps[b][:, 0:CAP], w1b[r].ap[:, c, q * 128:(q + 1) * 128], xgT.ap[:, c, :], start=(c == 0), stop=(c == 7)),
                         reads=[w1b[r].k(), xgT.k(c)], writes=[f"ps{b}"])
                for c in range(8):
                    P.op("pe", lambda e, c=c, q=q, b=b, r=r: e.matmul(ps[b][:, CAP:2 * CAP], w3b[r].ap[:, c, q * 128:(q + 1) * 128], xgT.ap[:, c, :], start=(c == 0), stop=(c == 7)),
                         reads=[w3b[r].k(), xgT.k(c)], writes=[f"ps{b}"])
                P.op("act", lambda e, b=b: e.activation(s1[b].ap, ps[b][:, 0:CAP], AF.Silu), reads=[f"ps{b}"], writes=[s1[b].k()])
                P.op("dve", lambda e, b=b, fc=fc: e.tensor_tensor(actT.ap[:, fc, :], s1[b].ap, ps[b][:, CAP:2 * CAP], op=ALU.mult), reads=[f"ps{b}", s1[b].k()], writes=[actT.k(fc)])
            for q in range(W2G):
                fc = fb * W2G + q
                for st in range(2):
                    for half in range(2):
                        yb_ = 2 + st * 2 + half
                        P.op("pe", lambda e, fc=fc, q=q, st=st, half=half, yb_=yb_, r=r: e.matmul(ps[yb_][:, :], actT.ap[:, fc, st * 128:(st + 1) * 128], w2b[r].ap[:, q, half * 512:(half + 1) * 512],
                                                                                           start=(fc == 0), stop=(fc == NFC - 1)),
                             reads=[actT.k(fc), w2b[r].k()], writes=[f"ps{yb_}"])
        for st in range(2):
            for half in range(2):
                yb_ = 2 + st * 2 + half
                P.op("act", lambda e, st=st, half=half, yb_=yb_: e.copy(ysb.ap[:, st, half * 512:(half + 1) * 512], ps[yb_][:, :]), reads=[f"ps{yb_}"], writes=[ysb.k(st, half)])
        for i in range(NT):
            for half in range(2):
                b = 6 + (2 * i + half) % 2
                for st in range(2):
                    P.op("pe", lambda e, i=i, half=half, st=st, b=b: e.matmul(ps[b][:, :], SelT.ap[:, st, i * 128:(i + 1) * 128], ysb.ap[:, st, half * 512:(half + 1) * 512], start=(st == 0), stop=(st == 1)),
                         reads=[SelT.k(st, i // 4), ysb.k(st, half)], writes=[f"ps{b}"])
                P.op("dve", lambda e, i=i, half=half, b=b, ex=ex: e.scalar_tensor_tensor(k.xres.ap[:, i, half * 512:(half + 1) * 512], ps[b][:, :], aff.ap[:, i, ex:ex + 1],
                                                                                     k.xres.ap[:, i, half * 512:(half + 1) * 512], op0=ALU.mult, op1=ALU.add),
                     reads=[f"ps{b}", aff.k(), k.xres.k(i)], writes=[k.xres.k(i)])
    A.release(m0)


_INPUT_NAMES = ["rel_bias", "ln1_g", "w_in", "w_out", "rk_mu", "rk_w0", "rk_w2", "rk_a0", "rk_a2", "rk_kk", "rk_ka",
                "rk_rk", "rk_g2", "rk_ln_g", "rk_ln_b", "ml_conv_w", "ml_conv_b", "ml_ib", "ml_fb", "ml_ln_g",
                "ln2_g", "router", "e_w1", "e_w3", "e_w2", "final_g"]


def make_in_maps(inputs, cores, names=None):
    consts = host_consts()
    shared = {n: np.ascontiguousarray(np.asarray(inputs[n], dtype=np.float32)) for n in _INPUT_NAMES if names is None or n in names}
    shared.update({n: v for n, v in consts.items() if names is None or n in names})
    if names is None or "c_pc" in names or "c_wx" in names:
        shared.update(host_rwkv_layout(inputs))
    x = np.asarray(inputs["x"], dtype=np.float32)
    maps = []
    for c in cores:
        m = dict(shared)
        m["x"] = np.ascontiguousarray(x[c])
        maps.append(m)
    return maps


def kernel(**inputs):
    nc, k = build()
    in_maps = make_in_maps(inputs, list(range(8)), set(k.IN.keys()))
    res = run_bass_kernel_spmd(nc, in_maps, core_ids=list(range(8)))
    return np.stack([np.asarray(r["out"], dtype=np.float32) for r in res.results], axis=0)
```

```python
from contextlib import ExitStack
import numpy as np
import concourse.bass as bass
import concourse.mybir as mybir

F32 = mybir.dt.float32
BF16 = mybir.dt.bfloat16
I32 = mybir.dt.int32
ALU = mybir.AluOpType
AF = mybir.ActivationFunctionType
AX = mybir.AxisListType

ENGS = ("pe", "act", "dve", "pool", "sp")


class Prog:
    def __init__(self, nc, strict_same_engine=False):
        self.nc = nc
        self.same_dist = 3
        self.ops = {e: [] for e in ENGS}
        self.keys = {}
        self.dma_cnt = {}
        self.es = ExitStack()
        self.n_ops = 0

    def _deps(self, reads, writes):
        deps = []
        for k in reads:
            deps.extend((ev, True) for ev in self._st(k)["w"])
        for k in writes:
            st = self._st(k)
            deps.extend((ev, True) for ev in st["w"])
            deps.extend((ev, False) for ev in st["r"].values())
        return deps

    def _st(self, k):
        st = self.keys.get(k)
        if st is None:
            inh = getattr(self, "inherit", {}).get(k[0], []) if isinstance(k, tuple) else []
            st = self.keys[k] = {"w": list(inh), "r": {}}
        return st

    def _record(self, ev, reads, writes):
        for k in reads:
            st = self._st(k)
            st["r"][(ev[0], ev[1])] = ev
        for k in writes:
            st = self._st(k)
            st["w"] = [ev]
            st["r"] = {}

    @staticmethod
    def _norm(reads, writes):
        r2, w2 = [], []
        for k in writes:
            if isinstance(k, str) and k.startswith("ps"):
                k = k.split("q")[0]
            if k not in w2:
                w2.append(k)
        for k in reads:
            if isinstance(k, str) and k.startswith("ps"):
                k = k.split("q")[0]
                if k not in w2:
                    w2.append(k)
            elif k not in r2:
                r2.append(k)
        return r2, w2

    def op(self, eng, fn, reads=(), writes=(), serial=False):
        reads, writes = self._norm(reads, writes)
        deps = self._deps(reads, writes)
        idx = len(self.ops[eng])
        if serial and idx > 0:
            deps.append((("eng", eng, idx - 1), "force"))
        ev = ("eng", eng, idx)
        self.ops[eng].append(dict(fn=fn, deps=deps, kind="c"))
        self._record(ev, reads, writes)
        self.n_ops += 1
        return ev

    def dma(self, q, out, in_, reads=(), writes=(), semkey=None, **kw):
        assert semkey is not None
        reads, writes = self._norm(reads, writes)
        deps = self._deps(reads, writes)
        c = self.dma_cnt.get(semkey, 0) + 1
        self.dma_cnt[semkey] = c
        ev = ("dma", semkey, c)
        fn = lambda e, out=out, in_=in_, kw=kw: e.dma_start(out=out, in_=in_, **kw)
        self.ops[q].append(dict(fn=fn, deps=deps, kind="d", semkey=semkey))
        self._record(ev, reads, writes)
        self.n_ops += 1
        return ev

    def emit(self, final_wait_events=()):
        nc = self.nc
        signal = {e: set() for e in ENGS}
        for e in ENGS:
            for i, o in enumerate(self.ops[e]):
                nd = []
                for (d, is_w) in o["deps"]:
                    if d[0] == "eng" and d[1] == e and is_w != "force":
                        if e == "pe" or not is_w or (i - d[2]) > self.same_dist:
                            continue
                    nd.append(d)
                    if d[0] == "eng":
                        signal[d[1]].add(d[2])
                o["deps"] = nd
        for d in final_wait_events:
            if d[0] == "eng":
                signal[d[1]].add(d[2])
        rank = {}
        for e in ENGS:
            r = 0
            for i in range(len(self.ops[e])):
                if i in signal[e]:
                    r += 1
                    rank[(e, i)] = r
        self.max_rank = {e: max([v for (ee, i), v in rank.items() if ee == e] + [0]) for e in ENGS}
        es = self.es
        sem_e = {e: es.enter_context(nc.semaphore("s_" + e)) for e in ENGS}
        sem_d = {k: es.enter_context(nc.semaphore("d_%d" % i)) for i, k in enumerate(self.dma_cnt)}
        self.n_sems = len(sem_e) + len(sem_d)

        def lower(ev):
            if ev[0] == "eng":
                return ("e_" + ev[1], sem_e[ev[1]], rank[(ev[1], ev[2])])
            return ("d_" + str(ev[1]), sem_d[ev[1]], 16 * ev[2])

        block = es.enter_context(nc.Block())
        engobj = {"pe": block.tensor, "act": block.scalar, "dve": block.vector,
                  "pool": block.gpsimd, "sp": block.sync}
        fw = self

        def make(e):
            def body(eng):
                known = {}
                for i, o in enumerate(fw.ops[e]):
                    need = {}
                    for d in o["deps"]:
                        nm, s, v = lower(d)
                        if known.get(nm, 0) >= v:
                            continue
                        if nm not in need or need[nm][1] < v:
                            need[nm] = (s, v)
                    for nm, (s, v) in need.items():
                        eng.wait_ge(s, v)
                        known[nm] = v
                    ins = o["fn"](eng)
                    if o["kind"] == "d":
                        ins.then_inc(sem_d[o["semkey"]], 16)
                    elif (e, i) in rank:
                        ins.then_inc(sem_e[e], 1)
                if e == "sp":
                    for d in final_wait_events:
                        nm, s, v = lower(d)
                        if known.get(nm, 0) < v:
                            eng.wait_ge(s, v)
                            known[nm] = v
            return body

        for e in ENGS:
            if self.ops[e] or e == "sp":
                engobj[e](make(e))
        es.close()


class Region:
    def __init__(self, uid, ap, lo, hi):
        self.uid, self.ap, self.lo, self.hi = uid, ap, lo, hi

    def k(self, *i):
        return (self.uid,) + tuple(i)

    def __getitem__(self, idx):
        return self.ap[idx]


class Arena:
    def __init__(self, P, tensor, ncols_f32):
        self.P, self.t, self.n = P, tensor, ncols_f32
        self.top = 0
        self.gen = 0
        self.pending = []
        self.live = []
        P.inherit = {}
        P._arena = self

    def alloc(self, name, free_shape, dtype=F32):
        nel = int(np.prod(free_shape))
        bpe = {F32: 4, BF16: 2, I32: 4}[dtype]
        ncol = (nel * bpe + 3) // 4
        lo, hi = self.top, self.top + ncol
        assert hi <= self.n, f"arena overflow allocating {name}: need {hi} cols of {self.n}"
        self.top = hi
        self.gen += 1
        uid = f"{name}#{self.gen}"
        ap = self.t[:, lo:hi]
        if dtype != F32:
            ap = ap.bitcast(dtype)
        ap = ap[:, 0:nel]
        if len(free_shape) > 1:
            names = " ".join(f"a{i}" for i in range(len(free_shape)))
            kw = {f"a{i}": int(s) for i, s in enumerate(free_shape)}
            ap = ap.rearrange(f"p ({names}) -> p {names}", **kw)
        evs = []
        keep = []
        for (plo, phi, pe) in self.pending:
            if plo < hi and lo < phi:
                evs.extend(pe)
            keep.append((plo, phi, pe))
        self.P.inherit[uid] = evs
        r = Region(uid, ap, lo, hi)
        self.live.append(r)
        return r

    def mark(self):
        return (self.top, len(self.live))

    def release(self, mark):
        top, nlive = mark
        P = self.P
        for r in self.live[nlive:]:
            evs = []
            for k in [k for k in P.keys if k[0] == r.uid]:
                st = P.keys.pop(k)
                evs.extend(st["w"])
                evs.extend(st["r"].values())
            evs.extend(P.inherit.get(r.uid, []))
            best = {}
            for ev in evs:
                kk = (ev[0], ev[1])
                if kk not in best or best[kk][2] < ev[2]:
                    best[kk] = ev
            self.pending.append((r.lo, r.hi, list(best.values())))
        del self.live[nlive:]
        self.top = top
from concourse.bass_utils import run_bass_kernel_spmd
S = 2048; D = 1024; NT = 16; INW = 3856; DEPTH = 2
ND = 3072; EC = 1535
NE = 16; CAP = 256; DFF = 2816


def t5_bucket_np(rel):
    nb = 16
    ret = np.where(rel > 0, nb, 0)
    n = np.abs(rel)
    max_exact = 8
    nf = np.maximum(n, 1).astype(np.float32)
    large = max_exact + (np.log(nf / np.float32(max_exact)) / np.float32(np.log(1024 / max_exact))
                         * np.float32(nb - max_exact)).astype(np.int32)
    large = np.minimum(large, nb - 1)
    return ret + np.where(n < max_exact, n, large)


def host_consts():
    d = np.arange(ND) - EC
    ad = np.abs(d)
    cnt = ((ad <= 64).astype(np.float32) + ((d % 4 == 0) & (ad <= 256)).astype(np.float32)
           + ((d % 16 == 0) & (ad <= 1024)).astype(np.float32))
    bk = t5_bucket_np(d)
    oh = np.zeros((32, ND), np.float32)
    oh[bk, np.arange(ND)] = 1.0
    c = {}
    c["c_oh"] = oh
    c["c_cnt"] = np.tile(cnt[None], (4, 1)).astype(np.float32)
    c["c_ident"] = np.eye(128, dtype=np.float32)
    c["c_jmat"] = np.eye(128, dtype=np.float32)[::-1].copy()
    c["c_triu"] = np.triu(np.ones((128, 128), np.float32))
    c["c_tril"] = np.tril(np.ones((128, 128), np.float32))
    ob = np.zeros((128, 128), np.float32); ob[:64, :64] = 1; ob[64:, 64:] = 1
    c["c_onesblk"] = ob
    tus = np.triu(np.ones((128, 128), np.float32), 1); tui = np.triu(np.ones((128, 128), np.float32), 0)
    c["c_mask4"] = np.concatenate([tus, tui, tus, tui], axis=1)
    c["c_trils"] = np.tril(np.ones((128, 128), np.float32), -1)
    rm = np.ones((128, 256), np.float32); rm[:, 0] = 0; rm[:, 128] = 0
    c["c_rmask"] = rm
    ohb = np.zeros((16, 2048), np.float32)
    for e_ in range(16):
        ohb[e_, e_ * 128:(e_ + 1) * 128] = 1.0
    c["c_ohb"] = ohb
    return c


class K:
    pass


def build(dbg=None, nlayers=DEPTH, stages=("attn", "mlstm", "rwkv", "moe")):
    nc = bass.Bass("TRN2", target_bir_lowering=False)
    k = K()
    k.nc = nc
    SH = dict(x=[S, D], rel_bias=[32, 4], ln1_g=[DEPTH, D], w_in=[DEPTH, D, INW], w_out=[DEPTH, D, D], rk_mu=[DEPTH, 2, 1280],
              rk_w0=[DEPTH, 2, 384], rk_w2=[DEPTH, 2, 64, 384], rk_a0=[DEPTH, 2, 384], rk_a2=[DEPTH, 2, 64, 384],
              rk_kk=[DEPTH, 384], rk_ka=[DEPTH, 384], rk_rk=[DEPTH, 6, 64], rk_g2=[DEPTH, 128, 384], rk_ln_g=[DEPTH, 384],
              rk_ln_b=[DEPTH, 384], ml_conv_w=[DEPTH, 5, 768], ml_conv_b=[DEPTH, 768], ml_ib=[DEPTH, 2, 4], ml_fb=[DEPTH, 2, 4],
              ml_ln_g=[DEPTH, 384], ln2_g=[DEPTH, D], router=[DEPTH, D, NE], e_w1=[DEPTH, NE, D, DFF], e_w3=[DEPTH, NE, D, DFF],
              e_w2=[DEPTH, NE, DFF, D], final_g=[D], c_oh=[32, ND], c_cnt=[4, ND], c_ident=[128, 128], c_jmat=[128, 128],
              c_triu=[128, 128], c_tril=[128, 128], c_onesblk=[128, 128], c_mask4=[128, 512], c_trils=[128, 128],
              c_rmask=[128, 256], c_ohb=[16, 2048], c_pc=[DEPTH, 128, NPC], c_wx=[DEPTH, D, 384])

    class _IN(dict):
        def __missing__(self, name):
            t = nc.dram_tensor(name, list(SH[name]), F32, kind="ExternalInput")
            self[name] = t
            return t
    IN = _IN()
    k.IN = IN
    out_t = nc.dram_tensor("out", [S, D], F32, kind="ExternalOutput")
    k.mscr = nc.dram_tensor("mscr", [4, ND], F32, kind="Internal")
    k.mtab_d = nc.dram_tensor("mtab_d", [4, 128, 23 * 128], F32, kind="Internal")
    k.xspill = nc.dram_tensor("xspill", [S, D], F32, kind="Internal")
    k.dbg = dbg
    k.dbg_out = {}
    with ExitStack() as es:
        ACOLS = 53000
        arena_t = es.enter_context(nc.sbuf_tensor("arena", [128, ACOLS], F32))
        k.ps = [es.enter_context(nc.psum_tensor(f"ps{i}", [128, 512], F32)) for i in range(8)]
        P = Prog(nc)
        A = Arena(P, arena_t, ACOLS)
        k.P, k.A = P, A
        k.final_events = []
        setup_consts(k)
        k.xres = A.alloc("xres", (NT, D))
        xin = IN["x"].ap().rearrange("(i p) d -> p i d", p=128)
        for i in range(NT):
            P.dma("sp" if i % 2 == 0 else "act", k.xres.ap[:, i, :], xin[:, i, :], writes=[k.xres.k(i)], semkey=("xld", i % 4))
        build_mask_table(k)
        for l in range(nlayers):
            m0 = A.mark()
            norm_to_hT(k, IN["ln1_g"].ap()[l], "hT")
            if "attn" in stages:
                attn_stage(k, l)
            if "mlstm" in stages:
                mlstm_stage(k, l)
            if "rwkv" in stages:
                rwkv_stage(k, l)
            A.release(m0)
            if "moe" in stages:
                moe_stage(k, l)
        final_stage(k, out_t)
        P.emit(final_wait_events=k.final_events)
    return nc, k


def dbg_dump(k, name, region_ap, shape, dt, reads):
    if not k.dbg or name not in k.dbg:
        return None
    t = k.nc.dram_tensor("dbg_" + name, list(shape), dt, kind="ExternalOutput")
    k.dbg_out[name] = t
    return t


def setup_consts(k):
    P, A, IN = k.P, k.A, k.IN
    k.ident = A.alloc("ident", (128,))
    k.jmat = A.alloc("jmat", (128,))
    k.ident16 = A.alloc("ident16", (128,), BF16)
    k.triu = A.alloc("triu", (128,))
    k.tril = A.alloc("tril", (128,))
    P.dma("sp", k.ident.ap, IN["c_ident"].ap(), writes=[k.ident.k()], semkey="c0")
    P.dma("sp", k.jmat.ap, IN["c_jmat"].ap(), writes=[k.jmat.k()], semkey="c1")
    P.dma("sp", k.triu.ap, IN["c_triu"].ap(), writes=[k.triu.k()], semkey="c2")
    P.dma("sp", k.tril.ap, IN["c_tril"].ap(), writes=[k.tril.k()], semkey="c3")
    P.op("dve", lambda e: e.tensor_copy(k.ident16.ap, k.ident.ap), reads=[k.ident.k()], writes=[k.ident16.k()])
    k.one = A.alloc("one", (1,))
    P.op("pool", lambda e: e.memset(k.one.ap, 1.0), writes=[k.one.k()])
    k.ones32 = A.alloc("ones32", (128,))
    P.op("pool", lambda e: e.memset(k.ones32.ap, 1.0), writes=[k.ones32.k()])
    k.negm = [A.alloc("negm0", (128,)), A.alloc("negm1", (128,))]
    P.op("dve", lambda e: e.tensor_scalar(k.negm[0].ap, k.triu.ap, -1.0, 30000.0, op0=ALU.add, op1=ALU.mult), reads=[k.triu.k()], writes=[k.negm[0].k()])
    P.op("dve", lambda e: e.tensor_scalar(k.negm[1].ap, k.tril.ap, -1.0, 30000.0, op0=ALU.add, op1=ALU.mult), reads=[k.tril.k()], writes=[k.negm[1].k()])
    k.epsgn = A.alloc("epsgn", (1,))
    P.op("pool", lambda e: e.memset(k.epsgn.ap, 64e-5), writes=[k.epsgn.k()])
    k.onesblk = A.alloc("onesblk", (128,)); k.onesblk64 = A.alloc("onesblk64", (128,))
    k.trils = A.alloc("trils", (128,))
    P.dma("act", k.onesblk.ap, IN["c_onesblk"].ap(), writes=[k.onesblk.k()], semkey="c4")
    P.dma("act", k.trils.ap, IN["c_trils"].ap(), writes=[k.trils.k()], semkey="c6")
    P.op("dve", lambda e: e.tensor_scalar(k.onesblk64.ap, k.onesblk.ap, 1.0 / 64, None, op0=ALU.mult), reads=[k.onesblk.k()], writes=[k.onesblk64.k()])
    k.iota256 = A.alloc("iota256", (256,)); k.slotidx = A.alloc("slotidx", (2,))
    k.ones16 = A.alloc("ones16", (128,), BF16); k.trius16 = A.alloc("trius16", (128,), BF16)
    P.op("pool", lambda e: e.iota(k.iota256.ap, [[1, 256]], base=0, channel_multiplier=0, allow_small_or_imprecise_dtypes=True), writes=[k.iota256.k()])
    P.op("pool", lambda e: e.iota(k.slotidx.ap, [[128, 2]], base=0, channel_multiplier=1, allow_small_or_imprecise_dtypes=True), writes=[k.slotidx.k()])
    P.op("dve", lambda e: e.tensor_copy(k.ones16.ap, k.ones32.ap), reads=[k.ones32.k()], writes=[k.ones16.k()])
    P.op("dve", lambda e: e.tensor_tensor(k.trius16.ap, k.triu.ap, k.ident.ap, op=ALU.subtract), reads=[k.triu.k(), k.ident.k()], writes=[k.trius16.k()])
    k.eps6 = A.alloc("eps6", (1,))
    P.op("pool", lambda e: e.memset(k.eps6.ap, 1e-6), writes=[k.eps6.k()])


def build_mask_table(k):
    P, A, IN, ps = k.P, k.A, k.IN, k.ps
    m0 = A.mark()
    rb = A.alloc("rb", (4,)); oh = A.alloc("oh", (ND,)); cnt = A.alloc("cnt", (ND,)); mm = A.alloc("mm", (ND,))
    P.dma("sp", rb.ap[0:32, :], IN["rel_bias"].ap(), writes=[rb.k()], semkey="mt0")
    P.dma("sp", oh.ap[0:32, :], IN["c_oh"].ap(), writes=[oh.k()], semkey="mt1")
    P.dma("act", cnt.ap[0:4, :], IN["c_cnt"].ap(), writes=[cnt.k()], semkey="mt2")
    for c in range(ND // 512):
        b = ps[c % 2]
        P.op("pe", lambda e, c=c, b=b: e.matmul(b[0:4, :], rb.ap[0:32, :], oh.ap[0:32, c * 512:(c + 1) * 512], start=True, stop=True),
             reads=[rb.k(), oh.k()], writes=[f"ps{c%2}"])
        P.op("act", lambda e, c=c, b=b: e.activation(mm.ap[0:4, c * 512:(c + 1) * 512], b[0:4, :], AF.Exp),
             reads=[f"ps{c%2}"], writes=[mm.k(c)])
        P.op("dve", lambda e, c=c: e.tensor_tensor(mm.ap[0:4, c * 512:(c + 1) * 512], mm.ap[0:4, c * 512:(c + 1) * 512],
                                                  cnt.ap[0:4, c * 512:(c + 1) * 512], op=ALU.mult),
             reads=[mm.k(c), cnt.k()], writes=[mm.k(c)])
    P.dma("sp", k.mscr.ap(), mm.ap[0:4, :], reads=[mm.k(c) for c in range(ND // 512)], writes=["mscr"], semkey="mt3")
    hanks = [A.alloc(f"hank{i}", (23, 128)) for i in range(2)]
    mts = [A.alloc(f"mt{i}", (23, 128)) for i in range(2)]
    for h in range(4):
        hank = hanks[h % 2]
        mt = mts[h % 2]
        src = bass.AP(k.mscr, h * ND, [[1, 128], [128, 23], [1, 128]])
        P.dma("sp" if h % 2 == 0 else "act", hank.ap, src, reads=["mscr"], writes=[hank.k()], semkey=("mt4", h))
        for j in range(23):
            jj = 22 - j
            b = 2 + (j // 4) % 2
            P.op("pe", lambda e, j=j, b=b, hank=hank: e.matmul(ps[b][:, (j % 4) * 128:(j % 4 + 1) * 128], hank.ap[:, j, :], k.jmat.ap, start=True, stop=True),
                 reads=[hank.k(), k.jmat.k()], writes=[f"ps{b}"])
            P.op("act" if j % 2 == 0 else "dve",
                 (lambda e, jj=jj, j=j, b=b, mt=mt: e.copy(mt.ap[:, jj, :], ps[b][:, (j % 4) * 128:(j % 4 + 1) * 128])) if j % 2 == 0 else
                 (lambda e, jj=jj, j=j, b=b, mt=mt: e.tensor_copy(mt.ap[:, jj, :], ps[b][:, (j % 4) * 128:(j % 4 + 1) * 128])),
                 reads=[f"ps{b}"], writes=[mt.k()])
        P.dma("sp", k.mtab_d.ap()[h].rearrange("p (j q) -> p j q", j=23), mt.ap, reads=[mt.k()], writes=[("mtab_d", h)], semkey=("mt5", h))
    A.release(m0)


def norm_to_hT(k, g_ap, name, want_f32T=False):
    P, A, ps = k.P, k.A, k.ps
    k.hT = A.alloc(name, (8, S), BF16)
    m0 = A.mark()
    gb = A.alloc("gb", (D,)); junk = A.alloc("junk", (D,)); ss = A.alloc("ss", (NT,)); rstd = A.alloc("rstd", (NT,))
    hb = [A.alloc(f"hb{i}", (D,), BF16) for i in range(2)]
    P.dma("sp", gb.ap, g_ap.partition_broadcast(128), writes=[gb.k()], semkey="gb")
    P.op("pool", lambda e: e.memset(ss.ap, 0.0), writes=[ss.k(i) for i in range(NT)])
    for i in range(NT):
        P.op("act", lambda e, i=i: e.activation(junk.ap, k.xres.ap[:, i, :], AF.Square, accum_out=ss.ap[:, i:i + 1]),
             reads=[k.xres.k(i)], writes=[junk.k(), ss.k(i)])
    P.op("act", lambda e: e.activation(rstd.ap, ss.ap, AF.Sqrt, scale=1.0 / D, bias=k.eps6.ap[:, 0:1]),
         reads=[ss.k(i) for i in range(NT)] + [k.eps6.k()], writes=[rstd.k()])
    P.op("dve", lambda e: e.reciprocal(rstd.ap, rstd.ap), reads=[rstd.k()], writes=[rstd.k()])
    for i in range(NT):
        h_ = hb[i % 2]
        P.op("dve", lambda e, i=i, h_=h_: e.scalar_tensor_tensor(h_.ap, k.xres.ap[:, i, :], rstd.ap[:, i:i + 1], gb.ap, op0=ALU.mult, op1=ALU.mult),
             reads=[k.xres.k(i), rstd.k(), gb.k()], writes=[h_.k()])
        b = 4 + i % 2
        pb = ps[b][:, :].bitcast(BF16)
        for c in range(8):
            P.op("pe", lambda e, c=c, pb=pb, h_=h_: e.transpose(pb[:, c * 128:(c + 1) * 128], h_.ap[:, c * 128:(c + 1) * 128], k.ident16.ap),
                 reads=[h_.k(), k.ident16.k()], writes=[f"ps{b}"])
        P.op("act", lambda e, i=i, pb=pb: e.copy(k.hT.ap[:, :, i * 128:(i + 1) * 128], pb.rearrange("p (c t) -> p c t", c=8)),
             reads=[f"ps{b}"], writes=[k.hT.k(i)])
    A.release(m0)


def load_w_bf16(k, name, src_ap, nchunks, ncols, semkey, split=None):
    P, A = k.P, k.A
    w = A.alloc(name, (nchunks, ncols), BF16)
    v = src_ap.rearrange("(c p) n -> p c n", p=128)
    for c in range(nchunks):
        P.dma("pool", w.ap[:, c, :], v[:, c, :], writes=[w.k(c)], semkey=(semkey, c % 4))
    return w


def outproj_partial(k, l, yT, nch, row0, tag):
    P, A, ps, IN = k.P, k.A, k.ps, k.IN
    wo = load_w_bf16(k, "wo_" + tag, IN["w_out"].ap()[l][row0:row0 + nch * 128, :], nch, D, "wo_" + tag)
    n = 0
    for i in range(NT):
        for half in range(2):
            b = 6 + n % 2
            n += 1
            for c in range(nch):
                P.op("pe", lambda e, i=i, half=half, c=c, b=b: e.matmul(ps[b][:, :], yT.ap[:, c, i * 128:(i + 1) * 128],
                                                                     wo.ap[:, c, half * 512:(half + 1) * 512], start=(c == 0), stop=(c == nch - 1)),
                     reads=[yT.k(i), wo.k(c)], writes=[f"ps{b}"])
            P.op("dve", lambda e, i=i, half=half, b=b: e.tensor_tensor(k.xres.ap[:, i, half * 512:(half + 1) * 512],
                                                                      k.xres.ap[:, i, half * 512:(half + 1) * 512], ps[b][:, :], op=ALU.add),
                 reads=[f"ps{b}", k.xres.k(i)], writes=[k.xres.k(i)])


def transpose_to_T(k, y, nch, yT):
    P, ps = k.P, k.ps
    for i in range(NT):
        b = 4 + i % 2
        pb = ps[b][:, :].bitcast(BF16)
        for c in range(nch):
            P.op("pe", lambda e, i=i, c=c, pb=pb: e.transpose(pb[:, c * 128:(c + 1) * 128], y.ap[:, i, c * 128:(c + 1) * 128], k.ident16.ap),
                 reads=[y.k(i), k.ident16.k()], writes=[f"ps{b}"])
        P.op("act", lambda e, i=i, pb=pb: e.copy(yT.ap[:, :, i * 128:(i + 1) * 128], pb[:, 0:nch * 128].rearrange("p (c t) -> p c t", c=nch)),
             reads=[f"ps{b}"], writes=[yT.k(i)])


def attn_stage(k, l):
    P, A, ps, IN = k.P, k.A, k.ps, k.IN
    m0 = A.mark()
    ya = A.alloc("ya", (NT, 256), BF16)
    m1 = A.mark()
    wa = load_w_bf16(k, "wa", IN["w_in"].ap()[l][:, 0:768], 8, 768, "wa")
    qT = A.alloc("qT", (2, S), BF16); kT = A.alloc("kT", (2, S), BF16)
    vp = A.alloc("vp", (NT, 4, 65), BF16)
    P.op("pool", lambda e: e.memset(vp.ap, 1.0), writes=[vp.k(i) for i in range(NT)])
    n = 0
    for cc in range(4):
        dst = qT if cc < 2 else kT
        for tg in range(4):
            b = n % 2; n += 1
            for c in range(8):
                P.op("pe", lambda e, c=c, cc=cc, tg=tg, b=b: e.matmul(ps[b][:, :], wa.ap[:, c, cc * 128:(cc + 1) * 128], k.hT.ap[:, c, tg * 512:(tg + 1) * 512],
                                                                  start=(c == 0), stop=(c == 7)),
                     reads=[wa.k(c)] + [k.hT.k(4 * tg + j) for j in range(4)], writes=[f"ps{b}"])
            P.op("act", lambda e, cc=cc, tg=tg, b=b, dst=dst: e.copy(dst.ap[:, cc % 2, tg * 512:(tg + 1) * 512], ps[b][:, :]),
                 reads=[f"ps{b}"], writes=[dst.k(tg)])
    for i in range(NT):
        b = 2 + i % 2
        for c in range(8):
            P.op("pe", lambda e, c=c, i=i, b=b: e.matmul(ps[b][:, 0:256], k.hT.ap[:, c, i * 128:(i + 1) * 128], wa.ap[:, c, 512:768], start=(c == 0), stop=(c == 7)),
                 reads=[wa.k(c), k.hT.k(i)], writes=[f"ps{b}"])
        P.op("dve", lambda e, i=i, b=b: e.tensor_copy(vp.ap[:, i, :, 0:64], ps[b][:, 0:256].rearrange("p (h c) -> p h c", h=4)),
             reads=[f"ps{b}"], writes=[vp.k(i)])
    mtab = [A.alloc(f"mtab{i}", (23, 128)) for i in range(2)]
    e32 = [A.alloc(f"e32_{i}", (512,)) for i in range(2)]
    p16 = [A.alloc(f"p16_{i}", (512,), BF16) for i in range(2)]
    rc = A.alloc("rc", (8,))
    iters = []
    for h in range(4):
        for g in range(4):
            kts = [kt for kt in range(NT) if -8 <= kt - 4 * g <= 11]
            for kt in kts:
                iters.append((h, g, kt, kt == kts[0], kt == kts[-1]))
    mts = {}

    def emit_score(n):
        h, g, kt, first, last = iters[n]
        sb = n % 2
        hp, hb_ = h // 2, (h % 2) * 64
        if h not in mts:
            mt = mtab[h % 2]
            P.dma("sp", mt.ap, k.mtab_d.ap()[h].rearrange("p (j q) -> p j q", j=23), reads=[("mtab_d", h)], writes=[mt.k()], semkey=("mtl", h % 2))
            mts[h] = mt
        P.op("pe", lambda e, kt=kt, g=g, sb=sb, hp=hp, hb_=hb_: e.matmul(ps[sb][:, :], kT.ap[hb_:hb_ + 64, hp, kt * 128:(kt + 1) * 128],
                                                                   qT.ap[hb_:hb_ + 64, hp, g * 512:(g + 1) * 512], start=True, stop=True),
             reads=[kT.k(kt // 4), qT.k(g)], writes=[f"ps{sb}"], serial=True)

    def emit_rest(n):
        h, g, kt, first, last = iters[n]
        sb = n % 2
        mt = mts[h]
        jj0 = 11 - kt + 4 * g
        P.op("act", lambda e, sb=sb: e.activation(e32[sb].ap, ps[sb][:, :], AF.Exp, scale=0.125),
             reads=[f"ps{sb}"], writes=[e32[sb].k()])
        P.op("dve", lambda e, sb=sb, jj0=jj0, mt=mt: e.tensor_tensor(p16[sb].ap, e32[sb].ap, mt.ap[:, jj0:jj0 + 4, :].rearrange("p j q -> p (j q)"), op=ALU.mult),
             reads=[e32[sb].k(), mt.k()], writes=[p16[sb].k()])
        for i in range(4):
            P.op("pe", lambda e, i=i, sb=sb, kt=kt, h=h, first=first, last=last: e.matmul(ps[2 + i][:, 0:65], p16[sb].ap[:, i * 128:(i + 1) * 128], vp.ap[:, kt, h, :],
                                                                                  start=first, stop=last),
                 reads=[p16[sb].k(), vp.k(kt)], writes=[f"ps{2+i}"])
        if last:
            for i in range(4):
                P.op("dve", lambda e, i=i: e.reciprocal(rc.ap[:, i:i + 1], ps[2 + i][:, 64:65]), reads=[f"ps{2+i}"], writes=[rc.k(i)])
                P.op("dve", lambda e, i=i, g=g, h=h: e.tensor_scalar(ya.ap[:, 4 * g + i, h * 64:(h + 1) * 64], ps[2 + i][:, 0:64], rc.ap[:, i:i + 1], None, op0=ALU.mult),
                     reads=[f"ps{2+i}", rc.k(i)], writes=[ya.k(4 * g + i)])

    emit_score(0)
    for n in range(len(iters)):
        if n + 1 < len(iters):
            emit_score(n + 1)
        emit_rest(n)
    A.release(m1)
    if k.dbg and "ya" in k.dbg:
        t = k.nc.dram_tensor("dbg_ya", [S, 256], BF16, kind="ExternalOutput")
        k.final_events.append(P.dma("sp", t.ap().rearrange("(i p) c -> p i c", p=128), ya.ap, reads=[ya.k(i) for i in range(NT)], semkey="dbg"))
    yaT = A.alloc("yaT", (2, S), BF16)
    transpose_to_T(k, ya, 2, yaT)
    outproj_partial(k, l, yaT, 2, 0, "a")
    A.release(m0)


def final_stage(k, out_t):
    P, A = k.P, k.A
    m0 = A.mark()
    gb = A.alloc("gbf", (D,)); junk = A.alloc("junkf", (D,)); ss = A.alloc("ssf", (NT,)); rstd = A.alloc("rstdf", (NT,))
    ob = [A.alloc(f"ob{i}", (D,)) for i in range(2)]
    P.dma("sp", gb.ap, k.IN["final_g"].ap().partition_broadcast(128), writes=[gb.k()], semkey="gbf")
    P.op("pool", lambda e: e.memset(ss.ap, 0.0), writes=[ss.k(i) for i in range(NT)])
    for i in range(NT):
        P.op("act", lambda e, i=i: e.activation(junk.ap, k.xres.ap[:, i, :], AF.Square, accum_out=ss.ap[:, i:i + 1]),
             reads=[k.xres.k(i)], writes=[junk.k(), ss.k(i)])
    P.op("act", lambda e: e.activation(rstd.ap, ss.ap, AF.Sqrt, scale=1.0 / D, bias=k.eps6.ap[:, 0:1]),
         reads=[ss.k(i) for i in range(NT)] + [k.eps6.k()], writes=[rstd.k()])
    P.op("dve", lambda e: e.reciprocal(rstd.ap, rstd.ap), reads=[rstd.k()], writes=[rstd.k()])
    ov = out_t.ap().rearrange("(i p) d -> p i d", p=128)
    for i in range(NT):
        o = ob[i % 2]
        P.op("dve", lambda e, i=i, o=o: e.scalar_tensor_tensor(o.ap, k.xres.ap[:, i, :], rstd.ap[:, i:i + 1], gb.ap, op0=ALU.mult, op1=ALU.mult),
             reads=[k.xres.k(i), rstd.k(), gb.k()], writes=[o.k()])
        k.final_events.append(P.dma("sp" if i % 2 == 0 else "act", ov[:, i, :], o.ap, reads=[o.k()], semkey=("out", i % 2)))
    A.release(m0)


OFF_MQK = 2304; OFF_MV = 3072; OFF_MO = 3456; OFF_MG = 3840


def mlstm_stage(k, l):
    P, A, ps, IN = k.P, k.A, k.ps, k.IN
    m0 = A.mark()
    hsum = A.alloc("hsum", (NT, 4, 96))
    m1 = A.mark()
    qkT = A.alloc("qkT", (8, S), BF16)
    vp = A.alloc("vpm", (NT, 4, 97), BF16)
    G = A.alloc("G", (NT, 16))
    BL = [A.alloc(f"BL{d}", (NT, 4)) for d in range(2)]
    EBL = [A.alloc(f"EBL{d}", (NT, 4)) for d in range(2)]
    IMB = [A.alloc(f"IMB{d}", (NT, 4)) for d in range(2)]
    m2 = A.mark()
    wm = load_w_bf16(k, "wm", IN["w_in"].ap()[l][:, OFF_MQK:OFF_MV], 8, OFF_MV - OFF_MQK, "wm")
    pre = A.alloc("pre", (S + 4,)); cacc = A.alloc("cacc", (S,))
    cw = A.alloc("cw", (8, 6))
    for g_ in range(8):
        P.dma("sp", cw.ap[0:96, g_, 0:5], IN["ml_conv_w"].ap()[l][:, g_ * 96:(g_ + 1) * 96].rearrange("j c -> c j"), writes=[cw.k()], semkey=("cw0", g_ % 4), allow_slow_non_contiguous=True)
    P.dma("sp", cw.ap[0:96, :, 5:6], IN["ml_conv_b"].ap()[l].rearrange("(g c o) -> c g o", c=96, o=1), writes=[cw.k()], semkey="cw1", allow_slow_non_contiguous=True)
    P.op("pool", lambda e: e.memset(pre.ap, 0.0), writes=[pre.k()])
    P.op("pool", lambda e: e.memset(vp.ap, 1.0), writes=[vp.k(i) for i in range(NT)])
    n = 0
    for j in range(8):
        for tg in range(4):
            b = n % 2; n += 1
            for c in range(8):
                P.op("pe", lambda e, c=c, j=j, tg=tg, b=b: e.matmul(ps[b][0:96, :], wm.ap[:, c, j * 96:(j + 1) * 96], k.hT.ap[:, c, tg * 512:(tg + 1) * 512],
                                                                 start=(c == 0), stop=(c == 7)),
                     reads=[wm.k(c)] + [k.hT.k(4 * tg + q) for q in range(4)], writes=[f"ps{b}"])
            P.op("act", lambda e, tg=tg, b=b: e.copy(pre.ap[0:96, 2 + tg * 512:2 + (tg + 1) * 512], ps[b][0:96, :]),
                 reads=[f"ps{b}"], writes=[pre.k()])
        P.op("dve", lambda e, j=j: e.tensor_scalar(cacc.ap[0:96, :], pre.ap[0:96, 0:S], cw.ap[0:96, j, 0:1], None, op0=ALU.mult),
             reads=[pre.k(), cw.k()], writes=[cacc.k()])
        for t in range(1, 5):
            P.op("dve", lambda e, j=j, t=t: e.scalar_tensor_tensor(cacc.ap[0:96, :], pre.ap[0:96, t:t + S], cw.ap[0:96, j, t:t + 1], cacc.ap[0:96, :],
                                                                 op0=ALU.mult, op1=ALU.add),
                 reads=[pre.k(), cw.k(), cacc.k()], writes=[cacc.k()])
        if j < 4:
            P.op("act", lambda e, j=j: e.activation(qkT.ap[0:96, j, :], cacc.ap[0:96, :], AF.Silu, bias=cw.ap[0:96, j, 5:6]),
                 reads=[cacc.k(), cw.k()], writes=[qkT.k(j)])
        else:
            P.op("act", lambda e, j=j: e.activation(cacc.ap[0:96, :], cacc.ap[0:96, :], AF.Silu, bias=cw.ap[0:96, j, 5:6]),
                 reads=[cacc.k(), cw.k()], writes=[cacc.k()])
            P.op("dve", lambda e, j=j: e.tensor_scalar(qkT.ap[0:96, j, :], cacc.ap[0:96, :], 96.0 ** -0.5, None, op0=ALU.mult),
                 reads=[cacc.k()], writes=[qkT.k(j)])
    A.release(m2)
    wv = load_w_bf16(k, "wv", IN["w_in"].ap()[l][:, OFF_MV:OFF_MO], 8, 384, "wv")
    wg = load_w_bf16(k, "wg", IN["w_in"].ap()[l][:, OFF_MG:INW], 8, 16, "wg")
    gbias = A.alloc("gbias", (16,))
    for d in range(2):
        P.dma("act", gbias.ap[:, 8 * d:8 * d + 4], IN["ml_ib"].ap()[l][d].partition_broadcast(128), writes=[gbias.k()], semkey=("gbi", d))
        P.dma("act", gbias.ap[:, 8 * d + 4:8 * d + 8], IN["ml_fb"].ap()[l][d].partition_broadcast(128), writes=[gbias.k()], semkey=("gbf_", d))
    for i in range(NT):
        b = 2 + i % 2
        for c in range(8):
            P.op("pe", lambda e, c=c, i=i, b=b: e.matmul(ps[b][:, 0:384], k.hT.ap[:, c, i * 128:(i + 1) * 128], wv.ap[:, c, :],
                                                     start=(c == 0), stop=(c == 7)),
                 reads=[wv.k(c), k.hT.k(i)], writes=[f"ps{b}"])
        P.op("dve", lambda e, i=i, b=b: e.tensor_copy(vp.ap[:, i, :, 0:96], ps[b][:, 0:384].rearrange("p (h c) -> p h c", h=4)),
             reads=[f"ps{b}"], writes=[vp.k(i)])
        b2 = 4 + i % 2
        for c in range(8):
            P.op("pe", lambda e, c=c, i=i, b2=b2: e.matmul(ps[b2][:, 0:16], k.hT.ap[:, c, i * 128:(i + 1) * 128], wg.ap[:, c, :],
                                                       start=(c == 0), stop=(c == 7)),
                 reads=[wg.k(c), k.hT.k(i)], writes=[f"ps{b2}"])
        P.op("dve", lambda e, i=i, b2=b2: e.tensor_tensor(G.ap[:, i, :], ps[b2][:, 0:16], gbias.ap, op=ALU.add),
             reads=[f"ps{b2}", gbias.k()], writes=[G.k()])
    A.release(m2)
    for d in range(2):
        v_ = G.ap[:, :, 8 * d + 4:8 * d + 8]
        P.op("act", lambda e, v_=v_: e.activation(v_, v_, AF.Exp, scale=-1.0), reads=[G.k()], writes=[G.k()])
        P.op("act", lambda e, v_=v_: e.activation(v_, v_, AF.Ln, bias=k.one.ap[:, 0:1]), reads=[G.k(), k.one.k()], writes=[G.k()])
        P.op("dve", lambda e, v_=v_: e.tensor_scalar(v_, v_, -1.0, None, op0=ALU.mult), reads=[G.k()], writes=[G.k()])
    for d in range(2):
        tri = k.triu if d == 0 else k.tril
        P.op("pe", lambda e, d=d, tri=tri: e.matmul(ps[7][:, 0:64].rearrange("p (i h) -> p i h", h=4), tri.ap, G.ap[:, :, 8 * d + 4:8 * d + 8], start=True, stop=True),
             reads=[tri.k(), G.k()], writes=["ps7"])
        P.op("dve", lambda e, d=d: e.tensor_copy(BL[d].ap, ps[7][:, 0:64].rearrange("p (i h) -> p i h", h=4)), reads=["ps7"], writes=[BL[d].k()])
        P.op("act", lambda e, d=d: e.activation(EBL[d].ap, BL[d].ap, AF.Exp), reads=[BL[d].k()], writes=[EBL[d].k()])
        P.op("dve", lambda e, d=d: e.tensor_tensor(IMB[d].ap, G.ap[:, :, 8 * d:8 * d + 4], BL[d].ap, op=ALU.subtract), reads=[G.k(), BL[d].k()], writes=[IMB[d].k()])
    C32 = A.alloc("C32", (4, 97)); Cb = A.alloc("Cb", (4, 97), BF16)
    HB = []
    for h in range(4):
        HB.append(dict(dg=A.alloc(f"dg{h}", (128,)), Dm=A.alloc(f"Dm{h}", (128,)), W16=A.alloc(f"W16{h}", (128,), BF16),
                       nis=A.alloc(f"nis{h}", (97,)), nsb=A.alloc(f"nsb{h}", (97,)), kh=A.alloc(f"kh{h}", (96,), BF16), sm=A.alloc(f"sm{h}", (4,))))

    def head_stream(h):
        hb = HB[h]
        dg, Dm, W16, nis, nsb, kh, sm = (hb[n_] for n_ in ("dg", "Dm", "W16", "nis", "nsb", "kh", "sm"))
        bA, bB = 2 * h, 2 * h + 1
        kA, kB = f"ps{bA}", f"ps{bB}"
        pbt = ps[bA][:, 384:432].bitcast(BF16)
        for d in range(2):
            P.op("pool", lambda e: e.memset(C32.ap[:, h, :], 0.0), writes=[C32.k(h)])
            P.op("pool", lambda e: e.memset(Cb.ap[:, h, :], 0.0), writes=[Cb.k(h)])
            nm = k.negm[d]
            endc = 127 if d == 0 else 0
            order = range(NT) if d == 0 else range(NT - 1, -1, -1)
            for i in order:
                yield from chunk(h, d, i, nm, endc, dg, Dm, W16, nis, nsb, kh, sm, bA, bB, kA, kB, pbt)

    def chunk(h, d, i, nm, endc, dg, Dm, W16, nis, nsb, kh, sm, bA, bB, kA, kB, pbt):
        tsl = slice(i * 128, (i + 1) * 128)
        P.op("pe", lambda e: e.matmul(ps[bA][:, 0:128], qkT.ap[0:96, 4 + h, tsl], qkT.ap[0:96, h, tsl], start=True, stop=True),
             reads=[qkT.k(4 + h), qkT.k(h)], writes=[kA])
        P.op("dve", lambda e: e.tensor_scalar(dg.ap, k.ident.ap, BL[d].ap[:, i, h:h + 1], None, op0=ALU.mult),
             reads=[k.ident.k(), BL[d].k()], writes=[dg.k()])
        P.op("pe", lambda e: e.matmul(ps[bA][:, 128:256], k.ones32.ap, dg.ap, start=True, stop=False), reads=[k.ones32.k(), dg.k()], writes=[kA])
        P.op("pe", lambda e: e.matmul(ps[bA][:, 128:256], k.ident.ap, nm.ap, start=False, stop=True), reads=[k.ident.k(), nm.k()], writes=[kA])
        yield
        P.op("act", lambda e: e.activation(Dm.ap, ps[bA][:, 128:256], AF.Exp, bias=IMB[d].ap[:, i, h:h + 1]), reads=[kA, IMB[d].k()], writes=[Dm.k()])
        P.op("act", lambda e: e.activation(sm.ap[:, 2:3], ps[bA][:, 128 + endc:129 + endc], AF.Exp), reads=[kA], writes=[sm.k(2)])
        P.op("dve", lambda e: e.tensor_tensor(W16.ap, ps[bA][:, 0:128], Dm.ap, op=ALU.mult), reads=[kA, Dm.k()], writes=[W16.k()])
        yield
        P.op("pe", lambda e: e.matmul(ps[bB][:, 0:97], W16.ap, vp.ap[:, i, h, :], start=True, stop=True), reads=[W16.k(), vp.k(i)], writes=[kB])
        P.op("pe", lambda e: e.matmul(ps[bB][:, 128:225], qkT.ap[0:96, h, tsl], Cb.ap[0:96, h, :], start=True, stop=True), reads=[qkT.k(h), Cb.k(h)], writes=[kB])
        P.op("pe", lambda e: e.transpose(pbt, qkT.ap[0:96, 4 + h, tsl], k.ident16.ap[0:96, 0:96]), reads=[qkT.k(4 + h), k.ident16.k()], writes=[kA])
        yield
        P.op("act", lambda e: e.copy(nis.ap, ps[bB][:, 0:97]), reads=[kB], writes=[nis.k()])
        P.op("dve", lambda e: e.tensor_scalar(kh.ap, pbt, Dm.ap[:, endc:endc + 1], None, op0=ALU.mult), reads=[kA, Dm.k()], writes=[kh.k()])
        P.op("dve", lambda e: e.scalar_tensor_tensor(nsb.ap, ps[bB][:, 128:225], EBL[d].ap[:, i, h:h + 1], nis.ap, op0=ALU.mult, op1=ALU.add),
             reads=[kB, EBL[d].k(), nis.k()], writes=[nsb.k()])
        P.op("pe", lambda e: e.matmul(ps[bA][0:96, 256:353], kh.ap, vp.ap[:, i, h, :], start=True, stop=True), reads=[kh.k(), vp.k(i)], writes=[kA])
        yield
        P.op("dve", lambda e: e.tensor_scalar(sm.ap[:, 0:1], nsb.ap[:, 96:97], -1.0, None, op0=ALU.mult), reads=[nsb.k()], writes=[sm.k(0)])
        P.op("dve", lambda e: e.scalar_tensor_tensor(sm.ap[:, 0:1], nsb.ap[:, 96:97], 1.0, sm.ap[:, 0:1], op0=ALU.max, op1=ALU.max), reads=[nsb.k(), sm.k(0)], writes=[sm.k(0)])
        P.op("dve", lambda e: e.reciprocal(sm.ap[:, 0:1], sm.ap[:, 0:1]), reads=[sm.k(0)], writes=[sm.k(0)])
        if d == 0:
            P.op("dve", lambda e: e.tensor_scalar(hsum.ap[:, i, h, :], nsb.ap[:, 0:96], sm.ap[:, 0:1], None, op0=ALU.mult), reads=[nsb.k(), sm.k(0)], writes=[hsum.k(i, h)])
        else:
            P.op("dve", lambda e: e.scalar_tensor_tensor(hsum.ap[:, i, h, :], nsb.ap[:, 0:96], sm.ap[:, 0:1], hsum.ap[:, i, h, :], op0=ALU.mult, op1=ALU.add),
                 reads=[nsb.k(), sm.k(0), hsum.k(i, h)], writes=[hsum.k(i, h)])
        P.op("dve", lambda e: e.scalar_tensor_tensor(C32.ap[0:96, h, :], C32.ap[0:96, h, :], sm.ap[0:96, 2:3], ps[bA][0:96, 256:353], op0=ALU.mult, op1=ALU.add),
             reads=[C32.k(h), sm.k(2), kA], writes=[C32.k(h)])
        P.op("act", lambda e: e.copy(Cb.ap[0:96, h, :], C32.ap[0:96, h, :]), reads=[C32.k(h)], writes=[Cb.k(h)])
        yield

    gens = [head_stream(h) for h in range(4)]
    while gens:
        for g_ in list(gens):
            try:
                next(g_)
            except StopIteration:
                gens.remove(g_)
    A.release(m1)
    yc = A.alloc("yc", (NT, 384), BF16)
    m3 = A.mark()
    wmo = load_w_bf16(k, "wmo", IN["w_in"].ap()[l][:, OFF_MO:OFF_MG], 8, 384, "wmo")
    lng = A.alloc("lng", (384,))
    P.dma("sp", lng.ap, IN["ml_ln_g"].ap()[l].partition_broadcast(128), writes=[lng.k()], semkey="lng")
    st = A.alloc("st", (16,)); cen = [A.alloc(f"cen{i}", (4, 96)) for i in range(2)]; junk = A.alloc("junkm", (96,))
    og = [A.alloc(f"og{i}", (384,)) for i in range(2)]
    for i in range(NT):
        r = i % 2
        b = 2 + r
        for c in range(8):
            P.op("pe", lambda e, c=c, i=i, b=b: e.matmul(ps[b][:, 0:384], k.hT.ap[:, c, i * 128:(i + 1) * 128], wmo.ap[:, c, :], start=(c == 0), stop=(c == 7)),
                 reads=[wmo.k(c), k.hT.k(i)], writes=[f"ps{b}"])
        P.op("act", lambda e, r=r, b=b: e.activation(og[r].ap, ps[b][:, 0:384], AF.Sigmoid), reads=[f"ps{b}"], writes=[og[r].k()])
        sk = st.k()
        P.op("dve", lambda e, i=i: e.tensor_reduce(st.ap[:, 0:4], hsum.ap[:, i, :, :], axis=AX.X, op=ALU.add), reads=[hsum.k(i, h_) for h_ in range(4)], writes=[sk])
        P.op("dve", lambda e: e.tensor_scalar(st.ap[:, 0:4], st.ap[:, 0:4], 1.0 / 96, None, op0=ALU.mult), reads=[sk], writes=[sk])
        P.op("pool", lambda e: e.memset(st.ap[:, 4:8], 0.0), writes=[sk])
        for h in range(4):
            P.op("dve", lambda e, i=i, h=h, r=r: e.tensor_scalar(cen[r].ap[:, h, :], hsum.ap[:, i, h, :], st.ap[:, h:h + 1], None, op0=ALU.subtract),
                 reads=[hsum.k(i, h), sk], writes=[cen[r].k(h)])
            P.op("act", lambda e, h=h, r=r: e.activation(junk.ap, cen[r].ap[:, h, :], AF.Square, accum_out=st.ap[:, 4 + h:5 + h]),
                 reads=[cen[r].k(h)], writes=[junk.k(), sk])
        P.op("act", lambda e: e.activation(st.ap[:, 8:12], st.ap[:, 4:8], AF.Sqrt, scale=1.0 / 96, bias=k.eps6.ap[:, 0:1]), reads=[sk, k.eps6.k()], writes=[sk])
        P.op("dve", lambda e: e.reciprocal(st.ap[:, 8:12], st.ap[:, 8:12]), reads=[sk], writes=[sk])
        for h in range(4):
            P.op("dve", lambda e, h=h, r=r: e.scalar_tensor_tensor(cen[r].ap[:, h, :], cen[r].ap[:, h, :], st.ap[:, 8 + h:9 + h], lng.ap[:, h * 96:(h + 1) * 96],
                                                                 op0=ALU.mult, op1=ALU.mult),
                 reads=[cen[r].k(h), sk, lng.k()], writes=[cen[r].k(h)])
        P.op("dve", lambda e, i=i, r=r: e.tensor_tensor(yc.ap[:, i, :], cen[r].ap.rearrange("p h c -> p (h c)"), og[r].ap, op=ALU.mult),
             reads=[cen[r].k(h) for h in range(4)] + [og[r].k()], writes=[yc.k(i)])
    A.release(m3)
    if k.dbg and "yc" in k.dbg:
        t = k.nc.dram_tensor("dbg_yc", [S, 384], BF16, kind="ExternalOutput")
        k.final_events.append(P.dma("sp", t.ap().rearrange("(i p) c -> p i c", p=128), yc.ap, reads=[yc.k(i) for i in range(NT)], semkey="dbg"))
    ycT = A.alloc("ycT", (3, S), BF16)
    transpose_to_T(k, yc, 3, ycT)
    outproj_partial(k, l, ycT, 3, 640, "c")
    A.release(m0)


OFF_RKV = 768
BLK = 256
PC_MU, PC_MUX, PC_W0, PC_A0, PC_KK, PC_KA, PC_RK, PC_LNG, PC_LNB, NPC = 0, 18, 20, 26, 32, 35, 38, 41, 44, 48


def host_rwkv_layout(inputs):
    out = {}
    pc = np.zeros((DEPTH, 128, NPC), np.float32)
    for l in range(DEPTH):
        mu = np.asarray(inputs["rk_mu"][l], np.float32)
        for d in range(2):
            for j in range(3):
                for p in range(3):
                    pc[l, :, PC_MU + d * 9 + j * 3 + p] = mu[d, j * 384 + p * 128: j * 384 + (p + 1) * 128]
            pc[l, 0:64, PC_MUX + d] = mu[d, 1152:1216]
            pc[l, 64:128, PC_MUX + d] = mu[d, 1216:1280]
            for p in range(3):
                pc[l, :, PC_W0 + d * 3 + p] = inputs["rk_w0"][l][d][p * 128:(p + 1) * 128]
                pc[l, :, PC_A0 + d * 3 + p] = inputs["rk_a0"][l][d][p * 128:(p + 1) * 128]
        for p in range(3):
            sl = slice(p * 128, (p + 1) * 128)
            pc[l, :, PC_KK + p] = inputs["rk_kk"][l][sl]
            pc[l, :, PC_KA + p] = inputs["rk_ka"][l][sl]
            pc[l, :, PC_RK + p] = np.asarray(inputs["rk_rk"][l]).reshape(384)[sl]
            pc[l, :, PC_LNG + p] = inputs["rk_ln_g"][l][sl]
            pc[l, :, PC_LNB + p] = inputs["rk_ln_b"][l][sl]
    out["c_pc"] = pc
    w_in = np.asarray(inputs["w_in"], np.float32)
    out["c_wx"] = np.ascontiguousarray(np.concatenate(
        [w_in[:, :, 1920:1984], w_in[:, :, 2048:2112], w_in[:, :, 1984:2048], w_in[:, :, 2112:2176], w_in[:, :, 2176:2304]], axis=2))
    return out


def rev_ap(ap, n):
    a = ap.ap
    return bass.AP(ap.tensor, ap.offset + (n - 1) * a[-1][0], [list(a[0]), [-a[-1][0], n]])


def psk(b, c0, c1):
    return [f"ps{b}q{q}" for q in range(c0 // 128, (c1 + 127) // 128)]


def rwkv_stage(k, l):
    P, A, ps, IN = k.P, k.A, k.ps, k.IN
    m0 = A.mark()
    k.mask4 = A.alloc("mask4", (512,)); k.rmask = A.alloc("rmask", (256,))
    P.dma("act", k.mask4.ap, IN["c_mask4"].ap(), writes=[k.mask4.k()], semkey="c5")
    P.dma("act", k.rmask.ap, IN["c_rmask"].ap(), writes=[k.rmask.k()], semkey="c7")
    ybT = A.alloc("ybT", (3, S), BF16)
    P.op("pool", lambda e: e.memset(ybT.ap, 0.0), writes=[ybT.k(i) for i in range(NT)])
    xsp = k.xspill.ap().rearrange("(i p) d -> p i d", p=128)
    for i in range(NT):
        P.dma("sp" if i % 2 == 0 else "act", xsp[:, i, :], k.xres.ap[:, i, :], reads=[k.xres.k(i)], writes=[("xsp", i)], semkey=("xso", i % 4))
    evs = []
    for kk_ in [q for q in P.keys if isinstance(q, tuple) and q[0] == k.xres.uid]:
        st = P.keys.pop(kk_)
        evs.extend(st["w"]); evs.extend(st["r"].values())
    AX = Arena.__new__(Arena)
    AX.P, AX.t, AX.n, AX.top, AX.gen, AX.live = P, A.t, k.xres.hi, k.xres.lo, 100000 + 1000 * l, []
    AX.pending = [(k.xres.lo, k.xres.hi, evs)]
    import os as _os
    LVL = int(_os.environ.get("RWKV_SETUP", "9"))
    pc = A.alloc("pc", (NPC + 4,))
    P.dma("sp", pc.ap[:, 0:NPC], IN["c_pc"].ap()[l], writes=[pc.k()], semkey="pc")
    PCO = NPC
    P.op("dve", lambda e: e.tensor_scalar(pc.ap[:, PCO:PCO + 3], pc.ap[:, PC_KA:PC_KA + 3], -1.0, 1.0, op0=ALU.mult, op1=ALU.add), reads=[pc.k()], writes=[pc.k()])
    wl = [A.alloc(f"wl{d}", (384,)) for d in range(2)]
    for d in range(2):
        P.dma("sp", wl[d].ap[0:64, :], IN["rk_w2"].ap()[l][d], writes=[wl[d].k()], semkey=("wl", d))
        P.dma("act", wl[d].ap[64:128, :], IN["rk_a2"].ap()[l][d], writes=[wl[d].k()], semkey=("wl2", d))
    g2b = A.alloc("g2b", (384,), BF16)
    P.dma("pool", g2b.ap, IN["rk_g2"].ap()[l], writes=[g2b.k()], semkey="g2b")
    siggd = A.alloc("siggd", (S,), BF16)
    wdad = [(A if int(_os.environ.get("WDAD_MAIN", "0")) else AX).alloc(f"wdad{d}", (S,)) for d in range(2)]
    mA = A.mark()
    wx = load_w_bf16(k, "wx", IN["c_wx"].ap()[l], 8, 384, "wx")
    zx = A.alloc("zx", (S + 2,)); tmp = A.alloc("tmpx", (S,))
    P.op("pool", lambda e: e.memset(zx.ap[:, 0:1], 0.0), writes=[zx.k()])
    P.op("pool", lambda e: e.memset(zx.ap[:, S + 1:S + 2], 0.0), writes=[zx.k()])
    n = 0
    for d in range(2 if LVL >= 2 else 0):
        for tg in range(4):
            b = n % 2; n += 1
            for c in range(8):
                P.op("pe", lambda e, c=c, d=d, tg=tg, b=b: e.matmul(ps[b][:, :], wx.ap[:, c, d * 128:(d + 1) * 128], k.hT.ap[:, c, tg * 512:(tg + 1) * 512], start=(c == 0), stop=(c == 7)),
                     reads=[wx.k(c)] + [k.hT.k(4 * tg + q) for q in range(4)], writes=[f"ps{b}"])
            P.op("act", lambda e, tg=tg, b=b: e.copy(zx.ap[:, 1 + tg * 512:1 + (tg + 1) * 512], ps[b][:, :]), reads=[f"ps{b}"], writes=[zx.k()])
        if LVL < 3:
            continue
        if d == 0:
            cur, prv = zx.ap[:, 1:S + 1], zx.ap[:, 0:S]
        else:
            cur, prv = rev_ap(zx.ap[:, 1:S + 1], S), rev_ap(zx.ap[:, 2:S + 2], S)
        P.op("dve", lambda e, cur=cur, prv=prv: e.tensor_tensor(tmp.ap, prv, cur, op=ALU.subtract), reads=[zx.k()], writes=[tmp.k()])
        P.op("dve", lambda e, cur=cur, d=d: e.scalar_tensor_tensor(wdad[d].ap, tmp.ap, pc.ap[:, PC_MUX + d:PC_MUX + d + 1], cur, op0=ALU.mult, op1=ALU.add),
             reads=[tmp.k(), pc.k(), zx.k()], writes=[wdad[d].k()])
        P.op("act", lambda e, d=d: e.activation(wdad[d].ap[0:64, :], wdad[d].ap[0:64, :], AF.Tanh), reads=[wdad[d].k()], writes=[wdad[d].k()])
    for tg in range(4 if LVL >= 4 else 0):
        b = n % 2; n += 1
        for c in range(8):
            P.op("pe", lambda e, c=c, tg=tg, b=b: e.matmul(ps[b][:, :], wx.ap[:, c, 256:384], k.hT.ap[:, c, tg * 512:(tg + 1) * 512], start=(c == 0), stop=(c == 7)),
                 reads=[wx.k(c)] + [k.hT.k(4 * tg + q) for q in range(4)], writes=[f"ps{b}"])
        P.op("act", lambda e, tg=tg, b=b: e.activation(siggd.ap[:, tg * 512:(tg + 1) * 512], ps[b][:, :], AF.Sigmoid), reads=[f"ps{b}"], writes=[siggd.k()])
    A.release(mA)
    NB = ["XR", "XK", "XV", "T1", "LW", "AA", "KK", "SQ", "CUM"]
    NB16 = ["XRb", "XKb", "KKb", "AAb", "XVb", "BH", "KH"]
    SB = []
    for s_ in range(2):
        sb = {nm: A.alloc(f"{nm}{s_}", (BLK,)) for nm in NB}
        sb.update({nm: A.alloc(f"{nm}{s_}", (BLK,), BF16) for nm in NB16})
        sb["PCc"] = A.alloc(f"PCc{s_}", (BLK // 128,))
        sb["FT"] = A.alloc(f"FT{s_}", (512,))
        sb["AM"] = [A.alloc(f"AM{s_}{x}", (512,), BF16) for x in range(2)]
        sb["M"] = [[A.alloc(f"M{s_}{x}{q}", (128,), BF16) for q in range(2)] for x in range(2)]
        sb["MT"] = [[A.alloc(f"MT{s_}{x}{q}", (128,), BF16) for q in range(2)] for x in range(2)]
        sb["MM"] = [[A.alloc(f"MM{s_}{x}{q}", (256,), BF16) for q in range(2)] for x in range(2)]
        sb["Q"] = [A.alloc(f"Q{s_}{x}", (128,)) for x in range(2)]
        sb["Q16"] = [A.alloc(f"Qb{s_}{x}", (128,), BF16) for x in range(2)]
        sb["RHS"] = A.alloc(f"RHS{s_}", (128,), BF16)
        sb["SAz"] = [A.alloc(f"SAz{s_}{x}", (128,), BF16) for x in range(2)]
        sb["Vz"] = [A.alloc(f"Vz{s_}{x}", (128,), BF16) for x in range(2)]
        sb["BHt"] = A.alloc(f"BHt{s_}", (128,), BF16); sb["KHt"] = A.alloc(f"KHt{s_}", (128,), BF16)
        sb["T"] = A.alloc(f"Tst{s_}", (128,)); sb["T16"] = A.alloc(f"Tsb{s_}", (128,), BF16)
        sb["pb"] = 4 * s_
        SB.append(sb)
    import os as _os
    for p in range(int(_os.environ.get('RWKV_PAIRS', '3'))):
        mX = AX.mark()
        wp = AX.alloc("wp", (8, 3, 128), BF16)
        wv_ = IN["w_in"].ap()[l].rearrange("(c q) n -> q c n", q=128)
        for j in range(3):
            c0 = OFF_RKV + j * 384 + p * 128
            for c in range(8):
                P.dma("pool", wp.ap[:, c, j, :], wv_[:, c, c0:c0 + 128], writes=[wp.k(j)], semkey=("wp", j))
        zp = AX.alloc("zp", (3, S + 2))
        yacc = AX.alloc("yacc", (S,)); bonacc = AX.alloc("bonacc", (S,))
        P.op("pool", lambda e: e.memset(yacc.ap, 0.0), writes=[yacc.k(c) for c in range(NT)])
        P.op("pool", lambda e: e.memset(bonacc.ap, 0.0), writes=[bonacc.k(c) for c in range(NT)])
        for j in range(3):
            P.op("pool", lambda e, j=j: e.memset(zp.ap[:, j, 0:1], 0.0), writes=[zp.k(j)])
            P.op("pool", lambda e, j=j: e.memset(zp.ap[:, j, S + 1:S + 2], 0.0), writes=[zp.k(j)])
            for tg in range(4):
                b = n % 2; n += 1
                for c in range(8):
                    P.op("pe", lambda e, c=c, j=j, tg=tg, b=b: e.matmul(ps[b][:, :], wp.ap[:, c, j, :], k.hT.ap[:, c, tg * 512:(tg + 1) * 512], start=(c == 0), stop=(c == 7)),
                         reads=[wp.k(j)] + [k.hT.k(4 * tg + q) for q in range(4)], writes=[f"ps{b}"] + psk(b, 0, 512))
                P.op("act", lambda e, j=j, tg=tg, b=b: e.copy(zp.ap[:, j, 1 + tg * 512:1 + (tg + 1) * 512], ps[b][:, :]), reads=[f"ps{b}"] + psk(b, 0, 512), writes=[zp.k(j)])
        gens = [rwkv_stream(k, l, p, d, SB[d], pc, wl[d], wdad[d], zp, yacc, bonacc, PCO) for d in range(int(_os.environ.get("RWKV_NDIR", "2")))]
        while gens:
            for g in list(gens):
                try:
                    next(g)
                except StopIteration:
                    gens.remove(g)
        T1 = SB[0]["FT"]; T2 = SB[1]["FT"]
        for tg in range(4 if int(_os.environ.get("RWKV_FIN", "1")) else 0):
            cs = slice(tg * 512, (tg + 1) * 512)
            yk = [yacc.k(c) for c in range(4 * tg, 4 * tg + 4)]
            P.op("pe", lambda e, cs=cs: e.matmul(ps[0][:, :], k.onesblk64.ap, yacc.ap[:, cs], start=True, stop=True), reads=[k.onesblk64.k()] + yk, writes=["ps0"] + psk(0, 0, 512))
            P.op("dve", lambda e, cs=cs: e.tensor_tensor(yacc.ap[:, cs], yacc.ap[:, cs], ps[0][:, :], op=ALU.subtract), reads=["ps0"] + psk(0, 0, 512) + yk, writes=yk)
            P.op("act", lambda e, cs=cs: e.activation(T1.ap, yacc.ap[:, cs], AF.Square), reads=yk, writes=[T1.k()])
            P.op("pe", lambda e: e.matmul(ps[1][:, :], k.onesblk64.ap, T1.ap, start=True, stop=True), reads=[k.onesblk64.k(), T1.k()], writes=["ps1"] + psk(1, 0, 512))
            P.op("act", lambda e: e.activation(T2.ap, ps[1][:, :], AF.Sqrt, bias=k.epsgn.ap[:, 0:1]), reads=["ps1", k.epsgn.k()] + psk(1, 0, 512), writes=[T2.k()])
            P.op("dve", lambda e: e.reciprocal(T2.ap, T2.ap), reads=[T2.k()], writes=[T2.k()])
            P.op("dve", lambda e, cs=cs: e.tensor_tensor(yacc.ap[:, cs], yacc.ap[:, cs], T2.ap, op=ALU.mult), reads=yk + [T2.k()], writes=yk)
            P.op("dve", lambda e, cs=cs, p=p: e.tensor_scalar(yacc.ap[:, cs], yacc.ap[:, cs], pc.ap[:, PC_LNG + p:PC_LNG + p + 1], pc.ap[:, PC_LNB + p:PC_LNB + p + 1], op0=ALU.mult, op1=ALU.add),
                 reads=yk + [pc.k()], writes=yk)
            P.op("dve", lambda e, cs=cs: e.tensor_tensor(yacc.ap[:, cs], yacc.ap[:, cs], bonacc.ap[:, cs], op=ALU.add), reads=yk + [bonacc.k(c) for c in range(4 * tg, 4 * tg + 4)], writes=yk)
            P.op("pe", lambda e, cs=cs, p=p: e.matmul(ps[2][:, :], g2b.ap[:, p * 128:(p + 1) * 128], siggd.ap[:, cs], start=True, stop=True), reads=[g2b.k(), siggd.k()], writes=["ps2"] + psk(2, 0, 512))
            P.op("dve", lambda e, cs=cs, p=p: e.tensor_tensor(ybT.ap[:, p, cs], yacc.ap[:, cs], ps[2][:, :], op=ALU.mult), reads=yk + ["ps2"] + psk(2, 0, 512), writes=[ybT.k(c) for c in range(4 * tg, 4 * tg + 4)])
        AX.release(mX)
    AX.release((k.xres.lo, 0))
    evs = []
    for (_, _, e_) in AX.pending:
        evs.extend(e_)
    P.inherit[k.xres.uid] = evs
    for i in range(NT):
        P.dma("sp" if i % 2 == 0 else "act", k.xres.ap[:, i, :], xsp[:, i, :], reads=[("xsp", i)], writes=[k.xres.k(i)], semkey=("xsi", i % 4))
    if k.dbg and "yb" in k.dbg:
        t = k.nc.dram_tensor("dbg_yb", [384, S], BF16, kind="ExternalOutput")
        k.final_events.append(P.dma("sp", t.ap().rearrange("(c p) s -> p c s", p=128), ybT.ap, reads=[ybT.k(i) for i in range(NT)], semkey="dbg"))
    if LVL >= 5:
        outproj_partial(k, l, ybT, 3, 256, "b")
    A.release(m0)


def rwkv_stream(k, l, p, d, sb, pc, wl, wdad, zp, yacc, bonacc, PCO):
    P, ps = k.P, k.ps
    pb = sb["pb"]
    B0, B1, B2, B3 = pb, pb + 1, pb + 2, pb + 3
    XR, XK, XV, T1, LW, AA, KK, SQ, CUM, BH, KH, PCc = (sb[n_] for n_ in ["XR", "XK", "XV", "T1", "LW", "AA", "KK", "SQ", "CUM", "BH", "KH", "PCc"])
    XRb, XKb, KKb, AAb, XVb = (sb[n_] for n_ in ["XRb", "XKb", "KKb", "AAb", "XVb"])
    T = sb["T"]; T16 = sb["T16"]
    P.op("pool", lambda e: e.memset(T16.ap, 0.0), writes=[T16.k()])
    col = lambda c: pc.ap[:, c:c + 1]
    P.op("pool", lambda e: e.memset(T.ap, 0.0), writes=[T.k()])
    for x in range(2):
        P.op("pool", lambda e, x=x: e.memset(sb["SAz"][x].ap, 0.0), writes=[sb["SAz"][x].k()])
        P.op("pool", lambda e, x=x: e.memset(sb["Vz"][x].ap, 0.0), writes=[sb["Vz"][x].k()])
    import os as _os
    NBLK = int(_os.environ.get("RWKV_NBLK", str(S // BLK))); PH = int(_os.environ.get("RWKV_PHASE", "9"))
    def _blk(bi):
        t0 = bi * BLK
        if d == 0:
            cur = lambda j: zp.ap[:, j, 1 + t0:1 + t0 + BLK]
            prv = lambda j: zp.ap[:, j, t0:t0 + BLK]
            nat = lambda buf: buf.ap[:, t0:t0 + BLK]
            nchunks = list(range(t0 // 128, (t0 + BLK) // 128))
        else:
            a_ = S - t0 - BLK
            cur = lambda j: rev_ap(zp.ap[:, j, 1 + a_:1 + a_ + BLK], BLK)
            prv = lambda j: rev_ap(zp.ap[:, j, 2 + a_:2 + a_ + BLK], BLK)
            nat = lambda buf: rev_ap(buf.ap[:, a_:a_ + BLK], BLK)
            nchunks = list(range(a_ // 128, (a_ + BLK) // 128))
        scols = slice(t0, t0 + BLK)
        P0 = int(_os.environ.get("RWKV_P0", "9"))
        for j, X in enumerate((XR, XK, XV)):
            if P0 < 2:
                break
            P.op("dve", lambda e, j=j, prv=prv, cur=cur: e.tensor_tensor(T1.ap, prv(j), cur(j), op=ALU.subtract), reads=[zp.k(j)], writes=[T1.k()])
            P.op("dve", lambda e, j=j, X=X, cur=cur: e.scalar_tensor_tensor(X.ap, T1.ap, col(PC_MU + d * 9 + j * 3 + p), cur(j), op0=ALU.mult, op1=ALU.add),
                 reads=[T1.k(), zp.k(j), pc.k()], writes=[X.k()])
        if P0 >= 3:
            P.op("pe", lambda e, scols=scols: e.matmul(ps[B3][:, 0:BLK], wl.ap[0:64, p * 128:(p + 1) * 128], wdad.ap[0:64, scols], start=True, stop=True),
                 reads=[wl.k(), wdad.k()], writes=psk(B3, 0, BLK), serial=True)
        if P0 >= 4:
            P.op("pe", lambda e, scols=scols: e.matmul(ps[B3][:, 256:256 + BLK], wl.ap[64:128, p * 128:(p + 1) * 128], wdad.ap[64:128, scols], start=True, stop=True),
                 reads=[wl.k(), wdad.k()], writes=psk(B3, 256, 256 + BLK), serial=True)
        if P0 >= 5:
            P.op("act", lambda e: e.activation(LW.ap, ps[B3][:, 0:BLK], AF.Sigmoid, bias=col(PC_W0 + d * 3 + p)), reads=psk(B3, 0, BLK) + [pc.k()], writes=[LW.k()])
            P.op("act", lambda e: e.activation(AA.ap, ps[B3][:, 256:256 + BLK], AF.Sigmoid, bias=col(PC_A0 + d * 3 + p)), reads=psk(B3, 256, 256 + BLK) + [pc.k()], writes=[AA.k()])
        if P0 >= 6:
            P.op("dve", lambda e: e.tensor_scalar(LW.ap, LW.ap, -0.6065306597126334, None, op0=ALU.mult), reads=[LW.k()], writes=[LW.k()])
        yield
        if PH <= 1:
            return
        P.op("dve", lambda e: e.tensor_scalar(KK.ap, XK.ap, col(PC_KK + p), None, op0=ALU.mult), reads=[XK.k(), pc.k()], writes=[KK.k()])
        P.op("act", lambda e: e.activation(SQ.ap, KK.ap, AF.Square), reads=[KK.k()], writes=[SQ.k()])
        P.op("pe", lambda e: e.matmul(ps[B2][:, 0:BLK], k.onesblk.ap, SQ.ap, start=True, stop=True), reads=[k.onesblk.k(), SQ.k()], writes=psk(B2, 0, BLK))
        P.op("act", lambda e: e.activation(SQ.ap, ps[B2][:, 0:BLK], AF.Sqrt), reads=psk(B2, 0, BLK), writes=[SQ.k()])
        P.op("dve", lambda e: e.tensor_scalar(SQ.ap, SQ.ap, 1e-12, None, op0=ALU.max), reads=[SQ.k()], writes=[SQ.k()])
        P.op("dve", lambda e: e.reciprocal(SQ.ap, SQ.ap), reads=[SQ.k()], writes=[SQ.k()])
        P.op("dve", lambda e: e.tensor_tensor(KK.ap, KK.ap, SQ.ap, op=ALU.mult), reads=[KK.k(), SQ.k()], writes=[KK.k()])
        P.op("dve", lambda e: e.tensor_scalar(T1.ap, AA.ap, col(PC_KA + p), col(PCO + p), op0=ALU.mult, op1=ALU.add), reads=[AA.k(), pc.k()], writes=[T1.k()])
        P.op("dve", lambda e: e.tensor_tensor(XK.ap, XK.ap, T1.ap, op=ALU.mult), reads=[XK.k(), T1.k()], writes=[XK.k()])
        P.op("dve", lambda e: e.scalar_tensor_tensor(T1.ap, XR.ap, col(PC_RK + p), XK.ap, op0=ALU.mult, op1=ALU.mult), reads=[XR.k(), XK.k(), pc.k()], writes=[T1.k()])
        P.op("pe", lambda e: e.matmul(ps[B2][:, 256:256 + BLK], k.onesblk.ap, T1.ap, start=True, stop=True), reads=[k.onesblk.k(), T1.k()], writes=psk(B2, 256, 256 + BLK))
        P.op("dve", lambda e: e.tensor_tensor(T1.ap, ps[B2][:, 256:256 + BLK], XV.ap, op=ALU.mult), reads=psk(B2, 256, 256 + BLK) + [XV.k()], writes=[T1.k()])
        bk = [bonacc.k(c) for c in nchunks]
        P.op("dve", lambda e: e.tensor_tensor(nat(bonacc), nat(bonacc), T1.ap, op=ALU.add), reads=bk + [T1.k()], writes=bk)
        P.op("dve", lambda e: e.tensor_tensor(AA.ap, AA.ap, KK.ap, op=ALU.mult), reads=[AA.k(), KK.k()], writes=[AA.k()])
        yield
        if PH <= 2:
            return
        P.op("dve", lambda e: e.tensor_tensor_scan(CUM.ap, k.rmask.ap[:, 0:BLK], LW.ap, 0.0, op0=ALU.mult, op1=ALU.add), reads=[k.rmask.k(), LW.k()], writes=[CUM.k()])
        P.op("dve", lambda e: e.tensor_tensor(LW.ap, CUM.ap, LW.ap, op=ALU.subtract), reads=[CUM.k(), LW.k()], writes=[LW.k()])
        P.op("act", lambda e: e.activation(T1.ap, CUM.ap, AF.Exp), reads=[CUM.k()], writes=[T1.k()])
        P.op("dve", lambda e: e.tensor_tensor(XR.ap, XR.ap, T1.ap, op=ALU.mult), reads=[XR.k(), T1.k()], writes=[XR.k()])
        P.op("act", lambda e: e.activation(SQ.ap, CUM.ap, AF.Exp, scale=-1.0), reads=[CUM.k()], writes=[SQ.k()])
        P.op("dve", lambda e: e.tensor_tensor(AA.ap, AA.ap, SQ.ap, op=ALU.mult), reads=[AA.k(), SQ.k()], writes=[AA.k()])
        P.op("dve", lambda e: e.tensor_tensor(XK.ap, XK.ap, SQ.ap, op=ALU.mult), reads=[XK.k(), SQ.k()], writes=[XK.k()])
        P.op("act", lambda e: e.activation(T1.ap, LW.ap, AF.Exp), reads=[LW.k()], writes=[T1.k()])
        P.op("dve", lambda e: e.scalar_tensor_tensor(KK.ap, KK.ap, -1.0, T1.ap, op0=ALU.mult, op1=ALU.mult), reads=[KK.k(), T1.k()], writes=[KK.k()])
        for src_, dst_ in ((XR, XRb), (XK, XKb), (KK, KKb), (AA, AAb), (XV, XVb)):
            P.op("act", lambda e, src_=src_, dst_=dst_: e.copy(dst_.ap, src_.ap), reads=[src_.k()], writes=[dst_.k()])
        P.op("act", lambda e: e.activation(PCc.ap, CUM.ap.rearrange("p (c t) -> p c t", t=128)[:, :, 127], AF.Exp), reads=[CUM.k()], writes=[PCc.k()])
        for ch in range(BLK // 128):
            cs = slice(ch * 128, (ch + 1) * 128)
            P.op("dve", lambda e, cs=cs, ch=ch: e.tensor_scalar(BH.ap[:, cs], AA.ap[:, cs], PCc.ap[:, ch:ch + 1], None, op0=ALU.mult), reads=[AA.k(), PCc.k()], writes=[BH.k()])
            P.op("dve", lambda e, cs=cs, ch=ch: e.tensor_scalar(KH.ap[:, cs], XK.ap[:, cs], PCc.ap[:, ch:ch + 1], None, op0=ALU.mult), reads=[XK.k(), PCc.k()], writes=[KH.k()])
        yield
        if PH <= 3:
            return
        def _chunk(ch):
            cs = slice(ch * 128, (ch + 1) * 128)
            AM, Mb, MTb, Q, RHS, SAz, Vz, BHt, KHt = sb["AM"], sb["M"], sb["MT"], sb["Q"], sb["RHS"], sb["SAz"], sb["Vz"], sb["BHt"], sb["KHt"]
            for x in range(2):
                hs = slice(64 * x, 64 * x + 64)
                bx = B0 + x
                for q, (lh, rh) in enumerate(((AAb, KKb), (AAb, XRb), (XKb, KKb), (XKb, XRb))):
                    P.op("pe", lambda e, lh=lh, rh=rh, q=q, hs=hs, bx=bx: e.matmul(ps[bx][:, q * 128:(q + 1) * 128], lh.ap[hs, cs], rh.ap[hs, cs], start=True, stop=True),
                         reads=[lh.k(), rh.k()], writes=psk(bx, q * 128, (q + 1) * 128), serial=True)
                P.op("pe", lambda e, hs=hs, x=x: e.matmul(ps[B2][:, x * 128:(x + 1) * 128], KKb.ap[hs, cs], AAb.ap[hs, cs], start=True, stop=True),
                     reads=[KKb.k(), AAb.k()], writes=psk(B2, x * 128, (x + 1) * 128), serial=True)
                P.op("dve", lambda e, x=x, bx=bx: e.tensor_tensor(AM[x].ap, ps[bx][:, :], k.mask4.ap, op=ALU.mult), reads=psk(bx, 0, 512) + [k.mask4.k()], writes=[AM[x].k()])
                P.op("dve", lambda e, x=x: e.tensor_tensor(MTb[x][0].ap, ps[B2][:, x * 128:(x + 1) * 128], k.trils.ap, op=ALU.mult), reads=psk(B2, x * 128, (x + 1) * 128) + [k.trils.k()], writes=[MTb[x][0].k()])
                P.op("dve", lambda e, x=x: e.tensor_tensor(Q[x].ap, AM[x].ap[:, 0:128], k.ident.ap, op=ALU.add), reads=[AM[x].k(), k.ident.k()], writes=[Q[x].k()])
                P.op("act", lambda e, x=x: e.copy(sb["Q16"][x].ap, Q[x].ap), reads=[Q[x].k()], writes=[sb["Q16"][x].k()])
            yield
            if PH <= 4:
                return
            pbt = ps[B3][:, 0:192].bitcast(BF16)
            for q, src in enumerate((XVb, BH, KH)):
                P.op("pe", lambda e, q=q, src=src: e.transpose(pbt[:, q * 128:(q + 1) * 128], src.ap[:, cs], k.ident16.ap), reads=[src.k(), k.ident16.k()], writes=psk(B3, 0, 192))
            P.op("act", lambda e: e.copy(Vz[0].ap[:, 0:64], pbt[:, 0:64]), reads=psk(B3, 0, 192), writes=[Vz[0].k()])
            P.op("act", lambda e: e.copy(Vz[1].ap[:, 64:128], pbt[:, 64:128]), reads=psk(B3, 0, 192), writes=[Vz[1].k()])
            P.op("dve", lambda e: e.tensor_copy(BHt.ap, pbt[:, 128:256]), reads=psk(B3, 0, 192), writes=[BHt.k()])
            P.op("dve", lambda e: e.tensor_copy(KHt.ap, pbt[:, 256:384]), reads=psk(B3, 0, 192), writes=[KHt.k()])
            MM = sb["MM"]
            for lev in range(6):
                po = lev % 2
                for x in range(2):
                    bx = B0 + x
                    if lev == 0:
                        Mi = (AM[x].ap[:, 0:128], AM[x].k()); MTi = (MTb[x][0].ap, MTb[x][0].k())
                    else:
                        Mi = (MM[x][1 - po].ap[:, 0:128], MM[x][1 - po].k()); MTi = (MM[x][1 - po].ap[:, 128:256], MM[x][1 - po].k())
                    if lev < 5:
                        P.op("pe", lambda e, bx=bx, Mi=Mi, MTi=MTi: e.matmul(ps[bx][:, 0:128], MTi[0], Mi[0], start=True, stop=True), reads=[Mi[1], MTi[1]], writes=psk(bx, 0, 128))
                    P.op("pe", lambda e, bx=bx, Mi=Mi, MTi=MTi: e.matmul(ps[bx][:, 128:256], Mi[0], MTi[0], start=True, stop=True), reads=[Mi[1], MTi[1]], writes=psk(bx, 128, 256))
                    lo_ = 0 if lev < 5 else 128
                    P.op("act", lambda e, bx=bx, x=x, po=po, lo_=lo_: e.copy(MM[x][po].ap[:, lo_:256], ps[bx][:, lo_:256]), reads=psk(bx, 0, 256), writes=[MM[x][po].k()])
                for x in range(2):
                    P.op("pe", lambda e, x=x, po=po: e.matmul(ps[B2][:, x * 128:(x + 1) * 128], MM[x][po].ap[:, 128:256], sb["Q16"][x].ap, start=True, stop=True),
                         reads=[MM[x][po].k(), sb["Q16"][x].k()], writes=psk(B2, 0, 256))
                    P.op("dve", lambda e, x=x: e.tensor_tensor(sb["Q16"][x].ap, Q[x].ap, ps[B2][:, x * 128:(x + 1) * 128], op=ALU.add), reads=psk(B2, 0, 256) + [Q[x].k()], writes=[sb["Q16"][x].k()])
                    if lev < 5:
                        P.op("dve", lambda e, x=x: e.tensor_tensor(Q[x].ap, Q[x].ap, ps[B2][:, x * 128:(x + 1) * 128], op=ALU.add), reads=psk(B2, 0, 256) + [Q[x].k()], writes=[Q[x].k()])
                yield
            for x in range(2):
                hs = slice(64 * x, 64 * x + 64)
                P.op("pe", lambda e, hs=hs: e.matmul(ps[B2][:, 256 + hs.start:256 + hs.stop], KKb.ap[hs, cs], T16.ap[hs, hs], start=True, stop=False), reads=[KKb.k(), T16.k()], writes=psk(B2, 256, 384), serial=True)
                P.op("pe", lambda e, hs=hs, x=x: e.matmul(ps[B2][:, 256 + hs.start:256 + hs.stop], AM[x].ap[:, 256:384], Vz[x].ap[:, hs], start=False, stop=True), reads=[AM[x].k(), Vz[x].k()], writes=psk(B2, 256, 384))
            P.op("act", lambda e: e.copy(RHS.ap, ps[B2][:, 256:384]), reads=psk(B2, 256, 384), writes=[RHS.k()])
            for x in range(2):
                hs = slice(64 * x, 64 * x + 64)
                P.op("pe", lambda e, hs=hs, x=x: e.matmul(ps[B2][:, 384 + hs.start:384 + hs.stop], sb["Q16"][x].ap, RHS.ap[:, hs], start=True, stop=True), reads=[sb["Q16"][x].k(), RHS.k()], writes=psk(B2, 384, 512))
                P.op("act" if x == 0 else "dve", (lambda e, hs=hs, x=x: e.copy(SAz[x].ap[:, hs], ps[B2][:, 384 + hs.start:384 + hs.stop])) if x == 0 else
                     (lambda e, hs=hs, x=x: e.tensor_copy(SAz[x].ap[:, hs], ps[B2][:, 384 + hs.start:384 + hs.stop])), reads=psk(B2, 384, 512), writes=[SAz[x].k()])
            yield
            if PH <= 6:
                return
            ops_ = []
            for x in range(2):
                hs = slice(64 * x, 64 * x + 64)
                ops_.append((T16.ap[hs, :], XRb.ap[hs, cs], [T16.k(), XRb.k()]))
                ops_.append((SAz[x].ap, AM[x].ap[:, 128:256], [SAz[x].k(), AM[x].k()]))
                ops_.append((Vz[x].ap, AM[x].ap[:, 384:512], [Vz[x].k(), AM[x].k()]))
            for q, (lh, rh, rd) in enumerate(ops_):
                P.op("pe", lambda e, lh=lh, rh=rh, q=q: e.matmul(ps[B3][:, 384:512], lh, rh, start=(q == 0), stop=(q == len(ops_) - 1)), reads=rd, writes=psk(B3, 384, 512), serial=(q % 3 == 0))
            cn = nchunks[ch] if d == 0 else nchunks[len(nchunks) - 1 - ch]
            if d == 0:
                ydst = yacc.ap[:, cn * 128:(cn + 1) * 128]
            else:
                ydst = rev_ap(yacc.ap[:, cn * 128:(cn + 1) * 128], 128)
            P.op("dve", lambda e, ydst=ydst: e.tensor_tensor(ydst, ydst, ps[B3][:, 384:512], op=ALU.add), reads=psk(B3, 384, 512) + [yacc.k(cn)], writes=[yacc.k(cn)])
            for x in range(2):
                hs = slice(64 * x, 64 * x + 64)
                P.op("pe", lambda e, hs=hs, x=x: e.matmul(ps[B2][:, hs], BHt.ap, SAz[x].ap[:, hs], start=True, stop=False), reads=[BHt.k(), SAz[x].k()], writes=psk(B2, 0, 128))
                P.op("pe", lambda e, hs=hs, x=x: e.matmul(ps[B2][:, hs], KHt.ap, Vz[x].ap[:, hs], start=False, stop=True), reads=[KHt.k(), Vz[x].k()], writes=psk(B2, 0, 128))
            for x in range(2):
                hs = slice(64 * x, 64 * x + 64)
                P.op("dve", lambda e, hs=hs, ch=ch: e.scalar_tensor_tensor(T.ap[hs, hs], T.ap[hs, hs], PCc.ap[hs, ch:ch + 1], ps[B2][hs, hs], op0=ALU.mult, op1=ALU.add),
                     reads=[T.k(), PCc.k()] + psk(B2, 0, 128), writes=[T.k()])
            P.op("act", lambda e: e.copy(T16.ap, T.ap), reads=[T.k()], writes=[T16.k()])
            yield
            if PH <= 7:
                return
        for ch in range(BLK // 128):
            yield from _chunk(ch)

    for bi in range(NBLK):
        yield from _blk(bi)


FB = 256
NFB = DFF // FB
NFC = DFF // 128
W2G = 2


def moe_stage(k, l):
    P, A, ps, IN = k.P, k.A, k.ps, k.IN
    m0 = A.mark()
    k.ohb = A.alloc("ohb", (2048,))
    P.dma("act", k.ohb.ap[0:16, :], IN["c_ohb"].ap(), writes=[k.ohb.k()], semkey="c4")
    x2b = A.alloc("x2b", (NT, D), BF16)
    aff = A.alloc("aff", (NT, NE))
    pm = A.alloc("pm", (NT, NE))
    pmT = A.alloc("pmT", (S,))
    m1 = A.mark()
    gb = A.alloc("gb2", (D,)); junk = A.alloc("junk2", (D,)); ss = A.alloc("ss2", (NT,)); rstd = A.alloc("rstd2", (NT,))
    x2f = [A.alloc(f"x2f{i}", (D,)) for i in range(2)]
    x2T = [A.alloc(f"x2T{i}", (8, 128)) for i in range(2)]
    rsb = A.alloc("rsb", (8, NE)); sm = A.alloc("smx", (NT, 4))
    affT = A.alloc("affT", (S,))
    P.dma("sp", gb.ap, IN["ln2_g"].ap()[l].partition_broadcast(128), writes=[gb.k()], semkey="gb")
    P.dma("act", rsb.ap, IN["router"].ap()[l].rearrange("(c p) e -> p c e", p=128), writes=[rsb.k()], semkey="rsb")
    P.op("pool", lambda e: e.memset(ss.ap, 0.0), writes=[ss.k(i) for i in range(NT)])
    P.op("pool", lambda e: e.memset(sm.ap, 0.0), writes=[sm.k()])
    for i in range(NT):
        P.op("act", lambda e, i=i: e.activation(junk.ap, k.xres.ap[:, i, :], AF.Square, accum_out=ss.ap[:, i:i + 1]),
             reads=[k.xres.k(i)], writes=[junk.k(), ss.k(i)])
    P.op("act", lambda e: e.activation(rstd.ap, ss.ap, AF.Sqrt, scale=1.0 / D, bias=k.eps6.ap[:, 0:1]),
         reads=[ss.k(i) for i in range(NT)] + [k.eps6.k()], writes=[rstd.k()])
    P.op("dve", lambda e: e.reciprocal(rstd.ap, rstd.ap), reads=[rstd.k()], writes=[rstd.k()])
    for i in range(NT):
        xf = x2f[i % 2]; xt = x2T[i % 2]
        P.op("dve", lambda e, i=i, xf=xf: e.scalar_tensor_tensor(xf.ap, k.xres.ap[:, i, :], rstd.ap[:, i:i + 1], gb.ap, op0=ALU.mult, op1=ALU.mult),
             reads=[k.xres.k(i), rstd.k(), gb.k()], writes=[xf.k()])
        P.op("act", lambda e, i=i, xf=xf: e.copy(x2b.ap[:, i, :], xf.ap), reads=[xf.k()], writes=[x2b.k(i)])
        for hb_ in range(2):
            b = 2 * (i % 2) + hb_
            for c in range(4):
                cc = hb_ * 4 + c
                P.op("pe", lambda e, b=b, c=c, cc=cc, xf=xf: e.transpose(ps[b][:, c * 128:(c + 1) * 128], xf.ap[:, cc * 128:(cc + 1) * 128], k.ident.ap),
                     reads=[xf.k(), k.ident.k()], writes=[f"ps{b}"])
            P.op("act" if hb_ == 0 else "dve",
                 (lambda e, b=b, hb_=hb_, xt=xt: e.copy(xt.ap[:, hb_ * 4:hb_ * 4 + 4, :], ps[b][:, :].rearrange("p (c t) -> p c t", c=4))) if hb_ == 0 else
                 (lambda e, b=b, hb_=hb_, xt=xt: e.tensor_copy(xt.ap[:, hb_ * 4:hb_ * 4 + 4, :], ps[b][:, :].rearrange("p (c t) -> p c t", c=4))),
                 reads=[f"ps{b}"], writes=[xt.k(hb_)])
        lb = 4 + i % 2
        for c in range(8):
            P.op("pe", lambda e, c=c, lb=lb, xt=xt: e.matmul(ps[lb][:, 0:NE], xt.ap[:, c, :], rsb.ap[:, c, :], start=(c == 0), stop=(c == 7)),
                 reads=[xt.k(0), xt.k(1), rsb.k()], writes=[f"ps{lb}"])
        P.op("dve", lambda e, i=i, lb=lb: e.tensor_reduce(sm.ap[:, i, 0:1], ps[lb][:, 0:NE], axis=AX.X, op=ALU.max), reads=[f"ps{lb}"], writes=[sm.k()])
        P.op("dve", lambda e, i=i: e.tensor_scalar(sm.ap[:, i, 0:1], sm.ap[:, i, 0:1], -1.0, None, op0=ALU.mult), reads=[sm.k()], writes=[sm.k()])
        P.op("act", lambda e, i=i, lb=lb: e.activation(aff.ap[:, i, :], ps[lb][:, 0:NE], AF.Exp, bias=sm.ap[:, i, 0:1], accum_out=sm.ap[:, i, 1:2]),
             reads=[f"ps{lb}", sm.k()], writes=[aff.k(), sm.k()])
        P.op("dve", lambda e, i=i: e.reciprocal(sm.ap[:, i, 2:3], sm.ap[:, i, 1:2]), reads=[sm.k()], writes=[sm.k()])
        P.op("dve", lambda e, i=i: e.tensor_scalar(aff.ap[:, i, :], aff.ap[:, i, :], sm.ap[:, i, 2:3], None, op0=ALU.mult), reads=[aff.k(), sm.k()], writes=[aff.k()])
        tb = 6 + (i // 4) % 2
        P.op("pe", lambda e, i=i, tb=tb: e.transpose(ps[tb][0:NE, (i % 4) * 128:(i % 4 + 1) * 128], aff.ap[:, i, :], k.ident.ap), reads=[aff.k(), k.ident.k()], writes=[f"ps{tb}"])
        if i % 4 == 3:
            P.op("act", lambda e, i=i, tb=tb: e.copy(affT.ap[0:NE, (i - 3) * 128:(i + 1) * 128], ps[tb][0:NE, :]), reads=[f"ps{tb}"], writes=[affT.k()])
    bs = A.alloc("bs", (8,)); bjunk = A.alloc("bjunk", (S,))
    LO, HI, MID, CNT, GE, D1 = (bs.ap[0:NE, j:j + 1] for j in range(6))
    P.op("pool", lambda e: e.memset(bs.ap, 0.0), writes=[bs.k()])
    P.op("pool", lambda e: e.memset(bs.ap[:, 1:2], 1.0), writes=[bs.k()])
    for it in range(30):
        P.op("dve", lambda e: e.tensor_tensor(MID, LO, HI, op=ALU.add), reads=[bs.k()], writes=[bs.k()])
        P.op("dve", lambda e: e.tensor_scalar(MID, MID, 0.5, None, op0=ALU.mult), reads=[bs.k()], writes=[bs.k()])
        P.op("dve", lambda e: e.tensor_scalar(bjunk.ap[0:NE, :], affT.ap[0:NE, :], MID, None, op0=ALU.is_gt, op1=ALU.add, accum_out=CNT),
             reads=[bs.k(), affT.k()], writes=[bs.k(), bjunk.k()])
        P.op("dve", lambda e: e.tensor_scalar(GE, CNT, float(CAP) - 0.5, None, op0=ALU.is_gt), reads=[bs.k()], writes=[bs.k()])
        P.op("dve", lambda e: e.tensor_tensor(D1, MID, LO, op=ALU.subtract), reads=[bs.k()], writes=[bs.k()])
        P.op("dve", lambda e: e.scalar_tensor_tensor(LO, D1, GE, LO, op0=ALU.mult, op1=ALU.add), reads=[bs.k()], writes=[bs.k()])
        P.op("dve", lambda e: e.tensor_tensor(D1, HI, MID, op=ALU.subtract), reads=[bs.k()], writes=[bs.k()])
        P.op("dve", lambda e: e.scalar_tensor_tensor(HI, D1, GE, MID, op0=ALU.mult, op1=ALU.add), reads=[bs.k()], writes=[bs.k()])
    dgt = A.alloc("dgt", (NE,)); thrb = A.alloc("thrb", (NE,))
    P.op("dve", lambda e: e.tensor_scalar(dgt.ap[0:NE, :], k.ident.ap[0:NE, 0:NE], LO, None, op0=ALU.mult), reads=[bs.k(), k.ident.k()], writes=[dgt.k()])
    P.op("pe", lambda e: e.matmul(ps[0][:, 0:NE], k.ones32.ap[0:NE, :], dgt.ap[0:NE, :], start=True, stop=True), reads=[k.ones32.k(), dgt.k()], writes=["ps0"])
    P.op("act", lambda e: e.copy(thrb.ap, ps[0][:, 0:NE]), reads=["ps0"], writes=[thrb.k()])
    mk16 = A.alloc("mk16", (NT, NE), BF16); mk32 = A.alloc("mk32", (NT, NE)); base = A.alloc("basec", (NE,))
    P.op("pool", lambda e: e.memset(base.ap, 0.0), writes=[base.k()])
    for i in range(NT):
        P.op("dve", lambda e, i=i: e.tensor_tensor(mk32.ap[:, i, :], aff.ap[:, i, :], thrb.ap, op=ALU.is_gt), reads=[aff.k(), thrb.k()], writes=[mk32.k(i)])
        P.op("act", lambda e, i=i: e.copy(mk16.ap[:, i, :], mk32.ap[:, i, :]), reads=[mk32.k(i)], writes=[mk16.k(i)])
        b = i % 2
        P.op("pe", lambda e, i=i, b=b: e.matmul(ps[b][:, 0:NE], k.trius16.ap, mk16.ap[:, i, :], start=True, stop=True), reads=[k.trius16.k(), mk16.k(i)], writes=[f"ps{b}"])
        P.op("pe", lambda e, i=i, b=b: e.matmul(ps[b][:, 128:128 + NE], k.ones16.ap, mk16.ap[:, i, :], start=True, stop=True), reads=[k.ones16.k(), mk16.k(i)], writes=[f"ps{b}"])
        P.op("dve", lambda e, i=i, b=b: e.scalar_tensor_tensor(pm.ap[:, i, :], ps[b][:, 0:NE], 1.0, base.ap, op0=ALU.add, op1=ALU.add), reads=[f"ps{b}", base.k()], writes=[pm.k(i)])
        P.op("dve", lambda e, i=i: e.tensor_tensor(pm.ap[:, i, :], pm.ap[:, i, :], mk32.ap[:, i, :], op=ALU.mult), reads=[pm.k(i), mk32.k(i)], writes=[pm.k(i)])
        P.op("dve", lambda e, i=i: e.tensor_scalar(pm.ap[:, i, :], pm.ap[:, i, :], -1.0, None, op0=ALU.add), reads=[pm.k(i)], writes=[pm.k(i)])
        P.op("dve", lambda e, b=b: e.tensor_tensor(base.ap, base.ap, ps[b][:, 128:128 + NE], op=ALU.add), reads=[f"ps{b}", base.k()], writes=[base.k()])
        tb = 6 + (i // 4) % 2
        P.op("pe", lambda e, i=i, tb=tb: e.transpose(ps[tb][0:NE, (i % 4) * 128:(i % 4 + 1) * 128], pm.ap[:, i, :], k.ident.ap), reads=[pm.k(i), k.ident.k()], writes=[f"ps{tb}"])
        if i % 4 == 3:
            P.op("act", lambda e, i=i, tb=tb: e.copy(pmT.ap[0:NE, (i - 3) * 128:(i + 1) * 128], ps[tb][0:NE, :]), reads=[f"ps{tb}"], writes=[pmT.k()])
    A.release(m1)
    SelE = A.alloc("SelE", (NT, CAP), BF16); SelT = A.alloc("SelT", (2, S), BF16)
    xgT = A.alloc("xgT", (8, CAP), BF16); actT = A.alloc("actT", (NFC, CAP), BF16)
    s1 = [A.alloc(f"s1_{i}", (CAP,)) for i in range(2)]
    ysb = A.alloc("ysb", (2, D), BF16)
    w1b = [A.alloc(f"w1b{i}", (8, FB), BF16) for i in range(2)]
    w3b = [A.alloc(f"w3b{i}", (8, FB), BF16) for i in range(2)]
    w2b = [A.alloc(f"w2b{i}", (W2G, D), BF16) for i in range(2)]
    wn = 0
    for ex in range(NE):
        w1v = IN["e_w1"].ap()[l][ex].rearrange("(c p) f -> p c f", p=128)
        w3v = IN["e_w3"].ap()[l][ex].rearrange("(c p) f -> p c f", p=128)
        w2v = IN["e_w2"].ap()[l][ex].rearrange("(g p) d -> p g d", p=128)
        for i in range(NT):
            P.op("dve", lambda e, i=i, ex=ex: e.tensor_scalar(SelE.ap[:, i, :], k.iota256.ap, pm.ap[:, i, ex:ex + 1], None, op0=ALU.is_equal),
                 reads=[k.iota256.k(), pm.k(i)], writes=[SelE.k(i)])
        for st in range(2):
            for tg in range(4):
                P.op("pe", lambda e, ex=ex, tg=tg: e.matmul(ps[6][:, :], k.ohb.ap[0:NE, ex * 128:(ex + 1) * 128], pmT.ap[0:NE, tg * 512:(tg + 1) * 512], start=True, stop=True),
                     reads=[k.ohb.k(), pmT.k()], writes=["ps6"])
                P.op("dve", lambda e, st=st, tg=tg: e.tensor_scalar(SelT.ap[:, st, tg * 512:(tg + 1) * 512], ps[6][:, :], k.slotidx.ap[:, st:st + 1], None, op0=ALU.is_equal),
                     reads=["ps6", k.slotidx.k()], writes=[SelT.k(st, tg)])
        for c in range(8):
            b = 6 + c % 2
            for i in range(NT):
                P.op("pe", lambda e, c=c, i=i, b=b: e.matmul(ps[b][:, 0:CAP], x2b.ap[:, i, c * 128:(c + 1) * 128], SelE.ap[:, i, :], start=(i == 0), stop=(i == NT - 1)),
                     reads=[x2b.k(i), SelE.k(i)], writes=[f"ps{b}"])
            P.op("act", lambda e, c=c, b=b: e.copy(xgT.ap[:, c, :], ps[b][:, 0:CAP]), reads=[f"ps{b}"], writes=[xgT.k(c)])
        for fb in range(NFB):
            r = wn % 2; wn += 1
            P.dma("pool", w1b[r].ap, w1v[:, :, fb * FB:(fb + 1) * FB], writes=[w1b[r].k()], semkey=("w1", r))
            P.dma("pool", w3b[r].ap, w3v[:, :, fb * FB:(fb + 1) * FB], writes=[w3b[r].k()], semkey=("w3", r))
            P.dma("pool", w2b[r].ap, w2v[:, fb * W2G:(fb + 1) * W2G, :], writes=[w2b[r].k()], semkey=("w2", r))
            for q in range(FB // 128):
                fc = fb * (FB // 128) + q
                b = fc % 2
                for c in range(8):
                    P.op("pe", lambda e, c=c, q=q, b=b, r=r: e.matmul(ps[b][:, 0:CAP], w1b[r].ap[:, c, q * 128:(q + 1) * 128], xgT.ap[:, c, :], start=(c == 0), stop=(c == 7)),
                         reads=[w1b[r].k(), xgT.k(c)], writes=[f"ps{b}"])
                for c in range(8):
                    P.op("pe", lambda e, c=c, q=q, b=b, r=r: e.matmul(ps[b][:, CAP:2 * CAP], w3b[r].ap[:, c, q * 128:(q + 1) * 128], xgT.ap[:, c, :], start=(c == 0), stop=(c == 7)),
                         reads=[w3b[r].k(), xgT.k(c)], writes=[f"ps{b}"])
                P.op("act", lambda e, b=b: e.activation(s1[b].ap, ps[b][:, 0:CAP], AF.Silu), reads=[f"ps{b}"], writes=[s1[b].k()])
                P.op("dve", lambda e, b=b, fc=fc: e.tensor_tensor(actT.ap[:, fc, :], s1[b].ap, ps[b][:, CAP:2 * CAP], op=ALU.mult), reads=[f"ps{b}", s1[b].k()], writes=[actT.k(fc)])
            for q in range(W2G):
                fc = fb * W2G + q
                for st in range(2):
                    for half in range(2):
                        yb_ = 2 + st * 2 + half
                        P.op("pe", lambda e, fc=fc, q=q, st=st, half=half, yb_=yb_, r=r: e.matmul(ps[yb_][:, :], actT.ap[:, fc, st * 128:(st + 1) * 128], w2b[r].ap[:, q, half * 512:(half + 1) * 512],
                                                                                           start=(fc == 0), stop=(fc == NFC - 1)),
                             reads=[actT.k(fc), w2b[r].k()], writes=[f"ps{yb_}"])
        for st in range(2):
            for half in range(2):
                yb_ = 2 + st * 2 + half
                P.op("act", lambda e, st=st, half=half, yb_=yb_: e.copy(ysb.ap[:, st, half * 512:(half + 1) * 512], ps[yb_][:, :]), reads=[f"ps{yb_}"], writes=[ysb.k(st, half)])
        for i in range(NT):
            for half in range(2):
                b = 6 + (2 * i + half) % 2
                for st in range(2):
                    P.op("pe", lambda e, i=i, half=half, st=st, b=b: e.matmul(ps[b][:, :], SelT.ap[:, st, i * 128:(i + 1) * 128], ysb.ap[:, st, half * 512:(half + 1) * 512], start=(st == 0), stop=(st == 1)),
                         reads=[SelT.k(st, i // 4), ysb.k(st, half)], writes=[f"ps{b}"])
                P.op("dve", lambda e, i=i, half=half, b=b, ex=ex: e.scalar_tensor_tensor(k.xres.ap[:, i, half * 512:(half + 1) * 512], ps[b][:, :], aff.ap[:, i, ex:ex + 1],
                                                                                     k.xres.ap[:, i, half * 512:(half + 1) * 512], op0=ALU.mult, op1=ALU.add),
                     reads=[f"ps{b}", aff.k(), k.xres.k(i)], writes=[k.xres.k(i)])
    A.release(m0)


_INPUT_NAMES = ["rel_bias", "ln1_g", "w_in", "w_out", "rk_mu", "rk_w0", "rk_w2", "rk_a0", "rk_a2", "rk_kk", "rk_ka",
                "rk_rk", "rk_g2", "rk_ln_g", "rk_ln_b", "ml_conv_w", "ml_conv_b", "ml_ib", "ml_fb", "ml_ln_g",
                "ln2_g", "router", "e_w1", "e_w3", "e_w2", "final_g"]


def make_in_maps(inputs, cores, names=None):
    consts = host_consts()
    shared = {n: np.ascontiguousarray(np.asarray(inputs[n], dtype=np.float32)) for n in _INPUT_NAMES if names is None or n in names}
    shared.update({n: v for n, v in consts.items() if names is None or n in names})
    if names is None or "c_pc" in names or "c_wx" in names:
        shared.update(host_rwkv_layout(inputs))
    x = np.asarray(inputs["x"], dtype=np.float32)
    maps = []
    for c in cores:
        m = dict(shared)
        m["x"] = np.ascontiguousarray(x[c])
        maps.append(m)
    return maps


def kernel(**inputs):
    nc, k = build()
    in_maps = make_in_maps(inputs, list(range(8)), set(k.IN.keys()))
    res = run_bass_kernel_spmd(nc, in_maps, core_ids=list(range(8)))
    return np.stack([np.asarray(r["out"], dtype=np.float32) for r in res.results], axis=0)
```

```python
from contextlib import ExitStack
import numpy as np
import concourse.bass as bass
import concourse.mybir as mybir

F32 = mybir.dt.float32
BF16 = mybir.dt.bfloat16
I32 = mybir.dt.int32
ALU = mybir.AluOpType
AF = mybir.ActivationFunctionType
AX = mybir.AxisListType

ENGS = ("pe", "act", "dve", "pool", "sp")


class Prog:
    def __init__(self, nc, strict_same_engine=False):
        self.nc = nc
        self.same_dist = 3
        self.ops = {e: [] for e in ENGS}
        self.keys = {}
        self.dma_cnt = {}
        self.es = ExitStack()
        self.n_ops = 0

    def _deps(self, reads, writes):
        deps = []
        for k in reads:
            deps.extend((ev, True) for ev in self._st(k)["w"])
        for k in writes:
            st = self._st(k)
            deps.extend((ev, True) for ev in st["w"])
            deps.extend((ev, False) for ev in st["r"].values())
        return deps

    def _st(self, k):
        st = self.keys.get(k)
        if st is None:
            inh = getattr(self, "inherit", {}).get(k[0], []) if isinstance(k, tuple) else []
            st = self.keys[k] = {"w": list(inh), "r": {}}
        return st

    def _record(self, ev, reads, writes):
        for k in reads:
            st = self._st(k)
            st["r"][(ev[0], ev[1])] = ev
        for k in writes:
            st = self._st(k)
            st["w"] = [ev]
            st["r"] = {}

    @staticmethod
    def _norm(reads, writes):
        r2, w2 = [], []
        for k in writes:
            if isinstance(k, str) and k.startswith("ps"):
                k = k.split("q")[0]
            if k not in w2:
                w2.append(k)
        for k in reads:
            if isinstance(k, str) and k.startswith("ps"):
                k = k.split("q")[0]
                if k not in w2:
                    w2.append(k)
            elif k not in r2:
                r2.append(k)
        return r2, w2

    def op(self, eng, fn, reads=(), writes=(), serial=False):
        reads, writes = self._norm(reads, writes)
        deps = self._deps(reads, writes)
        idx = len(self.ops[eng])
        if serial and idx > 0:
            deps.append((("eng", eng, idx - 1), "force"))
        ev = ("eng", eng, idx)
        self.ops[eng].append(dict(fn=fn, deps=deps, kind="c"))
        self._record(ev, reads, writes)
        self.n_ops += 1
        return ev

    def dma(self, q, out, in_, reads=(), writes=(), semkey=None, **kw):
        assert semkey is not None
        reads, writes = self._norm(reads, writes)
        deps = self._deps(reads, writes)
        c = self.dma_cnt.get(semkey, 0) + 1
        self.dma_cnt[semkey] = c
        if c > 1:
            deps.append((("dma", semkey, c - 1), "force"))
        ev = ("dma", semkey, c)
        fn = lambda e, out=out, in_=in_, kw=kw: e.dma_start(out=out, in_=in_, **kw)
        self.ops[q].append(dict(fn=fn, deps=deps, kind="d", semkey=semkey))
        self._record(ev, reads, writes)
        self.n_ops += 1
        return ev

    def emit(self, final_wait_events=()):
        nc = self.nc
        signal = {e: set() for e in ENGS}
        for e in ENGS:
            for i, o in enumerate(self.ops[e]):
                nd = []
                for (d, is_w) in o["deps"]:
                    if d[0] == "eng" and d[1] == e and is_w != "force":
                        if e == "pe" or not is_w or (i - d[2]) > self.same_dist:
                            continue
                    nd.append(d)
                    if d[0] == "eng":
                        signal[d[1]].add(d[2])
                o["deps"] = nd
        for d in final_wait_events:
            if d[0] == "eng":
                signal[d[1]].add(d[2])
        rank = {}
        for e in ENGS:
            r = 0
            for i in range(len(self.ops[e])):
                if i in signal[e]:
                    r += 1
                    rank[(e, i)] = r
        self.max_rank = {e: max([v for (ee, i), v in rank.items() if ee == e] + [0]) for e in ENGS}
        es = self.es
        sem_e = {e: es.enter_context(nc.semaphore("s_" + e)) for e in ENGS}
        sem_d = {k: es.enter_context(nc.semaphore("d_%d" % i)) for i, k in enumerate(self.dma_cnt)}
        self.n_sems = len(sem_e) + len(sem_d)

        def lower(ev):
            if ev[0] == "eng":
                return ("e_" + ev[1], sem_e[ev[1]], rank[(ev[1], ev[2])])
            return ("d_" + str(ev[1]), sem_d[ev[1]], 16 * ev[2])

        block = es.enter_context(nc.Block())
        engobj = {"pe": block.tensor, "act": block.scalar, "dve": block.vector,
                  "pool": block.gpsimd, "sp": block.sync}
        fw = self

        def make(e):
            def body(eng):
                known = {}
                for i, o in enumerate(fw.ops[e]):
                    need = {}
                    for d in o["deps"]:
                        nm, s, v = lower(d)
                        if known.get(nm, 0) >= v:
                            continue
                        if nm not in need or need[nm][1] < v:
                            need[nm] = (s, v)
                    for nm, (s, v) in need.items():
                        eng.wait_ge(s, v)
                        known[nm] = v
                    ins = o["fn"](eng)
                    if o["kind"] == "d":
                        ins.then_inc(sem_d[o["semkey"]], 16)
                    elif (e, i) in rank:
                        ins.then_inc(sem_e[e], 1)
                if e == "sp":
                    for d in final_wait_events:
                        nm, s, v = lower(d)
                        if known.get(nm, 0) < v:
                            eng.wait_ge(s, v)
                            known[nm] = v
            return body

        for e in ENGS:
            if self.ops[e] or e == "sp":
                engobj[e](make(e))
        es.close()


class Region:
    def __init__(self, uid, ap, lo, hi):
        self.uid, self.ap, self.lo, self.hi = uid, ap, lo, hi

    def k(self, *i):
        return (self.uid,) + tuple(i)

    def __getitem__(self, idx):
        return self.ap[idx]


class Arena:
    def __init__(self, P, tensor, ncols_f32):
        self.P, self.t, self.n = P, tensor, ncols_f32
        self.top = 0
        self.gen = 0
        self.pending = []
        self.live = []
        P.inherit = {}
        P._arena = self

    def alloc(self, name, free_shape, dtype=F32):
        nel = int(np.prod(free_shape))
        bpe = {F32: 4, BF16: 2, I32: 4}[dtype]
        ncol = (nel * bpe + 3) // 4
        lo, hi = self.top, self.top + ncol
        assert hi <= self.n, f"arena overflow allocating {name}: need {hi} cols of {self.n}"
        self.top = hi
        self.gen += 1
        uid = f"{name}#{self.gen}"
        ap = self.t[:, lo:hi]
        if dtype != F32:
            ap = ap.bitcast(dtype)
        ap = ap[:, 0:nel]
        if len(free_shape) > 1:
            names = " ".join(f"a{i}" for i in range(len(free_shape)))
            kw = {f"a{i}": int(s) for i, s in enumerate(free_shape)}
            ap = ap.rearrange(f"p ({names}) -> p {names}", **kw)
        evs = []
        keep = []
        for (plo, phi, pe) in self.pending:
            if plo < hi and lo < phi:
                evs.extend(pe)
            keep.append((plo, phi, pe))
        self.P.inherit[uid] = evs
        r = Region(uid, ap, lo, hi)
        self.live.append(r)
        return r

    def mark(self):
        return (self.top, len(self.live))

    def release(self, mark):
        top, nlive = mark
        P = self.P
        for r in self.live[nlive:]:
            evs = []
            for k in [k for k in P.keys if k[0] == r.uid]:
                st = P.keys.pop(k)
                evs.extend(st["w"])
                evs.extend(st["r"].values())
            evs.extend(P.inherit.get(r.uid, []))
            best = {}
            for ev in evs:
                kk = (ev[0], ev[1])
                if kk not in best or best[kk][2] < ev[2]:
                    best[kk] = ev
            self.pending.append((r.lo, r.hi, list(best.values())))
        del self.live[nlive:]
        self.top = top
from concourse.bass_utils import run_bass_kernel_spmd
S = 2048; D = 1024; NT = 16; INW = 3856; DEPTH = 2
ND = 3072; EC = 1535
NE = 16; CAP = 256; DFF = 2816


def t5_bucket_np(rel):
    nb = 16
    ret = np.where(rel > 0, nb, 0)
    n = np.abs(rel)
    max_exact = 8
    nf = np.maximum(n, 1).astype(np.float32)
    large = max_exact + (np.log(nf / np.float32(max_exact)) / np.float32(np.log(1024 / max_exact))
                         * np.float32(nb - max_exact)).astype(np.int32)
    large = np.minimum(large, nb - 1)
    return ret + np.where(n < max_exact, n, large)


def host_consts():
    d = np.arange(ND) - EC
    ad = np.abs(d)
    cnt = ((ad <= 64).astype(np.float32) + ((d % 4 == 0) & (ad <= 256)).astype(np.float32)
           + ((d % 16 == 0) & (ad <= 1024)).astype(np.float32))
    bk = t5_bucket_np(d)
    oh = np.zeros((32, ND), np.float32)
    oh[bk, np.arange(ND)] = 1.0
    c = {}
    c["c_oh"] = oh
    c["c_cnt"] = np.tile(cnt[None], (4, 1)).astype(np.float32)
    c["c_ident"] = np.eye(128, dtype=np.float32)
    c["c_jmat"] = np.eye(128, dtype=np.float32)[::-1].copy()
    c["c_triu"] = np.triu(np.ones((128, 128), np.float32))
    c["c_tril"] = np.tril(np.ones((128, 128), np.float32))
    ob = np.zeros((128, 128), np.float32); ob[:64, :64] = 1; ob[64:, 64:] = 1
    c["c_onesblk"] = ob
    tus = np.triu(np.ones((128, 128), np.float32), 1); tui = np.triu(np.ones((128, 128), np.float32), 0)
    c["c_mask4"] = np.concatenate([tus, tui, tus, tui], axis=1)
    c["c_trils"] = np.tril(np.ones((128, 128), np.float32), -1)
    rm = np.ones((128, 256), np.float32); rm[:, 0] = 0; rm[:, 128] = 0
    c["c_rmask"] = rm
    ohb = np.zeros((16, 2048), np.float32)
    for e_ in range(16):
        ohb[e_, e_ * 128:(e_ + 1) * 128] = 1.0
    c["c_ohb"] = ohb
    return c


class K:
    pass


def build(dbg=None, nlayers=DEPTH, stages=("attn", "mlstm", "rwkv", "moe")):
    nc = bass.Bass("TRN2", target_bir_lowering=False)
    k = K()
    k.nc = nc
    SH = dict(x=[S, D], rel_bias=[32, 4], ln1_g=[DEPTH, D], w_in=[DEPTH, D, INW], w_out=[DEPTH, D, D], rk_mu=[DEPTH, 2, 1280],
              rk_w0=[DEPTH, 2, 384], rk_w2=[DEPTH, 2, 64, 384], rk_a0=[DEPTH, 2, 384], rk_a2=[DEPTH, 2, 64, 384],
              rk_kk=[DEPTH, 384], rk_ka=[DEPTH, 384], rk_rk=[DEPTH, 6, 64], rk_g2=[DEPTH, 128, 384], rk_ln_g=[DEPTH, 384],
              rk_ln_b=[DEPTH, 384], ml_conv_w=[DEPTH, 5, 768], ml_conv_b=[DEPTH, 768], ml_ib=[DEPTH, 2, 4], ml_fb=[DEPTH, 2, 4],
              ml_ln_g=[DEPTH, 384], ln2_g=[DEPTH, D], router=[DEPTH, D, NE], e_w1=[DEPTH, NE, D, DFF], e_w3=[DEPTH, NE, D, DFF],
              e_w2=[DEPTH, NE, DFF, D], final_g=[D], c_oh=[32, ND], c_cnt=[4, ND], c_ident=[128, 128], c_jmat=[128, 128],
              c_triu=[128, 128], c_tril=[128, 128], c_onesblk=[128, 128], c_mask4=[128, 512], c_trils=[128, 128],
              c_rmask=[128, 256], c_ohb=[16, 2048], c_pc=[DEPTH, 128, NPC], c_wx=[DEPTH, D, 384])

    class _IN(dict):
        def __missing__(self, name):
            t = nc.dram_tensor(name, list(SH[name]), F32, kind="ExternalInput")
            self[name] = t
            return t
    IN = _IN()
    k.IN = IN
    out_t = nc.dram_tensor("out", [S, D], F32, kind="ExternalOutput")
    k.mscr = nc.dram_tensor("mscr", [4, ND], F32, kind="Internal")
    k.mtab_d = nc.dram_tensor("mtab_d", [4, 128, 23 * 128], F32, kind="Internal")
    k.xspill = nc.dram_tensor("xspill", [S, D], F32, kind="Internal")
    k.dbg = dbg
    k.dbg_out = {}
    with ExitStack() as es:
        ACOLS = 53000
        arena_t = es.enter_context(nc.sbuf_tensor("arena", [128, ACOLS], F32))
        k.ps = [es.enter_context(nc.psum_tensor(f"ps{i}", [128, 512], F32)) for i in range(8)]
        P = Prog(nc)
        A = Arena(P, arena_t, ACOLS)
        k.P, k.A = P, A
        k.final_events = []
        setup_consts(k)
        k.xres = A.alloc("xres", (NT, D))
        xin = IN["x"].ap().rearrange("(i p) d -> p i d", p=128)
        for i in range(NT):
            P.dma("sp" if i % 2 == 0 else "act", k.xres.ap[:, i, :], xin[:, i, :], writes=[k.xres.k(i)], semkey=("xld", i % 4))
        build_mask_table(k)
        for l in range(nlayers):
            m0 = A.mark()
            norm_to_hT(k, IN["ln1_g"].ap()[l], "hT")
            if "attn" in stages:
                attn_stage(k, l)
            if "mlstm" in stages:
                mlstm_stage(k, l)
            if "rwkv" in stages:
                rwkv_stage(k, l)
            A.release(m0)
            if "moe" in stages:
                moe_stage(k, l)
        final_stage(k, out_t)
        P.emit(final_wait_events=k.final_events)
    return nc, k


def dbg_dump(k, name, region_ap, shape, dt, reads):
    if not k.dbg or name not in k.dbg:
        return None
    t = k.nc.dram_tensor("dbg_" + name, list(shape), dt, kind="ExternalOutput")
    k.dbg_out[name] = t
    return t


def setup_consts(k):
    P, A, IN = k.P, k.A, k.IN
    k.ident = A.alloc("ident", (128,))
    k.jmat = A.alloc("jmat", (128,))
    k.ident16 = A.alloc("ident16", (128,), BF16)
    k.triu = A.alloc("triu", (128,))
    k.tril = A.alloc("tril", (128,))
    P.dma("sp", k.ident.ap, IN["c_ident"].ap(), writes=[k.ident.k()], semkey="c0")
    P.dma("sp", k.jmat.ap, IN["c_jmat"].ap(), writes=[k.jmat.k()], semkey="c1")
    P.dma("sp", k.triu.ap, IN["c_triu"].ap(), writes=[k.triu.k()], semkey="c2")
    P.dma("sp", k.tril.ap, IN["c_tril"].ap(), writes=[k.tril.k()], semkey="c3")
    P.op("dve", lambda e: e.tensor_copy(k.ident16.ap, k.ident.ap), reads=[k.ident.k()], writes=[k.ident16.k()])
    k.one = A.alloc("one", (1,))
    P.op("pool", lambda e: e.memset(k.one.ap, 1.0), writes=[k.one.k()])
    k.ones32 = A.alloc("ones32", (128,))
    P.op("pool", lambda e: e.memset(k.ones32.ap, 1.0), writes=[k.ones32.k()])
    k.negm = [A.alloc("negm0", (128,)), A.alloc("negm1", (128,))]
    P.op("dve", lambda e: e.tensor_scalar(k.negm[0].ap, k.triu.ap, -1.0, 30000.0, op0=ALU.add, op1=ALU.mult), reads=[k.triu.k()], writes=[k.negm[0].k()])
    P.op("dve", lambda e: e.tensor_scalar(k.negm[1].ap, k.tril.ap, -1.0, 30000.0, op0=ALU.add, op1=ALU.mult), reads=[k.tril.k()], writes=[k.negm[1].k()])
    k.epsgn = A.alloc("epsgn", (1,))
    P.op("pool", lambda e: e.memset(k.epsgn.ap, 64e-5), writes=[k.epsgn.k()])
    k.onesblk = A.alloc("onesblk", (128,)); k.onesblk64 = A.alloc("onesblk64", (128,))
    k.trils = A.alloc("trils", (128,))
    P.dma("act", k.onesblk.ap, IN["c_onesblk"].ap(), writes=[k.onesblk.k()], semkey="c4")
    P.dma("act", k.trils.ap, IN["c_trils"].ap(), writes=[k.trils.k()], semkey="c6")
    P.op("dve", lambda e: e.tensor_scalar(k.onesblk64.ap, k.onesblk.ap, 1.0 / 64, None, op0=ALU.mult), reads=[k.onesblk.k()], writes=[k.onesblk64.k()])
    k.iota256 = A.alloc("iota256", (256,)); k.slotidx = A.alloc("slotidx", (2,))
    k.ones16 = A.alloc("ones16", (128,), BF16); k.trius16 = A.alloc("trius16", (128,), BF16)
    P.op("pool", lambda e: e.iota(k.iota256.ap, [[1, 256]], base=0, channel_multiplier=0, allow_small_or_imprecise_dtypes=True), writes=[k.iota256.k()])
    P.op("pool", lambda e: e.iota(k.slotidx.ap, [[128, 2]], base=0, channel_multiplier=1, allow_small_or_imprecise_dtypes=True), writes=[k.slotidx.k()])
    P.op("dve", lambda e: e.tensor_copy(k.ones16.ap, k.ones32.ap), reads=[k.ones32.k()], writes=[k.ones16.k()])
    P.op("dve", lambda e: e.tensor_tensor(k.trius16.ap, k.triu.ap, k.ident.ap, op=ALU.subtract), reads=[k.triu.k(), k.ident.k()], writes=[k.trius16.k()])
    k.eps6 = A.alloc("eps6", (1,))
    P.op("pool", lambda e: e.memset(k.eps6.ap, 1e-6), writes=[k.eps6.k()])


def build_mask_table(k):
    P, A, IN, ps = k.P, k.A, k.IN, k.ps
    m0 = A.mark()
    rb = A.alloc("rb", (4,)); oh = A.alloc("oh", (ND,)); cnt = A.alloc("cnt", (ND,)); mm = A.alloc("mm", (ND,))
    P.dma("sp", rb.ap[0:32, :], IN["rel_bias"].ap(), writes=[rb.k()], semkey="mt0")
    P.dma("sp", oh.ap[0:32, :], IN["c_oh"].ap(), writes=[oh.k()], semkey="mt1")
    P.dma("act", cnt.ap[0:4, :], IN["c_cnt"].ap(), writes=[cnt.k()], semkey="mt2")
    for c in range(ND // 512):
        b = ps[c % 2]
        P.op("pe", lambda e, c=c, b=b: e.matmul(b[0:4, :], rb.ap[0:32, :], oh.ap[0:32, c * 512:(c + 1) * 512], start=True, stop=True),
             reads=[rb.k(), oh.k()], writes=[f"ps{c%2}"])
        P.op("act", lambda e, c=c, b=b: e.activation(mm.ap[0:4, c * 512:(c + 1) * 512], b[0:4, :], AF.Exp),
             reads=[f"ps{c%2}"], writes=[mm.k(c)])
        P.op("dve", lambda e, c=c: e.tensor_tensor(mm.ap[0:4, c * 512:(c + 1) * 512], mm.ap[0:4, c * 512:(c + 1) * 512],
                                                  cnt.ap[0:4, c * 512:(c + 1) * 512], op=ALU.mult),
             reads=[mm.k(c), cnt.k()], writes=[mm.k(c)])
    P.dma("sp", k.mscr.ap(), mm.ap[0:4, :], reads=[mm.k(c) for c in range(ND // 512)], writes=["mscr"], semkey="mt3")
    hanks = [A.alloc(f"hank{i}", (23, 128)) for i in range(2)]
    mts = [A.alloc(f"mt{i}", (23, 128)) for i in range(2)]
    for h in range(4):
        hank = hanks[h % 2]
        mt = mts[h % 2]
        src = bass.AP(k.mscr, h * ND, [[1, 128], [128, 23], [1, 128]])
        P.dma("sp" if h % 2 == 0 else "act", hank.ap, src, reads=["mscr"], writes=[hank.k()], semkey=("mt4", h))
        for j in range(23):
            jj = 22 - j
            b = 2 + (j // 4) % 2
            P.op("pe", lambda e, j=j, b=b, hank=hank: e.matmul(ps[b][:, (j % 4) * 128:(j % 4 + 1) * 128], hank.ap[:, j, :], k.jmat.ap, start=True, stop=True),
                 reads=[hank.k(), k.jmat.k()], writes=[f"ps{b}"])
            P.op("act" if j % 2 == 0 else "dve",
                 (lambda e, jj=jj, j=j, b=b, mt=mt: e.copy(mt.ap[:, jj, :], ps[b][:, (j % 4) * 128:(j % 4 + 1) * 128])) if j % 2 == 0 else
                 (lambda e, jj=jj, j=j, b=b, mt=mt: e.tensor_copy(mt.ap[:, jj, :], ps[b][:, (j % 4) * 128:(j % 4 + 1) * 128])),
                 reads=[f"ps{b}"], writes=[mt.k()])
        P.dma("sp", k.mtab_d.ap()[h].rearrange("p (j q) -> p j q", j=23), mt.ap, reads=[mt.k()], writes=[("mtab_d", h)], semkey=("mt5", h))
    A.release(m0)


def norm_to_hT(k, g_ap, name, want_f32T=False):
    P, A, ps = k.P, k.A, k.ps
    k.hT = A.alloc(name, (8, S), BF16)
    m0 = A.mark()
    gb = A.alloc("gb", (D,)); junk = A.alloc("junk", (D,)); ss = A.alloc("ss", (NT,)); rstd = A.alloc("rstd", (NT,))
    hb = [A.alloc(f"hb{i}", (D,), BF16) for i in range(2)]
    P.dma("sp", gb.ap, g_ap.partition_broadcast(128), writes=[gb.k()], semkey="gb")
    P.op("pool", lambda e: e.memset(ss.ap, 0.0), writes=[ss.k(i) for i in range(NT)])
    for i in range(NT):
        P.op("act", lambda e, i=i: e.activation(junk.ap, k.xres.ap[:, i, :], AF.Square, accum_out=ss.ap[:, i:i + 1]),
             reads=[k.xres.k(i)], writes=[junk.k(), ss.k(i)])
    P.op("act", lambda e: e.activation(rstd.ap, ss.ap, AF.Sqrt, scale=1.0 / D, bias=k.eps6.ap[:, 0:1]),
         reads=[ss.k(i) for i in range(NT)] + [k.eps6.k()], writes=[rstd.k()])
    P.op("dve", lambda e: e.reciprocal(rstd.ap, rstd.ap), reads=[rstd.k()], writes=[rstd.k()])
    for i in range(NT):
        h_ = hb[i % 2]
        P.op("dve", lambda e, i=i, h_=h_: e.scalar_tensor_tensor(h_.ap, k.xres.ap[:, i, :], rstd.ap[:, i:i + 1], gb.ap, op0=ALU.mult, op1=ALU.mult),
             reads=[k.xres.k(i), rstd.k(), gb.k()], writes=[h_.k()])
        b = 4 + i % 2
        pb = ps[b][:, :].bitcast(BF16)
        for c in range(8):
            P.op("pe", lambda e, c=c, pb=pb, h_=h_: e.transpose(pb[:, c * 128:(c + 1) * 128], h_.ap[:, c * 128:(c + 1) * 128], k.ident16.ap),
                 reads=[h_.k(), k.ident16.k()], writes=[f"ps{b}"])
        P.op("act", lambda e, i=i, pb=pb: e.copy(k.hT.ap[:, :, i * 128:(i + 1) * 128], pb.rearrange("p (c t) -> p c t", c=8)),
             reads=[f"ps{b}"], writes=[k.hT.k(i)])
    A.release(m0)


def load_w_bf16(k, name, src_ap, nchunks, ncols, semkey, split=None):
    P, A = k.P, k.A
    w = A.alloc(name, (nchunks, ncols), BF16)
    v = src_ap.rearrange("(c p) n -> p c n", p=128)
    for c in range(nchunks):
        P.dma("pool", w.ap[:, c, :], v[:, c, :], writes=[w.k(c)], semkey=(semkey, c % 4))
    return w


def outproj_partial(k, l, yT, nch, row0, tag):
    P, A, ps, IN = k.P, k.A, k.ps, k.IN
    wo = load_w_bf16(k, "wo_" + tag, IN["w_out"].ap()[l][row0:row0 + nch * 128, :], nch, D, "wo_" + tag)
    n = 0
    for i in range(NT):
        for half in range(2):
            b = 6 + n % 2
            n += 1
            for c in range(nch):
                P.op("pe", lambda e, i=i, half=half, c=c, b=b: e.matmul(ps[b][:, :], yT.ap[:, c, i * 128:(i + 1) * 128],
                                                                     wo.ap[:, c, half * 512:(half + 1) * 512], start=(c == 0), stop=(c == nch - 1)),
                     reads=[yT.k(i), wo.k(c)], writes=[f"ps{b}"])
            P.op("dve", lambda e, i=i, half=half, b=b: e.tensor_tensor(k.xres.ap[:, i, half * 512:(half + 1) * 512],
                                                                      k.xres.ap[:, i, half * 512:(half + 1) * 512], ps[b][:, :], op=ALU.add),
                 reads=[f"ps{b}", k.xres.k(i)], writes=[k.xres.k(i)])


def transpose_to_T(k, y, nch, yT):
    P, ps = k.P, k.ps
    for i in range(NT):
        b = 4 + i % 2
        pb = ps[b][:, :].bitcast(BF16)
        for c in range(nch):
            P.op("pe", lambda e, i=i, c=c, pb=pb: e.transpose(pb[:, c * 128:(c + 1) * 128], y.ap[:, i, c * 128:(c + 1) * 128], k.ident16.ap),
                 reads=[y.k(i), k.ident16.k()], writes=[f"ps{b}"])
        P.op("act", lambda e, i=i, pb=pb: e.copy(yT.ap[:, :, i * 128:(i + 1) * 128], pb[:, 0:nch * 128].rearrange("p (c t) -> p c t", c=nch)),
             reads=[f"ps{b}"], writes=[yT.k(i)])


def attn_stage(k, l):
    P, A, ps, IN = k.P, k.A, k.ps, k.IN
    m0 = A.mark()
    ya = A.alloc("ya", (NT, 256), BF16)
    m1 = A.mark()
    wa = load_w_bf16(k, "wa", IN["w_in"].ap()[l][:, 0:768], 8, 768, "wa")
    qT = A.alloc("qT", (2, S), BF16); kT = A.alloc("kT", (2, S), BF16)
    vp = A.alloc("vp", (NT, 4, 65), BF16)
    P.op("pool", lambda e: e.memset(vp.ap, 1.0), writes=[vp.k(i) for i in range(NT)])
    n = 0
    for cc in range(4):
        dst = qT if cc < 2 else kT
        for tg in range(4):
            b = n % 2; n += 1
            for c in range(8):
                P.op("pe", lambda e, c=c, cc=cc, tg=tg, b=b: e.matmul(ps[b][:, :], wa.ap[:, c, cc * 128:(cc + 1) * 128], k.hT.ap[:, c, tg * 512:(tg + 1) * 512],
                                                                  start=(c == 0), stop=(c == 7)),
                     reads=[wa.k(c)] + [k.hT.k(4 * tg + j) for j in range(4)], writes=[f"ps{b}"])
            P.op("act", lambda e, cc=cc, tg=tg, b=b, dst=dst: e.copy(dst.ap[:, cc % 2, tg * 512:(tg + 1) * 512], ps[b][:, :]),
                 reads=[f"ps{b}"], writes=[dst.k(tg)])
    for i in range(NT):
        b = 2 + i % 2
        for c in range(8):
            P.op("pe", lambda e, c=c, i=i, b=b: e.matmul(ps[b][:, 0:256], k.hT.ap[:, c, i * 128:(i + 1) * 128], wa.ap[:, c, 512:768], start=(c == 0), stop=(c == 7)),
                 reads=[wa.k(c), k.hT.k(i)], writes=[f"ps{b}"])
        P.op("dve", lambda e, i=i, b=b: e.tensor_copy(vp.ap[:, i, :, 0:64], ps[b][:, 0:256].rearrange("p (h c) -> p h c", h=4)),
             reads=[f"ps{b}"], writes=[vp.k(i)])
    mtab = [A.alloc(f"mtab{i}", (23, 128)) for i in range(2)]
    e32 = [A.alloc(f"e32_{i}", (512,)) for i in range(2)]
    p16 = [A.alloc(f"p16_{i}", (512,), BF16) for i in range(2)]
    rc = A.alloc("rc", (8,))
    iters = []
    for h in range(4):
        for g in range(4):
            kts = [kt for kt in range(NT) if -8 <= kt - 4 * g <= 11]
            for kt in kts:
                iters.append((h, g, kt, kt == kts[0], kt == kts[-1]))
    mts = {}

    def emit_score(n):
        h, g, kt, first, last = iters[n]
        sb = n % 2
        hp, hb_ = h // 2, (h % 2) * 64
        if h not in mts:
            mt = mtab[h % 2]
            P.dma("sp", mt.ap, k.mtab_d.ap()[h].rearrange("p (j q) -> p j q", j=23), reads=[("mtab_d", h)], writes=[mt.k()], semkey=("mtl", h % 2))
            mts[h] = mt
        P.op("pe", lambda e, kt=kt, g=g, sb=sb, hp=hp, hb_=hb_: e.matmul(ps[sb][:, :], kT.ap[hb_:hb_ + 64, hp, kt * 128:(kt + 1) * 128],
                                                                   qT.ap[hb_:hb_ + 64, hp, g * 512:(g + 1) * 512], start=True, stop=True),
             reads=[kT.k(kt // 4), qT.k(g)], writes=[f"ps{sb}"], serial=True)

    def emit_rest(n):
        h, g, kt, first, last = iters[n]
        sb = n % 2
        mt = mts[h]
        jj0 = 11 - kt + 4 * g
        P.op("act", lambda e, sb=sb: e.activation(e32[sb].ap, ps[sb][:, :], AF.Exp, scale=0.125),
             reads=[f"ps{sb}"], writes=[e32[sb].k()])
        P.op("dve", lambda e, sb=sb, jj0=jj0, mt=mt: e.tensor_tensor(p16[sb].ap, e32[sb].ap, mt.ap[:, jj0:jj0 + 4, :].rearrange("p j q -> p (j q)"), op=ALU.mult),
             reads=[e32[sb].k(), mt.k()], writes=[p16[sb].k()])
        for i in range(4):
            P.op("pe", lambda e, i=i, sb=sb, kt=kt, h=h, first=first, last=last: e.matmul(ps[2 + i][:, 0:65], p16[sb].ap[:, i * 128:(i + 1) * 128], vp.ap[:, kt, h, :],
                                                                                  start=first, stop=last),
                 reads=[p16[sb].k(), vp.k(kt)], writes=[f"ps{2+i}"])
        if last:
            for i in range(4):
                P.op("dve", lambda e, i=i: e.reciprocal(rc.ap[:, i:i + 1], ps[2 + i][:, 64:65]), reads=[f"ps{2+i}"], writes=[rc.k(i)])
                P.op("dve", lambda e, i=i, g=g, h=h: e.tensor_scalar(ya.ap[:, 4 * g + i, h * 64:(h + 1) * 64], ps[2 + i][:, 0:64], rc.ap[:, i:i + 1], None, op0=ALU.mult),
                     reads=[f"ps{2+i}", rc.k(i)], writes=[ya.k(4 * g + i)])

    emit_score(0)
    for n in range(len(iters)):
        if n + 1 < len(iters):
            emit_score(n + 1)
        emit_rest(n)
    A.release(m1)
    if k.dbg and "ya" in k.dbg:
        t = k.nc.dram_tensor("dbg_ya", [S, 256], BF16, kind="ExternalOutput")
        k.final_events.append(P.dma("sp", t.ap().rearrange("(i p) c -> p i c", p=128), ya.ap, reads=[ya.k(i) for i in range(NT)], semkey="dbg"))
    yaT = A.alloc("yaT", (2, S), BF16)
    transpose_to_T(k, ya, 2, yaT)
    outproj_partial(k, l, yaT, 2, 0, "a")
    A.release(m0)


def final_stage(k, out_t):
    P, A = k.P, k.A
    m0 = A.mark()
    gb = A.alloc("gbf", (D,)); junk = A.alloc("junkf", (D,)); ss = A.alloc("ssf", (NT,)); rstd = A.alloc("rstdf", (NT,))
    ob = [A.alloc(f"ob{i}", (D,)) for i in range(2)]
    P.dma("sp", gb.ap, k.IN["final_g"].ap().partition_broadcast(128), writes=[gb.k()], semkey="gbf")
    P.op("pool", lambda e: e.memset(ss.ap, 0.0), writes=[ss.k(i) for i in range(NT)])
    for i in range(NT):
        P.op("act", lambda e, i=i: e.activation(junk.ap, k.xres.ap[:, i, :], AF.Square, accum_out=ss.ap[:, i:i + 1]),
             reads=[k.xres.k(i)], writes=[junk.k(), ss.k(i)])
    P.op("act", lambda e: e.activation(rstd.ap, ss.ap, AF.Sqrt, scale=1.0 / D, bias=k.eps6.ap[:, 0:1]),
         reads=[ss.k(i) for i in range(NT)] + [k.eps6.k()], writes=[rstd.k()])
    P.op("dve", lambda e: e.reciprocal(rstd.ap, rstd.ap), reads=[rstd.k()], writes=[rstd.k()])
    ov = out_t.ap().rearrange("(i p) d -> p i d", p=128)
    for i in range(NT):
        o = ob[i % 2]
        P.op("dve", lambda e, i=i, o=o: e.scalar_tensor_tensor(o.ap, k.xres.ap[:, i, :], rstd.ap[:, i:i + 1], gb.ap, op0=ALU.mult, op1=ALU.mult),
             reads=[k.xres.k(i), rstd.k(), gb.k()], writes=[o.k()])
        k.final_events.append(P.dma("sp" if i % 2 == 0 else "act", ov[:, i, :], o.ap, reads=[o.k()], semkey=("out", i % 2)))
    A.release(m0)


OFF_MQK = 2304; OFF_MV = 3072; OFF_MO = 3456; OFF_MG = 3840


def mlstm_stage(k, l):
    P, A, ps, IN = k.P, k.A, k.ps, k.IN
    m0 = A.mark()
    hsum = A.alloc("hsum", (NT, 4, 96))
    m1 = A.mark()
    qkT = A.alloc("qkT", (8, S), BF16)
    vp = A.alloc("vpm", (NT, 4, 97), BF16)
    G = A.alloc("G", (NT, 16))
    BL = [A.alloc(f"BL{d}", (NT, 4)) for d in range(2)]
    EBL = [A.alloc(f"EBL{d}", (NT, 4)) for d in range(2)]
    IMB = [A.alloc(f"IMB{d}", (NT, 4)) for d in range(2)]
    m2 = A.mark()
    wm = load_w_bf16(k, "wm", IN["w_in"].ap()[l][:, OFF_MQK:OFF_MV], 8, OFF_MV - OFF_MQK, "wm")
    pre = A.alloc("pre", (S + 4,)); cacc = A.alloc("cacc", (S,))
    cw = A.alloc("cw", (8, 6))
    for g_ in range(8):
        P.dma("sp", cw.ap[0:96, g_, 0:5], IN["ml_conv_w"].ap()[l][:, g_ * 96:(g_ + 1) * 96].rearrange("j c -> c j"), writes=[cw.k()], semkey=("cw0", g_ % 4), allow_slow_non_contiguous=True)
    P.dma("sp", cw.ap[0:96, :, 5:6], IN["ml_conv_b"].ap()[l].rearrange("(g c o) -> c g o", c=96, o=1), writes=[cw.k()], semkey="cw1", allow_slow_non_contiguous=True)
    P.op("pool", lambda e: e.memset(pre.ap, 0.0), writes=[pre.k()])
    P.op("pool", lambda e: e.memset(vp.ap, 1.0), writes=[vp.k(i) for i in range(NT)])
    n = 0
    for j in range(8):
        for tg in range(4):
            b = n % 2; n += 1
            for c in range(8):
                P.op("pe", lambda e, c=c, j=j, tg=tg, b=b: e.matmul(ps[b][0:96, :], wm.ap[:, c, j * 96:(j + 1) * 96], k.hT.ap[:, c, tg * 512:(tg + 1) * 512],
                                                                 start=(c == 0), stop=(c == 7)),
                     reads=[wm.k(c)] + [k.hT.k(4 * tg + q) for q in range(4)], writes=[f"ps{b}"])
            P.op("act", lambda e, tg=tg, b=b: e.copy(pre.ap[0:96, 2 + tg * 512:2 + (tg + 1) * 512], ps[b][0:96, :]),
                 reads=[f"ps{b}"], writes=[pre.k()])
        P.op("dve", lambda e, j=j: e.tensor_scalar(cacc.ap[0:96, :], pre.ap[0:96, 0:S], cw.ap[0:96, j, 0:1], None, op0=ALU.mult),
             reads=[pre.k(), cw.k()], writes=[cacc.k()])
        for t in range(1, 5):
            P.op("dve", lambda e, j=j, t=t: e.scalar_tensor_tensor(cacc.ap[0:96, :], pre.ap[0:96, t:t + S], cw.ap[0:96, j, t:t + 1], cacc.ap[0:96, :],
                                                                 op0=ALU.mult, op1=ALU.add),
                 reads=[pre.k(), cw.k(), cacc.k()], writes=[cacc.k()])
        if j < 4:
            P.op("act", lambda e, j=j: e.activation(qkT.ap[0:96, j, :], cacc.ap[0:96, :], AF.Silu, bias=cw.ap[0:96, j, 5:6]),
                 reads=[cacc.k(), cw.k()], writes=[qkT.k(j)])
        else:
            P.op("act", lambda e, j=j: e.activation(cacc.ap[0:96, :], cacc.ap[0:96, :], AF.Silu, bias=cw.ap[0:96, j, 5:6]),
                 reads=[cacc.k(), cw.k()], writes=[cacc.k()])
            P.op("dve", lambda e, j=j: e.tensor_scalar(qkT.ap[0:96, j, :], cacc.ap[0:96, :], 96.0 ** -0.5, None, op0=ALU.mult),
                 reads=[cacc.k()], writes=[qkT.k(j)])
    A.release(m2)
    wv = load_w_bf16(k, "wv", IN["w_in"].ap()[l][:, OFF_MV:OFF_MO], 8, 384, "wv")
    wg = load_w_bf16(k, "wg", IN["w_in"].ap()[l][:, OFF_MG:INW], 8, 16, "wg")
    gbias = A.alloc("gbias", (16,))
    for d in range(2):
        P.dma("act", gbias.ap[:, 8 * d:8 * d + 4], IN["ml_ib"].ap()[l][d].partition_broadcast(128), writes=[gbias.k()], semkey=("gbi", d))
        P.dma("act", gbias.ap[:, 8 * d + 4:8 * d + 8], IN["ml_fb"].ap()[l][d].partition_broadcast(128), writes=[gbias.k()], semkey=("gbf_", d))
    for i in range(NT):
        b = 2 + i % 2
        for c in range(8):
            P.op("pe", lambda e, c=c, i=i, b=b: e.matmul(ps[b][:, 0:384], k.hT.ap[:, c, i * 128:(i + 1) * 128], wv.ap[:, c, :],
                                                     start=(c == 0), stop=(c == 7)),
                 reads=[wv.k(c), k.hT.k(i)], writes=[f"ps{b}"])
        P.op("dve", lambda e, i=i, b=b: e.tensor_copy(vp.ap[:, i, :, 0:96], ps[b][:, 0:384].rearrange("p (h c) -> p h c", h=4)),
             reads=[f"ps{b}"], writes=[vp.k(i)])
        b2 = 4 + i % 2
        for c in range(8):
            P.op("pe", lambda e, c=c, i=i, b2=b2: e.matmul(ps[b2][:, 0:16], k.hT.ap[:, c, i * 128:(i + 1) * 128], wg.ap[:, c, :],
                                                       start=(c == 0), stop=(c == 7)),
                 reads=[wg.k(c), k.hT.k(i)], writes=[f"ps{b2}"])
        P.op("dve", lambda e, i=i, b2=b2: e.tensor_tensor(G.ap[:, i, :], ps[b2][:, 0:16], gbias.ap, op=ALU.add),
             reads=[f"ps{b2}", gbias.k()], writes=[G.k()])
    A.release(m2)
    for d in range(2):
        v_ = G.ap[:, :, 8 * d + 4:8 * d + 8]
        P.op("act", lambda e, v_=v_: e.activation(v_, v_, AF.Exp, scale=-1.0), reads=[G.k()], writes=[G.k()])
        P.op("act", lambda e, v_=v_: e.activation(v_, v_, AF.Ln, bias=k.one.ap[:, 0:1]), reads=[G.k(), k.one.k()], writes=[G.k()])
        P.op("dve", lambda e, v_=v_: e.tensor_scalar(v_, v_, -1.0, None, op0=ALU.mult), reads=[G.k()], writes=[G.k()])
    for d in range(2):
        tri = k.triu if d == 0 else k.tril
        P.op("pe", lambda e, d=d, tri=tri: e.matmul(ps[7][:, 0:64].rearrange("p (i h) -> p i h", h=4), tri.ap, G.ap[:, :, 8 * d + 4:8 * d + 8], start=True, stop=True),
             reads=[tri.k(), G.k()], writes=["ps7"])
        P.op("dve", lambda e, d=d: e.tensor_copy(BL[d].ap, ps[7][:, 0:64].rearrange("p (i h) -> p i h", h=4)), reads=["ps7"], writes=[BL[d].k()])
        P.op("act", lambda e, d=d: e.activation(EBL[d].ap, BL[d].ap, AF.Exp), reads=[BL[d].k()], writes=[EBL[d].k()])
        P.op("dve", lambda e, d=d: e.tensor_tensor(IMB[d].ap, G.ap[:, :, 8 * d:8 * d + 4], BL[d].ap, op=ALU.subtract), reads=[G.k(), BL[d].k()], writes=[IMB[d].k()])
    C32 = A.alloc("C32", (4, 97)); Cb = A.alloc("Cb", (4, 97), BF16)
    HB = []
    for h in range(4):
        HB.append(dict(dg=A.alloc(f"dg{h}", (128,)), Dm=A.alloc(f"Dm{h}", (128,)), W16=A.alloc(f"W16{h}", (128,), BF16),
                       nis=A.alloc(f"nis{h}", (97,)), nsb=A.alloc(f"nsb{h}", (97,)), kh=A.alloc(f"kh{h}", (96,), BF16), sm=A.alloc(f"sm{h}", (4,))))

    def head_stream(h):
        hb = HB[h]
        dg, Dm, W16, nis, nsb, kh, sm = (hb[n_] for n_ in ("dg", "Dm", "W16", "nis", "nsb", "kh", "sm"))
        bA, bB = 2 * h, 2 * h + 1
        kA, kB = f"ps{bA}", f"ps{bB}"
        pbt = ps[bA][:, 384:432].bitcast(BF16)
        for d in range(2):
            P.op("pool", lambda e: e.memset(C32.ap[:, h, :], 0.0), writes=[C32.k(h)])
            P.op("pool", lambda e: e.memset(Cb.ap[:, h, :], 0.0), writes=[Cb.k(h)])
            nm = k.negm[d]
            endc = 127 if d == 0 else 0
            order = range(NT) if d == 0 else range(NT - 1, -1, -1)
            for i in order:
                yield from chunk(h, d, i, nm, endc, dg, Dm, W16, nis, nsb, kh, sm, bA, bB, kA, kB, pbt)

    def chunk(h, d, i, nm, endc, dg, Dm, W16, nis, nsb, kh, sm, bA, bB, kA, kB, pbt):
        tsl = slice(i * 128, (i + 1) * 128)
        P.op("pe", lambda e: e.matmul(ps[bA][:, 0:128], qkT.ap[0:96, 4 + h, tsl], qkT.ap[0:96, h, tsl], start=True, stop=True),
             reads=[qkT.k(4 + h), qkT.k(h)], writes=[kA])
        P.op("dve", lambda e: e.tensor_scalar(dg.ap, k.ident.ap, BL[d].ap[:, i, h:h + 1], None, op0=ALU.mult),
             reads=[k.ident.k(), BL[d].k()], writes=[dg.k()])
        P.op("pe", lambda e: e.matmul(ps[bA][:, 128:256], k.ones32.ap, dg.ap, start=True, stop=False), reads=[k.ones32.k(), dg.k()], writes=[kA])
        P.op("pe", lambda e: e.matmul(ps[bA][:, 128:256], k.ident.ap, nm.ap, start=False, stop=True), reads=[k.ident.k(), nm.k()], writes=[kA])
        yield
        P.op("act", lambda e: e.activation(Dm.ap, ps[bA][:, 128:256], AF.Exp, bias=IMB[d].ap[:, i, h:h + 1]), reads=[kA, IMB[d].k()], writes=[Dm.k()])
        P.op("act", lambda e: e.activation(sm.ap[:, 2:3], ps[bA][:, 128 + endc:129 + endc], AF.Exp), reads=[kA], writes=[sm.k(2)])
        P.op("dve", lambda e: e.tensor_tensor(W16.ap, ps[bA][:, 0:128], Dm.ap, op=ALU.mult), reads=[kA, Dm.k()], writes=[W16.k()])
        yield
        P.op("pe", lambda e: e.matmul(ps[bB][:, 0:97], W16.ap, vp.ap[:, i, h, :], start=True, stop=True), reads=[W16.k(), vp.k(i)], writes=[kB])
        P.op("pe", lambda e: e.matmul(ps[bB][:, 128:225], qkT.ap[0:96, h, tsl], Cb.ap[0:96, h, :], start=True, stop=True), reads=[qkT.k(h), Cb.k(h)], writes=[kB])
        P.op("pe", lambda e: e.transpose(pbt, qkT.ap[0:96, 4 + h, tsl], k.ident16.ap[0:96, 0:96]), reads=[qkT.k(4 + h), k.ident16.k()], writes=[kA])
        yield
        P.op("act", lambda e: e.copy(nis.ap, ps[bB][:, 0:97]), reads=[kB], writes=[nis.k()])
        P.op("dve", lambda e: e.tensor_scalar(kh.ap, pbt, Dm.ap[:, endc:endc + 1], None, op0=ALU.mult), reads=[kA, Dm.k()], writes=[kh.k()])
        P.op("dve", lambda e: e.scalar_tensor_tensor(nsb.ap, ps[bB][:, 128:225], EBL[d].ap[:, i, h:h + 1], nis.ap, op0=ALU.mult, op1=ALU.add),
             reads=[kB, EBL[d].k(), nis.k()], writes=[nsb.k()])
        P.op("pe", lambda e: e.matmul(ps[bA][0:96, 256:353], kh.ap, vp.ap[:, i, h, :], start=True, stop=True), reads=[kh.k(), vp.k(i)], writes=[kA])
        yield
        P.op("dve", lambda e: e.tensor_scalar(sm.ap[:, 0:1], nsb.ap[:, 96:97], -1.0, None, op0=ALU.mult), reads=[nsb.k()], writes=[sm.k(0)])
        P.op("dve", lambda e: e.scalar_tensor_tensor(sm.ap[:, 0:1], nsb.ap[:, 96:97], 1.0, sm.ap[:, 0:1], op0=ALU.max, op1=ALU.max), reads=[nsb.k(), sm.k(0)], writes=[sm.k(0)])
        P.op("dve", lambda e: e.reciprocal(sm.ap[:, 0:1], sm.ap[:, 0:1]), reads=[sm.k(0)], writes=[sm.k(0)])
        if d == 0:
            P.op("dve", lambda e: e.tensor_scalar(hsum.ap[:, i, h, :], nsb.ap[:, 0:96], sm.ap[:, 0:1], None, op0=ALU.mult), reads=[nsb.k(), sm.k(0)], writes=[hsum.k(i, h)])
        else:
            P.op("dve", lambda e: e.scalar_tensor_tensor(hsum.ap[:, i, h, :], nsb.ap[:, 0:96], sm.ap[:, 0:1], hsum.ap[:, i, h, :], op0=ALU.mult, op1=ALU.add),
                 reads=[nsb.k(), sm.k(0), hsum.k(i, h)], writes=[hsum.k(i, h)])
        P.op("dve", lambda e: e.scalar_tensor_tensor(C32.ap[0:96, h, :], C32.ap[0:96, h, :], sm.ap[0:96, 2:3], ps[bA][0:96, 256:353], op0=ALU.mult, op1=ALU.add),
             reads=[C32.k(h), sm.k(2), kA], writes=[C32.k(h)])
        P.op("act", lambda e: e.copy(Cb.ap[0:96, h, :], C32.ap[0:96, h, :]), reads=[C32.k(h)], writes=[Cb.k(h)])
        yield

    gens = [head_stream(h) for h in range(4)]
    while gens:
        for g_ in list(gens):
            try:
                next(g_)
            except StopIteration:
                gens.remove(g_)
    A.release(m1)
    yc = A.alloc("yc", (NT, 384), BF16)
    m3 = A.mark()
    wmo = load_w_bf16(k, "wmo", IN["w_in"].ap()[l][:, OFF_MO:OFF_MG], 8, 384, "wmo")
    lng = A.alloc("lng", (384,))
    P.dma("sp", lng.ap, IN["ml_ln_g"].ap()[l].partition_broadcast(128), writes=[lng.k()], semkey="lng")
    st = A.alloc("st", (16,)); cen = [A.alloc(f"cen{i}", (4, 96)) for i in range(2)]; junk = A.alloc("junkm", (96,))
    og = [A.alloc(f"og{i}", (384,)) for i in range(2)]
    for i in range(NT):
        r = i % 2
        b = 2 + r
        for c in range(8):
            P.op("pe", lambda e, c=c, i=i, b=b: e.matmul(ps[b][:, 0:384], k.hT.ap[:, c, i * 128:(i + 1) * 128], wmo.ap[:, c, :], start=(c == 0), stop=(c == 7)),
                 reads=[wmo.k(c), k.hT.k(i)], writes=[f"ps{b}"])
        P.op("act", lambda e, r=r, b=b: e.activation(og[r].ap, ps[b][:, 0:384], AF.Sigmoid), reads=[f"ps{b}"], writes=[og[r].k()])
        sk = st.k()
        P.op("dve", lambda e, i=i: e.tensor_reduce(st.ap[:, 0:4], hsum.ap[:, i, :, :], axis=AX.X, op=ALU.add), reads=[hsum.k(i, h_) for h_ in range(4)], writes=[sk])
        P.op("dve", lambda e: e.tensor_scalar(st.ap[:, 0:4], st.ap[:, 0:4], 1.0 / 96, None, op0=ALU.mult), reads=[sk], writes=[sk])
        P.op("pool", lambda e: e.memset(st.ap[:, 4:8], 0.0), writes=[sk])
        for h in range(4):
            P.op("dve", lambda e, i=i, h=h, r=r: e.tensor_scalar(cen[r].ap[:, h, :], hsum.ap[:, i, h, :], st.ap[:, h:h + 1], None, op0=ALU.subtract),
                 reads=[hsum.k(i, h), sk], writes=[cen[r].k(h)])
            P.op("act", lambda e, h=h, r=r: e.activation(junk.ap, cen[r].ap[:, h, :], AF.Square, accum_out=st.ap[:, 4 + h:5 + h]),
                 reads=[cen[r].k(h)], writes=[junk.k(), sk])
        P.op("act", lambda e: e.activation(st.ap[:, 8:12], st.ap[:, 4:8], AF.Sqrt, scale=1.0 / 96, bias=k.eps6.ap[:, 0:1]), reads=[sk, k.eps6.k()], writes=[sk])
        P.op("dve", lambda e: e.reciprocal(st.ap[:, 8:12], st.ap[:, 8:12]), reads=[sk], writes=[sk])
        for h in range(4):
            P.op("dve", lambda e, h=h, r=r: e.scalar_tensor_tensor(cen[r].ap[:, h, :], cen[r].ap[:, h, :], st.ap[:, 8 + h:9 + h], lng.ap[:, h * 96:(h + 1) * 96],
                                                                 op0=ALU.mult, op1=ALU.mult),
                 reads=[cen[r].k(h), sk, lng.k()], writes=[cen[r].k(h)])
        P.op("dve", lambda e, i=i, r=r: e.tensor_tensor(yc.ap[:, i, :], cen[r].ap.rearrange("p h c -> p (h c)"), og[r].ap, op=ALU.mult),
             reads=[cen[r].k(h) for h in range(4)] + [og[r].k()], writes=[yc.k(i)])
    A.release(m3)
    if k.dbg and "yc" in k.dbg:
        t = k.nc.dram_tensor("dbg_yc", [S, 384], BF16, kind="ExternalOutput")
        k.final_events.append(P.dma("sp", t.ap().rearrange("(i p) c -> p i c", p=128), yc.ap, reads=[yc.k(i) for i in range(NT)], semkey="dbg"))
    ycT = A.alloc("ycT", (3, S), BF16)
    transpose_to_T(k, yc, 3, ycT)
    outproj_partial(k, l, ycT, 3, 640, "c")
    A.release(m0)


OFF_RKV = 768
BLK = 256
PC_MU, PC_MUX, PC_W0, PC_A0, PC_KK, PC_KA, PC_RK, PC_LNG, PC_LNB, NPC = 0, 18, 20, 26, 32, 35, 38, 41, 44, 48


def host_rwkv_layout(inputs):
    out = {}
    pc = np.zeros((DEPTH, 128, NPC), np.float32)
    for l in range(DEPTH):
        mu = np.asarray(inputs["rk_mu"][l], np.float32)
        for d in range(2):
            for j in range(3):
                for p in range(3):
                    pc[l, :, PC_MU + d * 9 + j * 3 + p] = mu[d, j * 384 + p * 128: j * 384 + (p + 1) * 128]
            pc[l, 0:64, PC_MUX + d] = mu[d, 1152:1216]
            pc[l, 64:128, PC_MUX + d] = mu[d, 1216:1280]
            for p in range(3):
                pc[l, :, PC_W0 + d * 3 + p] = inputs["rk_w0"][l][d][p * 128:(p + 1) * 128]
                pc[l, :, PC_A0 + d * 3 + p] = inputs["rk_a0"][l][d][p * 128:(p + 1) * 128]
        for p in range(3):
            sl = slice(p * 128, (p + 1) * 128)
            pc[l, :, PC_KK + p] = inputs["rk_kk"][l][sl]
            pc[l, :, PC_KA + p] = inputs["rk_ka"][l][sl]
            pc[l, :, PC_RK + p] = np.asarray(inputs["rk_rk"][l]).reshape(384)[sl]
            pc[l, :, PC_LNG + p] = inputs["rk_ln_g"][l][sl]
            pc[l, :, PC_LNB + p] = inputs["rk_ln_b"][l][sl]
    out["c_pc"] = pc
    w_in = np.asarray(inputs["w_in"], np.float32)
    out["c_wx"] = np.ascontiguousarray(np.concatenate(
        [w_in[:, :, 1920:1984], w_in[:, :, 2048:2112], w_in[:, :, 1984:2048], w_in[:, :, 2112:2176], w_in[:, :, 2176:2304]], axis=2))
    return out


def rev_ap(ap, n):
    a = ap.ap
    return bass.AP(ap.tensor, ap.offset + (n - 1) * a[-1][0], [list(a[0]), [-a[-1][0], n]])


def psk(b, c0, c1):
    return [f"ps{b}q{q}" for q in range(c0 // 128, (c1 + 127) // 128)]


def rwkv_stage(k, l):
    P, A, ps, IN = k.P, k.A, k.ps, k.IN
    m0 = A.mark()
    k.mask4 = A.alloc("mask4", (512,)); k.rmask = A.alloc("rmask", (256,))
    P.dma("act", k.mask4.ap, IN["c_mask4"].ap(), writes=[k.mask4.k()], semkey="c5")
    P.dma("act", k.rmask.ap, IN["c_rmask"].ap(), writes=[k.rmask.k()], semkey="c7")
    ybT = A.alloc("ybT", (3, S), BF16)
    P.op("pool", lambda e: e.memset(ybT.ap, 0.0), writes=[ybT.k(i) for i in range(NT)])
    xsp = k.xspill.ap().rearrange("(i p) d -> p i d", p=128)
    for i in range(NT):
        P.dma("sp" if i % 2 == 0 else "act", xsp[:, i, :], k.xres.ap[:, i, :], reads=[k.xres.k(i)], writes=[("xsp", i)], semkey=("xso", i % 4))
    evs = []
    for kk_ in [q for q in P.keys if isinstance(q, tuple) and q[0] == k.xres.uid]:
        st = P.keys.pop(kk_)
        evs.extend(st["w"]); evs.extend(st["r"].values())
    AX = Arena.__new__(Arena)
    AX.P, AX.t, AX.n, AX.top, AX.gen, AX.live = P, A.t, k.xres.hi, k.xres.lo, 100000 + 1000 * l, []
    AX.pending = [(k.xres.lo, k.xres.hi, evs)]
    import os as _os
    LVL = int(_os.environ.get("RWKV_SETUP", "9"))
    pc = A.alloc("pc", (NPC + 4,))
    P.dma("sp", pc.ap[:, 0:NPC], IN["c_pc"].ap()[l], writes=[pc.k()], semkey="pc")
    PCO = NPC
    P.op("dve", lambda e: e.tensor_scalar(pc.ap[:, PCO:PCO + 3], pc.ap[:, PC_KA:PC_KA + 3], -1.0, 1.0, op0=ALU.mult, op1=ALU.add), reads=[pc.k()], writes=[pc.k()])
    wl = [A.alloc(f"wl{d}", (384,)) for d in range(2)]
    for d in range(2):
        P.dma("sp", wl[d].ap[0:64, :], IN["rk_w2"].ap()[l][d], writes=[wl[d].k()], semkey=("wl", d))
        P.dma("act", wl[d].ap[64:128, :], IN["rk_a2"].ap()[l][d], writes=[wl[d].k()], semkey=("wl2", d))
    g2b = A.alloc("g2b", (384,), BF16)
    P.dma("pool", g2b.ap, IN["rk_g2"].ap()[l], writes=[g2b.k()], semkey="g2b")
    siggd = A.alloc("siggd", (S,), BF16)
    wdad = [(A if int(_os.environ.get("WDAD_MAIN", "0")) else AX).alloc(f"wdad{d}", (S,)) for d in range(2)]
    mA = A.mark()
    wx = load_w_bf16(k, "wx", IN["c_wx"].ap()[l], 8, 384, "wx")
    zx = A.alloc("zx", (S + 2,)); tmp = A.alloc("tmpx", (S,))
    P.op("pool", lambda e: e.memset(zx.ap[:, 0:1], 0.0), writes=[zx.k()])
    P.op("pool", lambda e: e.memset(zx.ap[:, S + 1:S + 2], 0.0), writes=[zx.k()])
    n = 0
    for d in range(2 if LVL >= 2 else 0):
        for tg in range(4):
            b = n % 2; n += 1
            for c in range(8):
                P.op("pe", lambda e, c=c, d=d, tg=tg, b=b: e.matmul(ps[b][:, :], wx.ap[:, c, d * 128:(d + 1) * 128], k.hT.ap[:, c, tg * 512:(tg + 1) * 512], start=(c == 0), stop=(c == 7)),
                     reads=[wx.k(c)] + [k.hT.k(4 * tg + q) for q in range(4)], writes=[f"ps{b}"])
            P.op("act", lambda e, tg=tg, b=b: e.copy(zx.ap[:, 1 + tg * 512:1 + (tg + 1) * 512], ps[b][:, :]), reads=[f"ps{b}"], writes=[zx.k()])
        if LVL < 3:
            continue
        if d == 0:
            cur, prv = zx.ap[:, 1:S + 1], zx.ap[:, 0:S]
        else:
            cur, prv = rev_ap(zx.ap[:, 1:S + 1], S), rev_ap(zx.ap[:, 2:S + 2], S)
        P.op("dve", lambda e, cur=cur, prv=prv: e.tensor_tensor(tmp.ap, prv, cur, op=ALU.subtract), reads=[zx.k()], writes=[tmp.k()])
        P.op("dve", lambda e, cur=cur, d=d: e.scalar_tensor_tensor(wdad[d].ap, tmp.ap, pc.ap[:, PC_MUX + d:PC_MUX + d + 1], cur, op0=ALU.mult, op1=ALU.add),
             reads=[tmp.k(), pc.k(), zx.k()], writes=[wdad[d].k()])
        P.op("act", lambda e, d=d: e.activation(wdad[d].ap[0:64, :], wdad[d].ap[0:64, :], AF.Tanh), reads=[wdad[d].k()], writes=[wdad[d].k()])
    for tg in range(4 if LVL >= 4 else 0):
        b = n % 2; n += 1
        for c in range(8):
            P.op("pe", lambda e, c=c, tg=tg, b=b: e.matmul(ps[b][:, :], wx.ap[:, c, 256:384], k.hT.ap[:, c, tg * 512:(tg + 1) * 512], start=(c == 0), stop=(c == 7)),
                 reads=[wx.k(c)] + [k.hT.k(4 * tg + q) for q in range(4)], writes=[f"ps{b}"])
        P.op("act", lambda e, tg=tg, b=b: e.activation(siggd.ap[:, tg * 512:(tg + 1) * 512], ps[b][:, :], AF.Sigmoid), reads=[f"ps{b}"], writes=[siggd.k()])
    A.release(mA)
    NB = ["XR", "XK", "XV", "T1", "LW", "AA", "KK", "SQ", "CUM"]
    NB16 = ["XRb", "XKb", "KKb", "AAb", "XVb", "BH", "KH"]
    SB = []
    for s_ in range(2):
        sb = {nm: A.alloc(f"{nm}{s_}", (BLK,)) for nm in NB}
        sb.update({nm: A.alloc(f"{nm}{s_}", (BLK,), BF16) for nm in NB16})
        sb["PCc"] = A.alloc(f"PCc{s_}", (BLK // 128,))
        sb["AR"] = A.alloc(f"AR{s_}", (BLK // 128, 256), BF16)
        sb["FT"] = A.alloc(f"FT{s_}", (512,))
        sb["AM"] = [A.alloc(f"AM{s_}{x}", (512,), BF16) for x in range(2)]
        sb["M"] = [[A.alloc(f"M{s_}{x}{q}", (128,), BF16) for q in range(2)] for x in range(2)]
        sb["MT"] = [[A.alloc(f"MT{s_}{x}{q}", (128,), BF16) for q in range(2)] for x in range(2)]
        sb["B"] = [[A.alloc(f"Bq{s_}{x}{q}", (384,), BF16) for q in range(2)] for x in range(2)]
        sb["Q"] = [A.alloc(f"Q{s_}{x}", (128,)) for x in range(2)]
        sb["Q16"] = [A.alloc(f"Qb{s_}{x}", (128,), BF16) for x in range(2)]
        sb["RHS"] = A.alloc(f"RHS{s_}", (128,), BF16)
        sb["SAz"] = [A.alloc(f"SAz{s_}{x}", (128,), BF16) for x in range(2)]
        sb["Vz"] = [A.alloc(f"Vz{s_}{x}", (128,), BF16) for x in range(2)]
        sb["BHt"] = A.alloc(f"BHt{s_}", (128,), BF16); sb["KHt"] = A.alloc(f"KHt{s_}", (128,), BF16)
        sb["T"] = A.alloc(f"Tst{s_}", (128,)); sb["T16"] = A.alloc(f"Tsb{s_}", (128,), BF16)
        sb["pb"] = 4 * s_
        SB.append(sb)
    import os as _os
    for p in range(int(_os.environ.get('RWKV_PAIRS', '3'))):
        mX = AX.mark()
        wp = AX.alloc("wp", (8, 3, 128), BF16)
        wv_ = IN["w_in"].ap()[l].rearrange("(c q) n -> q c n", q=128)
        for j in range(3):
            c0 = OFF_RKV + j * 384 + p * 128
            for c in range(8):
                P.dma("pool", wp.ap[:, c, j, :], wv_[:, c, c0:c0 + 128], writes=[wp.k(j)], semkey=("wp", j))
        zp = AX.alloc("zp", (3, S + 2))
        yacc = AX.alloc("yacc", (S,)); bonacc = AX.alloc("bonacc", (S,))
        P.op("pool", lambda e: e.memset(yacc.ap, 0.0), writes=[yacc.k(c) for c in range(NT)])
        P.op("pool", lambda e: e.memset(bonacc.ap, 0.0), writes=[bonacc.k(c) for c in range(NT)])
        for j in range(3):
            P.op("pool", lambda e, j=j: e.memset(zp.ap[:, j, 0:1], 0.0), writes=[zp.k(j)])
            P.op("pool", lambda e, j=j: e.memset(zp.ap[:, j, S + 1:S + 2], 0.0), writes=[zp.k(j)])
            for tg in range(4):
                b = n % 2; n += 1
                for c in range(8):
                    P.op("pe", lambda e, c=c, j=j, tg=tg, b=b: e.matmul(ps[b][:, :], wp.ap[:, c, j, :], k.hT.ap[:, c, tg * 512:(tg + 1) * 512], start=(c == 0), stop=(c == 7)),
                         reads=[wp.k(j)] + [k.hT.k(4 * tg + q) for q in range(4)], writes=[f"ps{b}"] + psk(b, 0, 512))
                P.op("act", lambda e, j=j, tg=tg, b=b: e.copy(zp.ap[:, j, 1 + tg * 512:1 + (tg + 1) * 512], ps[b][:, :]), reads=[f"ps{b}"] + psk(b, 0, 512), writes=[zp.k(j)])
        gens = [rwkv_stream(k, l, p, d, SB[d], pc, wl[d], wdad[d], zp, yacc, bonacc, PCO) for d in range(int(_os.environ.get("RWKV_NDIR", "2")))]
        while gens:
            for g in list(gens):
                try:
                    next(g)
                except StopIteration:
                    gens.remove(g)
        T1 = SB[0]["FT"]; T2 = SB[1]["FT"]
        for tg in range(4 if int(_os.environ.get("RWKV_FIN", "1")) else 0):
            cs = slice(tg * 512, (tg + 1) * 512)
            yk = [yacc.k(c) for c in range(4 * tg, 4 * tg + 4)]
            P.op("pe", lambda e, cs=cs: e.matmul(ps[0][:, :], k.onesblk64.ap, yacc.ap[:, cs], start=True, stop=True), reads=[k.onesblk64.k()] + yk, writes=["ps0"] + psk(0, 0, 512))
            P.op("dve", lambda e, cs=cs: e.tensor_tensor(yacc.ap[:, cs], yacc.ap[:, cs], ps[0][:, :], op=ALU.subtract), reads=["ps0"] + psk(0, 0, 512) + yk, writes=yk)
            P.op("act", lambda e, cs=cs: e.activation(T1.ap, yacc.ap[:, cs], AF.Square), reads=yk, writes=[T1.k()])
            P.op("pe", lambda e: e.matmul(ps[1][:, :], k.onesblk64.ap, T1.ap, start=True, stop=True), reads=[k.onesblk64.k(), T1.k()], writes=["ps1"] + psk(1, 0, 512))
            P.op("act", lambda e: e.activation(T2.ap, ps[1][:, :], AF.Sqrt, bias=k.epsgn.ap[:, 0:1]), reads=["ps1", k.epsgn.k()] + psk(1, 0, 512), writes=[T2.k()])
            P.op("dve", lambda e: e.reciprocal(T2.ap, T2.ap), reads=[T2.k()], writes=[T2.k()])
            P.op("dve", lambda e, cs=cs: e.tensor_tensor(yacc.ap[:, cs], yacc.ap[:, cs], T2.ap, op=ALU.mult), reads=yk + [T2.k()], writes=yk)
            P.op("dve", lambda e, cs=cs, p=p: e.tensor_scalar(yacc.ap[:, cs], yacc.ap[:, cs], pc.ap[:, PC_LNG + p:PC_LNG + p + 1], pc.ap[:, PC_LNB + p:PC_LNB + p + 1], op0=ALU.mult, op1=ALU.add),
                 reads=yk + [pc.k()], writes=yk)
            P.op("dve", lambda e, cs=cs: e.tensor_tensor(yacc.ap[:, cs], yacc.ap[:, cs], bonacc.ap[:, cs], op=ALU.add), reads=yk + [bonacc.k(c) for c in range(4 * tg, 4 * tg + 4)], writes=yk)
            P.op("pe", lambda e, cs=cs, p=p: e.matmul(ps[2][:, :], g2b.ap[:, p * 128:(p + 1) * 128], siggd.ap[:, cs], start=True, stop=True), reads=[g2b.k(), siggd.k()], writes=["ps2"] + psk(2, 0, 512))
            P.op("dve", lambda e, cs=cs, p=p: e.tensor_tensor(ybT.ap[:, p, cs], yacc.ap[:, cs], ps[2][:, :], op=ALU.mult), reads=yk + ["ps2"] + psk(2, 0, 512), writes=[ybT.k(c) for c in range(4 * tg, 4 * tg + 4)])
        AX.release(mX)
    AX.release((k.xres.lo, 0))
    evs = []
    for (_, _, e_) in AX.pending:
        evs.extend(e_)
    P.inherit[k.xres.uid] = evs
    for i in range(NT):
        P.dma("sp" if i % 2 == 0 else "act", k.xres.ap[:, i, :], xsp[:, i, :], reads=[("xsp", i)], writes=[k.xres.k(i)], semkey=("xsi", i % 4))
    if k.dbg and "yb" in k.dbg:
        t = k.nc.dram_tensor("dbg_yb", [384, S], BF16, kind="ExternalOutput")
        k.final_events.append(P.dma("sp", t.ap().rearrange("(c p) s -> p c s", p=128), ybT.ap, reads=[ybT.k(i) for i in range(NT)], semkey="dbg"))
    if LVL >= 5:
        outproj_partial(k, l, ybT, 3, 256, "b")
    A.release(m0)


def rwkv_stream(k, l, p, d, sb, pc, wl, wdad, zp, yacc, bonacc, PCO):
    P, ps = k.P, k.ps
    pb = sb["pb"]
    B0, B1, B2, B3 = pb, pb + 1, pb + 2, pb + 3
    XR, XK, XV, T1, LW, AA, KK, SQ, CUM, BH, KH, PCc = (sb[n_] for n_ in ["XR", "XK", "XV", "T1", "LW", "AA", "KK", "SQ", "CUM", "BH", "KH", "PCc"])
    XRb, XKb, KKb, AAb, XVb = (sb[n_] for n_ in ["XRb", "XKb", "KKb", "AAb", "XVb"])
    T = sb["T"]; T16 = sb["T16"]
    P.op("pool", lambda e: e.memset(T16.ap, 0.0), writes=[T16.k()])
    col = lambda c: pc.ap[:, c:c + 1]
    P.op("pool", lambda e: e.memset(T.ap, 0.0), writes=[T.k()])
    for x in range(2):
        P.op("pool", lambda e, x=x: e.memset(sb["SAz"][x].ap, 0.0), writes=[sb["SAz"][x].k()])
        P.op("pool", lambda e, x=x: e.memset(sb["Vz"][x].ap, 0.0), writes=[sb["Vz"][x].k()])
    import os as _os
    NBLK = int(_os.environ.get("RWKV_NBLK", str(S // BLK))); PH = int(_os.environ.get("RWKV_PHASE", "9"))
    def _blk(bi):
        t0 = bi * BLK
        if d == 0:
            cur = lambda j: zp.ap[:, j, 1 + t0:1 + t0 + BLK]
            prv = lambda j: zp.ap[:, j, t0:t0 + BLK]
            nat = lambda buf: buf.ap[:, t0:t0 + BLK]
            nchunks = list(range(t0 // 128, (t0 + BLK) // 128))
        else:
            a_ = S - t0 - BLK
            cur = lambda j: rev_ap(zp.ap[:, j, 1 + a_:1 + a_ + BLK], BLK)
            prv = lambda j: rev_ap(zp.ap[:, j, 2 + a_:2 + a_ + BLK], BLK)
            nat = lambda buf: rev_ap(buf.ap[:, a_:a_ + BLK], BLK)
            nchunks = list(range(a_ // 128, (a_ + BLK) // 128))
        scols = slice(t0, t0 + BLK)
        P0 = int(_os.environ.get("RWKV_P0", "9"))
        for j, X in enumerate((XR, XK, XV)):
            if P0 < 2:
                break
            P.op("dve", lambda e, j=j, prv=prv, cur=cur: e.tensor_tensor(T1.ap, prv(j), cur(j), op=ALU.subtract), reads=[zp.k(j)], writes=[T1.k()])
            P.op("dve", lambda e, j=j, X=X, cur=cur: e.scalar_tensor_tensor(X.ap, T1.ap, col(PC_MU + d * 9 + j * 3 + p), cur(j), op0=ALU.mult, op1=ALU.add),
                 reads=[T1.k(), zp.k(j), pc.k()], writes=[X.k()])
        if P0 >= 3:
            P.op("pe", lambda e, scols=scols: e.matmul(ps[B3][:, 0:BLK], wl.ap[0:64, p * 128:(p + 1) * 128], wdad.ap[0:64, scols], start=True, stop=True),
                 reads=[wl.k(), wdad.k()], writes=psk(B3, 0, BLK), serial=True)
        if P0 >= 4:
            P.op("pe", lambda e, scols=scols: e.matmul(ps[B3][:, 256:256 + BLK], wl.ap[64:128, p * 128:(p + 1) * 128], wdad.ap[64:128, scols], start=True, stop=True),
                 reads=[wl.k(), wdad.k()], writes=psk(B3, 256, 256 + BLK), serial=True)
        if P0 >= 5:
            P.op("act", lambda e: e.activation(LW.ap, ps[B3][:, 0:BLK], AF.Sigmoid, bias=col(PC_W0 + d * 3 + p)), reads=psk(B3, 0, BLK) + [pc.k()], writes=[LW.k()])
            P.op("act", lambda e: e.activation(AA.ap, ps[B3][:, 256:256 + BLK], AF.Sigmoid, bias=col(PC_A0 + d * 3 + p)), reads=psk(B3, 256, 256 + BLK) + [pc.k()], writes=[AA.k()])
        if P0 >= 6:
            P.op("dve", lambda e: e.tensor_scalar(LW.ap, LW.ap, -0.6065306597126334, None, op0=ALU.mult), reads=[LW.k()], writes=[LW.k()])
        yield
        if PH <= 1:
            return
        P.op("dve", lambda e: e.tensor_scalar(KK.ap, XK.ap, col(PC_KK + p), None, op0=ALU.mult), reads=[XK.k(), pc.k()], writes=[KK.k()])
        P.op("act", lambda e: e.activation(SQ.ap, KK.ap, AF.Square), reads=[KK.k()], writes=[SQ.k()])
        P.op("pe", lambda e: e.matmul(ps[B2][:, 0:BLK], k.onesblk.ap, SQ.ap, start=True, stop=True), reads=[k.onesblk.k(), SQ.k()], writes=psk(B2, 0, BLK))
        P.op("act", lambda e: e.activation(SQ.ap, ps[B2][:, 0:BLK], AF.Sqrt), reads=psk(B2, 0, BLK), writes=[SQ.k()])
        P.op("dve", lambda e: e.tensor_scalar(SQ.ap, SQ.ap, 1e-12, None, op0=ALU.max), reads=[SQ.k()], writes=[SQ.k()])
        P.op("dve", lambda e: e.reciprocal(SQ.ap, SQ.ap), reads=[SQ.k()], writes=[SQ.k()])
        P.op("dve", lambda e: e.tensor_tensor(KK.ap, KK.ap, SQ.ap, op=ALU.mult), reads=[KK.k(), SQ.k()], writes=[KK.k()])
        P.op("dve", lambda e: e.tensor_scalar(T1.ap, AA.ap, col(PC_KA + p), col(PCO + p), op0=ALU.mult, op1=ALU.add), reads=[AA.k(), pc.k()], writes=[T1.k()])
        P.op("dve", lambda e: e.tensor_tensor(XK.ap, XK.ap, T1.ap, op=ALU.mult), reads=[XK.k(), T1.k()], writes=[XK.k()])
        P.op("dve", lambda e: e.scalar_tensor_tensor(T1.ap, XR.ap, col(PC_RK + p), XK.ap, op0=ALU.mult, op1=ALU.mult), reads=[XR.k(), XK.k(), pc.k()], writes=[T1.k()])
        P.op("pe", lambda e: e.matmul(ps[B2][:, 256:256 + BLK], k.onesblk.ap, T1.ap, start=True, stop=True), reads=[k.onesblk.k(), T1.k()], writes=psk(B2, 256, 256 + BLK))
        P.op("dve", lambda e: e.tensor_tensor(T1.ap, ps[B2][:, 256:256 + BLK], XV.ap, op=ALU.mult), reads=psk(B2, 256, 256 + BLK) + [XV.k()], writes=[T1.k()])
        bk = [bonacc.k(c) for c in nchunks]
        P.op("dve", lambda e: e.tensor_tensor(nat(bonacc), nat(bonacc), T1.ap, op=ALU.add), reads=bk + [T1.k()], writes=bk)
        P.op("dve", lambda e: e.tensor_tensor(AA.ap, AA.ap, KK.ap, op=ALU.mult), reads=[AA.k(), KK.k()], writes=[AA.k()])
        yield
        if PH <= 2:
            return
        P.op("dve", lambda e: e.tensor_tensor_scan(CUM.ap, k.rmask.ap[:, 0:BLK], LW.ap, 0.0, op0=ALU.mult, op1=ALU.add), reads=[k.rmask.k(), LW.k()], writes=[CUM.k()])
        P.op("dve", lambda e: e.tensor_tensor(LW.ap, CUM.ap, LW.ap, op=ALU.subtract), reads=[CUM.k(), LW.k()], writes=[LW.k()])
        P.op("act", lambda e: e.activation(T1.ap, CUM.ap, AF.Exp), reads=[CUM.k()], writes=[T1.k()])
        P.op("dve", lambda e: e.tensor_tensor(XR.ap, XR.ap, T1.ap, op=ALU.mult), reads=[XR.k(), T1.k()], writes=[XR.k()])
        P.op("act", lambda e: e.activation(SQ.ap, CUM.ap, AF.Exp, scale=-1.0), reads=[CUM.k()], writes=[SQ.k()])
        P.op("dve", lambda e: e.tensor_tensor(AA.ap, AA.ap, SQ.ap, op=ALU.mult), reads=[AA.k(), SQ.k()], writes=[AA.k()])
        P.op("dve", lambda e: e.tensor_tensor(XK.ap, XK.ap, SQ.ap, op=ALU.mult), reads=[XK.k(), SQ.k()], writes=[XK.k()])
        P.op("act", lambda e: e.activation(T1.ap, LW.ap, AF.Exp), reads=[LW.k()], writes=[T1.k()])
        P.op("dve", lambda e: e.scalar_tensor_tensor(KK.ap, KK.ap, -1.0, T1.ap, op0=ALU.mult, op1=ALU.mult), reads=[KK.k(), T1.k()], writes=[KK.k()])
        AR = sb["AR"]
        for src_, dst_ in ((XK, XKb), (AA, AAb), (XV, XVb)):
            P.op("act", lambda e, src_=src_, dst_=dst_: e.copy(dst_.ap, src_.ap), reads=[src_.k()], writes=[dst_.k()])
        P.op("act", lambda e: e.copy(AR.ap[:, :, 0:128], KK.ap.rearrange("p (c t) -> p c t", t=128)), reads=[KK.k()], writes=[AR.k()])
        P.op("act", lambda e: e.copy(AR.ap[:, :, 128:256], XR.ap.rearrange("p (c t) -> p c t", t=128)), reads=[XR.k()], writes=[AR.k()])
        P.op("act", lambda e: e.activation(PCc.ap, CUM.ap.rearrange("p (c t) -> p c t", t=128)[:, :, 127], AF.Exp), reads=[CUM.k()], writes=[PCc.k()])
        for ch in range(BLK // 128):
            cs = slice(ch * 128, (ch + 1) * 128)
            P.op("dve", lambda e, cs=cs, ch=ch: e.tensor_scalar(BH.ap[:, cs], AA.ap[:, cs], PCc.ap[:, ch:ch + 1], None, op0=ALU.mult), reads=[AA.k(), PCc.k()], writes=[BH.k()])
            P.op("dve", lambda e, cs=cs, ch=ch: e.tensor_scalar(KH.ap[:, cs], XK.ap[:, cs], PCc.ap[:, ch:ch + 1], None, op0=ALU.mult), reads=[XK.k(), PCc.k()], writes=[KH.k()])
        yield
        if PH <= 3:
            return
        def _chunk(ch):
            cs = slice(ch * 128, (ch + 1) * 128)
            AM, Mb, MTb, Q, RHS, SAz, Vz, BHt, KHt = sb["AM"], sb["M"], sb["MT"], sb["Q"], sb["RHS"], sb["SAz"], sb["Vz"], sb["BHt"], sb["KHt"]
            for x in range(2):
                hs = slice(64 * x, 64 * x + 64)
                bx = B0 + x
                for q, lh in enumerate((AAb, XKb)):
                    P.op("pe", lambda e, lh=lh, q=q, hs=hs, bx=bx: e.matmul(ps[bx][:, q * 256:(q + 1) * 256], lh.ap[hs, cs], sb["AR"].ap[hs, ch, :], start=True, stop=True),
                         reads=[lh.k(), sb["AR"].k()], writes=psk(bx, q * 256, (q + 1) * 256), serial=True)
                P.op("pe", lambda e, hs=hs, x=x: e.matmul(ps[B2][:, x * 128:(x + 1) * 128], sb["AR"].ap[hs, ch, 0:128], AAb.ap[hs, cs], start=True, stop=True),
                     reads=[sb["AR"].k(), AAb.k()], writes=psk(B2, x * 128, (x + 1) * 128), serial=True)
                P.op("dve", lambda e, x=x, bx=bx: e.tensor_tensor(AM[x].ap, ps[bx][:, :], k.mask4.ap, op=ALU.mult), reads=psk(bx, 0, 512) + [k.mask4.k()], writes=[AM[x].k()])
                P.op("dve", lambda e, x=x: e.tensor_tensor(MTb[x][0].ap, ps[B2][:, x * 128:(x + 1) * 128], k.trils.ap, op=ALU.mult), reads=psk(B2, x * 128, (x + 1) * 128) + [k.trils.k()], writes=[MTb[x][0].k()])
            yield
            if PH <= 4:
                return
            pbt = ps[B3][:, 0:192].bitcast(BF16)
            for q, src in enumerate((XVb, BH, KH)):
                P.op("pe", lambda e, q=q, src=src: e.transpose(pbt[:, q * 128:(q + 1) * 128], src.ap[:, cs], k.ident16.ap), reads=[src.k(), k.ident16.k()], writes=psk(B3, 0, 192))
            P.op("act", lambda e: e.copy(Vz[0].ap[:, 0:64], pbt[:, 0:64]), reads=psk(B3, 0, 192), writes=[Vz[0].k()])
            P.op("act", lambda e: e.copy(Vz[1].ap[:, 64:128], pbt[:, 64:128]), reads=psk(B3, 0, 192), writes=[Vz[1].k()])
            P.op("dve", lambda e: e.tensor_copy(BHt.ap, pbt[:, 128:256]), reads=psk(B3, 0, 192), writes=[BHt.k()])
            P.op("dve", lambda e: e.tensor_copy(KHt.ap, pbt[:, 256:384]), reads=psk(B3, 0, 192), writes=[KHt.k()])
            Bq = sb["B"]
            for x in range(2):
                bx = B0 + x
                P.op("pe", lambda e, bx=bx, x=x: e.matmul(ps[bx][:, 0:128], AM[x].ap[:, 0:128], MTb[x][0].ap, start=True, stop=True), reads=[AM[x].k(), MTb[x][0].k()], writes=psk(bx, 0, 128))
                P.op("pe", lambda e, bx=bx, x=x: e.matmul(ps[bx][:, 128:256], MTb[x][0].ap, AM[x].ap[:, 0:128], start=True, stop=True), reads=[AM[x].k(), MTb[x][0].k()], writes=psk(bx, 128, 256))
                P.op("act", lambda e, bx=bx, x=x: e.copy(Bq[x][1].ap[:, 0:256], ps[bx][:, 0:256]), reads=psk(bx, 0, 256), writes=[Bq[x][1].k()])
                P.op("dve", lambda e, x=x: e.tensor_tensor(Bq[x][1].ap[:, 256:384], AM[x].ap[:, 0:128], k.ident.ap, op=ALU.add), reads=[AM[x].k(), k.ident.k()], writes=[Bq[x][1].k()])
            yield
            for lev in range(1, 7):
                cur, nxt = lev % 2, 1 - lev % 2
                for x in range(2):
                    bx = B0 + x
                    Bc, Bn = Bq[x][cur], Bq[x][nxt]
                    if lev < 6:
                        P.op("pe", lambda e, bx=bx, Bc=Bc: e.matmul(ps[bx][:, 0:128], Bc.ap[:, 128:256], Bc.ap[:, 0:128], start=True, stop=True), reads=[Bc.k()], writes=psk(bx, 0, 128))
                        P.op("pe", lambda e, bx=bx, Bc=Bc: e.matmul(ps[bx][:, 128:384], Bc.ap[:, 0:128], Bc.ap[:, 128:384], start=True, stop=True), reads=[Bc.k()], writes=psk(bx, 128, 384))
                        P.op("act", lambda e, bx=bx, Bn=Bn: e.copy(Bn.ap[:, 0:256], ps[bx][:, 0:256]), reads=psk(bx, 0, 384), writes=[Bn.k()])
                        P.op("dve", lambda e, bx=bx, Bc=Bc, Bn=Bn: e.tensor_tensor(Bn.ap[:, 256:384], Bc.ap[:, 256:384], ps[bx][:, 256:384], op=ALU.add), reads=psk(bx, 0, 384) + [Bc.k()], writes=[Bn.k()])
                    else:
                        P.op("pe", lambda e, bx=bx, Bc=Bc: e.matmul(ps[bx][:, 256:384], Bc.ap[:, 0:128], Bc.ap[:, 256:384], start=True, stop=True), reads=[Bc.k()], writes=psk(bx, 256, 384))
                        P.op("dve", lambda e, bx=bx, Bc=Bc, x=x: e.tensor_tensor(sb["Q16"][x].ap, Bc.ap[:, 256:384], ps[bx][:, 256:384], op=ALU.add), reads=psk(bx, 0, 384) + [Bc.k()], writes=[sb["Q16"][x].k()])
                yield
            for x in range(2):
                hs = slice(64 * x, 64 * x + 64)
                P.op("pe", lambda e, hs=hs: e.matmul(ps[B2][:, 256 + hs.start:256 + hs.stop], sb["AR"].ap[hs, ch, 0:128], T16.ap[hs, hs], start=True, stop=False), reads=[sb["AR"].k(), T16.k()], writes=psk(B2, 256, 384), serial=True)
                P.op("pe", lambda e, hs=hs, x=x: e.matmul(ps[B2][:, 256 + hs.start:256 + hs.stop], AM[x].ap[:, 256:384], Vz[x].ap[:, hs], start=False, stop=True), reads=[AM[x].k(), Vz[x].k()], writes=psk(B2, 256, 384))
            P.op("act", lambda e: e.copy(RHS.ap, ps[B2][:, 256:384]), reads=psk(B2, 256, 384), writes=[RHS.k()])
            for x in range(2):
                hs = slice(64 * x, 64 * x + 64)
                P.op("pe", lambda e, hs=hs, x=x: e.matmul(ps[B2][:, 384 + hs.start:384 + hs.stop], sb["Q16"][x].ap, RHS.ap[:, hs], start=True, stop=True), reads=[sb["Q16"][x].k(), RHS.k()], writes=psk(B2, 384, 512))
                P.op("act" if x == 0 else "dve", (lambda e, hs=hs, x=x: e.copy(SAz[x].ap[:, hs], ps[B2][:, 384 + hs.start:384 + hs.stop])) if x == 0 else
                     (lambda e, hs=hs, x=x: e.tensor_copy(SAz[x].ap[:, hs], ps[B2][:, 384 + hs.start:384 + hs.stop])), reads=psk(B2, 384, 512), writes=[SAz[x].k()])
            yield
            if PH <= 6:
                return
            ops_ = []
            for x in range(2):
                hs = slice(64 * x, 64 * x + 64)
                ops_.append((T16.ap[hs, :], sb["AR"].ap[hs, ch, 128:256], [T16.k(), sb["AR"].k()]))
                ops_.append((SAz[x].ap, AM[x].ap[:, 128:256], [SAz[x].k(), AM[x].k()]))
                ops_.append((Vz[x].ap, AM[x].ap[:, 384:512], [Vz[x].k(), AM[x].k()]))
            for q, (lh, rh, rd) in enumerate(ops_):
                P.op("pe", lambda e, lh=lh, rh=rh, q=q: e.matmul(ps[B3][:, 384:512], lh, rh, start=(q == 0), stop=(q == len(ops_) - 1)), reads=rd, writes=psk(B3, 384, 512), serial=(q % 3 == 0))
            cn = nchunks[ch] if d == 0 else nchunks[len(nchunks) - 1 - ch]
            if d == 0:
                ydst = yacc.ap[:, cn * 128:(cn + 1) * 128]
            else:
                ydst = rev_ap(yacc.ap[:, cn * 128:(cn + 1) * 128], 128)
            P.op("dve", lambda e, ydst=ydst: e.tensor_tensor(ydst, ydst, ps[B3][:, 384:512], op=ALU.add), reads=psk(B3, 384, 512) + [yacc.k(cn)], writes=[yacc.k(cn)])
            for x in range(2):
                hs = slice(64 * x, 64 * x + 64)
                P.op("pe", lambda e, hs=hs, x=x: e.matmul(ps[B2][:, hs], BHt.ap, SAz[x].ap[:, hs], start=True, stop=False), reads=[BHt.k(), SAz[x].k()], writes=psk(B2, 0, 128))
                P.op("pe", lambda e, hs=hs, x=x: e.matmul(ps[B2][:, hs], KHt.ap, Vz[x].ap[:, hs], start=False, stop=True), reads=[KHt.k(), Vz[x].k()], writes=psk(B2, 0, 128))
            for x in range(2):
                hs = slice(64 * x, 64 * x + 64)
                P.op("dve", lambda e, hs=hs, ch=ch: e.scalar_tensor_tensor(T.ap[hs, hs], T.ap[hs, hs], PCc.ap[hs, ch:ch + 1], ps[B2][hs, hs], op0=ALU.mult, op1=ALU.add),
                     reads=[T.k(), PCc.k()] + psk(B2, 0, 128), writes=[T.k()])
            P.op("act", lambda e: e.copy(T16.ap, T.ap), reads=[T.k()], writes=[T16.k()])
            yield
            if PH <= 7:
                return
        for ch in range(BLK // 128):
            yield from _chunk(ch)

    for bi in range(NBLK):
        yield from _blk(bi)


FB = 256
NFB = DFF // FB
NFC = DFF // 128
W2G = 2


def moe_stage(k, l):
    P, A, ps, IN = k.P, k.A, k.ps, k.IN
    m0 = A.mark()
    k.ohb = A.alloc("ohb", (2048,))
    P.dma("act", k.ohb.ap[0:16, :], IN["c_ohb"].ap(), writes=[k.ohb.k()], semkey="c4")
    x2b = A.alloc("x2b", (NT, D), BF16)
    aff = A.alloc("aff", (NT, NE))
    pm = A.alloc("pm", (NT, NE))
    pmT = A.alloc("pmT", (S,))
    m1 = A.mark()
    gb = A.alloc("gb2", (D,)); junk = A.alloc("junk2", (D,)); ss = A.alloc("ss2", (NT,)); rstd = A.alloc("rstd2", (NT,))
    x2f = [A.alloc(f"x2f{i}", (D,)) for i in range(2)]
    x2T = [A.alloc(f"x2T{i}", (8, 128)) for i in range(2)]
    rsb = A.alloc("rsb", (8, NE)); sm = A.alloc("smx", (NT, 4))
    affT = A.alloc("affT", (S,))
    P.dma("sp", gb.ap, IN["ln2_g"].ap()[l].partition_broadcast(128), writes=[gb.k()], semkey="gb")
    P.dma("act", rsb.ap, IN["router"].ap()[l].rearrange("(c p) e -> p c e", p=128), writes=[rsb.k()], semkey="rsb")
    P.op("pool", lambda e: e.memset(ss.ap, 0.0), writes=[ss.k(i) for i in range(NT)])
    P.op("pool", lambda e: e.memset(sm.ap, 0.0), writes=[sm.k()])
    for i in range(NT):
        P.op("act", lambda e, i=i: e.activation(junk.ap, k.xres.ap[:, i, :], AF.Square, accum_out=ss.ap[:, i:i + 1]),
             reads=[k.xres.k(i)], writes=[junk.k(), ss.k(i)])
    P.op("act", lambda e: e.activation(rstd.ap, ss.ap, AF.Sqrt, scale=1.0 / D, bias=k.eps6.ap[:, 0:1]),
         reads=[ss.k(i) for i in range(NT)] + [k.eps6.k()], writes=[rstd.k()])
    P.op("dve", lambda e: e.reciprocal(rstd.ap, rstd.ap), reads=[rstd.k()], writes=[rstd.k()])
    for i in range(NT):
        xf = x2f[i % 2]; xt = x2T[i % 2]
        P.op("dve", lambda e, i=i, xf=xf: e.scalar_tensor_tensor(xf.ap, k.xres.ap[:, i, :], rstd.ap[:, i:i + 1], gb.ap, op0=ALU.mult, op1=ALU.mult),
             reads=[k.xres.k(i), rstd.k(), gb.k()], writes=[xf.k()])
        P.op("act", lambda e, i=i, xf=xf: e.copy(x2b.ap[:, i, :], xf.ap), reads=[xf.k()], writes=[x2b.k(i)])
        for hb_ in range(2):
            b = 2 * (i % 2) + hb_
            for c in range(4):
                cc = hb_ * 4 + c
                P.op("pe", lambda e, b=b, c=c, cc=cc, xf=xf: e.transpose(ps[b][:, c * 128:(c + 1) * 128], xf.ap[:, cc * 128:(cc + 1) * 128], k.ident.ap),
                     reads=[xf.k(), k.ident.k()], writes=[f"ps{b}"])
            P.op("act" if hb_ == 0 else "dve",
                 (lambda e, b=b, hb_=hb_, xt=xt: e.copy(xt.ap[:, hb_ * 4:hb_ * 4 + 4, :], ps[b][:, :].rearrange("p (c t) -> p c t", c=4))) if hb_ == 0 else
                 (lambda e, b=b, hb_=hb_, xt=xt: e.tensor_copy(xt.ap[:, hb_ * 4:hb_ * 4 + 4, :], ps[b][:, :].rearrange("p (c t) -> p c t", c=4))),
                 reads=[f"ps{b}"], writes=[xt.k(hb_)])
        lb = 4 + i % 2
        for c in range(8):
            P.op("pe", lambda e, c=c, lb=lb, xt=xt: e.matmul(ps[lb][:, 0:NE], xt.ap[:, c, :], rsb.ap[:, c, :], start=(c == 0), stop=(c == 7)),
                 reads=[xt.k(0), xt.k(1), rsb.k()], writes=[f"ps{lb}"])
        P.op("dve", lambda e, i=i, lb=lb: e.tensor_reduce(sm.ap[:, i, 0:1], ps[lb][:, 0:NE], axis=AX.X, op=ALU.max), reads=[f"ps{lb}"], writes=[sm.k()])
        P.op("dve", lambda e, i=i: e.tensor_scalar(sm.ap[:, i, 0:1], sm.ap[:, i, 0:1], -1.0, None, op0=ALU.mult), reads=[sm.k()], writes=[sm.k()])
        P.op("act", lambda e, i=i, lb=lb: e.activation(aff.ap[:, i, :], ps[lb][:, 0:NE], AF.Exp, bias=sm.ap[:, i, 0:1], accum_out=sm.ap[:, i, 1:2]),
             reads=[f"ps{lb}", sm.k()], writes=[aff.k(), sm.k()])
        P.op("dve", lambda e, i=i: e.reciprocal(sm.ap[:, i, 2:3], sm.ap[:, i, 1:2]), reads=[sm.k()], writes=[sm.k()])
        P.op("dve", lambda e, i=i: e.tensor_scalar(aff.ap[:, i, :], aff.ap[:, i, :], sm.ap[:, i, 2:3], None, op0=ALU.mult), reads=[aff.k(), sm.k()], writes=[aff.k()])
        tb = 6 + (i // 4) % 2
        P.op("pe", lambda e, i=i, tb=tb: e.transpose(ps[tb][0:NE, (i % 4) * 128:(i % 4 + 1) * 128], aff.ap[:, i, :], k.ident.ap), reads=[aff.k(), k.ident.k()], writes=[f"ps{tb}"])
        if i % 4 == 3:
            P.op("act", lambda e, i=i, tb=tb: e.copy(affT.ap[0:NE, (i - 3) * 128:(i + 1) * 128], ps[tb][0:NE, :]), reads=[f"ps{tb}"], writes=[affT.k()])
    bs = A.alloc("bs", (8,)); bjunk = A.alloc("bjunk", (S,))
    LO, HI, MID, CNT, GE, D1 = (bs.ap[0:NE, j:j + 1] for j in range(6))
    P.op("pool", lambda e: e.memset(bs.ap, 0.0), writes=[bs.k()])
    P.op("pool", lambda e: e.memset(bs.ap[:, 1:2], 1.0), writes=[bs.k()])
    for it in range(30):
        P.op("dve", lambda e: e.tensor_tensor(MID, LO, HI, op=ALU.add), reads=[bs.k()], writes=[bs.k()])
        P.op("dve", lambda e: e.tensor_scalar(MID, MID, 0.5, None, op0=ALU.mult), reads=[bs.k()], writes=[bs.k()])
        P.op("dve", lambda e: e.tensor_scalar(bjunk.ap[0:NE, :], affT.ap[0:NE, :], MID, None, op0=ALU.is_gt, op1=ALU.add, accum_out=CNT),
             reads=[bs.k(), affT.k()], writes=[bs.k(), bjunk.k()])
        P.op("dve", lambda e: e.tensor_scalar(GE, CNT, float(CAP) - 0.5, None, op0=ALU.is_gt), reads=[bs.k()], writes=[bs.k()])
        P.op("dve", lambda e: e.tensor_tensor(D1, MID, LO, op=ALU.subtract), reads=[bs.k()], writes=[bs.k()])
        P.op("dve", lambda e: e.scalar_tensor_tensor(LO, D1, GE, LO, op0=ALU.mult, op1=ALU.add), reads=[bs.k()], writes=[bs.k()])
        P.op("dve", lambda e: e.tensor_tensor(D1, HI, MID, op=ALU.subtract), reads=[bs.k()], writes=[bs.k()])
        P.op("dve", lambda e: e.scalar_tensor_tensor(HI, D1, GE, MID, op0=ALU.mult, op1=ALU.add), reads=[bs.k()], writes=[bs.k()])
    dgt = A.alloc("dgt", (NE,)); thrb = A.alloc("thrb", (NE,))
    P.op("dve", lambda e: e.tensor_scalar(dgt.ap[0:NE, :], k.ident.ap[0:NE, 0:NE], LO, None, op0=ALU.mult), reads=[bs.k(), k.ident.k()], writes=[dgt.k()])
    P.op("pe", lambda e: e.matmul(ps[0][:, 0:NE], k.ones32.ap[0:NE, :], dgt.ap[0:NE, :], start=True, stop=True), reads=[k.ones32.k(), dgt.k()], writes=["ps0"])
    P.op("act", lambda e: e.copy(thrb.ap, ps[0][:, 0:NE]), reads=["ps0"], writes=[thrb.k()])
    mk16 = A.alloc("mk16", (NT, NE), BF16); mk32 = A.alloc("mk32", (NT, NE)); base = A.alloc("basec", (NE,))
    P.op("pool", lambda e: e.memset(base.ap, 0.0), writes=[base.k()])
    for i in range(NT):
        P.op("dve", lambda e, i=i: e.tensor_tensor(mk32.ap[:, i, :], aff.ap[:, i, :], thrb.ap, op=ALU.is_gt), reads=[aff.k(), thrb.k()], writes=[mk32.k(i)])
        P.op("act", lambda e, i=i: e.copy(mk16.ap[:, i, :], mk32.ap[:, i, :]), reads=[mk32.k(i)], writes=[mk16.k(i)])
        b = i % 2
        P.op("pe", lambda e, i=i, b=b: e.matmul(ps[b][:, 0:NE], k.trius16.ap, mk16.ap[:, i, :], start=True, stop=True), reads=[k.trius16.k(), mk16.k(i)], writes=[f"ps{b}"])
        P.op("pe", lambda e, i=i, b=b: e.matmul(ps[b][:, 128:128 + NE], k.ones16.ap, mk16.ap[:, i, :], start=True, stop=True), reads=[k.ones16.k(), mk16.k(i)], writes=[f"ps{b}"])
        P.op("dve", lambda e, i=i, b=b: e.scalar_tensor_tensor(pm.ap[:, i, :], ps[b][:, 0:NE], 1.0, base.ap, op0=ALU.add, op1=ALU.add), reads=[f"ps{b}", base.k()], writes=[pm.k(i)])
        P.op("dve", lambda e, i=i: e.tensor_tensor(pm.ap[:, i, :], pm.ap[:, i, :], mk32.ap[:, i, :], op=ALU.mult), reads=[pm.k(i), mk32.k(i)], writes=[pm.k(i)])
        P.op("dve", lambda e, i=i: e.tensor_scalar(pm.ap[:, i, :], pm.ap[:, i, :], -1.0, None, op0=ALU.add), reads=[pm.k(i)], writes=[pm.k(i)])
        P.op("dve", lambda e, b=b: e.tensor_tensor(base.ap, base.ap, ps[b][:, 128:128 + NE], op=ALU.add), reads=[f"ps{b}", base.k()], writes=[base.k()])
        tb = 6 + (i // 4) % 2
        P.op("pe", lambda e, i=i, tb=tb: e.transpose(ps[tb][0:NE, (i % 4) * 128:(i % 4 + 1) * 128], pm.ap[:, i, :], k.ident.ap), reads=[pm.k(i), k.ident.k()], writes=[f"ps{tb}"])
        if i % 4 == 3:
            P.op("act", lambda e, i=i, tb=tb: e.copy(pmT.ap[0:NE, (i - 3) * 128:(i + 1) * 128], ps[tb][0:NE, :]), reads=[f"ps{tb}"], writes=[pmT.k()])
    A.release(m1)
    SelE = A.alloc("SelE", (NT, CAP), BF16); SelT = A.alloc("SelT", (2, S), BF16)
    xgT = A.alloc("xgT", (8, CAP), BF16); actT = A.alloc("actT", (NFC, CAP), BF16)
    s1 = [A.alloc(f"s1_{i}", (CAP,)) for i in range(2)]
    ysb = A.alloc("ysb", (2, D), BF16)
    w1b = [A.alloc(f"w1b{i}", (8, FB), BF16) for i in range(2)]
    w3b = [A.alloc(f"w3b{i}", (8, FB), BF16) for i in range(2)]
    w2b = [A.alloc(f"w2b{i}", (W2G, D), BF16) for i in range(2)]
    wn = 0
    for ex in range(NE):
        w1v = IN["e_w1"].ap()[l][ex].rearrange("(c p) f -> p c f", p=128)
        w3v = IN["e_w3"].ap()[l][ex].rearrange("(c p) f -> p c f", p=128)
        w2v = IN["e_w2"].ap()[l][ex].rearrange("(g p) d -> p g d", p=128)
        for i in range(NT):
            P.op("dve", lambda e, i=i, ex=ex: e.tensor_scalar(SelE.ap[:, i, :], k.iota256.ap, pm.ap[:, i, ex:ex + 1], None, op0=ALU.is_equal),
                 reads=[k.iota256.k(), pm.k(i)], writes=[SelE.k(i)])
        for st in range(2):
            for tg in range(4):
                P.op("pe", lambda e, ex=ex, tg=tg: e.matmul(ps[6][:, :], k.ohb.ap[0:NE, ex * 128:(ex + 1) * 128], pmT.ap[0:NE, tg * 512:(tg + 1) * 512], start=True, stop=True),
                     reads=[k.ohb.k(), pmT.k()], writes=["ps6"])
                P.op("dve", lambda e, st=st, tg=tg: e.tensor_scalar(SelT.ap[:, st, tg * 512:(tg + 1) * 512], ps[6][:, :], k.slotidx.ap[:, st:st + 1], None, op0=ALU.is_equal),
                     reads=["ps6", k.slotidx.k()], writes=[SelT.k(st, tg)])
        for c in range(8):
            b = 6 + c % 2
            for i in range(NT):
                P.op("pe", lambda e, c=c, i=i, b=b: e.matmul(ps[b][:, 0:CAP], x2b.ap[:, i, c * 128:(c + 1) * 128], SelE.ap[:, i, :], start=(i == 0), stop=(i == NT - 1)),
                     reads=[x2b.k(i), SelE.k(i)], writes=[f"ps{b}"])
            P.op("act", lambda e, c=c, b=b: e.copy(xgT.ap[:, c, :], ps[b][:, 0:CAP]), reads=[f"ps{b}"], writes=[xgT.k(c)])
        for fb in range(NFB):
            r = wn % 2; wn += 1
            P.dma("pool", w1b[r].ap, w1v[:, :, fb * FB:(fb + 1) * FB], writes=[w1b[r].k()], semkey=("w1", r))
            P.dma("pool", w3b[r].ap, w3v[:, :, fb * FB:(fb + 1) * FB], writes=[w3b[r].k()], semkey=("w3", r))
            P.dma("pool", w2b[r].ap, w2v[:, fb * W2G:(fb + 1) * W2G, :], writes=[w2b[r].k()], semkey=("w2", r))
            for q in range(FB // 128):
                fc = fb * (FB // 128) + q
                b = fc % 2
                for c in range(8):
                    P.op("pe", lambda e, c=c, q=q, b=b, r=r: e.matmul(ps[b][:, 0:CAP], w1b[r].ap[:, c, q * 128:(q + 1) * 128], xgT.ap[:, c, :], start=(c == 0), stop=(c == 7)),
                         reads=[w1b[r].k(), xgT.k(c)], writes=[f"ps{b}"])
                for c in range(8):
                    P.op("pe", lambda e, c=c, q=q, b=b, r=r: e.matmul(ps[b][:, CAP:2 * CAP], w3b[r].ap[:, c, q * 128:(q + 1) * 128], xgT.ap[:, c, :], start=(c == 0), stop=(c == 7)),
                         reads=[w3b[r].k(), xgT.k(c)], writes=[f"ps{b}"])
                P.op("act", lambda e, b=b: e.activation(s1[b].ap, ps[b][:, 0:CAP], AF.Silu), reads=[f"ps{b}"], writes=[s1[b].k()])
                P.op("dve", lambda e, b=b, fc=fc: e.tensor_tensor(actT.ap[:, fc, :], s1[b].ap, ps[b][:, CAP:2 * CAP], op=ALU.mult), reads=[f"ps{b}", s1[b].k()], writes=[actT.k(fc)])
            for q in range(W2G):
                fc = fb * W2G + q
                for st in range(2):
                    for half in range(2):
                        yb_ = 2 + st * 2 + half
                        P.op("pe", lambda e, fc=fc, q=q, st=st, half=half, yb_=yb_, r=r: e.matmul(ps[yb_][:, :], actT.ap[:, fc, st * 128:(st + 1) * 128], w2b[r].ap[:, q, half * 512:(half + 1) * 512],
                                                                                           start=(fc == 0), stop=(fc == NFC - 1)),
                             reads=[actT.k(fc), w2b[r].k()], writes=[f"ps{yb_}"])
        for st in range(2):
            for half in range(2):
                yb_ = 2 + st * 2 + half
                P.op("act", lambda e, st=st, half=half, yb_=yb_: e.copy(ysb.ap[:, st, half * 512:(half + 1) * 512], ps[yb_][:, :]), reads=[f"ps{yb_}"], writes=[ysb.k(st, half)])
        for i in range(NT):
            for half in range(2):
                b = 6 + (2 * i + half) % 2
                for st in range(2):
                    P.op("pe", lambda e, i=i, half=half, st=st, b=b: e.matmul(ps[b][:, :], SelT.ap[:, st, i * 128:(i + 1) * 128], ysb.ap[:, st, half * 512:(half + 1) * 512], start=(st == 0), stop=(st == 1)),
                         reads=[SelT.k(st, i // 4), ysb.k(st, half)], writes=[f"ps{b}"])
                P.op("dve", lambda e, i=i, half=half, b=b, ex=ex: e.scalar_tensor_tensor(k.xres.ap[:, i, half * 512:(half + 1) * 512], ps[b][:, :], aff.ap[:, i, ex:ex + 1],
                                                                                     k.xres.ap[:, i, half * 512:(half + 1) * 512], op0=ALU.mult, op1=ALU.add),
                     reads=[f"ps{b}", aff.k(), k.xres.k(i)], writes=[k.xres.k(i)])
    A.release(m0)


_INPUT_NAMES = ["rel_bias", "ln1_g", "w_in", "w_out", "rk_mu", "rk_w0", "rk_w2", "rk_a0", "rk_a2", "rk_kk", "rk_ka",
                "rk_rk", "rk_g2", "rk_ln_g", "rk_ln_b", "ml_conv_w", "ml_conv_b", "ml_ib", "ml_fb", "ml_ln_g",
                "ln2_g", "router", "e_w1", "e_w3", "e_w2", "final_g"]


def make_in_maps(inputs, cores, names=None):
    consts = host_consts()
    shared = {n: np.ascontiguousarray(np.asarray(inputs[n], dtype=np.float32)) for n in _INPUT_NAMES if names is None or n in names}
    shared.update({n: v for n, v in consts.items() if names is None or n in names})
    if names is None or "c_pc" in names or "c_wx" in names:
        shared.update(host_rwkv_layout(inputs))
    x = np.asarray(inputs["x"], dtype=np.float32)
    maps = []
    for c in cores:
        m = dict(shared)
        m["x"] = np.ascontiguousarray(x[c])
        maps.append(m)
    return maps


def kernel(**inputs):
    nc, k = build()
    in_maps = make_in_maps(inputs, list(range(8)), set(k.IN.keys()))
    res = run_bass_kernel_spmd(nc, in_maps, core_ids=list(range(8)))
    return np.stack([np.asarray(r["out"], dtype=np.float32) for r in res.results], axis=0)
```

```python
from contextlib import ExitStack
import numpy as np
import concourse.bass as bass
import concourse.mybir as mybir

F32 = mybir.dt.float32
BF16 = mybir.dt.bfloat16
I32 = mybir.dt.int32
ALU = mybir.AluOpType
AF = mybir.ActivationFunctionType
AX = mybir.AxisListType

ENGS = ("pe", "act", "dve", "pool", "sp")


class Prog:
    def __init__(self, nc, strict_same_engine=False):
        self.nc = nc
        self.same_dist = 3
        self.ops = {e: [] for e in ENGS}
        self.keys = {}
        self.dma_cnt = {}
        self.es = ExitStack()
        self.n_ops = 0

    def _deps(self, reads, writes):
        deps = []
        for k in reads:
            deps.extend((ev, True) for ev in self._st(k)["w"])
        for k in writes:
            st = self._st(k)
            deps.extend((ev, True) for ev in st["w"])
            deps.extend((ev, False) for ev in st["r"].values())
        return deps

    def _st(self, k):
        st = self.keys.get(k)
        if st is None:
            inh = getattr(self, "inherit", {}).get(k[0], []) if isinstance(k, tuple) else []
            st = self.keys[k] = {"w": list(inh), "r": {}}
        return st

    def _record(self, ev, reads, writes):
        for k in reads:
            st = self._st(k)
            st["r"][(ev[0], ev[1])] = ev
        for k in writes:
            st = self._st(k)
            st["w"] = [ev]
            st["r"] = {}

    @staticmethod
    def _norm(reads, writes):
        r2, w2 = [], []
        for k in writes:
            if isinstance(k, str) and k.startswith("ps"):
                k = k.split("q")[0]
            if k not in w2:
                w2.append(k)
        for k in reads:
            if isinstance(k, str) and k.startswith("ps"):
                k = k.split("q")[0]
                if k not in w2:
                    w2.append(k)
            elif k not in r2:
                r2.append(k)
        return r2, w2

    def op(self, eng, fn, reads=(), writes=(), serial=False):
        reads, writes = self._norm(reads, writes)
        deps = self._deps(reads, writes)
        idx = len(self.ops[eng])
        if serial and idx > 0:
            deps.append((("eng", eng, idx - 1), "force"))
        ev = ("eng", eng, idx)
        self.ops[eng].append(dict(fn=fn, deps=deps, kind="c"))
        self._record(ev, reads, writes)
        self.n_ops += 1
        return ev

    def dma(self, q, out, in_, reads=(), writes=(), semkey=None, **kw):
        assert semkey is not None
        reads, writes = self._norm(reads, writes)
        deps = self._deps(reads, writes)
        c = self.dma_cnt.get(semkey, 0) + 1
        self.dma_cnt[semkey] = c
        if c > 1:
            deps.append((("dma", semkey, c - 1), "force"))
        ev = ("dma", semkey, c)
        fn = lambda e, out=out, in_=in_, kw=kw: e.dma_start(out=out, in_=in_, **kw)
        self.ops[q].append(dict(fn=fn, deps=deps, kind="d", semkey=semkey))
        self._record(ev, reads, writes)
        self.n_ops += 1
        return ev

    def emit(self, final_wait_events=()):
        nc = self.nc
        signal = {e: set() for e in ENGS}
        for e in ENGS:
            for i, o in enumerate(self.ops[e]):
                nd = []
                for (d, is_w) in o["deps"]:
                    if d[0] == "eng" and d[1] == e and is_w != "force":
                        if e == "pe" or not is_w or (i - d[2]) > self.same_dist:
                            continue
                    nd.append(d)
                    if d[0] == "eng":
                        signal[d[1]].add(d[2])
                o["deps"] = nd
        for d in final_wait_events:
            if d[0] == "eng":
                signal[d[1]].add(d[2])
        rank = {}
        for e in ENGS:
            r = 0
            for i in range(len(self.ops[e])):
                if i in signal[e]:
                    r += 1
                    rank[(e, i)] = r
        self.max_rank = {e: max([v for (ee, i), v in rank.items() if ee == e] + [0]) for e in ENGS}
        es = self.es
        sem_e = {e: es.enter_context(nc.semaphore("s_" + e)) for e in ENGS}
        sem_d = {k: es.enter_context(nc.semaphore("d_%d" % i)) for i, k in enumerate(self.dma_cnt)}
        self.n_sems = len(sem_e) + len(sem_d)

        def lower(ev):
            if ev[0] == "eng":
                return ("e_" + ev[1], sem_e[ev[1]], rank[(ev[1], ev[2])])
            return ("d_" + str(ev[1]), sem_d[ev[1]], 16 * ev[2])

        block = es.enter_context(nc.Block())
        engobj = {"pe": block.tensor, "act": block.scalar, "dve": block.vector,
                  "pool": block.gpsimd, "sp": block.sync}
        fw = self

        def make(e):
            def body(eng):
                known = {}
                for i, o in enumerate(fw.ops[e]):
                    need = {}
                    for d in o["deps"]:
                        nm, s, v = lower(d)
                        if known.get(nm, 0) >= v:
                            continue
                        if nm not in need or need[nm][1] < v:
                            need[nm] = (s, v)
                    for nm, (s, v) in need.items():
                        eng.wait_ge(s, v)
                        known[nm] = v
                    ins = o["fn"](eng)
                    if o["kind"] == "d":
                        ins.then_inc(sem_d[o["semkey"]], 16)
                    elif (e, i) in rank:
                        ins.then_inc(sem_e[e], 1)
                if e == "sp":
                    for d in final_wait_events:
                        nm, s, v = lower(d)
                        if known.get(nm, 0) < v:
                            eng.wait_ge(s, v)
                            known[nm] = v
            return body

        for e in ENGS:
            if self.ops[e] or e == "sp":
                engobj[e](make(e))
        es.close()


class Region:
    def __init__(self, uid, ap, lo, hi):
        self.uid, self.ap, self.lo, self.hi = uid, ap, lo, hi

    def k(self, *i):
        return (self.uid,) + tuple(i)

    def __getitem__(self, idx):
        return self.ap[idx]


class Arena:
    def __init__(self, P, tensor, ncols_f32):
        self.P, self.t, self.n = P, tensor, ncols_f32
        self.top = 0
        self.gen = 0
        self.pending = []
        self.live = []
        P.inherit = {}
        P._arena = self

    def alloc(self, name, free_shape, dtype=F32):
        nel = int(np.prod(free_shape))
        bpe = {F32: 4, BF16: 2, I32: 4}[dtype]
        ncol = (nel * bpe + 3) // 4
        lo, hi = self.top, self.top + ncol
        assert hi <= self.n, f"arena overflow allocating {name}: need {hi} cols of {self.n}"
        self.top = hi
        self.gen += 1
        uid = f"{name}#{self.gen}"
        ap = self.t[:, lo:hi]
        if dtype != F32:
            ap = ap.bitcast(dtype)
        ap = ap[:, 0:nel]
        if len(free_shape) > 1:
            names = " ".join(f"a{i}" for i in range(len(free_shape)))
            kw = {f"a{i}": int(s) for i, s in enumerate(free_shape)}
            ap = ap.rearrange(f"p ({names}) -> p {names}", **kw)
        evs = []
        keep = []
        for (plo, phi, pe) in self.pending:
            if plo < hi and lo < phi:
                evs.extend(pe)
            keep.append((plo, phi, pe))
        self.P.inherit[uid] = evs
        r = Region(uid, ap, lo, hi)
        self.live.append(r)
        return r

    def mark(self):
        return (self.top, len(self.live))

    def release(self, mark):
        top, nlive = mark
        P = self.P
        for r in self.live[nlive:]:
            evs = []
            for k in [k for k in P.keys if k[0] == r.uid]:
                st = P.keys.pop(k)
                evs.extend(st["w"])
                evs.extend(st["r"].values())
            evs.extend(P.inherit.get(r.uid, []))
            best = {}
            for ev in evs:
                kk = (ev[0], ev[1])
                if kk not in best or best[kk][2] < ev[2]:
                    best[kk] = ev
            self.pending.append((r.lo, r.hi, list(best.values())))
        del self.live[nlive:]
        self.top = top
from concourse.bass_utils import run_bass_kernel_spmd
S = 2048; D = 1024; NT = 16; INW = 3856; DEPTH = 2
ND = 3072; EC = 1535
NE = 16; CAP = 256; DFF = 2816


def t5_bucket_np(rel):
    nb = 16
    ret = np.where(rel > 0, nb, 0)
    n = np.abs(rel)
    max_exact = 8
    nf = np.maximum(n, 1).astype(np.float32)
    large = max_exact + (np.log(nf / np.float32(max_exact)) / np.float32(np.log(1024 / max_exact))
                         * np.float32(nb - max_exact)).astype(np.int32)
    large = np.minimum(large, nb - 1)
    return ret + np.where(n < max_exact, n, large)


def host_consts():
    d = np.arange(ND) - EC
    ad = np.abs(d)
    cnt = ((ad <= 64).astype(np.float32) + ((d % 4 == 0) & (ad <= 256)).astype(np.float32)
           + ((d % 16 == 0) & (ad <= 1024)).astype(np.float32))
    bk = t5_bucket_np(d)
    oh = np.zeros((32, ND), np.float32)
    oh[bk, np.arange(ND)] = 1.0
    c = {}
    c["c_oh"] = oh
    c["c_cnt"] = np.tile(cnt[None], (4, 1)).astype(np.float32)
    c["c_ident"] = np.eye(128, dtype=np.float32)
    c["c_jmat"] = np.eye(128, dtype=np.float32)[::-1].copy()
    c["c_triu"] = np.triu(np.ones((128, 128), np.float32))
    c["c_tril"] = np.tril(np.ones((128, 128), np.float32))
    ob = np.zeros((128, 128), np.float32); ob[:64, :64] = 1; ob[64:, 64:] = 1
    c["c_onesblk"] = ob
    tus = np.triu(np.ones((128, 128), np.float32), 1); tui = np.triu(np.ones((128, 128), np.float32), 0)
    c["c_mask4"] = np.concatenate([tus, tui, tus, tui], axis=1)
    c["c_trils"] = np.tril(np.ones((128, 128), np.float32), -1)
    rm = np.ones((128, 256), np.float32); rm[:, 0] = 0; rm[:, 128] = 0
    c["c_rmask"] = rm
    ohb = np.zeros((16, 2048), np.float32)
    for e_ in range(16):
        ohb[e_, e_ * 128:(e_ + 1) * 128] = 1.0
    c["c_ohb"] = ohb
    return c


class K:
    pass


def build(dbg=None, nlayers=DEPTH, stages=("attn", "mlstm", "rwkv", "moe")):
    nc = bass.Bass("TRN2", target_bir_lowering=False)
    k = K()
    k.nc = nc
    SH = dict(x=[S, D], rel_bias=[32, 4], ln1_g=[DEPTH, D], w_in=[DEPTH, D, INW], w_out=[DEPTH, D, D], rk_mu=[DEPTH, 2, 1280],
              rk_w0=[DEPTH, 2, 384], rk_w2=[DEPTH, 2, 64, 384], rk_a0=[DEPTH, 2, 384], rk_a2=[DEPTH, 2, 64, 384],
              rk_kk=[DEPTH, 384], rk_ka=[DEPTH, 384], rk_rk=[DEPTH, 6, 64], rk_g2=[DEPTH, 128, 384], rk_ln_g=[DEPTH, 384],
              rk_ln_b=[DEPTH, 384], ml_conv_w=[DEPTH, 5, 768], ml_conv_b=[DEPTH, 768], ml_ib=[DEPTH, 2, 4], ml_fb=[DEPTH, 2, 4],
              ml_ln_g=[DEPTH, 384], ln2_g=[DEPTH, D], router=[DEPTH, D, NE], e_w1=[DEPTH, NE, D, DFF], e_w3=[DEPTH, NE, D, DFF],
              e_w2=[DEPTH, NE, DFF, D], final_g=[D], c_oh=[32, ND], c_cnt=[4, ND], c_ident=[128, 128], c_jmat=[128, 128],
              c_triu=[128, 128], c_tril=[128, 128], c_onesblk=[128, 128], c_mask4=[128, 512], c_trils=[128, 128],
              c_rmask=[128, 256], c_ohb=[16, 2048], c_pc=[DEPTH, 128, NPC], c_wx=[DEPTH, D, 384])

    class _IN(dict):
        def __missing__(self, name):
            t = nc.dram_tensor(name, list(SH[name]), F32, kind="ExternalInput")
            self[name] = t
            return t
    IN = _IN()
    k.IN = IN
    out_t = nc.dram_tensor("out", [S, D], F32, kind="ExternalOutput")
    k.mscr = nc.dram_tensor("mscr", [4, ND], F32, kind="Internal")
    k.mtab_d = nc.dram_tensor("mtab_d", [4, 128, 23 * 128], F32, kind="Internal")
    k.xspill = nc.dram_tensor("xspill", [S, D], F32, kind="Internal")
    k.dbg = dbg
    k.dbg_out = {}
    with ExitStack() as es:
        ACOLS = 53000
        arena_t = es.enter_context(nc.sbuf_tensor("arena", [128, ACOLS], F32))
        k.ps = [es.enter_context(nc.psum_tensor(f"ps{i}", [128, 512], F32)) for i in range(8)]
        P = Prog(nc)
        A = Arena(P, arena_t, ACOLS)
        k.P, k.A = P, A
        k.final_events = []
        setup_consts(k)
        k.xres = A.alloc("xres", (NT, D))
        xin = IN["x"].ap().rearrange("(i p) d -> p i d", p=128)
        for i in range(NT):
            P.dma("sp" if i % 2 == 0 else "act", k.xres.ap[:, i, :], xin[:, i, :], writes=[k.xres.k(i)], semkey=("xld", i % 4))
        build_mask_table(k)
        for l in range(nlayers):
            m0 = A.mark()
            norm_to_hT(k, IN["ln1_g"].ap()[l], "hT")
            if "attn" in stages:
                attn_stage(k, l)
            if "mlstm" in stages:
                mlstm_stage(k, l)
            if "rwkv" in stages:
                rwkv_stage(k, l)
            A.release(m0)
            if "moe" in stages:
                moe_stage(k, l)
        final_stage(k, out_t)
        P.emit(final_wait_events=k.final_events)
    return nc, k


def dbg_dump(k, name, region_ap, shape, dt, reads):
    if not k.dbg or name not in k.dbg:
        return None
    t = k.nc.dram_tensor("dbg_" + name, list(shape), dt, kind="ExternalOutput")
    k.dbg_out[name] = t
    return t


def setup_consts(k):
    P, A, IN = k.P, k.A, k.IN
    k.ident = A.alloc("ident", (128,))
    k.jmat = A.alloc("jmat", (128,))
    k.ident16 = A.alloc("ident16", (128,), BF16)
    k.triu = A.alloc("triu", (128,))
    k.tril = A.alloc("tril", (128,))
    P.dma("sp", k.ident.ap, IN["c_ident"].ap(), writes=[k.ident.k()], semkey="c0")
    P.dma("sp", k.jmat.ap, IN["c_jmat"].ap(), writes=[k.jmat.k()], semkey="c1")
    P.dma("sp", k.triu.ap, IN["c_triu"].ap(), writes=[k.triu.k()], semkey="c2")
    P.dma("sp", k.tril.ap, IN["c_tril"].ap(), writes=[k.tril.k()], semkey="c3")
    P.op("dve", lambda e: e.tensor_copy(k.ident16.ap, k.ident.ap), reads=[k.ident.k()], writes=[k.ident16.k()])
    k.one = A.alloc("one", (1,))
    P.op("pool", lambda e: e.memset(k.one.ap, 1.0), writes=[k.one.k()])
    k.ones32 = A.alloc("ones32", (128,))
    P.op("pool", lambda e: e.memset(k.ones32.ap, 1.0), writes=[k.ones32.k()])
    k.negm = [A.alloc("negm0", (128,)), A.alloc("negm1", (128,))]
    P.op("dve", lambda e: e.tensor_scalar(k.negm[0].ap, k.triu.ap, -1.0, 30000.0, op0=ALU.add, op1=ALU.mult), reads=[k.triu.k()], writes=[k.negm[0].k()])
    P.op("dve", lambda e: e.tensor_scalar(k.negm[1].ap, k.tril.ap, -1.0, 30000.0, op0=ALU.add, op1=ALU.mult), reads=[k.tril.k()], writes=[k.negm[1].k()])
    k.epsgn = A.alloc("epsgn", (1,))
    P.op("pool", lambda e: e.memset(k.epsgn.ap, 64e-5), writes=[k.epsgn.k()])
    k.onesblk = A.alloc("onesblk", (128,)); k.onesblk64 = A.alloc("onesblk64", (128,))
    k.trils = A.alloc("trils", (128,))
    P.dma("act", k.onesblk.ap, IN["c_onesblk"].ap(), writes=[k.onesblk.k()], semkey="c4")
    P.dma("act", k.trils.ap, IN["c_trils"].ap(), writes=[k.trils.k()], semkey="c6")
    P.op("dve", lambda e: e.tensor_scalar(k.onesblk64.ap, k.onesblk.ap, 1.0 / 64, None, op0=ALU.mult), reads=[k.onesblk.k()], writes=[k.onesblk64.k()])
    k.iota256 = A.alloc("iota256", (256,)); k.slotidx = A.alloc("slotidx", (2,))
    k.ones16 = A.alloc("ones16", (128,), BF16); k.trius16 = A.alloc("trius16", (128,), BF16)
    P.op("pool", lambda e: e.iota(k.iota256.ap, [[1, 256]], base=0, channel_multiplier=0, allow_small_or_imprecise_dtypes=True), writes=[k.iota256.k()])
    P.op("pool", lambda e: e.iota(k.slotidx.ap, [[128, 2]], base=0, channel_multiplier=1, allow_small_or_imprecise_dtypes=True), writes=[k.slotidx.k()])
    P.op("dve", lambda e: e.tensor_copy(k.ones16.ap, k.ones32.ap), reads=[k.ones32.k()], writes=[k.ones16.k()])
    P.op("dve", lambda e: e.tensor_tensor(k.trius16.ap, k.triu.ap, k.ident.ap, op=ALU.subtract), reads=[k.triu.k(), k.ident.k()], writes=[k.trius16.k()])
    k.eps6 = A.alloc("eps6", (1,))
    P.op("pool", lambda e: e.memset(k.eps6.ap, 1e-6), writes=[k.eps6.k()])


def build_mask_table(k):
    P, A, IN, ps = k.P, k.A, k.IN, k.ps
    m0 = A.mark()
    rb = A.alloc("rb", (4,)); oh = A.alloc("oh", (ND,)); cnt = A.alloc("cnt", (ND,)); mm = A.alloc("mm", (ND,))
    P.dma("sp", rb.ap[0:32, :], IN["rel_bias"].ap(), writes=[rb.k()], semkey="mt0")
    P.dma("sp", oh.ap[0:32, :], IN["c_oh"].ap(), writes=[oh.k()], semkey="mt1")
    P.dma("act", cnt.ap[0:4, :], IN["c_cnt"].ap(), writes=[cnt.k()], semkey="mt2")
    for c in range(ND // 512):
        b = ps[c % 2]
        P.op("pe", lambda e, c=c, b=b: e.matmul(b[0:4, :], rb.ap[0:32, :], oh.ap[0:32, c * 512:(c + 1) * 512], start=True, stop=True),
             reads=[rb.k(), oh.k()], writes=[f"ps{c%2}"])
        P.op("act", lambda e, c=c, b=b: e.activation(mm.ap[0:4, c * 512:(c + 1) * 512], b[0:4, :], AF.Exp),
             reads=[f"ps{c%2}"], writes=[mm.k(c)])
        P.op("dve", lambda e, c=c: e.tensor_tensor(mm.ap[0:4, c * 512:(c + 1) * 512], mm.ap[0:4, c * 512:(c + 1) * 512],
                                                  cnt.ap[0:4, c * 512:(c + 1) * 512], op=ALU.mult),
             reads=[mm.k(c), cnt.k()], writes=[mm.k(c)])
    P.dma("sp", k.mscr.ap(), mm.ap[0:4, :], reads=[mm.k(c) for c in range(ND // 512)], writes=["mscr"], semkey="mt3")
    hanks = [A.alloc(f"hank{i}", (23, 128)) for i in range(2)]
    mts = [A.alloc(f"mt{i}", (23, 128)) for i in range(2)]
    for h in range(4):
        hank = hanks[h % 2]
        mt = mts[h % 2]
        src = bass.AP(k.mscr, h * ND, [[1, 128], [128, 23], [1, 128]])
        P.dma("sp" if h % 2 == 0 else "act", hank.ap, src, reads=["mscr"], writes=[hank.k()], semkey=("mt4", h))
        for j in range(23):
            jj = 22 - j
            b = 2 + (j // 4) % 2
            P.op("pe", lambda e, j=j, b=b, hank=hank: e.matmul(ps[b][:, (j % 4) * 128:(j % 4 + 1) * 128], hank.ap[:, j, :], k.jmat.ap, start=True, stop=True),
                 reads=[hank.k(), k.jmat.k()], writes=[f"ps{b}"])
            P.op("act" if j % 2 == 0 else "dve",
                 (lambda e, jj=jj, j=j, b=b, mt=mt: e.copy(mt.ap[:, jj, :], ps[b][:, (j % 4) * 128:(j % 4 + 1) * 128])) if j % 2 == 0 else
                 (lambda e, jj=jj, j=j, b=b, mt=mt: e.tensor_copy(mt.ap[:, jj, :], ps[b][:, (j % 4) * 128:(j % 4 + 1) * 128])),
                 reads=[f"ps{b}"], writes=[mt.k()])
        P.dma("sp", k.mtab_d.ap()[h].rearrange("p (j q) -> p j q", j=23), mt.ap, reads=[mt.k()], writes=[("mtab_d", h)], semkey=("mt5", h))
    A.release(m0)


def norm_to_hT(k, g_ap, name, want_f32T=False):
    P, A, ps = k.P, k.A, k.ps
    k.hT = A.alloc(name, (8, S), BF16)
    m0 = A.mark()
    gb = A.alloc("gb", (D,)); junk = A.alloc("junk", (D,)); ss = A.alloc("ss", (NT,)); rstd = A.alloc("rstd", (NT,))
    hb = [A.alloc(f"hb{i}", (D,), BF16) for i in range(2)]
    P.dma("sp", gb.ap, g_ap.partition_broadcast(128), writes=[gb.k()], semkey="gb")
    P.op("pool", lambda e: e.memset(ss.ap, 0.0), writes=[ss.k(i) for i in range(NT)])
    for i in range(NT):
        P.op("act", lambda e, i=i: e.activation(junk.ap, k.xres.ap[:, i, :], AF.Square, accum_out=ss.ap[:, i:i + 1]),
             reads=[k.xres.k(i)], writes=[junk.k(), ss.k(i)])
    P.op("act", lambda e: e.activation(rstd.ap, ss.ap, AF.Sqrt, scale=1.0 / D, bias=k.eps6.ap[:, 0:1]),
         reads=[ss.k(i) for i in range(NT)] + [k.eps6.k()], writes=[rstd.k()])
    P.op("dve", lambda e: e.reciprocal(rstd.ap, rstd.ap), reads=[rstd.k()], writes=[rstd.k()])
    for i in range(NT):
        h_ = hb[i % 2]
        P.op("dve", lambda e, i=i, h_=h_: e.scalar_tensor_tensor(h_.ap, k.xres.ap[:, i, :], rstd.ap[:, i:i + 1], gb.ap, op0=ALU.mult, op1=ALU.mult),
             reads=[k.xres.k(i), rstd.k(), gb.k()], writes=[h_.k()])
        b = 4 + i % 2
        pb = ps[b][:, :].bitcast(BF16)
        for c in range(8):
            P.op("pe", lambda e, c=c, pb=pb, h_=h_: e.transpose(pb[:, c * 128:(c + 1) * 128], h_.ap[:, c * 128:(c + 1) * 128], k.ident16.ap),
                 reads=[h_.k(), k.ident16.k()], writes=[f"ps{b}"])
        P.op("act", lambda e, i=i, pb=pb: e.copy(k.hT.ap[:, :, i * 128:(i + 1) * 128], pb.rearrange("p (c t) -> p c t", c=8)),
             reads=[f"ps{b}"], writes=[k.hT.k(i)])
    A.release(m0)


def load_w_bf16(k, name, src_ap, nchunks, ncols, semkey, split=None):
    P, A = k.P, k.A
    w = A.alloc(name, (nchunks, ncols), BF16)
    v = src_ap.rearrange("(c p) n -> p c n", p=128)
    for c in range(nchunks):
        P.dma("pool", w.ap[:, c, :], v[:, c, :], writes=[w.k(c)], semkey=(semkey, c % 4))
    return w


def outproj_partial(k, l, yT, nch, row0, tag):
    P, A, ps, IN = k.P, k.A, k.ps, k.IN
    wo = load_w_bf16(k, "wo_" + tag, IN["w_out"].ap()[l][row0:row0 + nch * 128, :], nch, D, "wo_" + tag)
    n = 0
    for i in range(NT):
        for half in range(2):
            b = 6 + n % 2
            n += 1
            for c in range(nch):
                P.op("pe", lambda e, i=i, half=half, c=c, b=b: e.matmul(ps[b][:, :], yT.ap[:, c, i * 128:(i + 1) * 128],
                                                                     wo.ap[:, c, half * 512:(half + 1) * 512], start=(c == 0), stop=(c == nch - 1)),
                     reads=[yT.k(i), wo.k(c)], writes=[f"ps{b}"])
            P.op("dve", lambda e, i=i, half=half, b=b: e.tensor_tensor(k.xres.ap[:, i, half * 512:(half + 1) * 512],
                                                                      k.xres.ap[:, i, half * 512:(half + 1) * 512], ps[b][:, :], op=ALU.add),
                 reads=[f"ps{b}", k.xres.k(i)], writes=[k.xres.k(i)])


def transpose_to_T(k, y, nch, yT):
    P, ps = k.P, k.ps
    for i in range(NT):
        b = 4 + i % 2
        pb = ps[b][:, :].bitcast(BF16)
        for c in range(nch):
            P.op("pe", lambda e, i=i, c=c, pb=pb: e.transpose(pb[:, c * 128:(c + 1) * 128], y.ap[:, i, c * 128:(c + 1) * 128], k.ident16.ap),
                 reads=[y.k(i), k.ident16.k()], writes=[f"ps{b}"])
        P.op("act", lambda e, i=i, pb=pb: e.copy(yT.ap[:, :, i * 128:(i + 1) * 128], pb[:, 0:nch * 128].rearrange("p (c t) -> p c t", c=nch)),
             reads=[f"ps{b}"], writes=[yT.k(i)])


def attn_stage(k, l):
    P, A, ps, IN = k.P, k.A, k.ps, k.IN
    m0 = A.mark()
    ya = A.alloc("ya", (NT, 256), BF16)
    m1 = A.mark()
    wa = load_w_bf16(k, "wa", IN["w_in"].ap()[l][:, 0:768], 8, 768, "wa")
    qT = A.alloc("qT", (2, S), BF16); kT = A.alloc("kT", (2, S), BF16)
    vp = A.alloc("vp", (NT, 4, 65), BF16)
    P.op("pool", lambda e: e.memset(vp.ap, 1.0), writes=[vp.k(i) for i in range(NT)])
    n = 0
    for cc in range(4):
        dst = qT if cc < 2 else kT
        for tg in range(4):
            b = n % 2; n += 1
            for c in range(8):
                P.op("pe", lambda e, c=c, cc=cc, tg=tg, b=b: e.matmul(ps[b][:, :], wa.ap[:, c, cc * 128:(cc + 1) * 128], k.hT.ap[:, c, tg * 512:(tg + 1) * 512],
                                                                  start=(c == 0), stop=(c == 7)),
                     reads=[wa.k(c)] + [k.hT.k(4 * tg + j) for j in range(4)], writes=[f"ps{b}"])
            P.op("act", lambda e, cc=cc, tg=tg, b=b, dst=dst: e.copy(dst.ap[:, cc % 2, tg * 512:(tg + 1) * 512], ps[b][:, :]),
                 reads=[f"ps{b}"], writes=[dst.k(tg)])
    for i in range(NT):
        b = 2 + i % 2
        for c in range(8):
            P.op("pe", lambda e, c=c, i=i, b=b: e.matmul(ps[b][:, 0:256], k.hT.ap[:, c, i * 128:(i + 1) * 128], wa.ap[:, c, 512:768], start=(c == 0), stop=(c == 7)),
                 reads=[wa.k(c), k.hT.k(i)], writes=[f"ps{b}"])
        P.op("dve", lambda e, i=i, b=b: e.tensor_copy(vp.ap[:, i, :, 0:64], ps[b][:, 0:256].rearrange("p (h c) -> p h c", h=4)),
             reads=[f"ps{b}"], writes=[vp.k(i)])
    mtab = [A.alloc(f"mtab{i}", (23, 128)) for i in range(2)]
    e32 = [A.alloc(f"e32_{i}", (512,)) for i in range(2)]
    p16 = [A.alloc(f"p16_{i}", (512,), BF16) for i in range(2)]
    rc = A.alloc("rc", (8,))
    iters = []
    for h in range(4):
        for g in range(4):
            kts = [kt for kt in range(NT) if -8 <= kt - 4 * g <= 11]
            for kt in kts:
                iters.append((h, g, kt, kt == kts[0], kt == kts[-1]))
    mts = {}

    def emit_score(n):
        h, g, kt, first, last = iters[n]
        sb = n % 2
        hp, hb_ = h // 2, (h % 2) * 64
        if h not in mts:
            mt = mtab[h % 2]
            P.dma("sp", mt.ap, k.mtab_d.ap()[h].rearrange("p (j q) -> p j q", j=23), reads=[("mtab_d", h)], writes=[mt.k()], semkey=("mtl", h % 2))
            mts[h] = mt
        P.op("pe", lambda e, kt=kt, g=g, sb=sb, hp=hp, hb_=hb_: e.matmul(ps[sb][:, :], kT.ap[hb_:hb_ + 64, hp, kt * 128:(kt + 1) * 128],
                                                                   qT.ap[hb_:hb_ + 64, hp, g * 512:(g + 1) * 512], start=True, stop=True),
             reads=[kT.k(kt // 4), qT.k(g)], writes=[f"ps{sb}"], serial=True)

    def emit_rest(n):
        h, g, kt, first, last = iters[n]
        sb = n % 2
        mt = mts[h]
        jj0 = 11 - kt + 4 * g
        P.op("act", lambda e, sb=sb: e.activation(e32[sb].ap, ps[sb][:, :], AF.Exp, scale=0.125),
             reads=[f"ps{sb}"], writes=[e32[sb].k()])
        P.op("dve", lambda e, sb=sb, jj0=jj0, mt=mt: e.tensor_tensor(p16[sb].ap, e32[sb].ap, mt.ap[:, jj0:jj0 + 4, :].rearrange("p j q -> p (j q)"), op=ALU.mult),
             reads=[e32[sb].k(), mt.k()], writes=[p16[sb].k()])
        for i in range(4):
            P.op("pe", lambda e, i=i, sb=sb, kt=kt, h=h, first=first, last=last: e.matmul(ps[2 + i][:, 0:65], p16[sb].ap[:, i * 128:(i + 1) * 128], vp.ap[:, kt, h, :],
                                                                                  start=first, stop=last),
                 reads=[p16[sb].k(), vp.k(kt)], writes=[f"ps{2+i}"])
        if last:
            for i in range(4):
                P.op("dve", lambda e, i=i: e.reciprocal(rc.ap[:, i:i + 1], ps[2 + i][:, 64:65]), reads=[f"ps{2+i}"], writes=[rc.k(i)])
                P.op("dve", lambda e, i=i, g=g, h=h: e.tensor_scalar(ya.ap[:, 4 * g + i, h * 64:(h + 1) * 64], ps[2 + i][:, 0:64], rc.ap[:, i:i + 1], None, op0=ALU.mult),
                     reads=[f"ps{2+i}", rc.k(i)], writes=[ya.k(4 * g + i)])

    emit_score(0)
    for n in range(len(iters)):
        if n + 1 < len(iters):
            emit_score(n + 1)
        emit_rest(n)
    A.release(m1)
    if k.dbg and "ya" in k.dbg:
        t = k.nc.dram_tensor("dbg_ya", [S, 256], BF16, kind="ExternalOutput")
        k.final_events.append(P.dma("sp", t.ap().rearrange("(i p) c -> p i c", p=128), ya.ap, reads=[ya.k(i) for i in range(NT)], semkey="dbg"))
    yaT = A.alloc("yaT", (2, S), BF16)
    transpose_to_T(k, ya, 2, yaT)
    outproj_partial(k, l, yaT, 2, 0, "a")
    A.release(m0)


def final_stage(k, out_t):
    P, A = k.P, k.A
    m0 = A.mark()
    gb = A.alloc("gbf", (D,)); junk = A.alloc("junkf", (D,)); ss = A.alloc("ssf", (NT,)); rstd = A.alloc("rstdf", (NT,))
    ob = [A.alloc(f"ob{i}", (D,)) for i in range(2)]
    P.dma("sp", gb.ap, k.IN["final_g"].ap().partition_broadcast(128), writes=[gb.k()], semkey="gbf")
    P.op("pool", lambda e: e.memset(ss.ap, 0.0), writes=[ss.k(i) for i in range(NT)])
    for i in range(NT):
        P.op("act", lambda e, i=i: e.activation(junk.ap, k.xres.ap[:, i, :], AF.Square, accum_out=ss.ap[:, i:i + 1]),
             reads=[k.xres.k(i)], writes=[junk.k(), ss.k(i)])
    P.op("act", lambda e: e.activation(rstd.ap, ss.ap, AF.Sqrt, scale=1.0 / D, bias=k.eps6.ap[:, 0:1]),
         reads=[ss.k(i) for i in range(NT)] + [k.eps6.k()], writes=[rstd.k()])
    P.op("dve", lambda e: e.reciprocal(rstd.ap, rstd.ap), reads=[rstd.k()], writes=[rstd.k()])
    ov = out_t.ap().rearrange("(i p) d -> p i d", p=128)
    for i in range(NT):
        o = ob[i % 2]
        P.op("dve", lambda e, i=i, o=o: e.scalar_tensor_tensor(o.ap, k.xres.ap[:, i, :], rstd.ap[:, i:i + 1], gb.ap, op0=ALU.mult, op1=ALU.mult),
             reads=[k.xres.k(i), rstd.k(), gb.k()], writes=[o.k()])
        k.final_events.append(P.dma("sp" if i % 2 == 0 else "act", ov[:, i, :], o.ap, reads=[o.k()], semkey=("out", i % 2)))
    A.release(m0)


OFF_MQK = 2304; OFF_MV = 3072; OFF_MO = 3456; OFF_MG = 3840


def mlstm_stage(k, l):
    P, A, ps, IN = k.P, k.A, k.ps, k.IN
    m0 = A.mark()
    hsum = A.alloc("hsum", (NT, 4, 96))
    m1 = A.mark()
    qkT = A.alloc("qkT", (8, S), BF16)
    vp = A.alloc("vpm", (NT, 4, 97), BF16)
    G = A.alloc("G", (NT, 16))
    BL = [A.alloc(f"BL{d}", (NT, 4)) for d in range(2)]
    EBL = [A.alloc(f"EBL{d}", (NT, 4)) for d in range(2)]
    IMB = [A.alloc(f"IMB{d}", (NT, 4)) for d in range(2)]
    m2 = A.mark()
    wm = load_w_bf16(k, "wm", IN["w_in"].ap()[l][:, OFF_MQK:OFF_MV], 8, OFF_MV - OFF_MQK, "wm")
    pre = A.alloc("pre", (S + 4,)); cacc = A.alloc("cacc", (S,))
    cw = A.alloc("cw", (8, 6))
    for g_ in range(8):
        P.dma("sp", cw.ap[0:96, g_, 0:5], IN["ml_conv_w"].ap()[l][:, g_ * 96:(g_ + 1) * 96].rearrange("j c -> c j"), writes=[cw.k()], semkey=("cw0", g_ % 4), allow_slow_non_contiguous=True)
    P.dma("sp", cw.ap[0:96, :, 5:6], IN["ml_conv_b"].ap()[l].rearrange("(g c o) -> c g o", c=96, o=1), writes=[cw.k()], semkey="cw1", allow_slow_non_contiguous=True)
    P.op("pool", lambda e: e.memset(pre.ap, 0.0), writes=[pre.k()])
    P.op("pool", lambda e: e.memset(vp.ap, 1.0), writes=[vp.k(i) for i in range(NT)])
    n = 0
    for j in range(8):
        for tg in range(4):
            b = n % 2; n += 1
            for c in range(8):
                P.op("pe", lambda e, c=c, j=j, tg=tg, b=b: e.matmul(ps[b][0:96, :], wm.ap[:, c, j * 96:(j + 1) * 96], k.hT.ap[:, c, tg * 512:(tg + 1) * 512],
                                                                 start=(c == 0), stop=(c == 7)),
                     reads=[wm.k(c)] + [k.hT.k(4 * tg + q) for q in range(4)], writes=[f"ps{b}"])
            P.op("act", lambda e, tg=tg, b=b: e.copy(pre.ap[0:96, 2 + tg * 512:2 + (tg + 1) * 512], ps[b][0:96, :]),
                 reads=[f"ps{b}"], writes=[pre.k()])
        P.op("dve", lambda e, j=j: e.tensor_scalar(cacc.ap[0:96, :], pre.ap[0:96, 0:S], cw.ap[0:96, j, 0:1], None, op0=ALU.mult),
             reads=[pre.k(), cw.k()], writes=[cacc.k()])
        for t in range(1, 5):
            P.op("dve", lambda e, j=j, t=t: e.scalar_tensor_tensor(cacc.ap[0:96, :], pre.ap[0:96, t:t + S], cw.ap[0:96, j, t:t + 1], cacc.ap[0:96, :],
                                                                 op0=ALU.mult, op1=ALU.add),
                 reads=[pre.k(), cw.k(), cacc.k()], writes=[cacc.k()])
        if j < 4:
            P.op("act", lambda e, j=j: e.activation(qkT.ap[0:96, j, :], cacc.ap[0:96, :], AF.Silu, bias=cw.ap[0:96, j, 5:6]),
                 reads=[cacc.k(), cw.k()], writes=[qkT.k(j)])
        else:
            P.op("act", lambda e, j=j: e.activation(cacc.ap[0:96, :], cacc.ap[0:96, :], AF.Silu, bias=cw.ap[0:96, j, 5:6]),
                 reads=[cacc.k(), cw.k()], writes=[cacc.k()])
            P.op("dve", lambda e, j=j: e.tensor_scalar(qkT.ap[0:96, j, :], cacc.ap[0:96, :], 96.0 ** -0.5, None, op0=ALU.mult),
                 reads=[cacc.k()], writes=[qkT.k(j)])
    A.release(m2)
    wv = load_w_bf16(k, "wv", IN["w_in"].ap()[l][:, OFF_MV:OFF_MO], 8, 384, "wv")
    wg = load_w_bf16(k, "wg", IN["w_in"].ap()[l][:, OFF_MG:INW], 8, 16, "wg")
    gbias = A.alloc("gbias", (16,))
    for d in range(2):
        P.dma("act", gbias.ap[:, 8 * d:8 * d + 4], IN["ml_ib"].ap()[l][d].partition_broadcast(128), writes=[gbias.k()], semkey=("gbi", d))
        P.dma("act", gbias.ap[:, 8 * d + 4:8 * d + 8], IN["ml_fb"].ap()[l][d].partition_broadcast(128), writes=[gbias.k()], semkey=("gbf_", d))
    for i in range(NT):
        b = 2 + i % 2
        for c in range(8):
            P.op("pe", lambda e, c=c, i=i, b=b: e.matmul(ps[b][:, 0:384], k.hT.ap[:, c, i * 128:(i + 1) * 128], wv.ap[:, c, :],
                                                     start=(c == 0), stop=(c == 7)),
                 reads=[wv.k(c), k.hT.k(i)], writes=[f"ps{b}"])
        P.op("dve", lambda e, i=i, b=b: e.tensor_copy(vp.ap[:, i, :, 0:96], ps[b][:, 0:384].rearrange("p (h c) -> p h c", h=4)),
             reads=[f"ps{b}"], writes=[vp.k(i)])
        b2 = 4 + i % 2
        for c in range(8):
            P.op("pe", lambda e, c=c, i=i, b2=b2: e.matmul(ps[b2][:, 0:16], k.hT.ap[:, c, i * 128:(i + 1) * 128], wg.ap[:, c, :],
                                                       start=(c == 0), stop=(c == 7)),
                 reads=[wg.k(c), k.hT.k(i)], writes=[f"ps{b2}"])
        P.op("dve", lambda e, i=i, b2=b2: e.tensor_tensor(G.ap[:, i, :], ps[b2][:, 0:16], gbias.ap, op=ALU.add),
             reads=[f"ps{b2}", gbias.k()], writes=[G.k()])
    A.release(m2)
    for d in range(2):
        v_ = G.ap[:, :, 8 * d + 4:8 * d + 8]
        P.op("act", lambda e, v_=v_: e.activation(v_, v_, AF.Exp, scale=-1.0), reads=[G.k()], writes=[G.k()])
        P.op("act", lambda e, v_=v_: e.activation(v_, v_, AF.Ln, bias=k.one.ap[:, 0:1]), reads=[G.k(), k.one.k()], writes=[G.k()])
        P.op("dve", lambda e, v_=v_: e.tensor_scalar(v_, v_, -1.0, None, op0=ALU.mult), reads=[G.k()], writes=[G.k()])
    for d in range(2):
        tri = k.triu if d == 0 else k.tril
        P.op("pe", lambda e, d=d, tri=tri: e.matmul(ps[7][:, 0:64].rearrange("p (i h) -> p i h", h=4), tri.ap, G.ap[:, :, 8 * d + 4:8 * d + 8], start=True, stop=True),
             reads=[tri.k(), G.k()], writes=["ps7"])
        P.op("dve", lambda e, d=d: e.tensor_copy(BL[d].ap, ps[7][:, 0:64].rearrange("p (i h) -> p i h", h=4)), reads=["ps7"], writes=[BL[d].k()])
        P.op("act", lambda e, d=d: e.activation(EBL[d].ap, BL[d].ap, AF.Exp), reads=[BL[d].k()], writes=[EBL[d].k()])
        P.op("dve", lambda e, d=d: e.tensor_tensor(IMB[d].ap, G.ap[:, :, 8 * d:8 * d + 4], BL[d].ap, op=ALU.subtract), reads=[G.k(), BL[d].k()], writes=[IMB[d].k()])
    C32 = A.alloc("C32", (4, 97)); Cb = A.alloc("Cb", (4, 97), BF16)
    HB = []
    for h in range(4):
        HB.append(dict(dg=A.alloc(f"dg{h}", (128,)), Dm=A.alloc(f"Dm{h}", (128,)), W16=A.alloc(f"W16{h}", (128,), BF16),
                       nis=A.alloc(f"nis{h}", (97,)), nsb=A.alloc(f"nsb{h}", (97,)), kh=A.alloc(f"kh{h}", (96,), BF16), sm=A.alloc(f"sm{h}", (4,))))

    def head_stream(h):
        hb = HB[h]
        dg, Dm, W16, nis, nsb, kh, sm = (hb[n_] for n_ in ("dg", "Dm", "W16", "nis", "nsb", "kh", "sm"))
        bA, bB = 2 * h, 2 * h + 1
        kA, kB = f"ps{bA}", f"ps{bB}"
        pbt = ps[bA][:, 384:432].bitcast(BF16)
        for d in range(2):
            P.op("pool", lambda e: e.memset(C32.ap[:, h, :], 0.0), writes=[C32.k(h)])
            P.op("pool", lambda e: e.memset(Cb.ap[:, h, :], 0.0), writes=[Cb.k(h)])
            nm = k.negm[d]
            endc = 127 if d == 0 else 0
            order = range(NT) if d == 0 else range(NT - 1, -1, -1)
            for i in order:
                yield from chunk(h, d, i, nm, endc, dg, Dm, W16, nis, nsb, kh, sm, bA, bB, kA, kB, pbt)

    def chunk(h, d, i, nm, endc, dg, Dm, W16, nis, nsb, kh, sm, bA, bB, kA, kB, pbt):
        tsl = slice(i * 128, (i + 1) * 128)
        P.op("pe", lambda e: e.matmul(ps[bA][:, 0:128], qkT.ap[0:96, 4 + h, tsl], qkT.ap[0:96, h, tsl], start=True, stop=True),
             reads=[qkT.k(4 + h), qkT.k(h)], writes=[kA])
        P.op("dve", lambda e: e.tensor_scalar(dg.ap, k.ident.ap, BL[d].ap[:, i, h:h + 1], None, op0=ALU.mult),
             reads=[k.ident.k(), BL[d].k()], writes=[dg.k()])
        P.op("pe", lambda e: e.matmul(ps[bA][:, 128:256], k.ones32.ap, dg.ap, start=True, stop=False), reads=[k.ones32.k(), dg.k()], writes=[kA])
        P.op("pe", lambda e: e.matmul(ps[bA][:, 128:256], k.ident.ap, nm.ap, start=False, stop=True), reads=[k.ident.k(), nm.k()], writes=[kA])
        yield
        P.op("act", lambda e: e.activation(Dm.ap, ps[bA][:, 128:256], AF.Exp, bias=IMB[d].ap[:, i, h:h + 1]), reads=[kA, IMB[d].k()], writes=[Dm.k()])
        P.op("act", lambda e: e.activation(sm.ap[:, 2:3], ps[bA][:, 128 + endc:129 + endc], AF.Exp), reads=[kA], writes=[sm.k(2)])
        P.op("dve", lambda e: e.tensor_tensor(W16.ap, ps[bA][:, 0:128], Dm.ap, op=ALU.mult), reads=[kA, Dm.k()], writes=[W16.k()])
        yield
        P.op("pe", lambda e: e.matmul(ps[bB][:, 0:97], W16.ap, vp.ap[:, i, h, :], start=True, stop=True), reads=[W16.k(), vp.k(i)], writes=[kB])
        P.op("pe", lambda e: e.matmul(ps[bB][:, 128:225], qkT.ap[0:96, h, tsl], Cb.ap[0:96, h, :], start=True, stop=True), reads=[qkT.k(h), Cb.k(h)], writes=[kB])
        P.op("pe", lambda e: e.transpose(pbt, qkT.ap[0:96, 4 + h, tsl], k.ident16.ap[0:96, 0:96]), reads=[qkT.k(4 + h), k.ident16.k()], writes=[kA])
        yield
        P.op("act", lambda e: e.copy(nis.ap, ps[bB][:, 0:97]), reads=[kB], writes=[nis.k()])
        P.op("dve", lambda e: e.tensor_scalar(kh.ap, pbt, Dm.ap[:, endc:endc + 1], None, op0=ALU.mult), reads=[kA, Dm.k()], writes=[kh.k()])
        P.op("dve", lambda e: e.scalar_tensor_tensor(nsb.ap, ps[bB][:, 128:225], EBL[d].ap[:, i, h:h + 1], nis.ap, op0=ALU.mult, op1=ALU.add),
             reads=[kB, EBL[d].k(), nis.k()], writes=[nsb.k()])
        P.op("pe", lambda e: e.matmul(ps[bA][0:96, 256:353], kh.ap, vp.ap[:, i, h, :], start=True, stop=True), reads=[kh.k(), vp.k(i)], writes=[kA])
        yield
        P.op("dve", lambda e: e.tensor_scalar(sm.ap[:, 0:1], nsb.ap[:, 96:97], -1.0, None, op0=ALU.mult), reads=[nsb.k()], writes=[sm.k(0)])
        P.op("dve", lambda e: e.scalar_tensor_tensor(sm.ap[:, 0:1], nsb.ap[:, 96:97], 1.0, sm.ap[:, 0:1], op0=ALU.max, op1=ALU.max), reads=[nsb.k(), sm.k(0)], writes=[sm.k(0)])
        P.op("dve", lambda e: e.reciprocal(sm.ap[:, 0:1], sm.ap[:, 0:1]), reads=[sm.k(0)], writes=[sm.k(0)])
        if d == 0:
            P.op("dve", lambda e: e.tensor_scalar(hsum.ap[:, i, h, :], nsb.ap[:, 0:96], sm.ap[:, 0:1], None, op0=ALU.mult), reads=[nsb.k(), sm.k(0)], writes=[hsum.k(i, h)])
        else:
            P.op("dve", lambda e: e.scalar_tensor_tensor(hsum.ap[:, i, h, :], nsb.ap[:, 0:96], sm.ap[:, 0:1], hsum.ap[:, i, h, :], op0=ALU.mult, op1=ALU.add),
                 reads=[nsb.k(), sm.k(0), hsum.k(i, h)], writes=[hsum.k(i, h)])
        P.op("dve", lambda e: e.scalar_tensor_tensor(C32.ap[0:96, h, :], C32.ap[0:96, h, :], sm.ap[0:96, 2:3], ps[bA][0:96, 256:353], op0=ALU.mult, op1=ALU.add),
             reads=[C32.k(h), sm.k(2), kA], writes=[C32.k(h)])
        P.op("act", lambda e: e.copy(Cb.ap[0:96, h, :], C32.ap[0:96, h, :]), reads=[C32.k(h)], writes=[Cb.k(h)])
        yield

    gens = [head_stream(h) for h in range(4)]
    while gens:
        for g_ in list(gens):
            try:
                next(g_)
            except StopIteration:
                gens.remove(g_)
    A.release(m1)
    yc = A.alloc("yc", (NT, 384), BF16)
    m3 = A.mark()
    wmo = load_w_bf16(k, "wmo", IN["w_in"].ap()[l][:, OFF_MO:OFF_MG], 8, 384, "wmo")
    lng = A.alloc("lng", (384,))
    P.dma("sp", lng.ap, IN["ml_ln_g"].ap()[l].partition_broadcast(128), writes=[lng.k()], semkey="lng")
    st = A.alloc("st", (16,)); cen = [A.alloc(f"cen{i}", (4, 96)) for i in range(2)]; junk = A.alloc("junkm", (96,))
    og = [A.alloc(f"og{i}", (384,)) for i in range(2)]
    for i in range(NT):
        r = i % 2
        b = 2 + r
        for c in range(8):
            P.op("pe", lambda e, c=c, i=i, b=b: e.matmul(ps[b][:, 0:384], k.hT.ap[:, c, i * 128:(i + 1) * 128], wmo.ap[:, c, :], start=(c == 0), stop=(c == 7)),
                 reads=[wmo.k(c), k.hT.k(i)], writes=[f"ps{b}"])
        P.op("act", lambda e, r=r, b=b: e.activation(og[r].ap, ps[b][:, 0:384], AF.Sigmoid), reads=[f"ps{b}"], writes=[og[r].k()])
        sk = st.k()
        P.op("dve", lambda e, i=i: e.tensor_reduce(st.ap[:, 0:4], hsum.ap[:, i, :, :], axis=AX.X, op=ALU.add), reads=[hsum.k(i, h_) for h_ in range(4)], writes=[sk])
        P.op("dve", lambda e: e.tensor_scalar(st.ap[:, 0:4], st.ap[:, 0:4], 1.0 / 96, None, op0=ALU.mult), reads=[sk], writes=[sk])
        P.op("pool", lambda e: e.memset(st.ap[:, 4:8], 0.0), writes=[sk])
        for h in range(4):
            P.op("dve", lambda e, i=i, h=h, r=r: e.tensor_scalar(cen[r].ap[:, h, :], hsum.ap[:, i, h, :], st.ap[:, h:h + 1], None, op0=ALU.subtract),
                 reads=[hsum.k(i, h), sk], writes=[cen[r].k(h)])
            P.op("act", lambda e, h=h, r=r: e.activation(junk.ap, cen[r].ap[:, h, :], AF.Square, accum_out=st.ap[:, 4 + h:5 + h]),
                 reads=[cen[r].k(h)], writes=[junk.k(), sk])
        P.op("act", lambda e: e.activation(st.ap[:, 8:12], st.ap[:, 4:8], AF.Sqrt, scale=1.0 / 96, bias=k.eps6.ap[:, 0:1]), reads=[sk, k.eps6.k()], writes=[sk])
        P.op("dve", lambda e: e.reciprocal(st.ap[:, 8:12], st.ap[:, 8:12]), reads=[sk], writes=[sk])
        for h in range(4):
            P.op("dve", lambda e, h=h, r=r: e.scalar_tensor_tensor(cen[r].ap[:, h, :], cen[r].ap[:, h, :], st.ap[:, 8 + h:9 + h], lng.ap[:, h * 96:(h + 1) * 96],
                                                                 op0=ALU.mult, op1=ALU.mult),
                 reads=[cen[r].k(h), sk, lng.k()], writes=[cen[r].k(h)])
        P.op("dve", lambda e, i=i, r=r: e.tensor_tensor(yc.ap[:, i, :], cen[r].ap.rearrange("p h c -> p (h c)"), og[r].ap, op=ALU.mult),
             reads=[cen[r].k(h) for h in range(4)] + [og[r].k()], writes=[yc.k(i)])
    A.release(m3)
    if k.dbg and "yc" in k.dbg:
        t = k.nc.dram_tensor("dbg_yc", [S, 384], BF16, kind="ExternalOutput")
        k.final_events.append(P.dma("sp", t.ap().rearrange("(i p) c -> p i c", p=128), yc.ap, reads=[yc.k(i) for i in range(NT)], semkey="dbg"))
    ycT = A.alloc("ycT", (3, S), BF16)
    transpose_to_T(k, yc, 3, ycT)
    outproj_partial(k, l, ycT, 3, 640, "c")
    A.release(m0)


OFF_RKV = 768
BLK = 256
PC_MU, PC_MUX, PC_W0, PC_A0, PC_KK, PC_KA, PC_RK, PC_LNG, PC_LNB, NPC = 0, 18, 20, 26, 32, 35, 38, 41, 44, 48


def host_rwkv_layout(inputs):
    out = {}
    pc = np.zeros((DEPTH, 128, NPC), np.float32)
    for l in range(DEPTH):
        mu = np.asarray(inputs["rk_mu"][l], np.float32)
        for d in range(2):
            for j in range(3):
                for p in range(3):
                    pc[l, :, PC_MU + d * 9 + j * 3 + p] = mu[d, j * 384 + p * 128: j * 384 + (p + 1) * 128]
            pc[l, 0:64, PC_MUX + d] = mu[d, 1152:1216]
            pc[l, 64:128, PC_MUX + d] = mu[d, 1216:1280]
            for p in range(3):
                pc[l, :, PC_W0 + d * 3 + p] = inputs["rk_w0"][l][d][p * 128:(p + 1) * 128]
                pc[l, :, PC_A0 + d * 3 + p] = inputs["rk_a0"][l][d][p * 128:(p + 1) * 128]
        for p in range(3):
            sl = slice(p * 128, (p + 1) * 128)
            pc[l, :, PC_KK + p] = inputs["rk_kk"][l][sl]
            pc[l, :, PC_KA + p] = inputs["rk_ka"][l][sl]
            pc[l, :, PC_RK + p] = np.asarray(inputs["rk_rk"][l]).reshape(384)[sl]
            pc[l, :, PC_LNG + p] = inputs["rk_ln_g"][l][sl]
            pc[l, :, PC_LNB + p] = inputs["rk_ln_b"][l][sl]
    out["c_pc"] = pc
    w_in = np.asarray(inputs["w_in"], np.float32)
    out["c_wx"] = np.ascontiguousarray(np.concatenate(
        [w_in[:, :, 1920:1984], w_in[:, :, 2048:2112], w_in[:, :, 1984:2048], w_in[:, :, 2112:2176], w_in[:, :, 2176:2304]], axis=2))
    return out


def rev_ap(ap, n):
    a = ap.ap
    return bass.AP(ap.tensor, ap.offset + (n - 1) * a[-1][0], [list(a[0]), [-a[-1][0], n]])


def psk(b, c0, c1):
    return [f"ps{b}q{q}" for q in range(c0 // 128, (c1 + 127) // 128)]


def rwkv_stage(k, l):
    P, A, ps, IN = k.P, k.A, k.ps, k.IN
    m0 = A.mark()
    k.mask4 = A.alloc("mask4", (512,)); k.rmask = A.alloc("rmask", (256,))
    P.dma("act", k.mask4.ap, IN["c_mask4"].ap(), writes=[k.mask4.k()], semkey="c5")
    P.dma("act", k.rmask.ap, IN["c_rmask"].ap(), writes=[k.rmask.k()], semkey="c7")
    ybT = A.alloc("ybT", (3, S), BF16)
    P.op("pool", lambda e: e.memset(ybT.ap, 0.0), writes=[ybT.k(i) for i in range(NT)])
    xsp = k.xspill.ap().rearrange("(i p) d -> p i d", p=128)
    for i in range(NT):
        P.dma("sp" if i % 2 == 0 else "act", xsp[:, i, :], k.xres.ap[:, i, :], reads=[k.xres.k(i)], writes=[("xsp", i)], semkey=("xso", i % 4))
    evs = []
    for kk_ in [q for q in P.keys if isinstance(q, tuple) and q[0] == k.xres.uid]:
        st = P.keys.pop(kk_)
        evs.extend(st["w"]); evs.extend(st["r"].values())
    AX = Arena.__new__(Arena)
    AX.P, AX.t, AX.n, AX.top, AX.gen, AX.live = P, A.t, k.xres.hi, k.xres.lo, 100000 + 1000 * l, []
    AX.pending = [(k.xres.lo, k.xres.hi, evs)]
    import os as _os
    LVL = int(_os.environ.get("RWKV_SETUP", "9"))
    pc = A.alloc("pc", (NPC + 4,))
    P.dma("sp", pc.ap[:, 0:NPC], IN["c_pc"].ap()[l], writes=[pc.k()], semkey="pc")
    PCO = NPC
    P.op("dve", lambda e: e.tensor_scalar(pc.ap[:, PCO:PCO + 3], pc.ap[:, PC_KA:PC_KA + 3], -1.0, 1.0, op0=ALU.mult, op1=ALU.add), reads=[pc.k()], writes=[pc.k()])
    wl = [A.alloc(f"wl{d}", (384,)) for d in range(2)]
    for d in range(2):
        P.dma("sp", wl[d].ap[0:64, :], IN["rk_w2"].ap()[l][d], writes=[wl[d].k()], semkey=("wl", d))
        P.dma("act", wl[d].ap[64:128, :], IN["rk_a2"].ap()[l][d], writes=[wl[d].k()], semkey=("wl2", d))
    g2b = A.alloc("g2b", (384,), BF16)
    P.dma("pool", g2b.ap, IN["rk_g2"].ap()[l], writes=[g2b.k()], semkey="g2b")
    siggd = A.alloc("siggd", (S,), BF16)
    wdad = [(A if int(_os.environ.get("WDAD_MAIN", "0")) else AX).alloc(f"wdad{d}", (S,)) for d in range(2)]
    mA = A.mark()
    wx = load_w_bf16(k, "wx", IN["c_wx"].ap()[l], 8, 384, "wx")
    zx = A.alloc("zx", (S + 2,)); tmp = A.alloc("tmpx", (S,))
    P.op("pool", lambda e: e.memset(zx.ap[:, 0:1], 0.0), writes=[zx.k()])
    P.op("pool", lambda e: e.memset(zx.ap[:, S + 1:S + 2], 0.0), writes=[zx.k()])
    n = 0
    for d in range(2 if LVL >= 2 else 0):
        for tg in range(4):
            b = n % 2; n += 1
            for c in range(8):
                P.op("pe", lambda e, c=c, d=d, tg=tg, b=b: e.matmul(ps[b][:, :], wx.ap[:, c, d * 128:(d + 1) * 128], k.hT.ap[:, c, tg * 512:(tg + 1) * 512], start=(c == 0), stop=(c == 7)),
                     reads=[wx.k(c)] + [k.hT.k(4 * tg + q) for q in range(4)], writes=[f"ps{b}"])
            P.op("act", lambda e, tg=tg, b=b: e.copy(zx.ap[:, 1 + tg * 512:1 + (tg + 1) * 512], ps[b][:, :]), reads=[f"ps{b}"], writes=[zx.k()])
        if LVL < 3:
            continue
        if d == 0:
            cur, prv = zx.ap[:, 1:S + 1], zx.ap[:, 0:S]
        else:
            cur, prv = rev_ap(zx.ap[:, 1:S + 1], S), rev_ap(zx.ap[:, 2:S + 2], S)
        P.op("dve", lambda e, cur=cur, prv=prv: e.tensor_tensor(tmp.ap, prv, cur, op=ALU.subtract), reads=[zx.k()], writes=[tmp.k()])
        P.op("dve", lambda e, cur=cur, d=d: e.scalar_tensor_tensor(wdad[d].ap, tmp.ap, pc.ap[:, PC_MUX + d:PC_MUX + d + 1], cur, op0=ALU.mult, op1=ALU.add),
             reads=[tmp.k(), pc.k(), zx.k()], writes=[wdad[d].k()])
        P.op("act", lambda e, d=d: e.activation(wdad[d].ap[0:64, :], wdad[d].ap[0:64, :], AF.Tanh), reads=[wdad[d].k()], writes=[wdad[d].k()])
    for tg in range(4 if LVL >= 4 else 0):
        b = n % 2; n += 1
        for c in range(8):
            P.op("pe", lambda e, c=c, tg=tg, b=b: e.matmul(ps[b][:, :], wx.ap[:, c, 256:384], k.hT.ap[:, c, tg * 512:(tg + 1) * 512], start=(c == 0), stop=(c == 7)),
                 reads=[wx.k(c)] + [k.hT.k(4 * tg + q) for q in range(4)], writes=[f"ps{b}"])
        P.op("act", lambda e, tg=tg, b=b: e.activation(siggd.ap[:, tg * 512:(tg + 1) * 512], ps[b][:, :], AF.Sigmoid), reads=[f"ps{b}"], writes=[siggd.k()])
    A.release(mA)
    NB = ["XR", "XK", "XV", "T1", "LW", "AA", "KK", "SQ", "CUM"]
    NB16 = ["XRb", "XKb", "KKb", "AAb", "XVb", "BH", "KH"]
    SB = []
    for s_ in range(2):
        sb = {nm: A.alloc(f"{nm}{s_}", (BLK,)) for nm in NB}
        sb.update({nm: A.alloc(f"{nm}{s_}", (BLK,), BF16) for nm in NB16})
        sb["PCc"] = A.alloc(f"PCc{s_}", (BLK // 128,))
        sb["AR"] = A.alloc(f"AR{s_}", (BLK // 128, 256), BF16)
        sb["FT"] = A.alloc(f"FT{s_}", (512,))
        sb["AM"] = [A.alloc(f"AM{s_}{x}", (512,), BF16) for x in range(2)]
        sb["M"] = [[A.alloc(f"M{s_}{x}{q}", (128,), BF16) for q in range(2)] for x in range(2)]
        sb["MT"] = [[A.alloc(f"MT{s_}{x}{q}", (128,), BF16) for q in range(2)] for x in range(2)]
        sb["B"] = [[A.alloc(f"Bq{s_}{x}{q}", (384,), BF16) for q in range(2)] for x in range(2)]
        sb["Q"] = [A.alloc(f"Q{s_}{x}", (128,)) for x in range(2)]
        sb["Q16"] = [A.alloc(f"Qb{s_}{x}", (128,), BF16) for x in range(2)]
        sb["RHS"] = A.alloc(f"RHS{s_}", (128,), BF16)
        sb["SAz"] = [A.alloc(f"SAz{s_}{x}", (128,), BF16) for x in range(2)]
        sb["Vz"] = [A.alloc(f"Vz{s_}{x}", (128,), BF16) for x in range(2)]
        sb["BHt"] = A.alloc(f"BHt{s_}", (128,), BF16); sb["KHt"] = A.alloc(f"KHt{s_}", (128,), BF16)
        sb["T"] = A.alloc(f"Tst{s_}", (128,)); sb["T16"] = A.alloc(f"Tsb{s_}", (128,), BF16)
        sb["pb"] = 4 * s_
        SB.append(sb)
    import os as _os
    for p in range(int(_os.environ.get('RWKV_PAIRS', '3'))):
        mX = AX.mark()
        wp = AX.alloc("wp", (8, 3, 128), BF16)
        wv_ = IN["w_in"].ap()[l].rearrange("(c q) n -> q c n", q=128)
        for j in range(3):
            c0 = OFF_RKV + j * 384 + p * 128
            P.dma("pool", wp.ap[:, :, j, :], wv_[:, :, c0:c0 + 128], writes=[wp.k(j)], semkey=("wp", j))
        zp = AX.alloc("zp", (3, S + 2))
        yacc = AX.alloc("yacc", (S,)); bonacc = AX.alloc("bonacc", (S,))
        P.op("pool", lambda e: e.memset(yacc.ap, 0.0), writes=[yacc.k(c) for c in range(NT)])
        P.op("pool", lambda e: e.memset(bonacc.ap, 0.0), writes=[bonacc.k(c) for c in range(NT)])
        for j in range(3):
            P.op("pool", lambda e, j=j: e.memset(zp.ap[:, j, 0:1], 0.0), writes=[zp.k(j)])
            P.op("pool", lambda e, j=j: e.memset(zp.ap[:, j, S + 1:S + 2], 0.0), writes=[zp.k(j)])
            for tg in range(4):
                b = n % 2; n += 1
                for c in range(8):
                    P.op("pe", lambda e, c=c, j=j, tg=tg, b=b: e.matmul(ps[b][:, :], wp.ap[:, c, j, :], k.hT.ap[:, c, tg * 512:(tg + 1) * 512], start=(c == 0), stop=(c == 7)),
                         reads=[wp.k(j)] + [k.hT.k(4 * tg + q) for q in range(4)], writes=[f"ps{b}"] + psk(b, 0, 512))
                P.op("act", lambda e, j=j, tg=tg, b=b: e.copy(zp.ap[:, j, 1 + tg * 512:1 + (tg + 1) * 512], ps[b][:, :]), reads=[f"ps{b}"] + psk(b, 0, 512), writes=[zp.k(j)])
        gens = [rwkv_stream(k, l, p, d, SB[d], pc, wl[d], wdad[d], zp, yacc, bonacc, PCO) for d in range(int(_os.environ.get("RWKV_NDIR", "2")))]
        while gens:
            for g in list(gens):
                try:
                    next(g)
                except StopIteration:
                    gens.remove(g)
        T1 = SB[0]["FT"]; T2 = SB[1]["FT"]
        for tg in range(4 if int(_os.environ.get("RWKV_FIN", "1")) else 0):
            cs = slice(tg * 512, (tg + 1) * 512)
            yk = [yacc.k(c) for c in range(4 * tg, 4 * tg + 4)]
            P.op("pe", lambda e, cs=cs: e.matmul(ps[0][:, :], k.onesblk64.ap, yacc.ap[:, cs], start=True, stop=True), reads=[k.onesblk64.k()] + yk, writes=["ps0"] + psk(0, 0, 512))
            P.op("dve", lambda e, cs=cs: e.tensor_tensor(yacc.ap[:, cs], yacc.ap[:, cs], ps[0][:, :], op=ALU.subtract), reads=["ps0"] + psk(0, 0, 512) + yk, writes=yk)
            P.op("act", lambda e, cs=cs: e.activation(T1.ap, yacc.ap[:, cs], AF.Square), reads=yk, writes=[T1.k()])
            P.op("pe", lambda e: e.matmul(ps[1][:, :], k.onesblk64.ap, T1.ap, start=True, stop=True), reads=[k.onesblk64.k(), T1.k()], writes=["ps1"] + psk(1, 0, 512))
            P.op("act", lambda e: e.activation(T2.ap, ps[1][:, :], AF.Sqrt, bias=k.epsgn.ap[:, 0:1]), reads=["ps1", k.epsgn.k()] + psk(1, 0, 512), writes=[T2.k()])
            P.op("dve", lambda e: e.reciprocal(T2.ap, T2.ap), reads=[T2.k()], writes=[T2.k()])
            P.op("dve", lambda e, cs=cs: e.tensor_tensor(yacc.ap[:, cs], yacc.ap[:, cs], T2.ap, op=ALU.mult), reads=yk + [T2.k()], writes=yk)
            P.op("dve", lambda e, cs=cs, p=p: e.tensor_scalar(yacc.ap[:, cs], yacc.ap[:, cs], pc.ap[:, PC_LNG + p:PC_LNG + p + 1], pc.ap[:, PC_LNB + p:PC_LNB + p + 1], op0=ALU.mult, op1=ALU.add),
                 reads=yk + [pc.k()], writes=yk)
            P.op("dve", lambda e, cs=cs: e.tensor_tensor(yacc.ap[:, cs], yacc.ap[:, cs], bonacc.ap[:, cs], op=ALU.add), reads=yk + [bonacc.k(c) for c in range(4 * tg, 4 * tg + 4)], writes=yk)
            P.op("pe", lambda e, cs=cs, p=p: e.matmul(ps[2][:, :], g2b.ap[:, p * 128:(p + 1) * 128], siggd.ap[:, cs], start=True, stop=True), reads=[g2b.k(), siggd.k()], writes=["ps2"] + psk(2, 0, 512))
            P.op("dve", lambda e, cs=cs, p=p: e.tensor_tensor(ybT.ap[:, p, cs], yacc.ap[:, cs], ps[2][:, :], op=ALU.mult), reads=yk + ["ps2"] + psk(2, 0, 512), writes=[ybT.k(c) for c in range(4 * tg, 4 * tg + 4)])
        AX.release(mX)
    AX.release((k.xres.lo, 0))
    evs = []
    for (_, _, e_) in AX.pending:
        evs.extend(e_)
    P.inherit[k.xres.uid] = evs
    for i in range(NT):
        P.dma("sp" if i % 2 == 0 else "act", k.xres.ap[:, i, :], xsp[:, i, :], reads=[("xsp", i)], writes=[k.xres.k(i)], semkey=("xsi", i % 4))
    if k.dbg and "yb" in k.dbg:
        t = k.nc.dram_tensor("dbg_yb", [384, S], BF16, kind="ExternalOutput")
        k.final_events.append(P.dma("sp", t.ap().rearrange("(c p) s -> p c s", p=128), ybT.ap, reads=[ybT.k(i) for i in range(NT)], semkey="dbg"))
    if LVL >= 5:
        outproj_partial(k, l, ybT, 3, 256, "b")
    A.release(m0)


def rwkv_stream(k, l, p, d, sb, pc, wl, wdad, zp, yacc, bonacc, PCO):
    P, ps = k.P, k.ps
    pb = sb["pb"]
    B0, B1, B2, B3 = pb, pb + 1, pb + 2, pb + 3
    XR, XK, XV, T1, LW, AA, KK, SQ, CUM, BH, KH, PCc = (sb[n_] for n_ in ["XR", "XK", "XV", "T1", "LW", "AA", "KK", "SQ", "CUM", "BH", "KH", "PCc"])
    XRb, XKb, KKb, AAb, XVb = (sb[n_] for n_ in ["XRb", "XKb", "KKb", "AAb", "XVb"])
    T = sb["T"]; T16 = sb["T16"]
    P.op("pool", lambda e: e.memset(T16.ap, 0.0), writes=[T16.k()])
    col = lambda c: pc.ap[:, c:c + 1]
    P.op("pool", lambda e: e.memset(T.ap, 0.0), writes=[T.k()])
    for x in range(2):
        P.op("pool", lambda e, x=x: e.memset(sb["SAz"][x].ap, 0.0), writes=[sb["SAz"][x].k()])
        P.op("pool", lambda e, x=x: e.memset(sb["Vz"][x].ap, 0.0), writes=[sb["Vz"][x].k()])
    import os as _os
    NBLK = int(_os.environ.get("RWKV_NBLK", str(S // BLK))); PH = int(_os.environ.get("RWKV_PHASE", "9"))
    def _blk(bi):
        t0 = bi * BLK
        if d == 0:
            cur = lambda j: zp.ap[:, j, 1 + t0:1 + t0 + BLK]
            prv = lambda j: zp.ap[:, j, t0:t0 + BLK]
            nat = lambda buf: buf.ap[:, t0:t0 + BLK]
            nchunks = list(range(t0 // 128, (t0 + BLK) // 128))
        else:
            a_ = S - t0 - BLK
            cur = lambda j: rev_ap(zp.ap[:, j, 1 + a_:1 + a_ + BLK], BLK)
            prv = lambda j: rev_ap(zp.ap[:, j, 2 + a_:2 + a_ + BLK], BLK)
            nat = lambda buf: rev_ap(buf.ap[:, a_:a_ + BLK], BLK)
            nchunks = list(range(a_ // 128, (a_ + BLK) // 128))
        scols = slice(t0, t0 + BLK)
        P0 = int(_os.environ.get("RWKV_P0", "9"))
        for j, X in enumerate((XR, XK, XV)):
            if P0 < 2:
                break
            P.op("dve", lambda e, j=j, prv=prv, cur=cur: e.tensor_tensor(T1.ap, prv(j), cur(j), op=ALU.subtract), reads=[zp.k(j)], writes=[T1.k()])
            P.op("dve", lambda e, j=j, X=X, cur=cur: e.scalar_tensor_tensor(X.ap, T1.ap, col(PC_MU + d * 9 + j * 3 + p), cur(j), op0=ALU.mult, op1=ALU.add),
                 reads=[T1.k(), zp.k(j), pc.k()], writes=[X.k()])
        if P0 >= 3:
            P.op("pe", lambda e, scols=scols: e.matmul(ps[B3][:, 0:BLK], wl.ap[0:64, p * 128:(p + 1) * 128], wdad.ap[0:64, scols], start=True, stop=True),
                 reads=[wl.k(), wdad.k()], writes=psk(B3, 0, BLK), serial=True)
        if P0 >= 4:
            P.op("pe", lambda e, scols=scols: e.matmul(ps[B3][:, 256:256 + BLK], wl.ap[64:128, p * 128:(p + 1) * 128], wdad.ap[64:128, scols], start=True, stop=True),
                 reads=[wl.k(), wdad.k()], writes=psk(B3, 256, 256 + BLK), serial=True)
        if P0 >= 5:
            P.op("act", lambda e: e.activation(LW.ap, ps[B3][:, 0:BLK], AF.Sigmoid, bias=col(PC_W0 + d * 3 + p)), reads=psk(B3, 0, BLK) + [pc.k()], writes=[LW.k()])
            P.op("act", lambda e: e.activation(AA.ap, ps[B3][:, 256:256 + BLK], AF.Sigmoid, bias=col(PC_A0 + d * 3 + p)), reads=psk(B3, 256, 256 + BLK) + [pc.k()], writes=[AA.k()])
        if P0 >= 6:
            P.op("dve", lambda e: e.tensor_scalar(LW.ap, LW.ap, -0.6065306597126334, None, op0=ALU.mult), reads=[LW.k()], writes=[LW.k()])
        yield
        if PH <= 1:
            return
        P.op("act", lambda e: e.activation(KK.ap, XK.ap, AF.Copy, scale=col(PC_KK + p)), reads=[XK.k(), pc.k()], writes=[KK.k()])
        P.op("act", lambda e: e.activation(SQ.ap, KK.ap, AF.Square), reads=[KK.k()], writes=[SQ.k()])
        P.op("pe", lambda e: e.matmul(ps[B2][:, 0:BLK], k.onesblk.ap, SQ.ap, start=True, stop=True), reads=[k.onesblk.k(), SQ.k()], writes=psk(B2, 0, BLK))
        P.op("act", lambda e: e.activation(SQ.ap, ps[B2][:, 0:BLK], AF.Sqrt), reads=psk(B2, 0, BLK), writes=[SQ.k()])
        P.op("dve", lambda e: e.tensor_scalar(SQ.ap, SQ.ap, 1e-12, None, op0=ALU.max), reads=[SQ.k()], writes=[SQ.k()])
        P.op("dve", lambda e: e.reciprocal(SQ.ap, SQ.ap), reads=[SQ.k()], writes=[SQ.k()])
        P.op("dve", lambda e: e.tensor_tensor(KK.ap, KK.ap, SQ.ap, op=ALU.mult), reads=[KK.k(), SQ.k()], writes=[KK.k()])
        P.op("dve", lambda e: e.tensor_scalar(T1.ap, AA.ap, col(PC_KA + p), col(PCO + p), op0=ALU.mult, op1=ALU.add), reads=[AA.k(), pc.k()], writes=[T1.k()])
        P.op("dve", lambda e: e.tensor_tensor(XK.ap, XK.ap, T1.ap, op=ALU.mult), reads=[XK.k(), T1.k()], writes=[XK.k()])
        P.op("dve", lambda e: e.scalar_tensor_tensor(T1.ap, XR.ap, col(PC_RK + p), XK.ap, op0=ALU.mult, op1=ALU.mult), reads=[XR.k(), XK.k(), pc.k()], writes=[T1.k()])
        P.op("pe", lambda e: e.matmul(ps[B2][:, 256:256 + BLK], k.onesblk.ap, T1.ap, start=True, stop=True), reads=[k.onesblk.k(), T1.k()], writes=psk(B2, 256, 256 + BLK))
        P.op("dve", lambda e: e.tensor_tensor(T1.ap, ps[B2][:, 256:256 + BLK], XV.ap, op=ALU.mult), reads=psk(B2, 256, 256 + BLK) + [XV.k()], writes=[T1.k()])
        bk = [bonacc.k(c) for c in nchunks]
        P.op("dve", lambda e: e.tensor_tensor(nat(bonacc), nat(bonacc), T1.ap, op=ALU.add), reads=bk + [T1.k()], writes=bk)
        P.op("dve", lambda e: e.tensor_tensor(AA.ap, AA.ap, KK.ap, op=ALU.mult), reads=[AA.k(), KK.k()], writes=[AA.k()])
        yield
        if PH <= 2:
            return
        P.op("dve", lambda e: e.tensor_tensor_scan(CUM.ap, k.rmask.ap[:, 0:BLK], LW.ap, 0.0, op0=ALU.mult, op1=ALU.add), reads=[k.rmask.k(), LW.k()], writes=[CUM.k()])
        P.op("dve", lambda e: e.tensor_tensor(LW.ap, CUM.ap, LW.ap, op=ALU.subtract), reads=[CUM.k(), LW.k()], writes=[LW.k()])
        P.op("act", lambda e: e.activation(T1.ap, CUM.ap, AF.Exp), reads=[CUM.k()], writes=[T1.k()])
        P.op("dve", lambda e: e.tensor_tensor(XR.ap, XR.ap, T1.ap, op=ALU.mult), reads=[XR.k(), T1.k()], writes=[XR.k()])
        P.op("act", lambda e: e.activation(SQ.ap, CUM.ap, AF.Exp, scale=-1.0), reads=[CUM.k()], writes=[SQ.k()])
        P.op("dve", lambda e: e.tensor_tensor(AA.ap, AA.ap, SQ.ap, op=ALU.mult), reads=[AA.k(), SQ.k()], writes=[AA.k()])
        P.op("dve", lambda e: e.tensor_tensor(XK.ap, XK.ap, SQ.ap, op=ALU.mult), reads=[XK.k(), SQ.k()], writes=[XK.k()])
        P.op("act", lambda e: e.activation(T1.ap, LW.ap, AF.Exp), reads=[LW.k()], writes=[T1.k()])
        P.op("dve", lambda e: e.scalar_tensor_tensor(KK.ap, KK.ap, -1.0, T1.ap, op0=ALU.mult, op1=ALU.mult), reads=[KK.k(), T1.k()], writes=[KK.k()])
        AR = sb["AR"]
        for src_, dst_ in ((XK, XKb), (AA, AAb), (XV, XVb)):
            P.op("act", lambda e, src_=src_, dst_=dst_: e.copy(dst_.ap, src_.ap), reads=[src_.k()], writes=[dst_.k()])
        P.op("act", lambda e: e.copy(AR.ap[:, :, 0:128], KK.ap.rearrange("p (c t) -> p c t", t=128)), reads=[KK.k()], writes=[AR.k()])
        P.op("act", lambda e: e.copy(AR.ap[:, :, 128:256], XR.ap.rearrange("p (c t) -> p c t", t=128)), reads=[XR.k()], writes=[AR.k()])
        P.op("act", lambda e: e.activation(PCc.ap, CUM.ap.rearrange("p (c t) -> p c t", t=128)[:, :, 127], AF.Exp), reads=[CUM.k()], writes=[PCc.k()])
        for ch in range(BLK // 128):
            cs = slice(ch * 128, (ch + 1) * 128)
            P.op("act", lambda e, cs=cs, ch=ch: e.activation(BH.ap[:, cs], AA.ap[:, cs], AF.Copy, scale=PCc.ap[:, ch:ch + 1]), reads=[AA.k(), PCc.k()], writes=[BH.k()])
            P.op("act", lambda e, cs=cs, ch=ch: e.activation(KH.ap[:, cs], XK.ap[:, cs], AF.Copy, scale=PCc.ap[:, ch:ch + 1]), reads=[XK.k(), PCc.k()], writes=[KH.k()])
        yield
        if PH <= 3:
            return
        def _chunk(ch):
            cs = slice(ch * 128, (ch + 1) * 128)
            AM, Mb, MTb, Q, RHS, SAz, Vz, BHt, KHt = sb["AM"], sb["M"], sb["MT"], sb["Q"], sb["RHS"], sb["SAz"], sb["Vz"], sb["BHt"], sb["KHt"]
            for x in range(2):
                hs = slice(64 * x, 64 * x + 64)
                bx = B0 + x
                for q, lh in enumerate((AAb, XKb)):
                    P.op("pe", lambda e, lh=lh, q=q, hs=hs, bx=bx: e.matmul(ps[bx][:, q * 256:(q + 1) * 256], lh.ap[hs, cs], sb["AR"].ap[hs, ch, :], start=True, stop=True),
                         reads=[lh.k(), sb["AR"].k()], writes=psk(bx, q * 256, (q + 1) * 256), serial=True)
                P.op("pe", lambda e, hs=hs, x=x: e.matmul(ps[B2][:, x * 128:(x + 1) * 128], sb["AR"].ap[hs, ch, 0:128], AAb.ap[hs, cs], start=True, stop=True),
                     reads=[sb["AR"].k(), AAb.k()], writes=psk(B2, x * 128, (x + 1) * 128), serial=True)
                P.op("dve", lambda e, x=x, bx=bx: e.tensor_tensor(AM[x].ap, ps[bx][:, :], k.mask4.ap, op=ALU.mult), reads=psk(bx, 0, 512) + [k.mask4.k()], writes=[AM[x].k()])
                P.op("dve", lambda e, x=x: e.tensor_tensor(MTb[x][0].ap, ps[B2][:, x * 128:(x + 1) * 128], k.trils.ap, op=ALU.mult), reads=psk(B2, x * 128, (x + 1) * 128) + [k.trils.k()], writes=[MTb[x][0].k()])
            yield
            if PH <= 4:
                return
            pbt = ps[B3][:, 0:192].bitcast(BF16)
            for q, src in enumerate((XVb, BH, KH)):
                P.op("pe", lambda e, q=q, src=src: e.transpose(pbt[:, q * 128:(q + 1) * 128], src.ap[:, cs], k.ident16.ap), reads=[src.k(), k.ident16.k()], writes=psk(B3, 0, 192))
            P.op("act", lambda e: e.copy(Vz[0].ap[:, 0:64], pbt[:, 0:64]), reads=psk(B3, 0, 192), writes=[Vz[0].k()])
            P.op("act", lambda e: e.copy(Vz[1].ap[:, 64:128], pbt[:, 64:128]), reads=psk(B3, 0, 192), writes=[Vz[1].k()])
            P.op("dve", lambda e: e.tensor_copy(BHt.ap, pbt[:, 128:256]), reads=psk(B3, 0, 192), writes=[BHt.k()])
            P.op("dve", lambda e: e.tensor_copy(KHt.ap, pbt[:, 256:384]), reads=psk(B3, 0, 192), writes=[KHt.k()])
            Bq = sb["B"]
            for x in range(2):
                bx = B0 + x
                P.op("pe", lambda e, bx=bx, x=x: e.matmul(ps[bx][:, 0:128], AM[x].ap[:, 0:128], MTb[x][0].ap, start=True, stop=True), reads=[AM[x].k(), MTb[x][0].k()], writes=psk(bx, 0, 128))
                P.op("pe", lambda e, bx=bx, x=x: e.matmul(ps[bx][:, 128:256], MTb[x][0].ap, AM[x].ap[:, 0:128], start=True, stop=True), reads=[AM[x].k(), MTb[x][0].k()], writes=psk(bx, 128, 256))
                P.op("act", lambda e, bx=bx, x=x: e.copy(Bq[x][1].ap[:, 0:256], ps[bx][:, 0:256]), reads=psk(bx, 0, 256), writes=[Bq[x][1].k()])
                P.op("dve", lambda e, x=x: e.tensor_tensor(Bq[x][1].ap[:, 256:384], AM[x].ap[:, 0:128], k.ident.ap, op=ALU.add), reads=[AM[x].k(), k.ident.k()], writes=[Bq[x][1].k()])
            yield
            for lev in range(1, 7):
                cur, nxt = lev % 2, 1 - lev % 2
                for x in range(2):
                    bx = B0 + x
                    Bc, Bn = Bq[x][cur], Bq[x][nxt]
                    if lev < 6:
                        P.op("pe", lambda e, bx=bx, Bc=Bc: e.matmul(ps[bx][:, 0:128], Bc.ap[:, 128:256], Bc.ap[:, 0:128], start=True, stop=True), reads=[Bc.k()], writes=psk(bx, 0, 128))
                        P.op("pe", lambda e, bx=bx, Bc=Bc: e.matmul(ps[bx][:, 128:384], Bc.ap[:, 0:128], Bc.ap[:, 128:384], start=True, stop=True), reads=[Bc.k()], writes=psk(bx, 128, 384))
                        P.op("act", lambda e, bx=bx, Bn=Bn: e.copy(Bn.ap[:, 0:256], ps[bx][:, 0:256]), reads=psk(bx, 0, 384), writes=[Bn.k()])
                        P.op("dve", lambda e, bx=bx, Bc=Bc, Bn=Bn: e.tensor_tensor(Bn.ap[:, 256:384], Bc.ap[:, 256:384], ps[bx][:, 256:384], op=ALU.add), reads=psk(bx, 0, 384) + [Bc.k()], writes=[Bn.k()])
                    else:
                        P.op("pe", lambda e, bx=bx, Bc=Bc: e.matmul(ps[bx][:, 256:384], Bc.ap[:, 0:128], Bc.ap[:, 256:384], start=True, stop=True), reads=[Bc.k()], writes=psk(bx, 256, 384))
                        P.op("dve", lambda e, bx=bx, Bc=Bc, x=x: e.tensor_tensor(sb["Q16"][x].ap, Bc.ap[:, 256:384], ps[bx][:, 256:384], op=ALU.add), reads=psk(bx, 0, 384) + [Bc.k()], writes=[sb["Q16"][x].k()])
                yield
            for x in range(2):
                hs = slice(64 * x, 64 * x + 64)
                P.op("pe", lambda e, hs=hs: e.matmul(ps[B2][:, 256 + hs.start:256 + hs.stop], sb["AR"].ap[hs, ch, 0:128], T16.ap[hs, hs], start=True, stop=False), reads=[sb["AR"].k(), T16.k()], writes=psk(B2, 256, 384), serial=True)
                P.op("pe", lambda e, hs=hs, x=x: e.matmul(ps[B2][:, 256 + hs.start:256 + hs.stop], AM[x].ap[:, 256:384], Vz[x].ap[:, hs], start=False, stop=True), reads=[AM[x].k(), Vz[x].k()], writes=psk(B2, 256, 384))
            P.op("act", lambda e: e.copy(RHS.ap, ps[B2][:, 256:384]), reads=psk(B2, 256, 384), writes=[RHS.k()])
            for x in range(2):
                hs = slice(64 * x, 64 * x + 64)
                P.op("pe", lambda e, hs=hs, x=x: e.matmul(ps[B2][:, 384 + hs.start:384 + hs.stop], sb["Q16"][x].ap, RHS.ap[:, hs], start=True, stop=True), reads=[sb["Q16"][x].k(), RHS.k()], writes=psk(B2, 384, 512))
                P.op("act" if x == 0 else "dve", (lambda e, hs=hs, x=x: e.copy(SAz[x].ap[:, hs], ps[B2][:, 384 + hs.start:384 + hs.stop])) if x == 0 else
                     (lambda e, hs=hs, x=x: e.tensor_copy(SAz[x].ap[:, hs], ps[B2][:, 384 + hs.start:384 + hs.stop])), reads=psk(B2, 384, 512), writes=[SAz[x].k()])
            yield
            if PH <= 6:
                return
            ops_ = []
            for x in range(2):
                hs = slice(64 * x, 64 * x + 64)
                ops_.append((T16.ap[hs, :], sb["AR"].ap[hs, ch, 128:256], [T16.k(), sb["AR"].k()]))
                ops_.append((SAz[x].ap, AM[x].ap[:, 128:256], [SAz[x].k(), AM[x].k()]))
                ops_.append((Vz[x].ap, AM[x].ap[:, 384:512], [Vz[x].k(), AM[x].k()]))
            for q, (lh, rh, rd) in enumerate(ops_):
                P.op("pe", lambda e, lh=lh, rh=rh, q=q: e.matmul(ps[B3][:, 384:512], lh, rh, start=(q == 0), stop=(q == len(ops_) - 1)), reads=rd, writes=psk(B3, 384, 512), serial=(q % 3 == 0))
            cn = nchunks[ch] if d == 0 else nchunks[len(nchunks) - 1 - ch]
            if d == 0:
                ydst = yacc.ap[:, cn * 128:(cn + 1) * 128]
            else:
                ydst = rev_ap(yacc.ap[:, cn * 128:(cn + 1) * 128], 128)
            P.op("dve", lambda e, ydst=ydst: e.tensor_tensor(ydst, ydst, ps[B3][:, 384:512], op=ALU.add), reads=psk(B3, 384, 512) + [yacc.k(cn)], writes=[yacc.k(cn)])
            for x in range(2):
                hs = slice(64 * x, 64 * x + 64)
                P.op("pe", lambda e, hs=hs, x=x: e.matmul(ps[B2][:, hs], BHt.ap, SAz[x].ap[:, hs], start=True, stop=False), reads=[BHt.k(), SAz[x].k()], writes=psk(B2, 0, 128))
                P.op("pe", lambda e, hs=hs, x=x: e.matmul(ps[B2][:, hs], KHt.ap, Vz[x].ap[:, hs], start=False, stop=True), reads=[KHt.k(), Vz[x].k()], writes=psk(B2, 0, 128))
            for x in range(2):
                hs = slice(64 * x, 64 * x + 64)
                P.op("dve", lambda e, hs=hs, ch=ch: e.scalar_tensor_tensor(T.ap[hs, hs], T.ap[hs, hs], PCc.ap[hs, ch:ch + 1], ps[B2][hs, hs], op0=ALU.mult, op1=ALU.add),
                     reads=[T.k(), PCc.k()] + psk(B2, 0, 128), writes=[T.k()])
            P.op("act", lambda e: e.copy(T16.ap, T.ap), reads=[T.k()], writes=[T16.k()])
            yield
            if PH <= 7:
                return
        for ch in range(BLK // 128):
            yield from _chunk(ch)

    for bi in range(NBLK):
        yield from _blk(bi)


FB = 256
NFB = DFF // FB
NFC = DFF // 128
W2G = 2


def moe_stage(k, l):
    P, A, ps, IN = k.P, k.A, k.ps, k.IN
    m0 = A.mark()
    k.ohb = A.alloc("ohb", (2048,))
    P.dma("act", k.ohb.ap[0:16, :], IN["c_ohb"].ap(), writes=[k.ohb.k()], semkey="c4")
    x2b = A.alloc("x2b", (NT, D), BF16)
    aff = A.alloc("aff", (NT, NE))
    pm = A.alloc("pm", (NT, NE))
    pmT = A.alloc("pmT", (S,))
    m1 = A.mark()
    gb = A.alloc("gb2", (D,)); junk = A.alloc("junk2", (D,)); ss = A.alloc("ss2", (NT,)); rstd = A.alloc("rstd2", (NT,))
    x2f = [A.alloc(f"x2f{i}", (D,)) for i in range(2)]
    x2T = [A.alloc(f"x2T{i}", (8, 128)) for i in range(2)]
    rsb = A.alloc("rsb", (8, NE)); sm = A.alloc("smx", (NT, 4))
    affT = A.alloc("affT", (S,))
    P.dma("sp", gb.ap, IN["ln2_g"].ap()[l].partition_broadcast(128), writes=[gb.k()], semkey="gb")
    P.dma("act", rsb.ap, IN["router"].ap()[l].rearrange("(c p) e -> p c e", p=128), writes=[rsb.k()], semkey="rsb")
    P.op("pool", lambda e: e.memset(ss.ap, 0.0), writes=[ss.k(i) for i in range(NT)])
    P.op("pool", lambda e: e.memset(sm.ap, 0.0), writes=[sm.k()])
    for i in range(NT):
        P.op("act", lambda e, i=i: e.activation(junk.ap, k.xres.ap[:, i, :], AF.Square, accum_out=ss.ap[:, i:i + 1]),
             reads=[k.xres.k(i)], writes=[junk.k(), ss.k(i)])
    P.op("act", lambda e: e.activation(rstd.ap, ss.ap, AF.Sqrt, scale=1.0 / D, bias=k.eps6.ap[:, 0:1]),
         reads=[ss.k(i) for i in range(NT)] + [k.eps6.k()], writes=[rstd.k()])
    P.op("dve", lambda e: e.reciprocal(rstd.ap, rstd.ap), reads=[rstd.k()], writes=[rstd.k()])
    for i in range(NT):
        xf = x2f[i % 2]; xt = x2T[i % 2]
        P.op("dve", lambda e, i=i, xf=xf: e.scalar_tensor_tensor(xf.ap, k.xres.ap[:, i, :], rstd.ap[:, i:i + 1], gb.ap, op0=ALU.mult, op1=ALU.mult),
             reads=[k.xres.k(i), rstd.k(), gb.k()], writes=[xf.k()])
        P.op("act", lambda e, i=i, xf=xf: e.copy(x2b.ap[:, i, :], xf.ap), reads=[xf.k()], writes=[x2b.k(i)])
        for hb_ in range(2):
            b = 2 * (i % 2) + hb_
            for c in range(4):
                cc = hb_ * 4 + c
                P.op("pe", lambda e, b=b, c=c, cc=cc, xf=xf: e.transpose(ps[b][:, c * 128:(c + 1) * 128], xf.ap[:, cc * 128:(cc + 1) * 128], k.ident.ap),
                     reads=[xf.k(), k.ident.k()], writes=[f"ps{b}"])
            P.op("act" if hb_ == 0 else "dve",
                 (lambda e, b=b, hb_=hb_, xt=xt: e.copy(xt.ap[:, hb_ * 4:hb_ * 4 + 4, :], ps[b][:, :].rearrange("p (c t) -> p c t", c=4))) if hb_ == 0 else
                 (lambda e, b=b, hb_=hb_, xt=xt: e.tensor_copy(xt.ap[:, hb_ * 4:hb_ * 4 + 4, :], ps[b][:, :].rearrange("p (c t) -> p c t", c=4))),
                 reads=[f"ps{b}"], writes=[xt.k(hb_)])
        lb = 4 + i % 2
        for c in range(8):
            P.op("pe", lambda e, c=c, lb=lb, xt=xt: e.matmul(ps[lb][:, 0:NE], xt.ap[:, c, :], rsb.ap[:, c, :], start=(c == 0), stop=(c == 7)),
                 reads=[xt.k(0), xt.k(1), rsb.k()], writes=[f"ps{lb}"])
        P.op("dve", lambda e, i=i, lb=lb: e.tensor_reduce(sm.ap[:, i, 0:1], ps[lb][:, 0:NE], axis=AX.X, op=ALU.max), reads=[f"ps{lb}"], writes=[sm.k()])
        P.op("dve", lambda e, i=i: e.tensor_scalar(sm.ap[:, i, 0:1], sm.ap[:, i, 0:1], -1.0, None, op0=ALU.mult), reads=[sm.k()], writes=[sm.k()])
        P.op("act", lambda e, i=i, lb=lb: e.activation(aff.ap[:, i, :], ps[lb][:, 0:NE], AF.Exp, bias=sm.ap[:, i, 0:1], accum_out=sm.ap[:, i, 1:2]),
             reads=[f"ps{lb}", sm.k()], writes=[aff.k(), sm.k()])
        P.op("dve", lambda e, i=i: e.reciprocal(sm.ap[:, i, 2:3], sm.ap[:, i, 1:2]), reads=[sm.k()], writes=[sm.k()])
        P.op("dve", lambda e, i=i: e.tensor_scalar(aff.ap[:, i, :], aff.ap[:, i, :], sm.ap[:, i, 2:3], None, op0=ALU.mult), reads=[aff.k(), sm.k()], writes=[aff.k()])
        tb = 6 + (i // 4) % 2
        P.op("pe", lambda e, i=i, tb=tb: e.transpose(ps[tb][0:NE, (i % 4) * 128:(i % 4 + 1) * 128], aff.ap[:, i, :], k.ident.ap), reads=[aff.k(), k.ident.k()], writes=[f"ps{tb}"])
        if i % 4 == 3:
            P.op("act", lambda e, i=i, tb=tb: e.copy(affT.ap[0:NE, (i - 3) * 128:(i + 1) * 128], ps[tb][0:NE, :]), reads=[f"ps{tb}"], writes=[affT.k()])
    bs = A.alloc("bs", (8,)); bjunk = A.alloc("bjunk", (S,))
    LO, HI, MID, CNT, GE, D1 = (bs.ap[0:NE, j:j + 1] for j in range(6))
    P.op("pool", lambda e: e.memset(bs.ap, 0.0), writes=[bs.k()])
    P.op("pool", lambda e: e.memset(bs.ap[:, 1:2], 1.0), writes=[bs.k()])
    for it in range(30):
        P.op("dve", lambda e: e.tensor_tensor(MID, LO, HI, op=ALU.add), reads=[bs.k()], writes=[bs.k()])
        P.op("dve", lambda e: e.tensor_scalar(MID, MID, 0.5, None, op0=ALU.mult), reads=[bs.k()], writes=[bs.k()])
        P.op("dve", lambda e: e.tensor_scalar(bjunk.ap[0:NE, :], affT.ap[0:NE, :], MID, None, op0=ALU.is_gt, op1=ALU.add, accum_out=CNT),
             reads=[bs.k(), affT.k()], writes=[bs.k(), bjunk.k()])
        P.op("dve", lambda e: e.tensor_scalar(GE, CNT, float(CAP) - 0.5, None, op0=ALU.is_gt), reads=[bs.k()], writes=[bs.k()])
        P.op("dve", lambda e: e.tensor_tensor(D1, MID, LO, op=ALU.subtract), reads=[bs.k()], writes=[bs.k()])
        P.op("dve", lambda e: e.scalar_tensor_tensor(LO, D1, GE, LO, op0=ALU.mult, op1=ALU.add), reads=[bs.k()], writes=[bs.k()])
        P.op("dve", lambda e: e.tensor_tensor(D1, HI, MID, op=ALU.subtract), reads=[bs.k()], writes=[bs.k()])
        P.op("dve", lambda e: e.scalar_tensor_tensor(HI, D1, GE, MID, op0=ALU.mult, op1=ALU.add), reads=[bs.k()], writes=[bs.k()])
    dgt = A.alloc("dgt", (NE,)); thrb = A.alloc("thrb", (NE,))
    P.op("dve", lambda e: e.tensor_scalar(dgt.ap[0:NE, :], k.ident.ap[0:NE, 0:NE], LO, None, op0=ALU.mult), reads=[bs.k(), k.ident.k()], writes=[dgt.k()])
    P.op("pe", lambda e: e.matmul(ps[0][:, 0:NE], k.ones32.ap[0:NE, :], dgt.ap[0:NE, :], start=True, stop=True), reads=[k.ones32.k(), dgt.k()], writes=["ps0"])
    P.op("act", lambda e: e.copy(thrb.ap, ps[0][:, 0:NE]), reads=["ps0"], writes=[thrb.k()])
    mk16 = A.alloc("mk16", (NT, NE), BF16); mk32 = A.alloc("mk32", (NT, NE)); base = A.alloc("basec", (NE,))
    P.op("pool", lambda e: e.memset(base.ap, 0.0), writes=[base.k()])
    for i in range(NT):
        P.op("dve", lambda e, i=i: e.tensor_tensor(mk32.ap[:, i, :], aff.ap[:, i, :], thrb.ap, op=ALU.is_gt), reads=[aff.k(), thrb.k()], writes=[mk32.k(i)])
        P.op("act", lambda e, i=i: e.copy(mk16.ap[:, i, :], mk32.ap[:, i, :]), reads=[mk32.k(i)], writes=[mk16.k(i)])
        b = i % 2
        P.op("pe", lambda e, i=i, b=b: e.matmul(ps[b][:, 0:NE], k.trius16.ap, mk16.ap[:, i, :], start=True, stop=True), reads=[k.trius16.k(), mk16.k(i)], writes=[f"ps{b}"])
        P.op("pe", lambda e, i=i, b=b: e.matmul(ps[b][:, 128:128 + NE], k.ones16.ap, mk16.ap[:, i, :], start=True, stop=True), reads=[k.ones16.k(), mk16.k(i)], writes=[f"ps{b}"])
        P.op("dve", lambda e, i=i, b=b: e.scalar_tensor_tensor(pm.ap[:, i, :], ps[b][:, 0:NE], 1.0, base.ap, op0=ALU.add, op1=ALU.add), reads=[f"ps{b}", base.k()], writes=[pm.k(i)])
        P.op("dve", lambda e, i=i: e.tensor_tensor(pm.ap[:, i, :], pm.ap[:, i, :], mk32.ap[:, i, :], op=ALU.mult), reads=[pm.k(i), mk32.k(i)], writes=[pm.k(i)])
        P.op("dve", lambda e, i=i: e.tensor_scalar(pm.ap[:, i, :], pm.ap[:, i, :], -1.0, None, op0=ALU.add), reads=[pm.k(i)], writes=[pm.k(i)])
        P.op("dve", lambda e, b=b: e.tensor_tensor(base.ap, base.ap, ps[b][:, 128:128 + NE], op=ALU.add), reads=[f"ps{b}", base.k()], writes=[base.k()])
        tb = 6 + (i // 4) % 2
        P.op("pe", lambda e, i=i, tb=tb: e.transpose(ps[tb][0:NE, (i % 4) * 128:(i % 4 + 1) * 128], pm.ap[:, i, :], k.ident.ap), reads=[pm.k(i), k.ident.k()], writes=[f"ps{tb}"])
        if i % 4 == 3:
            P.op("act", lambda e, i=i, tb=tb: e.copy(pmT.ap[0:NE, (i - 3) * 128:(i + 1) * 128], ps[tb][0:NE, :]), reads=[f"ps{tb}"], writes=[pmT.k()])
    A.release(m1)
    SelE = A.alloc("SelE", (NT, CAP), BF16)
    SelT = [A.alloc(f"SelT{i}", (2, S), BF16) for i in range(2)]
    xgT = A.alloc("xgT", (8, CAP), BF16); actT = A.alloc("actT", (NFC, CAP), BF16)
    s1 = [A.alloc(f"s1_{i}", (CAP,)) for i in range(2)]
    ysb = A.alloc("ysb", (2, D), BF16)
    w1b = [A.alloc(f"w1b{i}", (8, FB), BF16) for i in range(2)]
    w3b = [A.alloc(f"w3b{i}", (8, FB), BF16) for i in range(2)]
    w2b = [A.alloc(f"w2b{i}", (W2G, D), BF16) for i in range(2)]
    pmT16 = A.alloc("pmT16", (S,), BF16); ohb16 = A.alloc("ohb16", (2048,), BF16)
    P.op("act", lambda e: e.copy(pmT16.ap[0:NE, :], pmT.ap[0:NE, :]), reads=[pmT.k()], writes=[pmT16.k()])
    P.op("act", lambda e: e.copy(ohb16.ap[0:NE, :], k.ohb.ap[0:NE, :]), reads=[k.ohb.k()], writes=[ohb16.k()])
    state = {"wn": 0}

    def sel_and_gather(ex):
        st_ = SelT[ex % 2]
        for i in range(NT):
            P.op("dve", lambda e, i=i, ex=ex: e.tensor_scalar(SelE.ap[:, i, :], k.iota256.ap, pm.ap[:, i, ex:ex + 1], None, op0=ALU.is_equal),
                 reads=[k.iota256.k(), pm.k(i)], writes=[SelE.k(i)])
        for st in range(2):
            for tg in range(4):
                P.op("pe", lambda e, ex=ex, tg=tg: e.matmul(ps[6][:, :], ohb16.ap[0:NE, ex * 128:(ex + 1) * 128], pmT16.ap[0:NE, tg * 512:(tg + 1) * 512], start=True, stop=True),
                     reads=[ohb16.k(), pmT16.k()], writes=["ps6"])
                P.op("dve", lambda e, st=st, tg=tg, st_=st_: e.tensor_scalar(st_.ap[:, st, tg * 512:(tg + 1) * 512], ps[6][:, :], k.slotidx.ap[:, st:st + 1], None, op0=ALU.is_equal),
                     reads=["ps6", k.slotidx.k()], writes=[st_.k(st, tg)])
        for c in range(8):
            b = 6 + c % 2
            for i in range(NT):
                P.op("pe", lambda e, c=c, i=i, b=b: e.matmul(ps[b][:, 0:CAP], x2b.ap[:, i, c * 128:(c + 1) * 128], SelE.ap[:, i, :], start=(i == 0), stop=(i == NT - 1)),
                     reads=[x2b.k(i), SelE.k(i)], writes=[f"ps{b}"])
            P.op("act", lambda e, c=c, b=b: e.copy(xgT.ap[:, c, :], ps[b][:, 0:CAP]), reads=[f"ps{b}"], writes=[xgT.k(c)])

    def ffn1(ex, fb):
        r = state["wn"] % 2; state["wn"] += 1
        w1v = IN["e_w1"].ap()[l][ex].rearrange("(c p) f -> p c f", p=128)
        w3v = IN["e_w3"].ap()[l][ex].rearrange("(c p) f -> p c f", p=128)
        w2v = IN["e_w2"].ap()[l][ex].rearrange("(g p) d -> p g d", p=128)
        P.dma("pool", w1b[r].ap, w1v[:, :, fb * FB:(fb + 1) * FB], writes=[w1b[r].k()], semkey=("w1", r))
        P.dma("pool", w3b[r].ap, w3v[:, :, fb * FB:(fb + 1) * FB], writes=[w3b[r].k()], semkey=("w3", r))
        P.dma("pool", w2b[r].ap, w2v[:, fb * W2G:(fb + 1) * W2G, :], writes=[w2b[r].k()], semkey=("w2", r))
        for q in range(FB // 128):
            fc = fb * (FB // 128) + q
            b = fc % 2
            for c in range(8):
                P.op("pe", lambda e, c=c, q=q, b=b, r=r: e.matmul(ps[b][:, 0:CAP], w1b[r].ap[:, c, q * 128:(q + 1) * 128], xgT.ap[:, c, :], start=(c == 0), stop=(c == 7)),
                     reads=[w1b[r].k(), xgT.k(c)], writes=[f"ps{b}"])
            for c in range(8):
                P.op("pe", lambda e, c=c, q=q, b=b, r=r: e.matmul(ps[b][:, CAP:2 * CAP], w3b[r].ap[:, c, q * 128:(q + 1) * 128], xgT.ap[:, c, :], start=(c == 0), stop=(c == 7)),
                     reads=[w3b[r].k(), xgT.k(c)], writes=[f"ps{b}"])
            P.op("act", lambda e, b=b: e.activation(s1[b].ap, ps[b][:, 0:CAP], AF.Silu), reads=[f"ps{b}"], writes=[s1[b].k()])
            P.op("dve", lambda e, b=b, fc=fc: e.tensor_tensor(actT.ap[:, fc, :], s1[b].ap, ps[b][:, CAP:2 * CAP], op=ALU.mult), reads=[f"ps{b}", s1[b].k()], writes=[actT.k(fc)])
        return r

    def ffn2(fb, r):
        for q in range(W2G):
            fc = fb * W2G + q
            for st in range(2):
                for half in range(2):
                    yb_ = 2 + st * 2 + half
                    P.op("pe", lambda e, fc=fc, q=q, st=st, half=half, yb_=yb_, r=r: e.matmul(ps[yb_][:, :], actT.ap[:, fc, st * 128:(st + 1) * 128], w2b[r].ap[:, q, half * 512:(half + 1) * 512],
                                                                                       start=(fc == 0), stop=(fc == NFC - 1)),
                         reads=[actT.k(fc), w2b[r].k()], writes=[f"ps{yb_}"])

    def scatter(ex):
        st_ = SelT[ex % 2]
        for i in range(NT):
            for half in range(2):
                b = 6 + (2 * i + half) % 2
                for st in range(2):
                    P.op("pe", lambda e, i=i, half=half, st=st, b=b, st_=st_: e.matmul(ps[b][:, :], st_.ap[:, st, i * 128:(i + 1) * 128], ysb.ap[:, st, half * 512:(half + 1) * 512], start=(st == 0), stop=(st == 1)),
                         reads=[st_.k(st, i // 4), ysb.k(st, half)], writes=[f"ps{b}"])
                P.op("dve", lambda e, i=i, half=half, b=b, ex=ex: e.scalar_tensor_tensor(k.xres.ap[:, i, half * 512:(half + 1) * 512], ps[b][:, :], aff.ap[:, i, ex:ex + 1],
                                                                                     k.xres.ap[:, i, half * 512:(half + 1) * 512], op0=ALU.mult, op1=ALU.add),
                     reads=[f"ps{b}", aff.k(), k.xres.k(i)], writes=[k.xres.k(i)])

    sel_and_gather(0)
    for ex in range(NE):
        rprev = ffn1(ex, 0)
        for fb in range(1, NFB):
            rcur = ffn1(ex, fb)
            ffn2(fb - 1, rprev)
            rprev = rcur
        ffn2(NFB - 1, rprev)
        for st in range(2):
            for half in range(2):
                yb_ = 2 + st * 2 + half
                P.op("act", lambda e, st=st, half=half, yb_=yb_: e.copy(ysb.ap[:, st, half * 512:(half + 1) * 512], ps[yb_][:, :]), reads=[f"ps{yb_}"], writes=[ysb.k(st, half)])
        if ex + 1 < NE:
            sel_and_gather(ex + 1)
        scatter(ex)
    A.release(m0)


_INPUT_NAMES = ["rel_bias", "ln1_g", "w_in", "w_out", "rk_mu", "rk_w0", "rk_w2", "rk_a0", "rk_a2", "rk_kk", "rk_ka",
                "rk_rk", "rk_g2", "rk_ln_g", "rk_ln_b", "ml_conv_w", "ml_conv_b", "ml_ib", "ml_fb", "ml_ln_g",
                "ln2_g", "router", "e_w1", "e_w3", "e_w2", "final_g"]


def make_in_maps(inputs, cores, names=None):
    consts = host_consts()
    shared = {n: np.ascontiguousarray(np.asarray(inputs[n], dtype=np.float32)) for n in _INPUT_NAMES if names is None or n in names}
    shared.update({n: v for n, v in consts.items() if names is None or n in names})
    if names is None or "c_pc" in names or "c_wx" in names:
        shared.update(host_rwkv_layout(inputs))
    x = np.asarray(inputs["x"], dtype=np.float32)
    maps = []
    for c in cores:
        m = dict(shared)
        m["x"] = np.ascontiguousarray(x[c])
        maps.append(m)
    return maps


def kernel(**inputs):
    nc, k = build()
    in_maps = make_in_maps(inputs, list(range(8)), set(k.IN.keys()))
    res = run_bass_kernel_spmd(nc, in_maps, core_ids=list(range(8)))
    return np.stack([np.asarray(r["out"], dtype=np.float32) for r in res.results], axis=0)
```

```python
from contextlib import ExitStack
import numpy as np
import concourse.bass as bass
import concourse.mybir as mybir

F32 = mybir.dt.float32
BF16 = mybir.dt.bfloat16
I32 = mybir.dt.int32
ALU = mybir.AluOpType
AF = mybir.ActivationFunctionType
AX = mybir.AxisListType

ENGS = ("pe", "act", "dve", "pool", "sp")


class Prog:
    def __init__(self, nc, strict_same_engine=False):
        self.nc = nc
        self.same_dist = 3
        self.ops = {e: [] for e in ENGS}
        self.keys = {}
        self.dma_cnt = {}
        self.es = ExitStack()
        self.n_ops = 0

    def _deps(self, reads, writes):
        deps = []
        for k in reads:
            deps.extend((ev, True) for ev in self._st(k)["w"])
        for k in writes:
            st = self._st(k)
            deps.extend((ev, True) for ev in st["w"])
            deps.extend((ev, False) for ev in st["r"].values())
        return deps

    def _st(self, k):
        st = self.keys.get(k)
        if st is None:
            inh = getattr(self, "inherit", {}).get(k[0], []) if isinstance(k, tuple) else []
            st = self.keys[k] = {"w": list(inh), "r": {}}
        return st

    def _record(self, ev, reads, writes):
        for k in reads:
            st = self._st(k)
            st["r"][(ev[0], ev[1])] = ev
        for k in writes:
            st = self._st(k)
            st["w"] = [ev]
            st["r"] = {}

    @staticmethod
    def _norm(reads, writes):
        r2, w2 = [], []
        for k in writes:
            if isinstance(k, str) and k.startswith("ps"):
                k = k.split("q")[0]
            if k not in w2:
                w2.append(k)
        for k in reads:
            if isinstance(k, str) and k.startswith("ps"):
                k = k.split("q")[0]
                if k not in w2:
                    w2.append(k)
            elif k not in r2:
                r2.append(k)
        return r2, w2

    def op(self, eng, fn, reads=(), writes=(), serial=False):
        reads, writes = self._norm(reads, writes)
        deps = self._deps(reads, writes)
        idx = len(self.ops[eng])
        if serial and idx > 0:
            deps.append((("eng", eng, idx - 1), "force"))
        ev = ("eng", eng, idx)
        self.ops[eng].append(dict(fn=fn, deps=deps, kind="c"))
        self._record(ev, reads, writes)
        self.n_ops += 1
        return ev

    def dma(self, q, out, in_, reads=(), writes=(), semkey=None, **kw):
        assert semkey is not None
        reads, writes = self._norm(reads, writes)
        deps = self._deps(reads, writes)
        c = self.dma_cnt.get(semkey, 0) + 1
        self.dma_cnt[semkey] = c
        if c > 1:
            deps.append((("dma", semkey, c - 1), "force"))
        ev = ("dma", semkey, c)
        fn = lambda e, out=out, in_=in_, kw=kw: e.dma_start(out=out, in_=in_, **kw)
        self.ops[q].append(dict(fn=fn, deps=deps, kind="d", semkey=semkey))
        self._record(ev, reads, writes)
        self.n_ops += 1
        return ev

    def emit(self, final_wait_events=()):
        nc = self.nc
        signal = {e: set() for e in ENGS}
        for e in ENGS:
            for i, o in enumerate(self.ops[e]):
                nd = []
                for (d, is_w) in o["deps"]:
                    if d[0] == "eng" and d[1] == e and is_w != "force":
                        if e == "pe" or not is_w or (i - d[2]) > self.same_dist:
                            continue
                    nd.append(d)
                    if d[0] == "eng":
                        signal[d[1]].add(d[2])
                o["deps"] = nd
        for d in final_wait_events:
            if d[0] == "eng":
                signal[d[1]].add(d[2])
        rank = {}
        for e in ENGS:
            r = 0
            for i in range(len(self.ops[e])):
                if i in signal[e]:
                    r += 1
                    rank[(e, i)] = r
        self.max_rank = {e: max([v for (ee, i), v in rank.items() if ee == e] + [0]) for e in ENGS}
        es = self.es
        sem_e = {e: es.enter_context(nc.semaphore("s_" + e)) for e in ENGS}
        sem_d = {k: es.enter_context(nc.semaphore("d_%d" % i)) for i, k in enumerate(self.dma_cnt)}
        self.n_sems = len(sem_e) + len(sem_d)

        def lower(ev):
            if ev[0] == "eng":
                return ("e_" + ev[1], sem_e[ev[1]], rank[(ev[1], ev[2])])
            return ("d_" + str(ev[1]), sem_d[ev[1]], 16 * ev[2])

        block = es.enter_context(nc.Block())
        engobj = {"pe": block.tensor, "act": block.scalar, "dve": block.vector,
                  "pool": block.gpsimd, "sp": block.sync}
        fw = self

        def make(e):
            def body(eng):
                known = {}
                for i, o in enumerate(fw.ops[e]):
                    need = {}
                    for d in o["deps"]:
                        nm, s, v = lower(d)
                        if known.get(nm, 0) >= v:
                            continue
                        if nm not in need or need[nm][1] < v:
                            need[nm] = (s, v)
                    for nm, (s, v) in need.items():
                        eng.wait_ge(s, v)
                        known[nm] = v
                    ins = o["fn"](eng)
                    if o["kind"] == "d":
                        ins.then_inc(sem_d[o["semkey"]], 16)
                    elif (e, i) in rank:
                        ins.then_inc(sem_e[e], 1)
                if e == "sp":
                    for d in final_wait_events:
                        nm, s, v = lower(d)
                        if known.get(nm, 0) < v:
                            eng.wait_ge(s, v)
                            known[nm] = v
            return body

        for e in ENGS:
            if self.ops[e] or e == "sp":
                engobj[e](make(e))
        es.close()


class Region:
    def __init__(self, uid, ap, lo, hi):
        self.uid, self.ap, self.lo, self.hi = uid, ap, lo, hi

    def k(self, *i):
        return (self.uid,) + tuple(i)

    def __getitem__(self, idx):
        return self.ap[idx]


class Arena:
    def __init__(self, P, tensor, ncols_f32):
        self.P, self.t, self.n = P, tensor, ncols_f32
        self.top = 0
        self.gen = 0
        self.pending = []
        self.live = []
        P.inherit = {}
        P._arena = self

    def alloc(self, name, free_shape, dtype=F32):
        nel = int(np.prod(free_shape))
        bpe = {F32: 4, BF16: 2, I32: 4}[dtype]
        ncol = (nel * bpe + 3) // 4
        lo, hi = self.top, self.top + ncol
        assert hi <= self.n, f"arena overflow allocating {name}: need {hi} cols of {self.n}"
        self.top = hi
        self.gen += 1
        uid = f"{name}#{self.gen}"
        ap = self.t[:, lo:hi]
        if dtype != F32:
            ap = ap.bitcast(dtype)
        ap = ap[:, 0:nel]
        if len(free_shape) > 1:
            names = " ".join(f"a{i}" for i in range(len(free_shape)))
            kw = {f"a{i}": int(s) for i, s in enumerate(free_shape)}
            ap = ap.rearrange(f"p ({names}) -> p {names}", **kw)
        evs = []
        keep = []
        for (plo, phi, pe) in self.pending:
            if plo < hi and lo < phi:
                evs.extend(pe)
            keep.append((plo, phi, pe))
        self.P.inherit[uid] = evs
        r = Region(uid, ap, lo, hi)
        self.live.append(r)
        return r

    def mark(self):
        return (self.top, len(self.live))

    def release(self, mark):
        top, nlive = mark
        P = self.P
        for r in self.live[nlive:]:
            evs = []
            for k in [k for k in P.keys if k[0] == r.uid]:
                st = P.keys.pop(k)
                evs.extend(st["w"])
                evs.extend(st["r"].values())
            evs.extend(P.inherit.get(r.uid, []))
            best = {}
            for ev in evs:
                kk = (ev[0], ev[1])
                if kk not in best or best[kk][2] < ev[2]:
                    best[kk] = ev
            self.pending.append((r.lo, r.hi, list(best.values())))
        del self.live[nlive:]
        self.top = top
from concourse.bass_utils import run_bass_kernel_spmd
S = 2048; D = 1024; NT = 16; INW = 3856; DEPTH = 2
ND = 3072; EC = 1535
NE = 16; CAP = 256; DFF = 2816


def t5_bucket_np(rel):
    nb = 16
    ret = np.where(rel > 0, nb, 0)
    n = np.abs(rel)
    max_exact = 8
    nf = np.maximum(n, 1).astype(np.float32)
    large = max_exact + (np.log(nf / np.float32(max_exact)) / np.float32(np.log(1024 / max_exact))
                         * np.float32(nb - max_exact)).astype(np.int32)
    large = np.minimum(large, nb - 1)
    return ret + np.where(n < max_exact, n, large)


def host_consts():
    d = np.arange(ND) - EC
    ad = np.abs(d)
    cnt = ((ad <= 64).astype(np.float32) + ((d % 4 == 0) & (ad <= 256)).astype(np.float32)
           + ((d % 16 == 0) & (ad <= 1024)).astype(np.float32))
    bk = t5_bucket_np(d)
    oh = np.zeros((32, ND), np.float32)
    oh[bk, np.arange(ND)] = 1.0
    c = {}
    c["c_oh"] = oh
    c["c_cnt"] = np.tile(cnt[None], (4, 1)).astype(np.float32)
    c["c_ident"] = np.eye(128, dtype=np.float32)
    c["c_jmat"] = np.eye(128, dtype=np.float32)[::-1].copy()
    c["c_triu"] = np.triu(np.ones((128, 128), np.float32))
    c["c_tril"] = np.tril(np.ones((128, 128), np.float32))
    ob = np.zeros((128, 128), np.float32); ob[:64, :64] = 1; ob[64:, 64:] = 1
    c["c_onesblk"] = ob
    tus = np.triu(np.ones((128, 128), np.float32), 1); tui = np.triu(np.ones((128, 128), np.float32), 0)
    c["c_mask4"] = np.concatenate([tus, tui, tus, tui], axis=1)
    c["c_trils"] = np.tril(np.ones((128, 128), np.float32), -1)
    rm = np.ones((128, 256), np.float32); rm[:, 0] = 0; rm[:, 128] = 0
    c["c_rmask"] = rm
    ohb = np.zeros((16, 2048), np.float32)
    for e_ in range(16):
        ohb[e_, e_ * 128:(e_ + 1) * 128] = 1.0
    c["c_ohb"] = ohb
    return c


class K:
    pass


def build(dbg=None, nlayers=DEPTH, stages=("attn", "mlstm", "rwkv", "moe")):
    nc = bass.Bass("TRN2", target_bir_lowering=False)
    k = K()
    k.nc = nc
    SH = dict(x=[S, D], rel_bias=[32, 4], ln1_g=[DEPTH, D], w_in=[DEPTH, D, INW], w_out=[DEPTH, D, D], rk_mu=[DEPTH, 2, 1280],
              rk_w0=[DEPTH, 2, 384], rk_w2=[DEPTH, 2, 64, 384], rk_a0=[DEPTH, 2, 384], rk_a2=[DEPTH, 2, 64, 384],
              rk_kk=[DEPTH, 384], rk_ka=[DEPTH, 384], rk_rk=[DEPTH, 6, 64], rk_g2=[DEPTH, 128, 384], rk_ln_g=[DEPTH, 384],
              rk_ln_b=[DEPTH, 384], ml_conv_w=[DEPTH, 5, 768], ml_conv_b=[DEPTH, 768], ml_ib=[DEPTH, 2, 4], ml_fb=[DEPTH, 2, 4],
              ml_ln_g=[DEPTH, 384], ln2_g=[DEPTH, D], router=[DEPTH, D, NE], e_w1=[DEPTH, NE, D, DFF], e_w3=[DEPTH, NE, D, DFF],
              e_w2=[DEPTH, NE, DFF, D], final_g=[D], c_oh=[32, ND], c_cnt=[4, ND], c_ident=[128, 128], c_jmat=[128, 128],
              c_triu=[128, 128], c_tril=[128, 128], c_onesblk=[128, 128], c_mask4=[128, 512], c_trils=[128, 128],
              c_rmask=[128, 256], c_ohb=[16, 2048], c_pc=[DEPTH, 128, NPC], c_wx=[DEPTH, D, 384])

    class _IN(dict):
        def __missing__(self, name):
            t = nc.dram_tensor(name, list(SH[name]), F32, kind="ExternalInput")
            self[name] = t
            return t
    IN = _IN()
    k.IN = IN
    out_t = nc.dram_tensor("out", [S, D], F32, kind="ExternalOutput")
    k.mscr = nc.dram_tensor("mscr", [4, ND], F32, kind="Internal")
    k.mtab_d = nc.dram_tensor("mtab_d", [4, 128, 23 * 128], F32, kind="Internal")
    k.xspill = nc.dram_tensor("xspill", [S, D], F32, kind="Internal")
    k.dbg = dbg
    k.dbg_out = {}
    with ExitStack() as es:
        ACOLS = 53000
        arena_t = es.enter_context(nc.sbuf_tensor("arena", [128, ACOLS], F32))
        k.ps = [es.enter_context(nc.psum_tensor(f"ps{i}", [128, 512], F32)) for i in range(8)]
        P = Prog(nc)
        A = Arena(P, arena_t, ACOLS)
        k.P, k.A = P, A
        k.final_events = []
        setup_consts(k)
        k.xres = A.alloc("xres", (NT, D))
        xin = IN["x"].ap().rearrange("(i p) d -> p i d", p=128)
        for i in range(NT):
            P.dma("sp" if i % 2 == 0 else "act", k.xres.ap[:, i, :], xin[:, i, :], writes=[k.xres.k(i)], semkey=("xld", i % 4))
        build_mask_table(k)
        for l in range(nlayers):
            m0 = A.mark()
            norm_to_hT(k, IN["ln1_g"].ap()[l], "hT")
            if "attn" in stages:
                attn_stage(k, l)
            if "mlstm" in stages:
                mlstm_stage(k, l)
            if "rwkv" in stages:
                rwkv_stage(k, l)
            A.release(m0)
            if "moe" in stages:
                moe_stage(k, l)
        final_stage(k, out_t)
        P.emit(final_wait_events=k.final_events)
    return nc, k


def dbg_dump(k, name, region_ap, shape, dt, reads):
    if not k.dbg or name not in k.dbg:
        return None
    t = k.nc.dram_tensor("dbg_" + name, list(shape), dt, kind="ExternalOutput")
    k.dbg_out[name] = t
    return t


def setup_consts(k):
    P, A, IN = k.P, k.A, k.IN
    k.ident = A.alloc("ident", (128,))
    k.jmat = A.alloc("jmat", (128,))
    k.ident16 = A.alloc("ident16", (128,), BF16)
    k.triu = A.alloc("triu", (128,))
    k.tril = A.alloc("tril", (128,))
    P.dma("sp", k.ident.ap, IN["c_ident"].ap(), writes=[k.ident.k()], semkey="c0")
    P.dma("sp", k.jmat.ap, IN["c_jmat"].ap(), writes=[k.jmat.k()], semkey="c1")
    P.dma("sp", k.triu.ap, IN["c_triu"].ap(), writes=[k.triu.k()], semkey="c2")
    P.dma("sp", k.tril.ap, IN["c_tril"].ap(), writes=[k.tril.k()], semkey="c3")
    P.op("dve", lambda e: e.tensor_copy(k.ident16.ap, k.ident.ap), reads=[k.ident.k()], writes=[k.ident16.k()])
    k.one = A.alloc("one", (1,))
    P.op("pool", lambda e: e.memset(k.one.ap, 1.0), writes=[k.one.k()])
    k.ones32 = A.alloc("ones32", (128,))
    P.op("pool", lambda e: e.memset(k.ones32.ap, 1.0), writes=[k.ones32.k()])
    k.negm = [A.alloc("negm0", (128,)), A.alloc("negm1", (128,))]
    P.op("dve", lambda e: e.tensor_scalar(k.negm[0].ap, k.triu.ap, -1.0, 30000.0, op0=ALU.add, op1=ALU.mult), reads=[k.triu.k()], writes=[k.negm[0].k()])
    P.op("dve", lambda e: e.tensor_scalar(k.negm[1].ap, k.tril.ap, -1.0, 30000.0, op0=ALU.add, op1=ALU.mult), reads=[k.tril.k()], writes=[k.negm[1].k()])
    k.epsgn = A.alloc("epsgn", (1,))
    P.op("pool", lambda e: e.memset(k.epsgn.ap, 64e-5), writes=[k.epsgn.k()])
    k.onesblk = A.alloc("onesblk", (128,)); k.onesblk64 = A.alloc("onesblk64", (128,))
    k.trils = A.alloc("trils", (128,))
    P.dma("act", k.onesblk.ap, IN["c_onesblk"].ap(), writes=[k.onesblk.k()], semkey="c4")
    P.dma("act", k.trils.ap, IN["c_trils"].ap(), writes=[k.trils.k()], semkey="c6")
    P.op("dve", lambda e: e.tensor_scalar(k.onesblk64.ap, k.onesblk.ap, 1.0 / 64, None, op0=ALU.mult), reads=[k.onesblk.k()], writes=[k.onesblk64.k()])
    k.iota256 = A.alloc("iota256", (256,)); k.slotidx = A.alloc("slotidx", (2,))
    k.ones16 = A.alloc("ones16", (128,), BF16); k.trius16 = A.alloc("trius16", (128,), BF16)
    P.op("pool", lambda e: e.iota(k.iota256.ap, [[1, 256]], base=0, channel_multiplier=0, allow_small_or_imprecise_dtypes=True), writes=[k.iota256.k()])
    P.op("pool", lambda e: e.iota(k.slotidx.ap, [[128, 2]], base=0, channel_multiplier=1, allow_small_or_imprecise_dtypes=True), writes=[k.slotidx.k()])
    P.op("dve", lambda e: e.tensor_copy(k.ones16.ap, k.ones32.ap), reads=[k.ones32.k()], writes=[k.ones16.k()])
    P.op("dve", lambda e: e.tensor_tensor(k.trius16.ap, k.triu.ap, k.ident.ap, op=ALU.subtract), reads=[k.triu.k(), k.ident.k()], writes=[k.trius16.k()])
    k.eps6 = A.alloc("eps6", (1,))
    P.op("pool", lambda e: e.memset(k.eps6.ap, 1e-6), writes=[k.eps6.k()])


def build_mask_table(k):
    P, A, IN, ps = k.P, k.A, k.IN, k.ps
    m0 = A.mark()
    rb = A.alloc("rb", (4,)); oh = A.alloc("oh", (ND,)); cnt = A.alloc("cnt", (ND,)); mm = A.alloc("mm", (ND,))
    P.dma("sp", rb.ap[0:32, :], IN["rel_bias"].ap(), writes=[rb.k()], semkey="mt0")
    P.dma("sp", oh.ap[0:32, :], IN["c_oh"].ap(), writes=[oh.k()], semkey="mt1")
    P.dma("act", cnt.ap[0:4, :], IN["c_cnt"].ap(), writes=[cnt.k()], semkey="mt2")
    for c in range(ND // 512):
        b = ps[c % 2]
        P.op("pe", lambda e, c=c, b=b: e.matmul(b[0:4, :], rb.ap[0:32, :], oh.ap[0:32, c * 512:(c + 1) * 512], start=True, stop=True),
             reads=[rb.k(), oh.k()], writes=[f"ps{c%2}"])
        P.op("act", lambda e, c=c, b=b: e.activation(mm.ap[0:4, c * 512:(c + 1) * 512], b[0:4, :], AF.Exp),
             reads=[f"ps{c%2}"], writes=[mm.k(c)])
        P.op("dve", lambda e, c=c: e.tensor_tensor(mm.ap[0:4, c * 512:(c + 1) * 512], mm.ap[0:4, c * 512:(c + 1) * 512],
                                                  cnt.ap[0:4, c * 512:(c + 1) * 512], op=ALU.mult),
             reads=[mm.k(c), cnt.k()], writes=[mm.k(c)])
    P.dma("sp", k.mscr.ap(), mm.ap[0:4, :], reads=[mm.k(c) for c in range(ND // 512)], writes=["mscr"], semkey="mt3")
    hanks = [A.alloc(f"hank{i}", (23, 128)) for i in range(2)]
    mts = [A.alloc(f"mt{i}", (23, 128)) for i in range(2)]
    for h in range(4):
        hank = hanks[h % 2]
        mt = mts[h % 2]
        src = bass.AP(k.mscr, h * ND, [[1, 128], [128, 23], [1, 128]])
        P.dma("sp" if h % 2 == 0 else "act", hank.ap, src, reads=["mscr"], writes=[hank.k()], semkey=("mt4", h))
        for j in range(23):
            jj = 22 - j
            b = 2 + (j // 4) % 2
            P.op("pe", lambda e, j=j, b=b, hank=hank: e.matmul(ps[b][:, (j % 4) * 128:(j % 4 + 1) * 128], hank.ap[:, j, :], k.jmat.ap, start=True, stop=True),
                 reads=[hank.k(), k.jmat.k()], writes=[f"ps{b}"])
            P.op("act" if j % 2 == 0 else "dve",
                 (lambda e, jj=jj, j=j, b=b, mt=mt: e.copy(mt.ap[:, jj, :], ps[b][:, (j % 4) * 128:(j % 4 + 1) * 128])) if j % 2 == 0 else
                 (lambda e, jj=jj, j=j, b=b, mt=mt: e.tensor_copy(mt.ap[:, jj, :], ps[b][:, (j % 4) * 128:(j % 4 + 1) * 128])),
                 reads=[f"ps{b}"], writes=[mt.k()])
        P.dma("sp", k.mtab_d.ap()[h].rearrange("p (j q) -> p j q", j=23), mt.ap, reads=[mt.k()], writes=[("mtab_d", h)], semkey=("mt5", h))
    A.release(m0)


def norm_to_hT(k, g_ap, name, want_f32T=False):
    P, A, ps = k.P, k.A, k.ps
    k.hT = A.alloc(name, (8, S), BF16)
    m0 = A.mark()
    gb = A.alloc("gb", (D,)); junk = A.alloc("junk", (D,)); ss = A.alloc("ss", (NT,)); rstd = A.alloc("rstd", (NT,))
    hb = [A.alloc(f"hb{i}", (D,), BF16) for i in range(2)]
    P.dma("sp", gb.ap, g_ap.partition_broadcast(128), writes=[gb.k()], semkey="gb")
    P.op("pool", lambda e: e.memset(ss.ap, 0.0), writes=[ss.k(i) for i in range(NT)])
    for i in range(NT):
        P.op("act", lambda e, i=i: e.activation(junk.ap, k.xres.ap[:, i, :], AF.Square, accum_out=ss.ap[:, i:i + 1]),
             reads=[k.xres.k(i)], writes=[junk.k(), ss.k(i)])
    P.op("act", lambda e: e.activation(rstd.ap, ss.ap, AF.Sqrt, scale=1.0 / D, bias=k.eps6.ap[:, 0:1]),
         reads=[ss.k(i) for i in range(NT)] + [k.eps6.k()], writes=[rstd.k()])
    P.op("dve", lambda e: e.reciprocal(rstd.ap, rstd.ap), reads=[rstd.k()], writes=[rstd.k()])
    for i in range(NT):
        h_ = hb[i % 2]
        P.op("dve", lambda e, i=i, h_=h_: e.scalar_tensor_tensor(h_.ap, k.xres.ap[:, i, :], rstd.ap[:, i:i + 1], gb.ap, op0=ALU.mult, op1=ALU.mult),
             reads=[k.xres.k(i), rstd.k(), gb.k()], writes=[h_.k()])
        b = 4 + i % 2
        pb = ps[b][:, :].bitcast(BF16)
        for c in range(8):
            P.op("pe", lambda e, c=c, pb=pb, h_=h_: e.transpose(pb[:, c * 128:(c + 1) * 128], h_.ap[:, c * 128:(c + 1) * 128], k.ident16.ap),
                 reads=[h_.k(), k.ident16.k()], writes=[f"ps{b}"])
        P.op("act", lambda e, i=i, pb=pb: e.copy(k.hT.ap[:, :, i * 128:(i + 1) * 128], pb.rearrange("p (c t) -> p c t", c=8)),
             reads=[f"ps{b}"], writes=[k.hT.k(i)])
    A.release(m0)


def load_w_bf16(k, name, src_ap, nchunks, ncols, semkey, split=None):
    P, A = k.P, k.A
    w = A.alloc(name, (nchunks, ncols), BF16)
    v = src_ap.rearrange("(c p) n -> p c n", p=128)
    for c in range(nchunks):
        P.dma("pool", w.ap[:, c, :], v[:, c, :], writes=[w.k(c)], semkey=(semkey, c % 4))
    return w


def outproj_partial(k, l, yT, nch, row0, tag):
    P, A, ps, IN = k.P, k.A, k.ps, k.IN
    wo = load_w_bf16(k, "wo_" + tag, IN["w_out"].ap()[l][row0:row0 + nch * 128, :], nch, D, "wo_" + tag)
    n = 0
    for i in range(NT):
        for half in range(2):
            b = 6 + n % 2
            n += 1
            for c in range(nch):
                P.op("pe", lambda e, i=i, half=half, c=c, b=b: e.matmul(ps[b][:, :], yT.ap[:, c, i * 128:(i + 1) * 128],
                                                                     wo.ap[:, c, half * 512:(half + 1) * 512], start=(c == 0), stop=(c == nch - 1)),
                     reads=[yT.k(i), wo.k(c)], writes=[f"ps{b}"])
            P.op("dve", lambda e, i=i, half=half, b=b: e.tensor_tensor(k.xres.ap[:, i, half * 512:(half + 1) * 512],
                                                                      k.xres.ap[:, i, half * 512:(half + 1) * 512], ps[b][:, :], op=ALU.add),
                 reads=[f"ps{b}", k.xres.k(i)], writes=[k.xres.k(i)])


def transpose_to_T(k, y, nch, yT):
    P, ps = k.P, k.ps
    for i in range(NT):
        b = 4 + i % 2
        pb = ps[b][:, :].bitcast(BF16)
        for c in range(nch):
            P.op("pe", lambda e, i=i, c=c, pb=pb: e.transpose(pb[:, c * 128:(c + 1) * 128], y.ap[:, i, c * 128:(c + 1) * 128], k.ident16.ap),
                 reads=[y.k(i), k.ident16.k()], writes=[f"ps{b}"])
        P.op("act", lambda e, i=i, pb=pb: e.copy(yT.ap[:, :, i * 128:(i + 1) * 128], pb[:, 0:nch * 128].rearrange("p (c t) -> p c t", c=nch)),
             reads=[f"ps{b}"], writes=[yT.k(i)])


def attn_stage(k, l):
    P, A, ps, IN = k.P, k.A, k.ps, k.IN
    m0 = A.mark()
    ya = A.alloc("ya", (NT, 256), BF16)
    m1 = A.mark()
    wa = load_w_bf16(k, "wa", IN["w_in"].ap()[l][:, 0:768], 8, 768, "wa")
    qT = A.alloc("qT", (2, S), BF16); kT = A.alloc("kT", (2, S), BF16)
    vp = A.alloc("vp", (NT, 4, 65), BF16)
    P.op("pool", lambda e: e.memset(vp.ap, 1.0), writes=[vp.k(i) for i in range(NT)])
    n = 0
    for cc in range(4):
        dst = qT if cc < 2 else kT
        for tg in range(4):
            b = n % 2; n += 1
            for c in range(8):
                P.op("pe", lambda e, c=c, cc=cc, tg=tg, b=b: e.matmul(ps[b][:, :], wa.ap[:, c, cc * 128:(cc + 1) * 128], k.hT.ap[:, c, tg * 512:(tg + 1) * 512],
                                                                  start=(c == 0), stop=(c == 7)),
                     reads=[wa.k(c)] + [k.hT.k(4 * tg + j) for j in range(4)], writes=[f"ps{b}"])
            P.op("act", lambda e, cc=cc, tg=tg, b=b, dst=dst: e.copy(dst.ap[:, cc % 2, tg * 512:(tg + 1) * 512], ps[b][:, :]),
                 reads=[f"ps{b}"], writes=[dst.k(tg)])
    for i in range(NT):
        b = 2 + i % 2
        for c in range(8):
            P.op("pe", lambda e, c=c, i=i, b=b: e.matmul(ps[b][:, 0:256], k.hT.ap[:, c, i * 128:(i + 1) * 128], wa.ap[:, c, 512:768], start=(c == 0), stop=(c == 7)),
                 reads=[wa.k(c), k.hT.k(i)], writes=[f"ps{b}"])
        P.op("dve", lambda e, i=i, b=b: e.tensor_copy(vp.ap[:, i, :, 0:64], ps[b][:, 0:256].rearrange("p (h c) -> p h c", h=4)),
             reads=[f"ps{b}"], writes=[vp.k(i)])
    mtab = [A.alloc(f"mtab{i}", (23, 128)) for i in range(2)]
    e32 = [A.alloc(f"e32_{i}", (512,)) for i in range(2)]
    p16 = [A.alloc(f"p16_{i}", (512,), BF16) for i in range(2)]
    rc = A.alloc("rc", (8,))
    iters = []
    for h in range(4):
        for g in range(4):
            kts = [kt for kt in range(NT) if -8 <= kt - 4 * g <= 11]
            for kt in kts:
                iters.append((h, g, kt, kt == kts[0], kt == kts[-1]))
    mts = {}

    def emit_score(n):
        h, g, kt, first, last = iters[n]
        sb = n % 2
        hp, hb_ = h // 2, (h % 2) * 64
        if h not in mts:
            mt = mtab[h % 2]
            P.dma("sp", mt.ap, k.mtab_d.ap()[h].rearrange("p (j q) -> p j q", j=23), reads=[("mtab_d", h)], writes=[mt.k()], semkey=("mtl", h % 2))
            mts[h] = mt
        P.op("pe", lambda e, kt=kt, g=g, sb=sb, hp=hp, hb_=hb_: e.matmul(ps[sb][:, :], kT.ap[hb_:hb_ + 64, hp, kt * 128:(kt + 1) * 128],
                                                                   qT.ap[hb_:hb_ + 64, hp, g * 512:(g + 1) * 512], start=True, stop=True),
             reads=[kT.k(kt // 4), qT.k(g)], writes=[f"ps{sb}"], serial=True)

    def emit_rest(n):
        h, g, kt, first, last = iters[n]
        sb = n % 2
        mt = mts[h]
        jj0 = 11 - kt + 4 * g
        P.op("act", lambda e, sb=sb: e.activation(e32[sb].ap, ps[sb][:, :], AF.Exp, scale=0.125),
             reads=[f"ps{sb}"], writes=[e32[sb].k()])
        P.op("dve", lambda e, sb=sb, jj0=jj0, mt=mt: e.tensor_tensor(p16[sb].ap, e32[sb].ap, mt.ap[:, jj0:jj0 + 4, :].rearrange("p j q -> p (j q)"), op=ALU.mult),
             reads=[e32[sb].k(), mt.k()], writes=[p16[sb].k()])
        for i in range(4):
            P.op("pe", lambda e, i=i, sb=sb, kt=kt, h=h, first=first, last=last: e.matmul(ps[2 + i][:, 0:65], p16[sb].ap[:, i * 128:(i + 1) * 128], vp.ap[:, kt, h, :],
                                                                                  start=first, stop=last),
                 reads=[p16[sb].k(), vp.k(kt)], writes=[f"ps{2+i}"])
        if last:
            for i in range(4):
                P.op("dve", lambda e, i=i: e.reciprocal(rc.ap[:, i:i + 1], ps[2 + i][:, 64:65]), reads=[f"ps{2+i}"], writes=[rc.k(i)])
                P.op("dve", lambda e, i=i, g=g, h=h: e.tensor_scalar(ya.ap[:, 4 * g + i, h * 64:(h + 1) * 64], ps[2 + i][:, 0:64], rc.ap[:, i:i + 1], None, op0=ALU.mult),
                     reads=[f"ps{2+i}", rc.k(i)], writes=[ya.k(4 * g + i)])

    emit_score(0)
    for n in range(len(iters)):
        if n + 1 < len(iters):
            emit_score(n + 1)
        emit_rest(n)
    A.release(m1)
    if k.dbg and "ya" in k.dbg:
        t = k.nc.dram_tensor("dbg_ya", [S, 256], BF16, kind="ExternalOutput")
        k.final_events.append(P.dma("sp", t.ap().rearrange("(i p) c -> p i c", p=128), ya.ap, reads=[ya.k(i) for i in range(NT)], semkey="dbg"))
    yaT = A.alloc("yaT", (2, S), BF16)
    transpose_to_T(k, ya, 2, yaT)
    outproj_partial(k, l, yaT, 2, 0, "a")
    A.release(m0)


def final_stage(k, out_t):
    P, A = k.P, k.A
    m0 = A.mark()
    gb = A.alloc("gbf", (D,)); junk = A.alloc("junkf", (D,)); ss = A.alloc("ssf", (NT,)); rstd = A.alloc("rstdf", (NT,))
    ob = [A.alloc(f"ob{i}", (D,)) for i in range(2)]
    P.dma("sp", gb.ap, k.IN["final_g"].ap().partition_broadcast(128), writes=[gb.k()], semkey="gbf")
    P.op("pool", lambda e: e.memset(ss.ap, 0.0), writes=[ss.k(i) for i in range(NT)])
    for i in range(NT):
        P.op("act", lambda e, i=i: e.activation(junk.ap, k.xres.ap[:, i, :], AF.Square, accum_out=ss.ap[:, i:i + 1]),
             reads=[k.xres.k(i)], writes=[junk.k(), ss.k(i)])
    P.op("act", lambda e: e.activation(rstd.ap, ss.ap, AF.Sqrt, scale=1.0 / D, bias=k.eps6.ap[:, 0:1]),
         reads=[ss.k(i) for i in range(NT)] + [k.eps6.k()], writes=[rstd.k()])
    P.op("dve", lambda e: e.reciprocal(rstd.ap, rstd.ap), reads=[rstd.k()], writes=[rstd.k()])
    ov = out_t.ap().rearrange("(i p) d -> p i d", p=128)
    for i in range(NT):
        o = ob[i % 2]
        P.op("dve", lambda e, i=i, o=o: e.scalar_tensor_tensor(o.ap, k.xres.ap[:, i, :], rstd.ap[:, i:i + 1], gb.ap, op0=ALU.mult, op1=ALU.mult),
             reads=[k.xres.k(i), rstd.k(), gb.k()], writes=[o.k()])
        k.final_events.append(P.dma("sp" if i % 2 == 0 else "act", ov[:, i, :], o.ap, reads=[o.k()], semkey=("out", i % 2)))
    A.release(m0)


OFF_MQK = 2304; OFF_MV = 3072; OFF_MO = 3456; OFF_MG = 3840


def mlstm_stage(k, l):
    P, A, ps, IN = k.P, k.A, k.ps, k.IN
    m0 = A.mark()
    hsum = A.alloc("hsum", (NT, 4, 96))
    m1 = A.mark()
    qkT = A.alloc("qkT", (8, S), BF16)
    vp = A.alloc("vpm", (NT, 4, 97), BF16)
    G = A.alloc("G", (NT, 16))
    BL = [A.alloc(f"BL{d}", (NT, 4)) for d in range(2)]
    EBL = [A.alloc(f"EBL{d}", (NT, 4)) for d in range(2)]
    IMB = [A.alloc(f"IMB{d}", (NT, 4)) for d in range(2)]
    m2 = A.mark()
    wm = load_w_bf16(k, "wm", IN["w_in"].ap()[l][:, OFF_MQK:OFF_MV], 8, OFF_MV - OFF_MQK, "wm")
    pre = A.alloc("pre", (S + 4,)); cacc = A.alloc("cacc", (S,))
    cw = A.alloc("cw", (8, 6))
    for g_ in range(8):
        P.dma("sp", cw.ap[0:96, g_, 0:5], IN["ml_conv_w"].ap()[l][:, g_ * 96:(g_ + 1) * 96].rearrange("j c -> c j"), writes=[cw.k()], semkey=("cw0", g_ % 4), allow_slow_non_contiguous=True)
    P.dma("sp", cw.ap[0:96, :, 5:6], IN["ml_conv_b"].ap()[l].rearrange("(g c o) -> c g o", c=96, o=1), writes=[cw.k()], semkey="cw1", allow_slow_non_contiguous=True)
    P.op("pool", lambda e: e.memset(pre.ap, 0.0), writes=[pre.k()])
    P.op("pool", lambda e: e.memset(vp.ap, 1.0), writes=[vp.k(i) for i in range(NT)])
    n = 0
    for j in range(8):
        for tg in range(4):
            b = n % 2; n += 1
            for c in range(8):
                P.op("pe", lambda e, c=c, j=j, tg=tg, b=b: e.matmul(ps[b][0:96, :], wm.ap[:, c, j * 96:(j + 1) * 96], k.hT.ap[:, c, tg * 512:(tg + 1) * 512],
                                                                 start=(c == 0), stop=(c == 7)),
                     reads=[wm.k(c)] + [k.hT.k(4 * tg + q) for q in range(4)], writes=[f"ps{b}"])
            P.op("act", lambda e, tg=tg, b=b: e.copy(pre.ap[0:96, 2 + tg * 512:2 + (tg + 1) * 512], ps[b][0:96, :]),
                 reads=[f"ps{b}"], writes=[pre.k()])
        P.op("dve", lambda e, j=j: e.tensor_scalar(cacc.ap[0:96, :], pre.ap[0:96, 0:S], cw.ap[0:96, j, 0:1], None, op0=ALU.mult),
             reads=[pre.k(), cw.k()], writes=[cacc.k()])
        for t in range(1, 5):
            P.op("dve", lambda e, j=j, t=t: e.scalar_tensor_tensor(cacc.ap[0:96, :], pre.ap[0:96, t:t + S], cw.ap[0:96, j, t:t + 1], cacc.ap[0:96, :],
                                                                 op0=ALU.mult, op1=ALU.add),
                 reads=[pre.k(), cw.k(), cacc.k()], writes=[cacc.k()])
        if j < 4:
            P.op("act", lambda e, j=j: e.activation(qkT.ap[0:96, j, :], cacc.ap[0:96, :], AF.Silu, bias=cw.ap[0:96, j, 5:6]),
                 reads=[cacc.k(), cw.k()], writes=[qkT.k(j)])
        else:
            P.op("act", lambda e, j=j: e.activation(cacc.ap[0:96, :], cacc.ap[0:96, :], AF.Silu, bias=cw.ap[0:96, j, 5:6]),
                 reads=[cacc.k(), cw.k()], writes=[cacc.k()])
            P.op("dve", lambda e, j=j: e.tensor_scalar(qkT.ap[0:96, j, :], cacc.ap[0:96, :], 96.0 ** -0.5, None, op0=ALU.mult),
                 reads=[cacc.k()], writes=[qkT.k(j)])
    A.release(m2)
    wv = load_w_bf16(k, "wv", IN["w_in"].ap()[l][:, OFF_MV:OFF_MO], 8, 384, "wv")
    wg = load_w_bf16(k, "wg", IN["w_in"].ap()[l][:, OFF_MG:INW], 8, 16, "wg")
    gbias = A.alloc("gbias", (16,))
    for d in range(2):
        P.dma("act", gbias.ap[:, 8 * d:8 * d + 4], IN["ml_ib"].ap()[l][d].partition_broadcast(128), writes=[gbias.k()], semkey=("gbi", d))
        P.dma("act", gbias.ap[:, 8 * d + 4:8 * d + 8], IN["ml_fb"].ap()[l][d].partition_broadcast(128), writes=[gbias.k()], semkey=("gbf_", d))
    for i in range(NT):
        b = 2 + i % 2
        for c in range(8):
            P.op("pe", lambda e, c=c, i=i, b=b: e.matmul(ps[b][:, 0:384], k.hT.ap[:, c, i * 128:(i + 1) * 128], wv.ap[:, c, :],
                                                     start=(c == 0), stop=(c == 7)),
                 reads=[wv.k(c), k.hT.k(i)], writes=[f"ps{b}"])
        P.op("dve", lambda e, i=i, b=b: e.tensor_copy(vp.ap[:, i, :, 0:96], ps[b][:, 0:384].rearrange("p (h c) -> p h c", h=4)),
             reads=[f"ps{b}"], writes=[vp.k(i)])
        b2 = 4 + i % 2
        for c in range(8):
            P.op("pe", lambda e, c=c, i=i, b2=b2: e.matmul(ps[b2][:, 0:16], k.hT.ap[:, c, i * 128:(i + 1) * 128], wg.ap[:, c, :],
                                                       start=(c == 0), stop=(c == 7)),
                 reads=[wg.k(c), k.hT.k(i)], writes=[f"ps{b2}"])
        P.op("dve", lambda e, i=i, b2=b2: e.tensor_tensor(G.ap[:, i, :], ps[b2][:, 0:16], gbias.ap, op=ALU.add),
             reads=[f"ps{b2}", gbias.k()], writes=[G.k()])
    A.release(m2)
    for d in range(2):
        v_ = G.ap[:, :, 8 * d + 4:8 * d + 8]
        P.op("act", lambda e, v_=v_: e.activation(v_, v_, AF.Exp, scale=-1.0), reads=[G.k()], writes=[G.k()])
        P.op("act", lambda e, v_=v_: e.activation(v_, v_, AF.Ln, bias=k.one.ap[:, 0:1]), reads=[G.k(), k.one.k()], writes=[G.k()])
        P.op("dve", lambda e, v_=v_: e.tensor_scalar(v_, v_, -1.0, None, op0=ALU.mult), reads=[G.k()], writes=[G.k()])
    for d in range(2):
        tri = k.triu if d == 0 else k.tril
        P.op("pe", lambda e, d=d, tri=tri: e.matmul(ps[7][:, 0:64].rearrange("p (i h) -> p i h", h=4), tri.ap, G.ap[:, :, 8 * d + 4:8 * d + 8], start=True, stop=True),
             reads=[tri.k(), G.k()], writes=["ps7"])
        P.op("dve", lambda e, d=d: e.tensor_copy(BL[d].ap, ps[7][:, 0:64].rearrange("p (i h) -> p i h", h=4)), reads=["ps7"], writes=[BL[d].k()])
        P.op("act", lambda e, d=d: e.activation(EBL[d].ap, BL[d].ap, AF.Exp), reads=[BL[d].k()], writes=[EBL[d].k()])
        P.op("dve", lambda e, d=d: e.tensor_tensor(IMB[d].ap, G.ap[:, :, 8 * d:8 * d + 4], BL[d].ap, op=ALU.subtract), reads=[G.k(), BL[d].k()], writes=[IMB[d].k()])
    C32 = A.alloc("C32", (4, 97)); Cb = A.alloc("Cb", (4, 97), BF16)
    HB = []
    for h in range(4):
        HB.append(dict(dg=A.alloc(f"dg{h}", (128,)), Dm=A.alloc(f"Dm{h}", (128,)), W16=A.alloc(f"W16{h}", (128,), BF16),
                       nis=A.alloc(f"nis{h}", (97,)), nsb=A.alloc(f"nsb{h}", (97,)), kh=A.alloc(f"kh{h}", (96,), BF16), sm=A.alloc(f"sm{h}", (4,))))

    def head_stream(h):
        hb = HB[h]
        dg, Dm, W16, nis, nsb, kh, sm = (hb[n_] for n_ in ("dg", "Dm", "W16", "nis", "nsb", "kh", "sm"))
        bA, bB = 2 * h, 2 * h + 1
        kA, kB = f"ps{bA}", f"ps{bB}"
        pbt = ps[bA][:, 384:432].bitcast(BF16)
        for d in range(2):
            P.op("pool", lambda e: e.memset(C32.ap[:, h, :], 0.0), writes=[C32.k(h)])
            P.op("pool", lambda e: e.memset(Cb.ap[:, h, :], 0.0), writes=[Cb.k(h)])
            nm = k.negm[d]
            endc = 127 if d == 0 else 0
            order = range(NT) if d == 0 else range(NT - 1, -1, -1)
            for i in order:
                yield from chunk(h, d, i, nm, endc, dg, Dm, W16, nis, nsb, kh, sm, bA, bB, kA, kB, pbt)

    def chunk(h, d, i, nm, endc, dg, Dm, W16, nis, nsb, kh, sm, bA, bB, kA, kB, pbt):
        tsl = slice(i * 128, (i + 1) * 128)
        P.op("pe", lambda e: e.matmul(ps[bA][:, 0:128], qkT.ap[0:96, 4 + h, tsl], qkT.ap[0:96, h, tsl], start=True, stop=True),
             reads=[qkT.k(4 + h), qkT.k(h)], writes=[kA])
        P.op("dve", lambda e: e.tensor_scalar(dg.ap, k.ident.ap, BL[d].ap[:, i, h:h + 1], None, op0=ALU.mult),
             reads=[k.ident.k(), BL[d].k()], writes=[dg.k()])
        P.op("pe", lambda e: e.matmul(ps[bA][:, 128:256], k.ones32.ap, dg.ap, start=True, stop=False), reads=[k.ones32.k(), dg.k()], writes=[kA])
        P.op("pe", lambda e: e.matmul(ps[bA][:, 128:256], k.ident.ap, nm.ap, start=False, stop=True), reads=[k.ident.k(), nm.k()], writes=[kA])
        yield
        P.op("act", lambda e: e.activation(Dm.ap, ps[bA][:, 128:256], AF.Exp, bias=IMB[d].ap[:, i, h:h + 1]), reads=[kA, IMB[d].k()], writes=[Dm.k()])
        P.op("act", lambda e: e.activation(sm.ap[:, 2:3], ps[bA][:, 128 + endc:129 + endc], AF.Exp), reads=[kA], writes=[sm.k(2)])
        P.op("dve", lambda e: e.tensor_tensor(W16.ap, ps[bA][:, 0:128], Dm.ap, op=ALU.mult), reads=[kA, Dm.k()], writes=[W16.k()])
        yield
        P.op("pe", lambda e: e.matmul(ps[bB][:, 0:97], W16.ap, vp.ap[:, i, h, :], start=True, stop=True), reads=[W16.k(), vp.k(i)], writes=[kB])
        P.op("pe", lambda e: e.matmul(ps[bB][:, 128:225], qkT.ap[0:96, h, tsl], Cb.ap[0:96, h, :], start=True, stop=True), reads=[qkT.k(h), Cb.k(h)], writes=[kB])
        P.op("pe", lambda e: e.transpose(pbt, qkT.ap[0:96, 4 + h, tsl], k.ident16.ap[0:96, 0:96]), reads=[qkT.k(4 + h), k.ident16.k()], writes=[kA])
        yield
        P.op("act", lambda e: e.copy(nis.ap, ps[bB][:, 0:97]), reads=[kB], writes=[nis.k()])
        P.op("dve", lambda e: e.tensor_scalar(kh.ap, pbt, Dm.ap[:, endc:endc + 1], None, op0=ALU.mult), reads=[kA, Dm.k()], writes=[kh.k()])
        P.op("dve", lambda e: e.scalar_tensor_tensor(nsb.ap, ps[bB][:, 128:225], EBL[d].ap[:, i, h:h + 1], nis.ap, op0=ALU.mult, op1=ALU.add),
             reads=[kB, EBL[d].k(), nis.k()], writes=[nsb.k()])
        P.op("pe", lambda e: e.matmul(ps[bA][0:96, 256:353], kh.ap, vp.ap[:, i, h, :], start=True, stop=True), reads=[kh.k(), vp.k(i)], writes=[kA])
        yield
        P.op("dve", lambda e: e.tensor_scalar(sm.ap[:, 0:1], nsb.ap[:, 96:97], -1.0, None, op0=ALU.mult), reads=[nsb.k()], writes=[sm.k(0)])
        P.op("dve", lambda e: e.scalar_tensor_tensor(sm.ap[:, 0:1], nsb.ap[:, 96:97], 1.0, sm.ap[:, 0:1], op0=ALU.max, op1=ALU.max), reads=[nsb.k(), sm.k(0)], writes=[sm.k(0)])
        P.op("dve", lambda e: e.reciprocal(sm.ap[:, 0:1], sm.ap[:, 0:1]), reads=[sm.k(0)], writes=[sm.k(0)])
        if d == 0:
            P.op("dve", lambda e: e.tensor_scalar(hsum.ap[:, i, h, :], nsb.ap[:, 0:96], sm.ap[:, 0:1], None, op0=ALU.mult), reads=[nsb.k(), sm.k(0)], writes=[hsum.k(i, h)])
        else:
            P.op("dve", lambda e: e.scalar_tensor_tensor(hsum.ap[:, i, h, :], nsb.ap[:, 0:96], sm.ap[:, 0:1], hsum.ap[:, i, h, :], op0=ALU.mult, op1=ALU.add),
                 reads=[nsb.k(), sm.k(0), hsum.k(i, h)], writes=[hsum.k(i, h)])
        P.op("dve", lambda e: e.scalar_tensor_tensor(C32.ap[0:96, h, :], C32.ap[0:96, h, :], sm.ap[0:96, 2:3], ps[bA][0:96, 256:353], op0=ALU.mult, op1=ALU.add),
             reads=[C32.k(h), sm.k(2), kA], writes=[C32.k(h)])
        P.op("act", lambda e: e.copy(Cb.ap[0:96, h, :], C32.ap[0:96, h, :]), reads=[C32.k(h)], writes=[Cb.k(h)])
        yield

    gens = [head_stream(h) for h in range(4)]
    while gens:
        for g_ in list(gens):
            try:
                next(g_)
            except StopIteration:
                gens.remove(g_)
    A.release(m1)
    yc = A.alloc("yc", (NT, 384), BF16)
    m3 = A.mark()
    wmo = load_w_bf16(k, "wmo", IN["w_in"].ap()[l][:, OFF_MO:OFF_MG], 8, 384, "wmo")
    lng = A.alloc("lng", (384,))
    P.dma("sp", lng.ap, IN["ml_ln_g"].ap()[l].partition_broadcast(128), writes=[lng.k()], semkey="lng")
    st = A.alloc("st", (16,)); cen = [A.alloc(f"cen{i}", (4, 96)) for i in range(2)]; junk = A.alloc("junkm", (96,))
    og = [A.alloc(f"og{i}", (384,)) for i in range(2)]
    for i in range(NT):
        r = i % 2
        b = 2 + r
        for c in range(8):
            P.op("pe", lambda e, c=c, i=i, b=b: e.matmul(ps[b][:, 0:384], k.hT.ap[:, c, i * 128:(i + 1) * 128], wmo.ap[:, c, :], start=(c == 0), stop=(c == 7)),
                 reads=[wmo.k(c), k.hT.k(i)], writes=[f"ps{b}"])
        P.op("act", lambda e, r=r, b=b: e.activation(og[r].ap, ps[b][:, 0:384], AF.Sigmoid), reads=[f"ps{b}"], writes=[og[r].k()])
        sk = st.k()
        P.op("dve", lambda e, i=i: e.tensor_reduce(st.ap[:, 0:4], hsum.ap[:, i, :, :], axis=AX.X, op=ALU.add), reads=[hsum.k(i, h_) for h_ in range(4)], writes=[sk])
        P.op("dve", lambda e: e.tensor_scalar(st.ap[:, 0:4], st.ap[:, 0:4], 1.0 / 96, None, op0=ALU.mult), reads=[sk], writes=[sk])
        P.op("pool", lambda e: e.memset(st.ap[:, 4:8], 0.0), writes=[sk])
        for h in range(4):
            P.op("dve", lambda e, i=i, h=h, r=r: e.tensor_scalar(cen[r].ap[:, h, :], hsum.ap[:, i, h, :], st.ap[:, h:h + 1], None, op0=ALU.subtract),
                 reads=[hsum.k(i, h), sk], writes=[cen[r].k(h)])
            P.op("act", lambda e, h=h, r=r: e.activation(junk.ap, cen[r].ap[:, h, :], AF.Square, accum_out=st.ap[:, 4 + h:5 + h]),
                 reads=[cen[r].k(h)], writes=[junk.k(), sk])
        P.op("act", lambda e: e.activation(st.ap[:, 8:12], st.ap[:, 4:8], AF.Sqrt, scale=1.0 / 96, bias=k.eps6.ap[:, 0:1]), reads=[sk, k.eps6.k()], writes=[sk])
        P.op("dve", lambda e: e.reciprocal(st.ap[:, 8:12], st.ap[:, 8:12]), reads=[sk], writes=[sk])
        for h in range(4):
            P.op("dve", lambda e, h=h, r=r: e.scalar_tensor_tensor(cen[r].ap[:, h, :], cen[r].ap[:, h, :], st.ap[:, 8 + h:9 + h], lng.ap[:, h * 96:(h + 1) * 96],
                                                                 op0=ALU.mult, op1=ALU.mult),
                 reads=[cen[r].k(h), sk, lng.k()], writes=[cen[r].k(h)])
        P.op("dve", lambda e, i=i, r=r: e.tensor_tensor(yc.ap[:, i, :], cen[r].ap.rearrange("p h c -> p (h c)"), og[r].ap, op=ALU.mult),
             reads=[cen[r].k(h) for h in range(4)] + [og[r].k()], writes=[yc.k(i)])
    A.release(m3)
    if k.dbg and "yc" in k.dbg:
        t = k.nc.dram_tensor("dbg_yc", [S, 384], BF16, kind="ExternalOutput")
        k.final_events.append(P.dma("sp", t.ap().rearrange("(i p) c -> p i c", p=128), yc.ap, reads=[yc.k(i) for i in range(NT)], semkey="dbg"))
    ycT = A.alloc("ycT", (3, S), BF16)
    transpose_to_T(k, yc, 3, ycT)
    outproj_partial(k, l, ycT, 3, 640, "c")
    A.release(m0)


OFF_RKV = 768
BLK = 256
PC_MU, PC_MUX, PC_W0, PC_A0, PC_KK, PC_KA, PC_RK, PC_LNG, PC_LNB, NPC = 0, 18, 20, 26, 32, 35, 38, 41, 44, 48


def host_rwkv_layout(inputs):
    out = {}
    pc = np.zeros((DEPTH, 128, NPC), np.float32)
    for l in range(DEPTH):
        mu = np.asarray(inputs["rk_mu"][l], np.float32)
        for d in range(2):
            for j in range(3):
                for p in range(3):
                    pc[l, :, PC_MU + d * 9 + j * 3 + p] = mu[d, j * 384 + p * 128: j * 384 + (p + 1) * 128]
            pc[l, 0:64, PC_MUX + d] = mu[d, 1152:1216]
            pc[l, 64:128, PC_MUX + d] = mu[d, 1216:1280]
            for p in range(3):
                pc[l, :, PC_W0 + d * 3 + p] = inputs["rk_w0"][l][d][p * 128:(p + 1) * 128]
                pc[l, :, PC_A0 + d * 3 + p] = inputs["rk_a0"][l][d][p * 128:(p + 1) * 128]
        for p in range(3):
            sl = slice(p * 128, (p + 1) * 128)
            pc[l, :, PC_KK + p] = inputs["rk_kk"][l][sl]
            pc[l, :, PC_KA + p] = inputs["rk_ka"][l][sl]
            pc[l, :, PC_RK + p] = np.asarray(inputs["rk_rk"][l]).reshape(384)[sl]
            pc[l, :, PC_LNG + p] = inputs["rk_ln_g"][l][sl]
            pc[l, :, PC_LNB + p] = inputs["rk_ln_b"][l][sl]
    out["c_pc"] = pc
    w_in = np.asarray(inputs["w_in"], np.float32)
    out["c_wx"] = np.ascontiguousarray(np.concatenate(
        [w_in[:, :, 1920:1984], w_in[:, :, 2048:2112], w_in[:, :, 1984:2048], w_in[:, :, 2112:2176], w_in[:, :, 2176:2304]], axis=2))
    return out


def rev_ap(ap, n):
    a = ap.ap
    return bass.AP(ap.tensor, ap.offset + (n - 1) * a[-1][0], [list(a[0]), [-a[-1][0], n]])


def psk(b, c0, c1):
    return [f"ps{b}q{q}" for q in range(c0 // 128, (c1 + 127) // 128)]


def rwkv_stage(k, l):
    P, A, ps, IN = k.P, k.A, k.ps, k.IN
    m0 = A.mark()
    k.mask4 = A.alloc("mask4", (512,)); k.rmask = A.alloc("rmask", (256,))
    P.dma("act", k.mask4.ap, IN["c_mask4"].ap(), writes=[k.mask4.k()], semkey="c5")
    P.dma("act", k.rmask.ap, IN["c_rmask"].ap(), writes=[k.rmask.k()], semkey="c7")
    ybT = A.alloc("ybT", (3, S), BF16)
    P.op("pool", lambda e: e.memset(ybT.ap, 0.0), writes=[ybT.k(i) for i in range(NT)])
    xsp = k.xspill.ap().rearrange("(i p) d -> p i d", p=128)
    for i in range(NT):
        P.dma("sp" if i % 2 == 0 else "act", xsp[:, i, :], k.xres.ap[:, i, :], reads=[k.xres.k(i)], writes=[("xsp", i)], semkey=("xso", i % 4))
    evs = []
    for kk_ in [q for q in P.keys if isinstance(q, tuple) and q[0] == k.xres.uid]:
        st = P.keys.pop(kk_)
        evs.extend(st["w"]); evs.extend(st["r"].values())
    AX = Arena.__new__(Arena)
    AX.P, AX.t, AX.n, AX.top, AX.gen, AX.live = P, A.t, k.xres.hi, k.xres.lo, 100000 + 1000 * l, []
    AX.pending = [(k.xres.lo, k.xres.hi, evs)]
    import os as _os
    LVL = int(_os.environ.get("RWKV_SETUP", "9"))
    pc = A.alloc("pc", (NPC + 4,))
    P.dma("sp", pc.ap[:, 0:NPC], IN["c_pc"].ap()[l], writes=[pc.k()], semkey="pc")
    PCO = NPC
    P.op("dve", lambda e: e.tensor_scalar(pc.ap[:, PCO:PCO + 3], pc.ap[:, PC_KA:PC_KA + 3], -1.0, 1.0, op0=ALU.mult, op1=ALU.add), reads=[pc.k()], writes=[pc.k()])
    wl = [A.alloc(f"wl{d}", (384,)) for d in range(2)]
    for d in range(2):
        P.dma("sp", wl[d].ap[0:64, :], IN["rk_w2"].ap()[l][d], writes=[wl[d].k()], semkey=("wl", d))
        P.dma("act", wl[d].ap[64:128, :], IN["rk_a2"].ap()[l][d], writes=[wl[d].k()], semkey=("wl2", d))
    g2b = A.alloc("g2b", (384,), BF16)
    P.dma("pool", g2b.ap, IN["rk_g2"].ap()[l], writes=[g2b.k()], semkey="g2b")
    siggd = A.alloc("siggd", (S,), BF16)
    wdad = [(A if int(_os.environ.get("WDAD_MAIN", "0")) else AX).alloc(f"wdad{d}", (S,)) for d in range(2)]
    mA = A.mark()
    wx = load_w_bf16(k, "wx", IN["c_wx"].ap()[l], 8, 384, "wx")
    zx = A.alloc("zx", (S + 2,)); tmp = A.alloc("tmpx", (S,))
    P.op("pool", lambda e: e.memset(zx.ap[:, 0:1], 0.0), writes=[zx.k()])
    P.op("pool", lambda e: e.memset(zx.ap[:, S + 1:S + 2], 0.0), writes=[zx.k()])
    n = 0
    for d in range(2 if LVL >= 2 else 0):
        for tg in range(4):
            b = n % 2; n += 1
            for c in range(8):
                P.op("pe", lambda e, c=c, d=d, tg=tg, b=b: e.matmul(ps[b][:, :], wx.ap[:, c, d * 128:(d + 1) * 128], k.hT.ap[:, c, tg * 512:(tg + 1) * 512], start=(c == 0), stop=(c == 7)),
                     reads=[wx.k(c)] + [k.hT.k(4 * tg + q) for q in range(4)], writes=[f"ps{b}"])
            P.op("act", lambda e, tg=tg, b=b: e.copy(zx.ap[:, 1 + tg * 512:1 + (tg + 1) * 512], ps[b][:, :]), reads=[f"ps{b}"], writes=[zx.k()])
        if LVL < 3:
            continue
        if d == 0:
            cur, prv = zx.ap[:, 1:S + 1], zx.ap[:, 0:S]
        else:
            cur, prv = rev_ap(zx.ap[:, 1:S + 1], S), rev_ap(zx.ap[:, 2:S + 2], S)
        P.op("dve", lambda e, cur=cur, prv=prv: e.tensor_tensor(tmp.ap, prv, cur, op=ALU.subtract), reads=[zx.k()], writes=[tmp.k()])
        P.op("dve", lambda e, cur=cur, d=d: e.scalar_tensor_tensor(wdad[d].ap, tmp.ap, pc.ap[:, PC_MUX + d:PC_MUX + d + 1], cur, op0=ALU.mult, op1=ALU.add),
             reads=[tmp.k(), pc.k(), zx.k()], writes=[wdad[d].k()])
        P.op("act", lambda e, d=d: e.activation(wdad[d].ap[0:64, :], wdad[d].ap[0:64, :], AF.Tanh), reads=[wdad[d].k()], writes=[wdad[d].k()])
    for tg in range(4 if LVL >= 4 else 0):
        b = n % 2; n += 1
        for c in range(8):
            P.op("pe", lambda e, c=c, tg=tg, b=b: e.matmul(ps[b][:, :], wx.ap[:, c, 256:384], k.hT.ap[:, c, tg * 512:(tg + 1) * 512], start=(c == 0), stop=(c == 7)),
                 reads=[wx.k(c)] + [k.hT.k(4 * tg + q) for q in range(4)], writes=[f"ps{b}"])
        P.op("act", lambda e, tg=tg, b=b: e.activation(siggd.ap[:, tg * 512:(tg + 1) * 512], ps[b][:, :], AF.Sigmoid), reads=[f"ps{b}"], writes=[siggd.k()])
    A.release(mA)
    NB = ["XR", "XK", "XV", "T1", "LW", "AA", "KK", "SQ", "CUM"]
    NB16 = ["XRb", "XKb", "KKb", "AAb", "XVb", "BH", "KH"]
    SB = []
    for s_ in range(2):
        sb = {nm: A.alloc(f"{nm}{s_}", (BLK,)) for nm in NB}
        sb.update({nm: A.alloc(f"{nm}{s_}", (BLK,), BF16) for nm in NB16})
        sb["PCc"] = A.alloc(f"PCc{s_}", (BLK // 128,))
        sb["AR"] = A.alloc(f"AR{s_}", (BLK // 128, 256), BF16)
        sb["FT"] = A.alloc(f"FT{s_}", (512,))
        sb["AM"] = [A.alloc(f"AM{s_}{x}", (512,), BF16) for x in range(2)]
        sb["M"] = [[A.alloc(f"M{s_}{x}{q}", (128,), BF16) for q in range(2)] for x in range(2)]
        sb["MT"] = [[A.alloc(f"MT{s_}{x}{q}", (128,), BF16) for q in range(2)] for x in range(2)]
        sb["B"] = [[A.alloc(f"Bq{s_}{x}{q}", (384,), BF16) for q in range(2)] for x in range(2)]
        sb["Q"] = [A.alloc(f"Q{s_}{x}", (128,)) for x in range(2)]
        sb["Q16"] = [A.alloc(f"Qb{s_}{x}", (128,), BF16) for x in range(2)]
        sb["RHS"] = A.alloc(f"RHS{s_}", (128,), BF16)
        sb["SAz"] = [A.alloc(f"SAz{s_}{x}", (128,), BF16) for x in range(2)]
        sb["Vz"] = [A.alloc(f"Vz{s_}{x}", (128,), BF16) for x in range(2)]
        sb["BHt"] = A.alloc(f"BHt{s_}", (128,), BF16); sb["KHt"] = A.alloc(f"KHt{s_}", (128,), BF16)
        sb["T"] = A.alloc(f"Tst{s_}", (128,)); sb["T16"] = A.alloc(f"Tsb{s_}", (128,), BF16)
        sb["pb"] = 4 * s_
        SB.append(sb)
    import os as _os
    for p in range(int(_os.environ.get('RWKV_PAIRS', '3'))):
        mX = AX.mark()
        wp = AX.alloc("wp", (8, 3, 128), BF16)
        wv_ = IN["w_in"].ap()[l].rearrange("(c q) n -> q c n", q=128)
        for j in range(3):
            c0 = OFF_RKV + j * 384 + p * 128
            P.dma("pool", wp.ap[:, :, j, :], wv_[:, :, c0:c0 + 128], writes=[wp.k(j)], semkey=("wp", j))
        zp = AX.alloc("zp", (3, S + 2))
        yacc = AX.alloc("yacc", (S,)); bonacc = AX.alloc("bonacc", (S,))
        P.op("pool", lambda e: e.memset(yacc.ap, 0.0), writes=[yacc.k(c) for c in range(NT)])
        P.op("pool", lambda e: e.memset(bonacc.ap, 0.0), writes=[bonacc.k(c) for c in range(NT)])
        for j in range(3):
            P.op("pool", lambda e, j=j: e.memset(zp.ap[:, j, 0:1], 0.0), writes=[zp.k(j)])
            P.op("pool", lambda e, j=j: e.memset(zp.ap[:, j, S + 1:S + 2], 0.0), writes=[zp.k(j)])
            for tg in range(4):
                b = n % 2; n += 1
                for c in range(8):
                    P.op("pe", lambda e, c=c, j=j, tg=tg, b=b: e.matmul(ps[b][:, :], wp.ap[:, c, j, :], k.hT.ap[:, c, tg * 512:(tg + 1) * 512], start=(c == 0), stop=(c == 7)),
                         reads=[wp.k(j)] + [k.hT.k(4 * tg + q) for q in range(4)], writes=[f"ps{b}"] + psk(b, 0, 512))
                P.op("act", lambda e, j=j, tg=tg, b=b: e.copy(zp.ap[:, j, 1 + tg * 512:1 + (tg + 1) * 512], ps[b][:, :]), reads=[f"ps{b}"] + psk(b, 0, 512), writes=[zp.k(j)])
        gens = [rwkv_stream(k, l, p, d, SB[d], pc, wl[d], wdad[d], zp, yacc, bonacc, PCO) for d in range(int(_os.environ.get("RWKV_NDIR", "2")))]
        while gens:
            for g in list(gens):
                try:
                    next(g)
                except StopIteration:
                    gens.remove(g)
        T1 = SB[0]["FT"]; T2 = SB[1]["FT"]
        for tg in range(4 if int(_os.environ.get("RWKV_FIN", "1")) else 0):
            cs = slice(tg * 512, (tg + 1) * 512)
            yk = [yacc.k(c) for c in range(4 * tg, 4 * tg + 4)]
            P.op("pe", lambda e, cs=cs: e.matmul(ps[0][:, :], k.onesblk64.ap, yacc.ap[:, cs], start=True, stop=True), reads=[k.onesblk64.k()] + yk, writes=["ps0"] + psk(0, 0, 512))
            P.op("dve", lambda e, cs=cs: e.tensor_tensor(yacc.ap[:, cs], yacc.ap[:, cs], ps[0][:, :], op=ALU.subtract), reads=["ps0"] + psk(0, 0, 512) + yk, writes=yk)
            P.op("act", lambda e, cs=cs: e.activation(T1.ap, yacc.ap[:, cs], AF.Square), reads=yk, writes=[T1.k()])
            P.op("pe", lambda e: e.matmul(ps[1][:, :], k.onesblk64.ap, T1.ap, start=True, stop=True), reads=[k.onesblk64.k(), T1.k()], writes=["ps1"] + psk(1, 0, 512))
            P.op("act", lambda e: e.activation(T2.ap, ps[1][:, :], AF.Sqrt, bias=k.epsgn.ap[:, 0:1]), reads=["ps1", k.epsgn.k()] + psk(1, 0, 512), writes=[T2.k()])
            P.op("dve", lambda e: e.reciprocal(T2.ap, T2.ap), reads=[T2.k()], writes=[T2.k()])
            P.op("dve", lambda e, cs=cs: e.tensor_tensor(yacc.ap[:, cs], yacc.ap[:, cs], T2.ap, op=ALU.mult), reads=yk + [T2.k()], writes=yk)
            P.op("dve", lambda e, cs=cs, p=p: e.tensor_scalar(yacc.ap[:, cs], yacc.ap[:, cs], pc.ap[:, PC_LNG + p:PC_LNG + p + 1], pc.ap[:, PC_LNB + p:PC_LNB + p + 1], op0=ALU.mult, op1=ALU.add),
                 reads=yk + [pc.k()], writes=yk)
            P.op("dve", lambda e, cs=cs: e.tensor_tensor(yacc.ap[:, cs], yacc.ap[:, cs], bonacc.ap[:, cs], op=ALU.add), reads=yk + [bonacc.k(c) for c in range(4 * tg, 4 * tg + 4)], writes=yk)
            P.op("pe", lambda e, cs=cs, p=p: e.matmul(ps[2][:, :], g2b.ap[:, p * 128:(p + 1) * 128], siggd.ap[:, cs], start=True, stop=True), reads=[g2b.k(), siggd.k()], writes=["ps2"] + psk(2, 0, 512))
            P.op("dve", lambda e, cs=cs, p=p: e.tensor_tensor(ybT.ap[:, p, cs], yacc.ap[:, cs], ps[2][:, :], op=ALU.mult), reads=yk + ["ps2"] + psk(2, 0, 512), writes=[ybT.k(c) for c in range(4 * tg, 4 * tg + 4)])
        AX.release(mX)
    AX.release((k.xres.lo, 0))
    evs = []
    for (_, _, e_) in AX.pending:
        evs.extend(e_)
    P.inherit[k.xres.uid] = evs
    for i in range(NT):
        P.dma("sp" if i % 2 == 0 else "act", k.xres.ap[:, i, :], xsp[:, i, :], reads=[("xsp", i)], writes=[k.xres.k(i)], semkey=("xsi", i % 4))
    if k.dbg and "yb" in k.dbg:
        t = k.nc.dram_tensor("dbg_yb", [384, S], BF16, kind="ExternalOutput")
        k.final_events.append(P.dma("sp", t.ap().rearrange("(c p) s -> p c s", p=128), ybT.ap, reads=[ybT.k(i) for i in range(NT)], semkey="dbg"))
    if LVL >= 5:
        outproj_partial(k, l, ybT, 3, 256, "b")
    A.release(m0)


def rwkv_stream(k, l, p, d, sb, pc, wl, wdad, zp, yacc, bonacc, PCO):
    P, ps = k.P, k.ps
    pb = sb["pb"]
    B0, B1, B2, B3 = pb, pb + 1, pb + 2, pb + 3
    XR, XK, XV, T1, LW, AA, KK, SQ, CUM, BH, KH, PCc = (sb[n_] for n_ in ["XR", "XK", "XV", "T1", "LW", "AA", "KK", "SQ", "CUM", "BH", "KH", "PCc"])
    XRb, XKb, KKb, AAb, XVb = (sb[n_] for n_ in ["XRb", "XKb", "KKb", "AAb", "XVb"])
    T = sb["T"]; T16 = sb["T16"]
    P.op("pool", lambda e: e.memset(T16.ap, 0.0), writes=[T16.k()])
    col = lambda c: pc.ap[:, c:c + 1]
    P.op("pool", lambda e: e.memset(T.ap, 0.0), writes=[T.k()])
    for x in range(2):
        P.op("pool", lambda e, x=x: e.memset(sb["SAz"][x].ap, 0.0), writes=[sb["SAz"][x].k()])
        P.op("pool", lambda e, x=x: e.memset(sb["Vz"][x].ap, 0.0), writes=[sb["Vz"][x].k()])
    import os as _os
    NBLK = int(_os.environ.get("RWKV_NBLK", str(S // BLK))); PH = int(_os.environ.get("RWKV_PHASE", "9"))
    def _blk(bi):
        t0 = bi * BLK
        if d == 0:
            cur = lambda j: zp.ap[:, j, 1 + t0:1 + t0 + BLK]
            prv = lambda j: zp.ap[:, j, t0:t0 + BLK]
            nat = lambda buf: buf.ap[:, t0:t0 + BLK]
            nchunks = list(range(t0 // 128, (t0 + BLK) // 128))
        else:
            a_ = S - t0 - BLK
            cur = lambda j: rev_ap(zp.ap[:, j, 1 + a_:1 + a_ + BLK], BLK)
            prv = lambda j: rev_ap(zp.ap[:, j, 2 + a_:2 + a_ + BLK], BLK)
            nat = lambda buf: rev_ap(buf.ap[:, a_:a_ + BLK], BLK)
            nchunks = list(range(a_ // 128, (a_ + BLK) // 128))
        scols = slice(t0, t0 + BLK)
        P0 = int(_os.environ.get("RWKV_P0", "9"))
        for j, X in enumerate((XR, XK, XV)):
            if P0 < 2:
                break
            P.op("dve", lambda e, j=j, prv=prv, cur=cur: e.tensor_tensor(T1.ap, prv(j), cur(j), op=ALU.subtract), reads=[zp.k(j)], writes=[T1.k()])
            P.op("dve", lambda e, j=j, X=X, cur=cur: e.scalar_tensor_tensor(X.ap, T1.ap, col(PC_MU + d * 9 + j * 3 + p), cur(j), op0=ALU.mult, op1=ALU.add),
                 reads=[T1.k(), zp.k(j), pc.k()], writes=[X.k()])
        if P0 >= 3:
            P.op("pe", lambda e, scols=scols: e.matmul(ps[B3][:, 0:BLK], wl.ap[0:64, p * 128:(p + 1) * 128], wdad.ap[0:64, scols], start=True, stop=True),
                 reads=[wl.k(), wdad.k()], writes=psk(B3, 0, BLK), serial=True)
        if P0 >= 4:
            P.op("pe", lambda e, scols=scols: e.matmul(ps[B3][:, 256:256 + BLK], wl.ap[64:128, p * 128:(p + 1) * 128], wdad.ap[64:128, scols], start=True, stop=True),
                 reads=[wl.k(), wdad.k()], writes=psk(B3, 256, 256 + BLK), serial=True)
        if P0 >= 5:
            P.op("act", lambda e: e.activation(LW.ap, ps[B3][:, 0:BLK], AF.Sigmoid, bias=col(PC_W0 + d * 3 + p)), reads=psk(B3, 0, BLK) + [pc.k()], writes=[LW.k()])
            P.op("act", lambda e: e.activation(AA.ap, ps[B3][:, 256:256 + BLK], AF.Sigmoid, bias=col(PC_A0 + d * 3 + p)), reads=psk(B3, 256, 256 + BLK) + [pc.k()], writes=[AA.k()])
        if P0 >= 6:
            P.op("pool", lambda e: e.tensor_scalar(LW.ap, LW.ap, -0.6065306597126334, None, op0=ALU.mult), reads=[LW.k()], writes=[LW.k()])
        yield
        if PH <= 1:
            return
        P.op("act", lambda e: e.activation(KK.ap, XK.ap, AF.Copy, scale=col(PC_KK + p)), reads=[XK.k(), pc.k()], writes=[KK.k()])
        P.op("act", lambda e: e.activation(SQ.ap, KK.ap, AF.Square), reads=[KK.k()], writes=[SQ.k()])
        P.op("pe", lambda e: e.matmul(ps[B2][:, 0:BLK], k.onesblk.ap, SQ.ap, start=True, stop=True), reads=[k.onesblk.k(), SQ.k()], writes=psk(B2, 0, BLK))
        P.op("act", lambda e: e.activation(SQ.ap, ps[B2][:, 0:BLK], AF.Sqrt), reads=psk(B2, 0, BLK), writes=[SQ.k()])
        P.op("dve", lambda e: e.tensor_scalar(SQ.ap, SQ.ap, 1e-12, None, op0=ALU.max), reads=[SQ.k()], writes=[SQ.k()])
        P.op("dve", lambda e: e.reciprocal(SQ.ap, SQ.ap), reads=[SQ.k()], writes=[SQ.k()])
        P.op("dve", lambda e: e.tensor_tensor(KK.ap, KK.ap, SQ.ap, op=ALU.mult), reads=[KK.k(), SQ.k()], writes=[KK.k()])
        P.op("pool", lambda e: e.tensor_scalar(T1.ap, AA.ap, col(PC_KA + p), col(PCO + p), op0=ALU.mult, op1=ALU.add), reads=[AA.k(), pc.k()], writes=[T1.k()])
        P.op("pool", lambda e: e.tensor_tensor(XK.ap, XK.ap, T1.ap, op=ALU.mult), reads=[XK.k(), T1.k()], writes=[XK.k()])
        P.op("dve", lambda e: e.scalar_tensor_tensor(T1.ap, XR.ap, col(PC_RK + p), XK.ap, op0=ALU.mult, op1=ALU.mult), reads=[XR.k(), XK.k(), pc.k()], writes=[T1.k()])
        P.op("pe", lambda e: e.matmul(ps[B2][:, 256:256 + BLK], k.onesblk.ap, T1.ap, start=True, stop=True), reads=[k.onesblk.k(), T1.k()], writes=psk(B2, 256, 256 + BLK))
        P.op("dve", lambda e: e.tensor_tensor(T1.ap, ps[B2][:, 256:256 + BLK], XV.ap, op=ALU.mult), reads=psk(B2, 256, 256 + BLK) + [XV.k()], writes=[T1.k()])
        bk = [bonacc.k(c) for c in nchunks]
        P.op("dve", lambda e: e.tensor_tensor(nat(bonacc), nat(bonacc), T1.ap, op=ALU.add), reads=bk + [T1.k()], writes=bk)
        P.op("pool", lambda e: e.tensor_tensor(AA.ap, AA.ap, KK.ap, op=ALU.mult), reads=[AA.k(), KK.k()], writes=[AA.k()])
        yield
        if PH <= 2:
            return
        P.op("dve", lambda e: e.tensor_tensor_scan(CUM.ap, k.rmask.ap[:, 0:BLK], LW.ap, 0.0, op0=ALU.mult, op1=ALU.add), reads=[k.rmask.k(), LW.k()], writes=[CUM.k()])
        P.op("pool", lambda e: e.tensor_tensor(LW.ap, CUM.ap, LW.ap, op=ALU.subtract), reads=[CUM.k(), LW.k()], writes=[LW.k()])
        P.op("act", lambda e: e.activation(T1.ap, CUM.ap, AF.Exp), reads=[CUM.k()], writes=[T1.k()])
        P.op("dve", lambda e: e.tensor_tensor(XR.ap, XR.ap, T1.ap, op=ALU.mult), reads=[XR.k(), T1.k()], writes=[XR.k()])
        P.op("act", lambda e: e.activation(SQ.ap, CUM.ap, AF.Exp, scale=-1.0), reads=[CUM.k()], writes=[SQ.k()])
        P.op("dve", lambda e: e.tensor_tensor(AA.ap, AA.ap, SQ.ap, op=ALU.mult), reads=[AA.k(), SQ.k()], writes=[AA.k()])
        P.op("dve", lambda e: e.tensor_tensor(XK.ap, XK.ap, SQ.ap, op=ALU.mult), reads=[XK.k(), SQ.k()], writes=[XK.k()])
        P.op("act", lambda e: e.activation(T1.ap, LW.ap, AF.Exp), reads=[LW.k()], writes=[T1.k()])
        P.op("dve", lambda e: e.scalar_tensor_tensor(KK.ap, KK.ap, -1.0, T1.ap, op0=ALU.mult, op1=ALU.mult), reads=[KK.k(), T1.k()], writes=[KK.k()])
        AR = sb["AR"]
        for src_, dst_ in ((XK, XKb), (AA, AAb), (XV, XVb)):
            P.op("act", lambda e, src_=src_, dst_=dst_: e.copy(dst_.ap, src_.ap), reads=[src_.k()], writes=[dst_.k()])
        P.op("act", lambda e: e.copy(AR.ap[:, :, 0:128], KK.ap.rearrange("p (c t) -> p c t", t=128)), reads=[KK.k()], writes=[AR.k()])
        P.op("act", lambda e: e.copy(AR.ap[:, :, 128:256], XR.ap.rearrange("p (c t) -> p c t", t=128)), reads=[XR.k()], writes=[AR.k()])
        P.op("act", lambda e: e.activation(PCc.ap, CUM.ap.rearrange("p (c t) -> p c t", t=128)[:, :, 127], AF.Exp), reads=[CUM.k()], writes=[PCc.k()])
        for ch in range(BLK // 128):
            cs = slice(ch * 128, (ch + 1) * 128)
            P.op("act", lambda e, cs=cs, ch=ch: e.activation(BH.ap[:, cs], AA.ap[:, cs], AF.Copy, scale=PCc.ap[:, ch:ch + 1]), reads=[AA.k(), PCc.k()], writes=[BH.k()])
            P.op("act", lambda e, cs=cs, ch=ch: e.activation(KH.ap[:, cs], XK.ap[:, cs], AF.Copy, scale=PCc.ap[:, ch:ch + 1]), reads=[XK.k(), PCc.k()], writes=[KH.k()])
        yield
        if PH <= 3:
            return
        def _chunk(ch):
            cs = slice(ch * 128, (ch + 1) * 128)
            AM, Mb, MTb, Q, RHS, SAz, Vz, BHt, KHt = sb["AM"], sb["M"], sb["MT"], sb["Q"], sb["RHS"], sb["SAz"], sb["Vz"], sb["BHt"], sb["KHt"]
            for x in range(2):
                hs = slice(64 * x, 64 * x + 64)
                bx = B0 + x
                for q, lh in enumerate((AAb, XKb)):
                    P.op("pe", lambda e, lh=lh, q=q, hs=hs, bx=bx: e.matmul(ps[bx][:, q * 256:(q + 1) * 256], lh.ap[hs, cs], sb["AR"].ap[hs, ch, :], start=True, stop=True),
                         reads=[lh.k(), sb["AR"].k()], writes=psk(bx, q * 256, (q + 1) * 256), serial=True)
                P.op("pe", lambda e, hs=hs, x=x: e.matmul(ps[B2][:, x * 128:(x + 1) * 128], sb["AR"].ap[hs, ch, 0:128], AAb.ap[hs, cs], start=True, stop=True),
                     reads=[sb["AR"].k(), AAb.k()], writes=psk(B2, x * 128, (x + 1) * 128), serial=True)
                P.op("dve", lambda e, x=x, bx=bx: e.tensor_tensor(AM[x].ap, ps[bx][:, :], k.mask4.ap, op=ALU.mult), reads=psk(bx, 0, 512) + [k.mask4.k()], writes=[AM[x].k()])
                P.op("dve", lambda e, x=x: e.tensor_tensor(MTb[x][0].ap, ps[B2][:, x * 128:(x + 1) * 128], k.trils.ap, op=ALU.mult), reads=psk(B2, x * 128, (x + 1) * 128) + [k.trils.k()], writes=[MTb[x][0].k()])
            yield
            if PH <= 4:
                return
            pbt = ps[B3][:, 0:192].bitcast(BF16)
            for q, src in enumerate((XVb, BH, KH)):
                P.op("pe", lambda e, q=q, src=src: e.transpose(pbt[:, q * 128:(q + 1) * 128], src.ap[:, cs], k.ident16.ap), reads=[src.k(), k.ident16.k()], writes=psk(B3, 0, 192))
            P.op("act", lambda e: e.copy(Vz[0].ap[:, 0:64], pbt[:, 0:64]), reads=psk(B3, 0, 192), writes=[Vz[0].k()])
            P.op("act", lambda e: e.copy(Vz[1].ap[:, 64:128], pbt[:, 64:128]), reads=psk(B3, 0, 192), writes=[Vz[1].k()])
            P.op("dve", lambda e: e.tensor_copy(BHt.ap, pbt[:, 128:256]), reads=psk(B3, 0, 192), writes=[BHt.k()])
            P.op("dve", lambda e: e.tensor_copy(KHt.ap, pbt[:, 256:384]), reads=psk(B3, 0, 192), writes=[KHt.k()])
            Bq = sb["B"]
            for x in range(2):
                bx = B0 + x
                P.op("pe", lambda e, bx=bx, x=x: e.matmul(ps[bx][:, 0:128], AM[x].ap[:, 0:128], MTb[x][0].ap, start=True, stop=True), reads=[AM[x].k(), MTb[x][0].k()], writes=psk(bx, 0, 128))
                P.op("pe", lambda e, bx=bx, x=x: e.matmul(ps[bx][:, 128:256], MTb[x][0].ap, AM[x].ap[:, 0:128], start=True, stop=True), reads=[AM[x].k(), MTb[x][0].k()], writes=psk(bx, 128, 256))
                P.op("act", lambda e, bx=bx, x=x: e.copy(Bq[x][1].ap[:, 0:256], ps[bx][:, 0:256]), reads=psk(bx, 0, 256), writes=[Bq[x][1].k()])
                P.op("dve", lambda e, x=x: e.tensor_tensor(Bq[x][1].ap[:, 256:384], AM[x].ap[:, 0:128], k.ident.ap, op=ALU.add), reads=[AM[x].k(), k.ident.k()], writes=[Bq[x][1].k()])
            yield
            for lev in range(1, 7):
                cur, nxt = lev % 2, 1 - lev % 2
                for x in range(2):
                    bx = B0 + x
                    Bc, Bn = Bq[x][cur], Bq[x][nxt]
                    if lev < 6:
                        P.op("pe", lambda e, bx=bx, Bc=Bc: e.matmul(ps[bx][:, 0:128], Bc.ap[:, 128:256], Bc.ap[:, 0:128], start=True, stop=True), reads=[Bc.k()], writes=psk(bx, 0, 128))
                        P.op("pe", lambda e, bx=bx, Bc=Bc: e.matmul(ps[bx][:, 128:384], Bc.ap[:, 0:128], Bc.ap[:, 128:384], start=True, stop=True), reads=[Bc.k()], writes=psk(bx, 128, 384))
                        P.op("act", lambda e, bx=bx, Bn=Bn: e.copy(Bn.ap[:, 0:256], ps[bx][:, 0:256]), reads=psk(bx, 0, 384), writes=[Bn.k()])
                        P.op("dve", lambda e, bx=bx, Bc=Bc, Bn=Bn: e.tensor_tensor(Bn.ap[:, 256:384], Bc.ap[:, 256:384], ps[bx][:, 256:384], op=ALU.add), reads=psk(bx, 0, 384) + [Bc.k()], writes=[Bn.k()])
                    else:
                        P.op("pe", lambda e, bx=bx, Bc=Bc: e.matmul(ps[bx][:, 256:384], Bc.ap[:, 0:128], Bc.ap[:, 256:384], start=True, stop=True), reads=[Bc.k()], writes=psk(bx, 256, 384))
                        P.op("dve", lambda e, bx=bx, Bc=Bc, x=x: e.tensor_tensor(sb["Q16"][x].ap, Bc.ap[:, 256:384], ps[bx][:, 256:384], op=ALU.add), reads=psk(bx, 0, 384) + [Bc.k()], writes=[sb["Q16"][x].k()])
                yield
            for x in range(2):
                hs = slice(64 * x, 64 * x + 64)
                P.op("pe", lambda e, hs=hs: e.matmul(ps[B2][:, 256 + hs.start:256 + hs.stop], sb["AR"].ap[hs, ch, 0:128], T16.ap[hs, hs], start=True, stop=False), reads=[sb["AR"].k(), T16.k()], writes=psk(B2, 256, 384), serial=True)
                P.op("pe", lambda e, hs=hs, x=x: e.matmul(ps[B2][:, 256 + hs.start:256 + hs.stop], AM[x].ap[:, 256:384], Vz[x].ap[:, hs], start=False, stop=True), reads=[AM[x].k(), Vz[x].k()], writes=psk(B2, 256, 384))
            P.op("act", lambda e: e.copy(RHS.ap, ps[B2][:, 256:384]), reads=psk(B2, 256, 384), writes=[RHS.k()])
            for x in range(2):
                hs = slice(64 * x, 64 * x + 64)
                P.op("pe", lambda e, hs=hs, x=x: e.matmul(ps[B2][:, 384 + hs.start:384 + hs.stop], sb["Q16"][x].ap, RHS.ap[:, hs], start=True, stop=True), reads=[sb["Q16"][x].k(), RHS.k()], writes=psk(B2, 384, 512))
                P.op("act" if x == 0 else "dve", (lambda e, hs=hs, x=x: e.copy(SAz[x].ap[:, hs], ps[B2][:, 384 + hs.start:384 + hs.stop])) if x == 0 else
                     (lambda e, hs=hs, x=x: e.tensor_copy(SAz[x].ap[:, hs], ps[B2][:, 384 + hs.start:384 + hs.stop])), reads=psk(B2, 384, 512), writes=[SAz[x].k()])
            yield
            if PH <= 6:
                return
            ops_ = []
            for x in range(2):
                hs = slice(64 * x, 64 * x + 64)
                ops_.append((T16.ap[hs, :], sb["AR"].ap[hs, ch, 128:256], [T16.k(), sb["AR"].k()]))
                ops_.append((SAz[x].ap, AM[x].ap[:, 128:256], [SAz[x].k(), AM[x].k()]))
                ops_.append((Vz[x].ap, AM[x].ap[:, 384:512], [Vz[x].k(), AM[x].k()]))
            for q, (lh, rh, rd) in enumerate(ops_):
                P.op("pe", lambda e, lh=lh, rh=rh, q=q: e.matmul(ps[B3][:, 384:512], lh, rh, start=(q == 0), stop=(q == len(ops_) - 1)), reads=rd, writes=psk(B3, 384, 512), serial=(q % 3 == 0))
            cn = nchunks[ch] if d == 0 else nchunks[len(nchunks) - 1 - ch]
            if d == 0:
                ydst = yacc.ap[:, cn * 128:(cn + 1) * 128]
            else:
                ydst = rev_ap(yacc.ap[:, cn * 128:(cn + 1) * 128], 128)
            P.op("dve", lambda e, ydst=ydst: e.tensor_tensor(ydst, ydst, ps[B3][:, 384:512], op=ALU.add), reads=psk(B3, 384, 512) + [yacc.k(cn)], writes=[yacc.k(cn)])
            for x in range(2):
                hs = slice(64 * x, 64 * x + 64)
                P.op("pe", lambda e, hs=hs, x=x: e.matmul(ps[B2][:, hs], BHt.ap, SAz[x].ap[:, hs], start=True, stop=False), reads=[BHt.k(), SAz[x].k()], writes=psk(B2, 0, 128))
                P.op("pe", lambda e, hs=hs, x=x: e.matmul(ps[B2][:, hs], KHt.ap, Vz[x].ap[:, hs], start=False, stop=True), reads=[KHt.k(), Vz[x].k()], writes=psk(B2, 0, 128))
            for x in range(2):
                hs = slice(64 * x, 64 * x + 64)
                P.op("dve", lambda e, hs=hs, ch=ch: e.scalar_tensor_tensor(T.ap[hs, hs], T.ap[hs, hs], PCc.ap[hs, ch:ch + 1], ps[B2][hs, hs], op0=ALU.mult, op1=ALU.add),
                     reads=[T.k(), PCc.k()] + psk(B2, 0, 128), writes=[T.k()])
            P.op("act", lambda e: e.copy(T16.ap, T.ap), reads=[T.k()], writes=[T16.k()])
            yield
            if PH <= 7:
                return
        for ch in range(BLK // 128):
            yield from _chunk(ch)

    for bi in range(NBLK):
        yield from _blk(bi)


FB = 256
NFB = DFF // FB
NFC = DFF // 128
W2G = 2


def moe_stage(k, l):
    P, A, ps, IN = k.P, k.A, k.ps, k.IN
    m0 = A.mark()
    k.ohb = A.alloc("ohb", (2048,))
    P.dma("act", k.ohb.ap[0:16, :], IN["c_ohb"].ap(), writes=[k.ohb.k()], semkey="c4")
    x2b = A.alloc("x2b", (NT, D), BF16)
    aff = A.alloc("aff", (NT, NE))
    pm = A.alloc("pm", (NT, NE))
    pmT = A.alloc("pmT", (S,))
    m1 = A.mark()
    gb = A.alloc("gb2", (D,)); junk = A.alloc("junk2", (D,)); ss = A.alloc("ss2", (NT,)); rstd = A.alloc("rstd2", (NT,))
    x2f = [A.alloc(f"x2f{i}", (D,)) for i in range(2)]
    x2T = [A.alloc(f"x2T{i}", (8, 128)) for i in range(2)]
    rsb = A.alloc("rsb", (8, NE)); sm = A.alloc("smx", (NT, 4))
    affT = A.alloc("affT", (S,))
    P.dma("sp", gb.ap, IN["ln2_g"].ap()[l].partition_broadcast(128), writes=[gb.k()], semkey="gb")
    P.dma("act", rsb.ap, IN["router"].ap()[l].rearrange("(c p) e -> p c e", p=128), writes=[rsb.k()], semkey="rsb")
    P.op("pool", lambda e: e.memset(ss.ap, 0.0), writes=[ss.k(i) for i in range(NT)])
    P.op("pool", lambda e: e.memset(sm.ap, 0.0), writes=[sm.k()])
    for i in range(NT):
        P.op("act", lambda e, i=i: e.activation(junk.ap, k.xres.ap[:, i, :], AF.Square, accum_out=ss.ap[:, i:i + 1]),
             reads=[k.xres.k(i)], writes=[junk.k(), ss.k(i)])
    P.op("act", lambda e: e.activation(rstd.ap, ss.ap, AF.Sqrt, scale=1.0 / D, bias=k.eps6.ap[:, 0:1]),
         reads=[ss.k(i) for i in range(NT)] + [k.eps6.k()], writes=[rstd.k()])
    P.op("dve", lambda e: e.reciprocal(rstd.ap, rstd.ap), reads=[rstd.k()], writes=[rstd.k()])
    for i in range(NT):
        xf = x2f[i % 2]; xt = x2T[i % 2]
        P.op("dve", lambda e, i=i, xf=xf: e.scalar_tensor_tensor(xf.ap, k.xres.ap[:, i, :], rstd.ap[:, i:i + 1], gb.ap, op0=ALU.mult, op1=ALU.mult),
             reads=[k.xres.k(i), rstd.k(), gb.k()], writes=[xf.k()])
        P.op("act", lambda e, i=i, xf=xf: e.copy(x2b.ap[:, i, :], xf.ap), reads=[xf.k()], writes=[x2b.k(i)])
        for hb_ in range(2):
            b = 2 * (i % 2) + hb_
            for c in range(4):
                cc = hb_ * 4 + c
                P.op("pe", lambda e, b=b, c=c, cc=cc, xf=xf: e.transpose(ps[b][:, c * 128:(c + 1) * 128], xf.ap[:, cc * 128:(cc + 1) * 128], k.ident.ap),
                     reads=[xf.k(), k.ident.k()], writes=[f"ps{b}"])
            P.op("act" if hb_ == 0 else "dve",
                 (lambda e, b=b, hb_=hb_, xt=xt: e.copy(xt.ap[:, hb_ * 4:hb_ * 4 + 4, :], ps[b][:, :].rearrange("p (c t) -> p c t", c=4))) if hb_ == 0 else
                 (lambda e, b=b, hb_=hb_, xt=xt: e.tensor_copy(xt.ap[:, hb_ * 4:hb_ * 4 + 4, :], ps[b][:, :].rearrange("p (c t) -> p c t", c=4))),
                 reads=[f"ps{b}"], writes=[xt.k(hb_)])
        lb = 4 + i % 2
        for c in range(8):
            P.op("pe", lambda e, c=c, lb=lb, xt=xt: e.matmul(ps[lb][:, 0:NE], xt.ap[:, c, :], rsb.ap[:, c, :], start=(c == 0), stop=(c == 7)),
                 reads=[xt.k(0), xt.k(1), rsb.k()], writes=[f"ps{lb}"])
        P.op("dve", lambda e, i=i, lb=lb: e.tensor_reduce(sm.ap[:, i, 0:1], ps[lb][:, 0:NE], axis=AX.X, op=ALU.max), reads=[f"ps{lb}"], writes=[sm.k()])
        P.op("dve", lambda e, i=i: e.tensor_scalar(sm.ap[:, i, 0:1], sm.ap[:, i, 0:1], -1.0, None, op0=ALU.mult), reads=[sm.k()], writes=[sm.k()])
        P.op("act", lambda e, i=i, lb=lb: e.activation(aff.ap[:, i, :], ps[lb][:, 0:NE], AF.Exp, bias=sm.ap[:, i, 0:1], accum_out=sm.ap[:, i, 1:2]),
             reads=[f"ps{lb}", sm.k()], writes=[aff.k(), sm.k()])
        P.op("dve", lambda e, i=i: e.reciprocal(sm.ap[:, i, 2:3], sm.ap[:, i, 1:2]), reads=[sm.k()], writes=[sm.k()])
        P.op("dve", lambda e, i=i: e.tensor_scalar(aff.ap[:, i, :], aff.ap[:, i, :], sm.ap[:, i, 2:3], None, op0=ALU.mult), reads=[aff.k(), sm.k()], writes=[aff.k()])
        tb = 6 + (i // 4) % 2
        P.op("pe", lambda e, i=i, tb=tb: e.transpose(ps[tb][0:NE, (i % 4) * 128:(i % 4 + 1) * 128], aff.ap[:, i, :], k.ident.ap), reads=[aff.k(), k.ident.k()], writes=[f"ps{tb}"])
        if i % 4 == 3:
            P.op("act", lambda e, i=i, tb=tb: e.copy(affT.ap[0:NE, (i - 3) * 128:(i + 1) * 128], ps[tb][0:NE, :]), reads=[f"ps{tb}"], writes=[affT.k()])
    bs = A.alloc("bs", (8,)); bjunk = A.alloc("bjunk", (S,))
    LO, HI, MID, CNT, GE, D1 = (bs.ap[0:NE, j:j + 1] for j in range(6))
    P.op("pool", lambda e: e.memset(bs.ap, 0.0), writes=[bs.k()])
    P.op("pool", lambda e: e.memset(bs.ap[:, 1:2], 1.0), writes=[bs.k()])
    for it in range(30):
        P.op("dve", lambda e: e.tensor_tensor(MID, LO, HI, op=ALU.add), reads=[bs.k()], writes=[bs.k()])
        P.op("dve", lambda e: e.tensor_scalar(MID, MID, 0.5, None, op0=ALU.mult), reads=[bs.k()], writes=[bs.k()])
        P.op("dve", lambda e: e.tensor_scalar(bjunk.ap[0:NE, :], affT.ap[0:NE, :], MID, None, op0=ALU.is_gt, op1=ALU.add, accum_out=CNT),
             reads=[bs.k(), affT.k()], writes=[bs.k(), bjunk.k()])
        P.op("dve", lambda e: e.tensor_scalar(GE, CNT, float(CAP) - 0.5, None, op0=ALU.is_gt), reads=[bs.k()], writes=[bs.k()])
        P.op("dve", lambda e: e.tensor_tensor(D1, MID, LO, op=ALU.subtract), reads=[bs.k()], writes=[bs.k()])
        P.op("dve", lambda e: e.scalar_tensor_tensor(LO, D1, GE, LO, op0=ALU.mult, op1=ALU.add), reads=[bs.k()], writes=[bs.k()])
        P.op("dve", lambda e: e.tensor_tensor(D1, HI, MID, op=ALU.subtract), reads=[bs.k()], writes=[bs.k()])
        P.op("dve", lambda e: e.scalar_tensor_tensor(HI, D1, GE, MID, op0=ALU.mult, op1=ALU.add), reads=[bs.k()], writes=[bs.k()])
    dgt = A.alloc("dgt", (NE,)); thrb = A.alloc("thrb", (NE,))
    P.op("dve", lambda e: e.tensor_scalar(dgt.ap[0:NE, :], k.ident.ap[0:NE, 0:NE], LO, None, op0=ALU.mult), reads=[bs.k(), k.ident.k()], writes=[dgt.k()])
    P.op("pe", lambda e: e.matmul(ps[0][:, 0:NE], k.ones32.ap[0:NE, :], dgt.ap[0:NE, :], start=True, stop=True), reads=[k.ones32.k(), dgt.k()], writes=["ps0"])
    P.op("act", lambda e: e.copy(thrb.ap, ps[0][:, 0:NE]), reads=["ps0"], writes=[thrb.k()])
    mk16 = A.alloc("mk16", (NT, NE), BF16); mk32 = A.alloc("mk32", (NT, NE)); base = A.alloc("basec", (NE,))
    P.op("pool", lambda e: e.memset(base.ap, 0.0), writes=[base.k()])
    for i in range(NT):
        P.op("dve", lambda e, i=i: e.tensor_tensor(mk32.ap[:, i, :], aff.ap[:, i, :], thrb.ap, op=ALU.is_gt), reads=[aff.k(), thrb.k()], writes=[mk32.k(i)])
        P.op("act", lambda e, i=i: e.copy(mk16.ap[:, i, :], mk32.ap[:, i, :]), reads=[mk32.k(i)], writes=[mk16.k(i)])
        b = i % 2
        P.op("pe", lambda e, i=i, b=b: e.matmul(ps[b][:, 0:NE], k.trius16.ap, mk16.ap[:, i, :], start=True, stop=True), reads=[k.trius16.k(), mk16.k(i)], writes=[f"ps{b}"])
        P.op("pe", lambda e, i=i, b=b: e.matmul(ps[b][:, 128:128 + NE], k.ones16.ap, mk16.ap[:, i, :], start=True, stop=True), reads=[k.ones16.k(), mk16.k(i)], writes=[f"ps{b}"])
        P.op("dve", lambda e, i=i, b=b: e.scalar_tensor_tensor(pm.ap[:, i, :], ps[b][:, 0:NE], 1.0, base.ap, op0=ALU.add, op1=ALU.add), reads=[f"ps{b}", base.k()], writes=[pm.k(i)])
        P.op("dve", lambda e, i=i: e.tensor_tensor(pm.ap[:, i, :], pm.ap[:, i, :], mk32.ap[:, i, :], op=ALU.mult), reads=[pm.k(i), mk32.k(i)], writes=[pm.k(i)])
        P.op("dve", lambda e, i=i: e.tensor_scalar(pm.ap[:, i, :], pm.ap[:, i, :], -1.0, None, op0=ALU.add), reads=[pm.k(i)], writes=[pm.k(i)])
        P.op("dve", lambda e, b=b: e.tensor_tensor(base.ap, base.ap, ps[b][:, 128:128 + NE], op=ALU.add), reads=[f"ps{b}", base.k()], writes=[base.k()])
        tb = 6 + (i // 4) % 2
        P.op("pe", lambda e, i=i, tb=tb: e.transpose(ps[tb][0:NE, (i % 4) * 128:(i % 4 + 1) * 128], pm.ap[:, i, :], k.ident.ap), reads=[pm.k(i), k.ident.k()], writes=[f"ps{tb}"])
        if i % 4 == 3:
            P.op("act", lambda e, i=i, tb=tb: e.copy(pmT.ap[0:NE, (i - 3) * 128:(i + 1) * 128], ps[tb][0:NE, :]), reads=[f"ps{tb}"], writes=[pmT.k()])
    A.release(m1)
    SelE = A.alloc("SelE", (NT, CAP), BF16)
    SelT = [A.alloc(f"SelT{i}", (2, S), BF16) for i in range(2)]
    xgT = A.alloc("xgT", (8, CAP), BF16); actT = A.alloc("actT", (NFC, CAP), BF16)
    s1 = [A.alloc(f"s1_{i}", (CAP,)) for i in range(2)]
    ysb = A.alloc("ysb", (2, D), BF16)
    w1b = [A.alloc(f"w1b{i}", (8, FB), BF16) for i in range(2)]
    w3b = [A.alloc(f"w3b{i}", (8, FB), BF16) for i in range(2)]
    w2b = [A.alloc(f"w2b{i}", (W2G, D), BF16) for i in range(2)]
    pmT16 = A.alloc("pmT16", (S,), BF16); ohb16 = A.alloc("ohb16", (2048,), BF16)
    P.op("act", lambda e: e.copy(pmT16.ap[0:NE, :], pmT.ap[0:NE, :]), reads=[pmT.k()], writes=[pmT16.k()])
    P.op("act", lambda e: e.copy(ohb16.ap[0:NE, :], k.ohb.ap[0:NE, :]), reads=[k.ohb.k()], writes=[ohb16.k()])
    state = {"wn": 0}

    def sel_and_gather(ex):
        st_ = SelT[ex % 2]
        for i in range(NT):
            P.op("dve", lambda e, i=i, ex=ex: e.tensor_scalar(SelE.ap[:, i, :], k.iota256.ap, pm.ap[:, i, ex:ex + 1], None, op0=ALU.is_equal),
                 reads=[k.iota256.k(), pm.k(i)], writes=[SelE.k(i)])
        for st in range(2):
            for tg in range(4):
                P.op("pe", lambda e, ex=ex, tg=tg: e.matmul(ps[6][:, :], ohb16.ap[0:NE, ex * 128:(ex + 1) * 128], pmT16.ap[0:NE, tg * 512:(tg + 1) * 512], start=True, stop=True),
                     reads=[ohb16.k(), pmT16.k()], writes=["ps6"])
                P.op("dve", lambda e, st=st, tg=tg, st_=st_: e.tensor_scalar(st_.ap[:, st, tg * 512:(tg + 1) * 512], ps[6][:, :], k.slotidx.ap[:, st:st + 1], None, op0=ALU.is_equal),
                     reads=["ps6", k.slotidx.k()], writes=[st_.k(st, tg)])
        for c in range(8):
            b = 6 + c % 2
            for i in range(NT):
                P.op("pe", lambda e, c=c, i=i, b=b: e.matmul(ps[b][:, 0:CAP], x2b.ap[:, i, c * 128:(c + 1) * 128], SelE.ap[:, i, :], start=(i == 0), stop=(i == NT - 1)),
                     reads=[x2b.k(i), SelE.k(i)], writes=[f"ps{b}"])
            P.op("act", lambda e, c=c, b=b: e.copy(xgT.ap[:, c, :], ps[b][:, 0:CAP]), reads=[f"ps{b}"], writes=[xgT.k(c)])

    def ffn1(ex, fb):
        r = state["wn"] % 2; state["wn"] += 1
        w1v = IN["e_w1"].ap()[l][ex].rearrange("(c p) f -> p c f", p=128)
        w3v = IN["e_w3"].ap()[l][ex].rearrange("(c p) f -> p c f", p=128)
        w2v = IN["e_w2"].ap()[l][ex].rearrange("(g p) d -> p g d", p=128)
        P.dma("pool", w1b[r].ap, w1v[:, :, fb * FB:(fb + 1) * FB], writes=[w1b[r].k()], semkey=("w1", r))
        P.dma("pool", w3b[r].ap, w3v[:, :, fb * FB:(fb + 1) * FB], writes=[w3b[r].k()], semkey=("w3", r))
        P.dma("pool", w2b[r].ap, w2v[:, fb * W2G:(fb + 1) * W2G, :], writes=[w2b[r].k()], semkey=("w2", r))
        for q in range(FB // 128):
            fc = fb * (FB // 128) + q
            b = fc % 2
            for c in range(8):
                P.op("pe", lambda e, c=c, q=q, b=b, r=r: e.matmul(ps[b][:, 0:CAP], w1b[r].ap[:, c, q * 128:(q + 1) * 128], xgT.ap[:, c, :], start=(c == 0), stop=(c == 7)),
                     reads=[w1b[r].k(), xgT.k(c)], writes=[f"ps{b}"])
            for c in range(8):
                P.op("pe", lambda e, c=c, q=q, b=b, r=r: e.matmul(ps[b][:, CAP:2 * CAP], w3b[r].ap[:, c, q * 128:(q + 1) * 128], xgT.ap[:, c, :], start=(c == 0), stop=(c == 7)),
                     reads=[w3b[r].k(), xgT.k(c)], writes=[f"ps{b}"])
            P.op("act", lambda e, b=b: e.activation(s1[b].ap, ps[b][:, 0:CAP], AF.Silu), reads=[f"ps{b}"], writes=[s1[b].k()])
            P.op("dve", lambda e, b=b, fc=fc: e.tensor_tensor(actT.ap[:, fc, :], s1[b].ap, ps[b][:, CAP:2 * CAP], op=ALU.mult), reads=[f"ps{b}", s1[b].k()], writes=[actT.k(fc)])
        return r

    def ffn2(fb, r):
        for q in range(W2G):
            fc = fb * W2G + q
            for st in range(2):
                for half in range(2):
                    yb_ = 2 + st * 2 + half
                    P.op("pe", lambda e, fc=fc, q=q, st=st, half=half, yb_=yb_, r=r: e.matmul(ps[yb_][:, :], actT.ap[:, fc, st * 128:(st + 1) * 128], w2b[r].ap[:, q, half * 512:(half + 1) * 512],
                                                                                       start=(fc == 0), stop=(fc == NFC - 1)),
                         reads=[actT.k(fc), w2b[r].k()], writes=[f"ps{yb_}"])

    def scatter(ex):
        st_ = SelT[ex % 2]
        for i in range(NT):
            for half in range(2):
                b = 6 + (2 * i + half) % 2
                for st in range(2):
                    P.op("pe", lambda e, i=i, half=half, st=st, b=b, st_=st_: e.matmul(ps[b][:, :], st_.ap[:, st, i * 128:(i + 1) * 128], ysb.ap[:, st, half * 512:(half + 1) * 512], start=(st == 0), stop=(st == 1)),
                         reads=[st_.k(st, i // 4), ysb.k(st, half)], writes=[f"ps{b}"])
                P.op("dve", lambda e, i=i, half=half, b=b, ex=ex: e.scalar_tensor_tensor(k.xres.ap[:, i, half * 512:(half + 1) * 512], ps[b][:, :], aff.ap[:, i, ex:ex + 1],
                                                                                     k.xres.ap[:, i, half * 512:(half + 1) * 512], op0=ALU.mult, op1=ALU.add),
                     reads=[f"ps{b}", aff.k(), k.xres.k(i)], writes=[k.xres.k(i)])

    sel_and_gather(0)
    for ex in range(NE):
        rprev = ffn1(ex, 0)
        for fb in range(1, NFB):
            rcur = ffn1(ex, fb)
            ffn2(fb - 1, rprev)
            rprev = rcur
        ffn2(NFB - 1, rprev)
        for st in range(2):
            for half in range(2):
                yb_ = 2 + st * 2 + half
                P.op("act", lambda e, st=st, half=half, yb_=yb_: e.copy(ysb.ap[:, st, half * 512:(half + 1) * 512], ps[yb_][:, :]), reads=[f"ps{yb_}"], writes=[ysb.k(st, half)])
        if ex + 1 < NE:
            sel_and_gather(ex + 1)
        scatter(ex)
    A.release(m0)


_INPUT_NAMES = ["rel_bias", "ln1_g", "w_in", "w_out", "rk_mu", "rk_w0", "rk_w2", "rk_a0", "rk_a2", "rk_kk", "rk_ka",
                "rk_rk", "rk_g2", "rk_ln_g", "rk_ln_b", "ml_conv_w", "ml_conv_b", "ml_ib", "ml_fb", "ml_ln_g",
                "ln2_g", "router", "e_w1", "e_w3", "e_w2", "final_g"]


def make_in_maps(inputs, cores, names=None):
    consts = host_consts()
    shared = {n: np.ascontiguousarray(np.asarray(inputs[n], dtype=np.float32)) for n in _INPUT_NAMES if names is None or n in names}
    shared.update({n: v for n, v in consts.items() if names is None or n in names})
    if names is None or "c_pc" in names or "c_wx" in names:
        shared.update(host_rwkv_layout(inputs))
    x = np.asarray(inputs["x"], dtype=np.float32)
    maps = []
    for c in cores:
        m = dict(shared)
        m["x"] = np.ascontiguousarray(x[c])
        maps.append(m)
    return maps


def kernel(**inputs):
    nc, k = build()
    in_maps = make_in_maps(inputs, list(range(8)), set(k.IN.keys()))
    res = run_bass_kernel_spmd(nc, in_maps, core_ids=list(range(8)))
    return np.stack([np.asarray(r["out"], dtype=np.float32) for r in res.results], axis=0)
```

```python
from contextlib import ExitStack
import numpy as np
import concourse.bass as bass
import concourse.mybir as mybir

F32 = mybir.dt.float32
BF16 = mybir.dt.bfloat16
I32 = mybir.dt.int32
ALU = mybir.AluOpType
AF = mybir.ActivationFunctionType
AX = mybir.AxisListType

ENGS = ("pe", "act", "dve", "pool", "sp")


class Prog:
    def __init__(self, nc, strict_same_engine=False):
        self.nc = nc
        self.same_dist = 3
        self.ops = {e: [] for e in ENGS}
        self.keys = {}
        self.dma_cnt = {}
        self.es = ExitStack()
        self.n_ops = 0

    def _deps(self, reads, writes):
        deps = []
        for k in reads:
            deps.extend((ev, True) for ev in self._st(k)["w"])
        for k in writes:
            st = self._st(k)
            deps.extend((ev, True) for ev in st["w"])
            deps.extend((ev, False) for ev in st["r"].values())
        return deps

    def _st(self, k):
        st = self.keys.get(k)
        if st is None:
            inh = getattr(self, "inherit", {}).get(k[0], []) if isinstance(k, tuple) else []
            st = self.keys[k] = {"w": list(inh), "r": {}}
        return st

    def _record(self, ev, reads, writes):
        for k in reads:
            st = self._st(k)
            st["r"][(ev[0], ev[1])] = ev
        for k in writes:
            st = self._st(k)
            st["w"] = [ev]
            st["r"] = {}

    @staticmethod
    def _norm(reads, writes):
        r2, w2 = [], []
        for k in writes:
            if isinstance(k, str) and k.startswith("ps"):
                k = k.split("q")[0]
            if k not in w2:
                w2.append(k)
        for k in reads:
            if isinstance(k, str) and k.startswith("ps"):
                k = k.split("q")[0]
                if k not in w2:
                    w2.append(k)
            elif k not in r2:
                r2.append(k)
        return r2, w2

    def op(self, eng, fn, reads=(), writes=(), serial=False):
        reads, writes = self._norm(reads, writes)
        deps = self._deps(reads, writes)
        idx = len(self.ops[eng])
        if serial and idx > 0:
            deps.append((("eng", eng, idx - 1), "force"))
        ev = ("eng", eng, idx)
        self.ops[eng].append(dict(fn=fn, deps=deps, kind="c"))
        self._record(ev, reads, writes)
        self.n_ops += 1
        return ev

    def dma(self, q, out, in_, reads=(), writes=(), semkey=None, **kw):
        assert semkey is not None
        reads, writes = self._norm(reads, writes)
        deps = self._deps(reads, writes)
        c = self.dma_cnt.get(semkey, 0) + 1
        self.dma_cnt[semkey] = c
        if c > 1:
            deps.append((("dma", semkey, c - 1), "force"))
        ev = ("dma", semkey, c)
        fn = lambda e, out=out, in_=in_, kw=kw: e.dma_start(out=out, in_=in_, **kw)
        self.ops[q].append(dict(fn=fn, deps=deps, kind="d", semkey=semkey))
        self._record(ev, reads, writes)
        self.n_ops += 1
        return ev

    def emit(self, final_wait_events=()):
        nc = self.nc
        signal = {e: set() for e in ENGS}
        for e in ENGS:
            for i, o in enumerate(self.ops[e]):
                nd = []
                for (d, is_w) in o["deps"]:
                    if d[0] == "eng" and d[1] == e and is_w != "force":
                        if e == "pe" or not is_w or (i - d[2]) > self.same_dist:
                            continue
                    nd.append(d)
                    if d[0] == "eng":
                        signal[d[1]].add(d[2])
                o["deps"] = nd
        for d in final_wait_events:
            if d[0] == "eng":
                signal[d[1]].add(d[2])
        rank = {}
        for e in ENGS:
            r = 0
            for i in range(len(self.ops[e])):
                if i in signal[e]:
                    r += 1
                    rank[(e, i)] = r
        self.max_rank = {e: max([v for (ee, i), v in rank.items() if ee == e] + [0]) for e in ENGS}
        es = self.es
        sem_e = {e: es.enter_context(nc.semaphore("s_" + e)) for e in ENGS}
        sem_d = {k: es.enter_context(nc.semaphore("d_%d" % i)) for i, k in enumerate(self.dma_cnt)}
        self.n_sems = len(sem_e) + len(sem_d)

        def lower(ev):
            if ev[0] == "eng":
                return ("e_" + ev[1], sem_e[ev[1]], rank[(ev[1], ev[2])])
            return ("d_" + str(ev[1]), sem_d[ev[1]], 16 * ev[2])

        block = es.enter_context(nc.Block())
        engobj = {"pe": block.tensor, "act": block.scalar, "dve": block.vector,
                  "pool": block.gpsimd, "sp": block.sync}
        fw = self

        def make(e):
            def body(eng):
                known = {}
                for i, o in enumerate(fw.ops[e]):
                    need = {}
                    for d in o["deps"]:
                        nm, s, v = lower(d)
                        if known.get(nm, 0) >= v:
                            continue
                        if nm not in need or need[nm][1] < v:
                            need[nm] = (s, v)
                    for nm, (s, v) in need.items():
                        eng.wait_ge(s, v)
                        known[nm] = v
                    ins = o["fn"](eng)
                    if o["kind"] == "d":
                        ins.then_inc(sem_d[o["semkey"]], 16)
                    elif (e, i) in rank:
                        ins.then_inc(sem_e[e], 1)
                if e == "sp":
                    for d in final_wait_events:
                        nm, s, v = lower(d)
                        if known.get(nm, 0) < v:
                            eng.wait_ge(s, v)
                            known[nm] = v
            return body

        for e in ENGS:
            if self.ops[e] or e == "sp":
                engobj[e](make(e))
        es.close()


class Region:
    def __init__(self, uid, ap, lo, hi):
        self.uid, self.ap, self.lo, self.hi = uid, ap, lo, hi

    def k(self, *i):
        return (self.uid,) + tuple(i)

    def __getitem__(self, idx):
        return self.ap[idx]


class Arena:
    def __init__(self, P, tensor, ncols_f32):
        self.P, self.t, self.n = P, tensor, ncols_f32
        self.top = 0
        self.gen = 0
        self.pending = []
        self.live = []
        P.inherit = {}
        P._arena = self

    def alloc(self, name, free_shape, dtype=F32):
        nel = int(np.prod(free_shape))
        bpe = {F32: 4, BF16: 2, I32: 4}[dtype]
        ncol = (nel * bpe + 3) // 4
        lo, hi = self.top, self.top + ncol
        assert hi <= self.n, f"arena overflow allocating {name}: need {hi} cols of {self.n}"
        self.top = hi
        self.gen += 1
        uid = f"{name}#{self.gen}"
        ap = self.t[:, lo:hi]
        if dtype != F32:
            ap = ap.bitcast(dtype)
        ap = ap[:, 0:nel]
        if len(free_shape) > 1:
            names = " ".join(f"a{i}" for i in range(len(free_shape)))
            kw = {f"a{i}": int(s) for i, s in enumerate(free_shape)}
            ap = ap.rearrange(f"p ({names}) -> p {names}", **kw)
        evs = []
        keep = []
        for (plo, phi, pe) in self.pending:
            if plo < hi and lo < phi:
                evs.extend(pe)
            keep.append((plo, phi, pe))
        self.P.inherit[uid] = evs
        r = Region(uid, ap, lo, hi)
        self.live.append(r)
        return r

    def mark(self):
        return (self.top, len(self.live))

    def release(self, mark):
        top, nlive = mark
        P = self.P
        for r in self.live[nlive:]:
            evs = []
            for k in [k for k in P.keys if k[0] == r.uid]:
                st = P.keys.pop(k)
                evs.extend(st["w"])
                evs.extend(st["r"].values())
            evs.extend(P.inherit.get(r.uid, []))
            best = {}
            for ev in evs:
                kk = (ev[0], ev[1])
                if kk not in best or best[kk][2] < ev[2]:
                    best[kk] = ev
            self.pending.append((r.lo, r.hi, list(best.values())))
        del self.live[nlive:]
        self.top = top
from concourse.bass_utils import run_bass_kernel_spmd
S = 2048; D = 1024; NT = 16; INW = 3856; DEPTH = 2
ND = 3072; EC = 1535
NE = 16; CAP = 256; DFF = 2816


def t5_bucket_np(rel):
    nb = 16
    ret = np.where(rel > 0, nb, 0)
    n = np.abs(rel)
    max_exact = 8
    nf = np.maximum(n, 1).astype(np.float32)
    large = max_exact + (np.log(nf / np.float32(max_exact)) / np.float32(np.log(1024 / max_exact))
                         * np.float32(nb - max_exact)).astype(np.int32)
    large = np.minimum(large, nb - 1)
    return ret + np.where(n < max_exact, n, large)


def host_consts():
    d = np.arange(ND) - EC
    ad = np.abs(d)
    cnt = ((ad <= 64).astype(np.float32) + ((d % 4 == 0) & (ad <= 256)).astype(np.float32)
           + ((d % 16 == 0) & (ad <= 1024)).astype(np.float32))
    bk = t5_bucket_np(d)
    oh = np.zeros((32, ND), np.float32)
    oh[bk, np.arange(ND)] = 1.0
    c = {}
    c["c_oh"] = oh
    c["c_cnt"] = np.tile(cnt[None], (4, 1)).astype(np.float32)
    c["c_ident"] = np.eye(128, dtype=np.float32)
    c["c_jmat"] = np.eye(128, dtype=np.float32)[::-1].copy()
    c["c_triu"] = np.triu(np.ones((128, 128), np.float32))
    c["c_tril"] = np.tril(np.ones((128, 128), np.float32))
    ob = np.zeros((128, 128), np.float32); ob[:64, :64] = 1; ob[64:, 64:] = 1
    c["c_onesblk"] = ob
    tus = np.triu(np.ones((128, 128), np.float32), 1); tui = np.triu(np.ones((128, 128), np.float32), 0)
    c["c_mask4"] = np.concatenate([tus, tui, tus, tui], axis=1)
    c["c_trils"] = np.tril(np.ones((128, 128), np.float32), -1)
    rm = np.ones((128, 256), np.float32); rm[:, 0] = 0; rm[:, 128] = 0
    c["c_rmask"] = rm
    ohb = np.zeros((16, 2048), np.float32)
    for e_ in range(16):
        ohb[e_, e_ * 128:(e_ + 1) * 128] = 1.0
    c["c_ohb"] = ohb
    return c


class K:
    pass


def build(dbg=None, nlayers=DEPTH, stages=("attn", "mlstm", "rwkv", "moe")):
    nc = bass.Bass("TRN2", target_bir_lowering=False)
    k = K()
    k.nc = nc
    SH = dict(x=[S, D], rel_bias=[32, 4], ln1_g=[DEPTH, D], w_in=[DEPTH, D, INW], w_out=[DEPTH, D, D], rk_mu=[DEPTH, 2, 1280],
              rk_w0=[DEPTH, 2, 384], rk_w2=[DEPTH, 2, 64, 384], rk_a0=[DEPTH, 2, 384], rk_a2=[DEPTH, 2, 64, 384],
              rk_kk=[DEPTH, 384], rk_ka=[DEPTH, 384], rk_rk=[DEPTH, 6, 64], rk_g2=[DEPTH, 128, 384], rk_ln_g=[DEPTH, 384],
              rk_ln_b=[DEPTH, 384], ml_conv_w=[DEPTH, 5, 768], ml_conv_b=[DEPTH, 768], ml_ib=[DEPTH, 2, 4], ml_fb=[DEPTH, 2, 4],
              ml_ln_g=[DEPTH, 384], ln2_g=[DEPTH, D], router=[DEPTH, D, NE], e_w1=[DEPTH, NE, D, DFF], e_w3=[DEPTH, NE, D, DFF],
              e_w2=[DEPTH, NE, DFF, D], final_g=[D], c_oh=[32, ND], c_cnt=[4, ND], c_ident=[128, 128], c_jmat=[128, 128],
              c_triu=[128, 128], c_tril=[128, 128], c_onesblk=[128, 128], c_mask4=[128, 512], c_trils=[128, 128],
              c_rmask=[128, 256], c_ohb=[16, 2048], c_pc=[DEPTH, 128, NPC], c_wx=[DEPTH, D, 384])

    class _IN(dict):
        def __missing__(self, name):
            t = nc.dram_tensor(name, list(SH[name]), F32, kind="ExternalInput")
            self[name] = t
            return t
    IN = _IN()
    k.IN = IN
    out_t = nc.dram_tensor("out", [S, D], F32, kind="ExternalOutput")
    k.mscr = nc.dram_tensor("mscr", [4, ND], F32, kind="Internal")
    k.mtab_d = nc.dram_tensor("mtab_d", [4, 128, 23 * 128], F32, kind="Internal")
    k.xspill = nc.dram_tensor("xspill", [S, D], F32, kind="Internal")
    k.dbg = dbg
    k.dbg_out = {}
    with ExitStack() as es:
        ACOLS = 53000
        arena_t = es.enter_context(nc.sbuf_tensor("arena", [128, ACOLS], F32))
        k.ps = [es.enter_context(nc.psum_tensor(f"ps{i}", [128, 512], F32)) for i in range(8)]
        P = Prog(nc)
        A = Arena(P, arena_t, ACOLS)
        k.P, k.A = P, A
        k.final_events = []
        setup_consts(k)
        k.xres = A.alloc("xres", (NT, D))
        xin = IN["x"].ap().rearrange("(i p) d -> p i d", p=128)
        for i in range(NT):
            P.dma("sp" if i % 2 == 0 else "act", k.xres.ap[:, i, :], xin[:, i, :], writes=[k.xres.k(i)], semkey=("xld", i % 4))
        build_mask_table(k)
        for l in range(nlayers):
            m0 = A.mark()
            norm_to_hT(k, IN["ln1_g"].ap()[l], "hT")
            if "attn" in stages:
                attn_stage(k, l)
            if "mlstm" in stages:
                mlstm_stage(k, l)
            if "rwkv" in stages:
                rwkv_stage(k, l)
            A.release(m0)
            if "moe" in stages:
                moe_stage(k, l)
        final_stage(k, out_t)
        P.emit(final_wait_events=k.final_events)
    return nc, k


def dbg_dump(k, name, region_ap, shape, dt, reads):
    if not k.dbg or name not in k.dbg:
        return None
    t = k.nc.dram_tensor("dbg_" + name, list(shape), dt, kind="ExternalOutput")
    k.dbg_out[name] = t
    return t


def setup_consts(k):
    P, A, IN = k.P, k.A, k.IN
    k.ident = A.alloc("ident", (128,))
    k.jmat = A.alloc("jmat", (128,))
    k.ident16 = A.alloc("ident16", (128,), BF16)
    k.triu = A.alloc("triu", (128,))
    k.tril = A.alloc("tril", (128,))
    P.dma("sp", k.ident.ap, IN["c_ident"].ap(), writes=[k.ident.k()], semkey="c0")
    P.dma("sp", k.jmat.ap, IN["c_jmat"].ap(), writes=[k.jmat.k()], semkey="c1")
    P.dma("sp", k.triu.ap, IN["c_triu"].ap(), writes=[k.triu.k()], semkey="c2")
    P.dma("sp", k.tril.ap, IN["c_tril"].ap(), writes=[k.tril.k()], semkey="c3")
    P.op("dve", lambda e: e.tensor_copy(k.ident16.ap, k.ident.ap), reads=[k.ident.k()], writes=[k.ident16.k()])
    k.one = A.alloc("one", (1,))
    P.op("pool", lambda e: e.memset(k.one.ap, 1.0), writes=[k.one.k()])
    k.ones32 = A.alloc("ones32", (128,))
    P.op("pool", lambda e: e.memset(k.ones32.ap, 1.0), writes=[k.ones32.k()])
    k.negm = [A.alloc("negm0", (128,)), A.alloc("negm1", (128,))]
    P.op("dve", lambda e: e.tensor_scalar(k.negm[0].ap, k.triu.ap, -1.0, 30000.0, op0=ALU.add, op1=ALU.mult), reads=[k.triu.k()], writes=[k.negm[0].k()])
    P.op("dve", lambda e: e.tensor_scalar(k.negm[1].ap, k.tril.ap, -1.0, 30000.0, op0=ALU.add, op1=ALU.mult), reads=[k.tril.k()], writes=[k.negm[1].k()])
    k.epsgn = A.alloc("epsgn", (1,))
    P.op("pool", lambda e: e.memset(k.epsgn.ap, 64e-5), writes=[k.epsgn.k()])
    k.onesblk = A.alloc("onesblk", (128,)); k.onesblk64 = A.alloc("onesblk64", (128,))
    k.trils = A.alloc("trils", (128,))
    P.dma("act", k.onesblk.ap, IN["c_onesblk"].ap(), writes=[k.onesblk.k()], semkey="c4")
    P.dma("act", k.trils.ap, IN["c_trils"].ap(), writes=[k.trils.k()], semkey="c6")
    P.op("dve", lambda e: e.tensor_scalar(k.onesblk64.ap, k.onesblk.ap, 1.0 / 64, None, op0=ALU.mult), reads=[k.onesblk.k()], writes=[k.onesblk64.k()])
    k.iota256 = A.alloc("iota256", (256,)); k.slotidx = A.alloc("slotidx", (2,))
    k.ones16 = A.alloc("ones16", (128,), BF16); k.trius16 = A.alloc("trius16", (128,), BF16)
    P.op("pool", lambda e: e.iota(k.iota256.ap, [[1, 256]], base=0, channel_multiplier=0, allow_small_or_imprecise_dtypes=True), writes=[k.iota256.k()])
    P.op("pool", lambda e: e.iota(k.slotidx.ap, [[128, 2]], base=0, channel_multiplier=1, allow_small_or_imprecise_dtypes=True), writes=[k.slotidx.k()])
    P.op("dve", lambda e: e.tensor_copy(k.ones16.ap, k.ones32.ap), reads=[k.ones32.k()], writes=[k.ones16.k()])
    P.op("dve", lambda e: e.tensor_tensor(k.trius16.ap, k.triu.ap, k.ident.ap, op=ALU.subtract), reads=[k.triu.k(), k.ident.k()], writes=[k.trius16.k()])
    k.eps6 = A.alloc("eps6", (1,))
    P.op("pool", lambda e: e.memset(k.eps6.ap, 1e-6), writes=[k.eps6.k()])


def build_mask_table(k):
    P, A, IN, ps = k.P, k.A, k.IN, k.ps
    m0 = A.mark()
    rb = A.alloc("rb", (4,)); oh = A.alloc("oh", (ND,)); cnt = A.alloc("cnt", (ND,)); mm = A.alloc("mm", (ND,))
    P.dma("sp", rb.ap[0:32, :], IN["rel_bias"].ap(), writes=[rb.k()], semkey="mt0")
    P.dma("sp", oh.ap[0:32, :], IN["c_oh"].ap(), writes=[oh.k()], semkey="mt1")
    P.dma("act", cnt.ap[0:4, :], IN["c_cnt"].ap(), writes=[cnt.k()], semkey="mt2")
    for c in range(ND // 512):
        b = ps[c % 2]
        P.op("pe", lambda e, c=c, b=b: e.matmul(b[0:4, :], rb.ap[0:32, :], oh.ap[0:32, c * 512:(c + 1) * 512], start=True, stop=True),
             reads=[rb.k(), oh.k()], writes=[f"ps{c%2}"])
        P.op("act", lambda e, c=c, b=b: e.activation(mm.ap[0:4, c * 512:(c + 1) * 512], b[0:4, :], AF.Exp),
             reads=[f"ps{c%2}"], writes=[mm.k(c)])
        P.op("dve", lambda e, c=c: e.tensor_tensor(mm.ap[0:4, c * 512:(c + 1) * 512], mm.ap[0:4, c * 512:(c + 1) * 512],
                                                  cnt.ap[0:4, c * 512:(c + 1) * 512], op=ALU.mult),
             reads=[mm.k(c), cnt.k()], writes=[mm.k(c)])
    P.dma("sp", k.mscr.ap(), mm.ap[0:4, :], reads=[mm.k(c) for c in range(ND // 512)], writes=["mscr"], semkey="mt3")
    hanks = [A.alloc(f"hank{i}", (23, 128)) for i in range(2)]
    mts = [A.alloc(f"mt{i}", (23, 128)) for i in range(2)]
    for h in range(4):
        hank = hanks[h % 2]
        mt = mts[h % 2]
        src = bass.AP(k.mscr, h * ND, [[1, 128], [128, 23], [1, 128]])
        P.dma("sp" if h % 2 == 0 else "act", hank.ap, src, reads=["mscr"], writes=[hank.k()], semkey=("mt4", h))
        for j in range(23):
            jj = 22 - j
            b = 2 + (j // 4) % 2
            P.op("pe", lambda e, j=j, b=b, hank=hank: e.matmul(ps[b][:, (j % 4) * 128:(j % 4 + 1) * 128], hank.ap[:, j, :], k.jmat.ap, start=True, stop=True),
                 reads=[hank.k(), k.jmat.k()], writes=[f"ps{b}"])
            P.op("act" if j % 2 == 0 else "dve",
                 (lambda e, jj=jj, j=j, b=b, mt=mt: e.copy(mt.ap[:, jj, :], ps[b][:, (j % 4) * 128:(j % 4 + 1) * 128])) if j % 2 == 0 else
                 (lambda e, jj=jj, j=j, b=b, mt=mt: e.tensor_copy(mt.ap[:, jj, :], ps[b][:, (j % 4) * 128:(j % 4 + 1) * 128])),
                 reads=[f"ps{b}"], writes=[mt.k()])
        P.dma("sp", k.mtab_d.ap()[h].rearrange("p (j q) -> p j q", j=23), mt.ap, reads=[mt.k()], writes=[("mtab_d", h)], semkey=("mt5", h))
    A.release(m0)


def norm_to_hT(k, g_ap, name, want_f32T=False):
    P, A, ps = k.P, k.A, k.ps
    k.hT = A.alloc(name, (8, S), BF16)
    m0 = A.mark()
    gb = A.alloc("gb", (D,)); junk = A.alloc("junk", (D,)); ss = A.alloc("ss", (NT,)); rstd = A.alloc("rstd", (NT,))
    hb = [A.alloc(f"hb{i}", (D,), BF16) for i in range(2)]
    P.dma("sp", gb.ap, g_ap.partition_broadcast(128), writes=[gb.k()], semkey="gb")
    P.op("pool", lambda e: e.memset(ss.ap, 0.0), writes=[ss.k(i) for i in range(NT)])
    for i in range(NT):
        P.op("act", lambda e, i=i: e.activation(junk.ap, k.xres.ap[:, i, :], AF.Square, accum_out=ss.ap[:, i:i + 1]),
             reads=[k.xres.k(i)], writes=[junk.k(), ss.k(i)])
    P.op("act", lambda e: e.activation(rstd.ap, ss.ap, AF.Sqrt, scale=1.0 / D, bias=k.eps6.ap[:, 0:1]),
         reads=[ss.k(i) for i in range(NT)] + [k.eps6.k()], writes=[rstd.k()])
    P.op("dve", lambda e: e.reciprocal(rstd.ap, rstd.ap), reads=[rstd.k()], writes=[rstd.k()])
    for i in range(NT):
        h_ = hb[i % 2]
        P.op("dve", lambda e, i=i, h_=h_: e.scalar_tensor_tensor(h_.ap, k.xres.ap[:, i, :], rstd.ap[:, i:i + 1], gb.ap, op0=ALU.mult, op1=ALU.mult),
             reads=[k.xres.k(i), rstd.k(), gb.k()], writes=[h_.k()])
        b = 4 + i % 2
        pb = ps[b][:, :].bitcast(BF16)
        for c in range(8):
            P.op("pe", lambda e, c=c, pb=pb, h_=h_: e.transpose(pb[:, c * 128:(c + 1) * 128], h_.ap[:, c * 128:(c + 1) * 128], k.ident16.ap),
                 reads=[h_.k(), k.ident16.k()], writes=[f"ps{b}"])
        P.op("act", lambda e, i=i, pb=pb: e.copy(k.hT.ap[:, :, i * 128:(i + 1) * 128], pb.rearrange("p (c t) -> p c t", c=8)),
             reads=[f"ps{b}"], writes=[k.hT.k(i)])
    A.release(m0)


def load_w_bf16(k, name, src_ap, nchunks, ncols, semkey, split=None):
    P, A = k.P, k.A
    w = A.alloc(name, (nchunks, ncols), BF16)
    v = src_ap.rearrange("(c p) n -> p c n", p=128)
    for c in range(nchunks):
        P.dma("pool", w.ap[:, c, :], v[:, c, :], writes=[w.k(c)], semkey=(semkey, c % 4))
    return w


def outproj_partial(k, l, yT, nch, row0, tag):
    P, A, ps, IN = k.P, k.A, k.ps, k.IN
    wo = load_w_bf16(k, "wo_" + tag, IN["w_out"].ap()[l][row0:row0 + nch * 128, :], nch, D, "wo_" + tag)
    n = 0
    for i in range(NT):
        for half in range(2):
            b = 6 + n % 2
            n += 1
            for c in range(nch):
                P.op("pe", lambda e, i=i, half=half, c=c, b=b: e.matmul(ps[b][:, :], yT.ap[:, c, i * 128:(i + 1) * 128],
                                                                     wo.ap[:, c, half * 512:(half + 1) * 512], start=(c == 0), stop=(c == nch - 1)),
                     reads=[yT.k(i), wo.k(c)], writes=[f"ps{b}"])
            P.op("dve", lambda e, i=i, half=half, b=b: e.tensor_tensor(k.xres.ap[:, i, half * 512:(half + 1) * 512],
                                                                      k.xres.ap[:, i, half * 512:(half + 1) * 512], ps[b][:, :], op=ALU.add),
                 reads=[f"ps{b}", k.xres.k(i)], writes=[k.xres.k(i)])


def transpose_to_T(k, y, nch, yT):
    P, ps = k.P, k.ps
    for i in range(NT):
        b = 4 + i % 2
        pb = ps[b][:, :].bitcast(BF16)
        for c in range(nch):
            P.op("pe", lambda e, i=i, c=c, pb=pb: e.transpose(pb[:, c * 128:(c + 1) * 128], y.ap[:, i, c * 128:(c + 1) * 128], k.ident16.ap),
                 reads=[y.k(i), k.ident16.k()], writes=[f"ps{b}"])
        P.op("act", lambda e, i=i, pb=pb: e.copy(yT.ap[:, :, i * 128:(i + 1) * 128], pb[:, 0:nch * 128].rearrange("p (c t) -> p c t", c=nch)),
             reads=[f"ps{b}"], writes=[yT.k(i)])


def attn_stage(k, l):
    P, A, ps, IN = k.P, k.A, k.ps, k.IN
    m0 = A.mark()
    ya = A.alloc("ya", (NT, 256), BF16)
    m1 = A.mark()
    wa = load_w_bf16(k, "wa", IN["w_in"].ap()[l][:, 0:768], 8, 768, "wa")
    qT = A.alloc("qT", (2, S), BF16); kT = A.alloc("kT", (2, S), BF16)
    vp = A.alloc("vp", (NT, 4, 65), BF16)
    P.op("pool", lambda e: e.memset(vp.ap, 1.0), writes=[vp.k(i) for i in range(NT)])
    n = 0
    for cc in range(4):
        dst = qT if cc < 2 else kT
        for tg in range(4):
            b = n % 2; n += 1
            for c in range(8):
                P.op("pe", lambda e, c=c, cc=cc, tg=tg, b=b: e.matmul(ps[b][:, :], wa.ap[:, c, cc * 128:(cc + 1) * 128], k.hT.ap[:, c, tg * 512:(tg + 1) * 512],
                                                                  start=(c == 0), stop=(c == 7)),
                     reads=[wa.k(c)] + [k.hT.k(4 * tg + j) for j in range(4)], writes=[f"ps{b}"])
            P.op("act", lambda e, cc=cc, tg=tg, b=b, dst=dst: e.copy(dst.ap[:, cc % 2, tg * 512:(tg + 1) * 512], ps[b][:, :]),
                 reads=[f"ps{b}"], writes=[dst.k(tg)])
    for i in range(NT):
        b = 2 + i % 2
        for c in range(8):
            P.op("pe", lambda e, c=c, i=i, b=b: e.matmul(ps[b][:, 0:256], k.hT.ap[:, c, i * 128:(i + 1) * 128], wa.ap[:, c, 512:768], start=(c == 0), stop=(c == 7)),
                 reads=[wa.k(c), k.hT.k(i)], writes=[f"ps{b}"])
        P.op("dve", lambda e, i=i, b=b: e.tensor_copy(vp.ap[:, i, :, 0:64], ps[b][:, 0:256].rearrange("p (h c) -> p h c", h=4)),
             reads=[f"ps{b}"], writes=[vp.k(i)])
    mtab = [A.alloc(f"mtab{i}", (23, 128)) for i in range(2)]
    e32 = [A.alloc(f"e32_{i}", (512,)) for i in range(2)]
    p16 = [A.alloc(f"p16_{i}", (512,), BF16) for i in range(2)]
    rc = A.alloc("rc", (8,))
    iters = []
    for h in range(4):
        for g in range(4):
            kts = [kt for kt in range(NT) if -8 <= kt - 4 * g <= 11]
            for kt in kts:
                iters.append((h, g, kt, kt == kts[0], kt == kts[-1]))
    mts = {}

    def emit_score(n):
        h, g, kt, first, last = iters[n]
        sb = n % 2
        hp, hb_ = h // 2, (h % 2) * 64
        if h not in mts:
            mt = mtab[h % 2]
            P.dma("sp", mt.ap, k.mtab_d.ap()[h].rearrange("p (j q) -> p j q", j=23), reads=[("mtab_d", h)], writes=[mt.k()], semkey=("mtl", h % 2))
            mts[h] = mt
        P.op("pe", lambda e, kt=kt, g=g, sb=sb, hp=hp, hb_=hb_: e.matmul(ps[sb][:, :], kT.ap[hb_:hb_ + 64, hp, kt * 128:(kt + 1) * 128],
                                                                   qT.ap[hb_:hb_ + 64, hp, g * 512:(g + 1) * 512], start=True, stop=True),
             reads=[kT.k(kt // 4), qT.k(g)], writes=[f"ps{sb}"])

    def emit_rest(n):
        h, g, kt, first, last = iters[n]
        sb = n % 2
        mt = mts[h]
        jj0 = 11 - kt + 4 * g
        P.op("act", lambda e, sb=sb: e.activation(e32[sb].ap, ps[sb][:, :], AF.Exp, scale=0.125),
             reads=[f"ps{sb}"], writes=[e32[sb].k()])
        P.op("dve", lambda e, sb=sb, jj0=jj0, mt=mt: e.tensor_tensor(p16[sb].ap, e32[sb].ap, mt.ap[:, jj0:jj0 + 4, :].rearrange("p j q -> p (j q)"), op=ALU.mult),
             reads=[e32[sb].k(), mt.k()], writes=[p16[sb].k()])
        for i in range(4):
            P.op("pe", lambda e, i=i, sb=sb, kt=kt, h=h, first=first, last=last: e.matmul(ps[2 + i][:, 0:65], p16[sb].ap[:, i * 128:(i + 1) * 128], vp.ap[:, kt, h, :],
                                                                                  start=first, stop=last),
                 reads=[p16[sb].k(), vp.k(kt)], writes=[f"ps{2+i}"])
        if last:
            for i in range(4):
                P.op("dve", lambda e, i=i: e.reciprocal(rc.ap[:, i:i + 1], ps[2 + i][:, 64:65]), reads=[f"ps{2+i}"], writes=[rc.k(i)])
                P.op("dve", lambda e, i=i, g=g, h=h: e.tensor_scalar(ya.ap[:, 4 * g + i, h * 64:(h + 1) * 64], ps[2 + i][:, 0:64], rc.ap[:, i:i + 1], None, op0=ALU.mult),
                     reads=[f"ps{2+i}", rc.k(i)], writes=[ya.k(4 * g + i)])

    emit_score(0)
    for n in range(len(iters)):
        if n + 1 < len(iters):
            emit_score(n + 1)
        emit_rest(n)
    A.release(m1)
    if k.dbg and "ya" in k.dbg:
        t = k.nc.dram_tensor("dbg_ya", [S, 256], BF16, kind="ExternalOutput")
        k.final_events.append(P.dma("sp", t.ap().rearrange("(i p) c -> p i c", p=128), ya.ap, reads=[ya.k(i) for i in range(NT)], semkey="dbg"))
    yaT = A.alloc("yaT", (2, S), BF16)
    transpose_to_T(k, ya, 2, yaT)
    outproj_partial(k, l, yaT, 2, 0, "a")
    A.release(m0)


def final_stage(k, out_t):
    P, A = k.P, k.A
    m0 = A.mark()
    gb = A.alloc("gbf", (D,)); junk = A.alloc("junkf", (D,)); ss = A.alloc("ssf", (NT,)); rstd = A.alloc("rstdf", (NT,))
    ob = [A.alloc(f"ob{i}", (D,)) for i in range(2)]
    P.dma("sp", gb.ap, k.IN["final_g"].ap().partition_broadcast(128), writes=[gb.k()], semkey="gbf")
    P.op("pool", lambda e: e.memset(ss.ap, 0.0), writes=[ss.k(i) for i in range(NT)])
    for i in range(NT):
        P.op("act", lambda e, i=i: e.activation(junk.ap, k.xres.ap[:, i, :], AF.Square, accum_out=ss.ap[:, i:i + 1]),
             reads=[k.xres.k(i)], writes=[junk.k(), ss.k(i)])
    P.op("act", lambda e: e.activation(rstd.ap, ss.ap, AF.Sqrt, scale=1.0 / D, bias=k.eps6.ap[:, 0:1]),
         reads=[ss.k(i) for i in range(NT)] + [k.eps6.k()], writes=[rstd.k()])
    P.op("dve", lambda e: e.reciprocal(rstd.ap, rstd.ap), reads=[rstd.k()], writes=[rstd.k()])
    ov = out_t.ap().rearrange("(i p) d -> p i d", p=128)
    for i in range(NT):
        o = ob[i % 2]
        P.op("dve", lambda e, i=i, o=o: e.scalar_tensor_tensor(o.ap, k.xres.ap[:, i, :], rstd.ap[:, i:i + 1], gb.ap, op0=ALU.mult, op1=ALU.mult),
             reads=[k.xres.k(i), rstd.k(), gb.k()], writes=[o.k()])
        k.final_events.append(P.dma("sp" if i % 2 == 0 else "act", ov[:, i, :], o.ap, reads=[o.k()], semkey=("out", i % 2)))
    A.release(m0)


OFF_MQK = 2304; OFF_MV = 3072; OFF_MO = 3456; OFF_MG = 3840


def mlstm_stage(k, l):
    P, A, ps, IN = k.P, k.A, k.ps, k.IN
    m0 = A.mark()
    hsum = A.alloc("hsum", (NT, 4, 96))
    m1 = A.mark()
    qkT = A.alloc("qkT", (8, S), BF16)
    vp = A.alloc("vpm", (NT, 4, 97), BF16)
    G = A.alloc("G", (NT, 16))
    BL = [A.alloc(f"BL{d}", (NT, 4)) for d in range(2)]
    EBL = [A.alloc(f"EBL{d}", (NT, 4)) for d in range(2)]
    IMB = [A.alloc(f"IMB{d}", (NT, 4)) for d in range(2)]
    m2 = A.mark()
    wm = load_w_bf16(k, "wm", IN["w_in"].ap()[l][:, OFF_MQK:OFF_MV], 8, OFF_MV - OFF_MQK, "wm")
    pre = A.alloc("pre", (S + 4,)); cacc = A.alloc("cacc", (S,))
    cw = A.alloc("cw", (8, 6))
    for g_ in range(8):
        P.dma("sp", cw.ap[0:96, g_, 0:5], IN["ml_conv_w"].ap()[l][:, g_ * 96:(g_ + 1) * 96].rearrange("j c -> c j"), writes=[cw.k()], semkey=("cw0", g_ % 4), allow_slow_non_contiguous=True)
    P.dma("sp", cw.ap[0:96, :, 5:6], IN["ml_conv_b"].ap()[l].rearrange("(g c o) -> c g o", c=96, o=1), writes=[cw.k()], semkey="cw1", allow_slow_non_contiguous=True)
    P.op("pool", lambda e: e.memset(pre.ap, 0.0), writes=[pre.k()])
    P.op("pool", lambda e: e.memset(vp.ap, 1.0), writes=[vp.k(i) for i in range(NT)])
    n = 0
    for j in range(8):
        for tg in range(4):
            b = n % 2; n += 1
            for c in range(8):
                P.op("pe", lambda e, c=c, j=j, tg=tg, b=b: e.matmul(ps[b][0:96, :], wm.ap[:, c, j * 96:(j + 1) * 96], k.hT.ap[:, c, tg * 512:(tg + 1) * 512],
                                                                 start=(c == 0), stop=(c == 7)),
                     reads=[wm.k(c)] + [k.hT.k(4 * tg + q) for q in range(4)], writes=[f"ps{b}"])
            P.op("act", lambda e, tg=tg, b=b: e.copy(pre.ap[0:96, 2 + tg * 512:2 + (tg + 1) * 512], ps[b][0:96, :]),
                 reads=[f"ps{b}"], writes=[pre.k()])
        P.op("dve", lambda e, j=j: e.tensor_scalar(cacc.ap[0:96, :], pre.ap[0:96, 0:S], cw.ap[0:96, j, 0:1], None, op0=ALU.mult),
             reads=[pre.k(), cw.k()], writes=[cacc.k()])
        for t in range(1, 5):
            P.op("dve", lambda e, j=j, t=t: e.scalar_tensor_tensor(cacc.ap[0:96, :], pre.ap[0:96, t:t + S], cw.ap[0:96, j, t:t + 1], cacc.ap[0:96, :],
                                                                 op0=ALU.mult, op1=ALU.add),
                 reads=[pre.k(), cw.k(), cacc.k()], writes=[cacc.k()])
        if j < 4:
            P.op("act", lambda e, j=j: e.activation(qkT.ap[0:96, j, :], cacc.ap[0:96, :], AF.Silu, bias=cw.ap[0:96, j, 5:6]),
                 reads=[cacc.k(), cw.k()], writes=[qkT.k(j)])
        else:
            P.op("act", lambda e, j=j: e.activation(cacc.ap[0:96, :], cacc.ap[0:96, :], AF.Silu, bias=cw.ap[0:96, j, 5:6]),
                 reads=[cacc.k(), cw.k()], writes=[cacc.k()])
            P.op("dve", lambda e, j=j: e.tensor_scalar(qkT.ap[0:96, j, :], cacc.ap[0:96, :], 96.0 ** -0.5, None, op0=ALU.mult),
                 reads=[cacc.k()], writes=[qkT.k(j)])
    A.release(m2)
    wv = load_w_bf16(k, "wv", IN["w_in"].ap()[l][:, OFF_MV:OFF_MO], 8, 384, "wv")
    wg = load_w_bf16(k, "wg", IN["w_in"].ap()[l][:, OFF_MG:INW], 8, 16, "wg")
    gbias = A.alloc("gbias", (16,))
    for d in range(2):
        P.dma("act", gbias.ap[:, 8 * d:8 * d + 4], IN["ml_ib"].ap()[l][d].partition_broadcast(128), writes=[gbias.k()], semkey=("gbi", d))
        P.dma("act", gbias.ap[:, 8 * d + 4:8 * d + 8], IN["ml_fb"].ap()[l][d].partition_broadcast(128), writes=[gbias.k()], semkey=("gbf_", d))
    for i in range(NT):
        b = 2 + i % 2
        for c in range(8):
            P.op("pe", lambda e, c=c, i=i, b=b: e.matmul(ps[b][:, 0:384], k.hT.ap[:, c, i * 128:(i + 1) * 128], wv.ap[:, c, :],
                                                     start=(c == 0), stop=(c == 7)),
                 reads=[wv.k(c), k.hT.k(i)], writes=[f"ps{b}"])
        P.op("dve", lambda e, i=i, b=b: e.tensor_copy(vp.ap[:, i, :, 0:96], ps[b][:, 0:384].rearrange("p (h c) -> p h c", h=4)),
             reads=[f"ps{b}"], writes=[vp.k(i)])
        b2 = 4 + i % 2
        for c in range(8):
            P.op("pe", lambda e, c=c, i=i, b2=b2: e.matmul(ps[b2][:, 0:16], k.hT.ap[:, c, i * 128:(i + 1) * 128], wg.ap[:, c, :],
                                                       start=(c == 0), stop=(c == 7)),
                 reads=[wg.k(c), k.hT.k(i)], writes=[f"ps{b2}"])
        P.op("dve", lambda e, i=i, b2=b2: e.tensor_tensor(G.ap[:, i, :], ps[b2][:, 0:16], gbias.ap, op=ALU.add),
             reads=[f"ps{b2}", gbias.k()], writes=[G.k()])
    A.release(m2)
    for d in range(2):
        v_ = G.ap[:, :, 8 * d + 4:8 * d + 8]
        P.op("act", lambda e, v_=v_: e.activation(v_, v_, AF.Exp, scale=-1.0), reads=[G.k()], writes=[G.k()])
        P.op("act", lambda e, v_=v_: e.activation(v_, v_, AF.Ln, bias=k.one.ap[:, 0:1]), reads=[G.k(), k.one.k()], writes=[G.k()])
        P.op("dve", lambda e, v_=v_: e.tensor_scalar(v_, v_, -1.0, None, op0=ALU.mult), reads=[G.k()], writes=[G.k()])
    for d in range(2):
        tri = k.triu if d == 0 else k.tril
        P.op("pe", lambda e, d=d, tri=tri: e.matmul(ps[7][:, 0:64].rearrange("p (i h) -> p i h", h=4), tri.ap, G.ap[:, :, 8 * d + 4:8 * d + 8], start=True, stop=True),
             reads=[tri.k(), G.k()], writes=["ps7"])
        P.op("dve", lambda e, d=d: e.tensor_copy(BL[d].ap, ps[7][:, 0:64].rearrange("p (i h) -> p i h", h=4)), reads=["ps7"], writes=[BL[d].k()])
        P.op("act", lambda e, d=d: e.activation(EBL[d].ap, BL[d].ap, AF.Exp), reads=[BL[d].k()], writes=[EBL[d].k()])
        P.op("dve", lambda e, d=d: e.tensor_tensor(IMB[d].ap, G.ap[:, :, 8 * d:8 * d + 4], BL[d].ap, op=ALU.subtract), reads=[G.k(), BL[d].k()], writes=[IMB[d].k()])
    C32 = A.alloc("C32", (4, 97)); Cb = A.alloc("Cb", (4, 97), BF16)
    HB = []
    for h in range(4):
        HB.append(dict(dg=A.alloc(f"dg{h}", (128,)), Dm=A.alloc(f"Dm{h}", (128,)), W16=A.alloc(f"W16{h}", (128,), BF16),
                       nis=A.alloc(f"nis{h}", (97,)), nsb=A.alloc(f"nsb{h}", (97,)), kh=A.alloc(f"kh{h}", (96,), BF16), sm=A.alloc(f"sm{h}", (4,))))

    def head_stream(h):
        hb = HB[h]
        dg, Dm, W16, nis, nsb, kh, sm = (hb[n_] for n_ in ("dg", "Dm", "W16", "nis", "nsb", "kh", "sm"))
        bA, bB = 2 * h, 2 * h + 1
        kA, kB = f"ps{bA}", f"ps{bB}"
        pbt = ps[bA][:, 384:432].bitcast(BF16)
        for d in range(2):
            P.op("pool", lambda e: e.memset(C32.ap[:, h, :], 0.0), writes=[C32.k(h)])
            P.op("pool", lambda e: e.memset(Cb.ap[:, h, :], 0.0), writes=[Cb.k(h)])
            nm = k.negm[d]
            endc = 127 if d == 0 else 0
            order = range(NT) if d == 0 else range(NT - 1, -1, -1)
            for i in order:
                yield from chunk(h, d, i, nm, endc, dg, Dm, W16, nis, nsb, kh, sm, bA, bB, kA, kB, pbt)

    def chunk(h, d, i, nm, endc, dg, Dm, W16, nis, nsb, kh, sm, bA, bB, kA, kB, pbt):
        tsl = slice(i * 128, (i + 1) * 128)
        P.op("pe", lambda e: e.matmul(ps[bA][:, 0:128], qkT.ap[0:96, 4 + h, tsl], qkT.ap[0:96, h, tsl], start=True, stop=True),
             reads=[qkT.k(4 + h), qkT.k(h)], writes=[kA])
        P.op("dve", lambda e: e.tensor_scalar(dg.ap, k.ident.ap, BL[d].ap[:, i, h:h + 1], None, op0=ALU.mult),
             reads=[k.ident.k(), BL[d].k()], writes=[dg.k()])
        P.op("pe", lambda e: e.matmul(ps[bA][:, 128:256], k.ones32.ap, dg.ap, start=True, stop=False), reads=[k.ones32.k(), dg.k()], writes=[kA])
        P.op("pe", lambda e: e.matmul(ps[bA][:, 128:256], k.ident.ap, nm.ap, start=False, stop=True), reads=[k.ident.k(), nm.k()], writes=[kA])
        yield
        P.op("act", lambda e: e.activation(Dm.ap, ps[bA][:, 128:256], AF.Exp, bias=IMB[d].ap[:, i, h:h + 1]), reads=[kA, IMB[d].k()], writes=[Dm.k()])
        P.op("act", lambda e: e.activation(sm.ap[:, 2:3], ps[bA][:, 128 + endc:129 + endc], AF.Exp), reads=[kA], writes=[sm.k(2)])
        P.op("dve", lambda e: e.tensor_tensor(W16.ap, ps[bA][:, 0:128], Dm.ap, op=ALU.mult), reads=[kA, Dm.k()], writes=[W16.k()])
        yield
        P.op("pe", lambda e: e.matmul(ps[bB][:, 0:97], W16.ap, vp.ap[:, i, h, :], start=True, stop=True), reads=[W16.k(), vp.k(i)], writes=[kB])
        P.op("pe", lambda e: e.matmul(ps[bB][:, 128:225], qkT.ap[0:96, h, tsl], Cb.ap[0:96, h, :], start=True, stop=True), reads=[qkT.k(h), Cb.k(h)], writes=[kB])
        P.op("pe", lambda e: e.transpose(pbt, qkT.ap[0:96, 4 + h, tsl], k.ident16.ap[0:96, 0:96]), reads=[qkT.k(4 + h), k.ident16.k()], writes=[kA])
        yield
        P.op("act", lambda e: e.copy(nis.ap, ps[bB][:, 0:97]), reads=[kB], writes=[nis.k()])
        P.op("dve", lambda e: e.tensor_scalar(kh.ap, pbt, Dm.ap[:, endc:endc + 1], None, op0=ALU.mult), reads=[kA, Dm.k()], writes=[kh.k()])
        P.op("dve", lambda e: e.scalar_tensor_tensor(nsb.ap, ps[bB][:, 128:225], EBL[d].ap[:, i, h:h + 1], nis.ap, op0=ALU.mult, op1=ALU.add),
             reads=[kB, EBL[d].k(), nis.k()], writes=[nsb.k()])
        P.op("pe", lambda e: e.matmul(ps[bA][0:96, 256:353], kh.ap, vp.ap[:, i, h, :], start=True, stop=True), reads=[kh.k(), vp.k(i)], writes=[kA])
        yield
        P.op("dve", lambda e: e.tensor_scalar(sm.ap[:, 0:1], nsb.ap[:, 96:97], -1.0, None, op0=ALU.mult), reads=[nsb.k()], writes=[sm.k(0)])
        P.op("dve", lambda e: e.scalar_tensor_tensor(sm.ap[:, 0:1], nsb.ap[:, 96:97], 1.0, sm.ap[:, 0:1], op0=ALU.max, op1=ALU.max), reads=[nsb.k(), sm.k(0)], writes=[sm.k(0)])
        P.op("dve", lambda e: e.reciprocal(sm.ap[:, 0:1], sm.ap[:, 0:1]), reads=[sm.k(0)], writes=[sm.k(0)])
        if d == 0:
            P.op("dve", lambda e: e.tensor_scalar(hsum.ap[:, i, h, :], nsb.ap[:, 0:96], sm.ap[:, 0:1], None, op0=ALU.mult), reads=[nsb.k(), sm.k(0)], writes=[hsum.k(i, h)])
        else:
            P.op("dve", lambda e: e.scalar_tensor_tensor(hsum.ap[:, i, h, :], nsb.ap[:, 0:96], sm.ap[:, 0:1], hsum.ap[:, i, h, :], op0=ALU.mult, op1=ALU.add),
                 reads=[nsb.k(), sm.k(0), hsum.k(i, h)], writes=[hsum.k(i, h)])
        P.op("dve", lambda e: e.scalar_tensor_tensor(C32.ap[0:96, h, :], C32.ap[0:96, h, :], sm.ap[0:96, 2:3], ps[bA][0:96, 256:353], op0=ALU.mult, op1=ALU.add),
             reads=[C32.k(h), sm.k(2), kA], writes=[C32.k(h)])
        P.op("act", lambda e: e.copy(Cb.ap[0:96, h, :], C32.ap[0:96, h, :]), reads=[C32.k(h)], writes=[Cb.k(h)])
        yield

    gens = [head_stream(h) for h in range(4)]
    while gens:
        for g_ in list(gens):
            try:
                next(g_)
            except StopIteration:
                gens.remove(g_)
    A.release(m1)
    yc = A.alloc("yc", (NT, 384), BF16)
    m3 = A.mark()
    wmo = load_w_bf16(k, "wmo", IN["w_in"].ap()[l][:, OFF_MO:OFF_MG], 8, 384, "wmo")
    lng = A.alloc("lng", (384,))
    P.dma("sp", lng.ap, IN["ml_ln_g"].ap()[l].partition_broadcast(128), writes=[lng.k()], semkey="lng")
    st = A.alloc("st", (16,)); cen = [A.alloc(f"cen{i}", (4, 96)) for i in range(2)]; junk = A.alloc("junkm", (96,))
    og = [A.alloc(f"og{i}", (384,)) for i in range(2)]
    for i in range(NT):
        r = i % 2
        b = 2 + r
        for c in range(8):
            P.op("pe", lambda e, c=c, i=i, b=b: e.matmul(ps[b][:, 0:384], k.hT.ap[:, c, i * 128:(i + 1) * 128], wmo.ap[:, c, :], start=(c == 0), stop=(c == 7)),
                 reads=[wmo.k(c), k.hT.k(i)], writes=[f"ps{b}"])
        P.op("act", lambda e, r=r, b=b: e.activation(og[r].ap, ps[b][:, 0:384], AF.Sigmoid), reads=[f"ps{b}"], writes=[og[r].k()])
        sk = st.k()
        P.op("dve", lambda e, i=i: e.tensor_reduce(st.ap[:, 0:4], hsum.ap[:, i, :, :], axis=AX.X, op=ALU.add), reads=[hsum.k(i, h_) for h_ in range(4)], writes=[sk])
        P.op("dve", lambda e: e.tensor_scalar(st.ap[:, 0:4], st.ap[:, 0:4], 1.0 / 96, None, op0=ALU.mult), reads=[sk], writes=[sk])
        P.op("pool", lambda e: e.memset(st.ap[:, 4:8], 0.0), writes=[sk])
        for h in range(4):
            P.op("dve", lambda e, i=i, h=h, r=r: e.tensor_scalar(cen[r].ap[:, h, :], hsum.ap[:, i, h, :], st.ap[:, h:h + 1], None, op0=ALU.subtract),
                 reads=[hsum.k(i, h), sk], writes=[cen[r].k(h)])
            P.op("act", lambda e, h=h, r=r: e.activation(junk.ap, cen[r].ap[:, h, :], AF.Square, accum_out=st.ap[:, 4 + h:5 + h]),
                 reads=[cen[r].k(h)], writes=[junk.k(), sk])
        P.op("act", lambda e: e.activation(st.ap[:, 8:12], st.ap[:, 4:8], AF.Sqrt, scale=1.0 / 96, bias=k.eps6.ap[:, 0:1]), reads=[sk, k.eps6.k()], writes=[sk])
        P.op("dve", lambda e: e.reciprocal(st.ap[:, 8:12], st.ap[:, 8:12]), reads=[sk], writes=[sk])
        for h in range(4):
            P.op("dve", lambda e, h=h, r=r: e.scalar_tensor_tensor(cen[r].ap[:, h, :], cen[r].ap[:, h, :], st.ap[:, 8 + h:9 + h], lng.ap[:, h * 96:(h + 1) * 96],
                                                                 op0=ALU.mult, op1=ALU.mult),
                 reads=[cen[r].k(h), sk, lng.k()], writes=[cen[r].k(h)])
        P.op("dve", lambda e, i=i, r=r: e.tensor_tensor(yc.ap[:, i, :], cen[r].ap.rearrange("p h c -> p (h c)"), og[r].ap, op=ALU.mult),
             reads=[cen[r].k(h) for h in range(4)] + [og[r].k()], writes=[yc.k(i)])
    A.release(m3)
    if k.dbg and "yc" in k.dbg:
        t = k.nc.dram_tensor("dbg_yc", [S, 384], BF16, kind="ExternalOutput")
        k.final_events.append(P.dma("sp", t.ap().rearrange("(i p) c -> p i c", p=128), yc.ap, reads=[yc.k(i) for i in range(NT)], semkey="dbg"))
    ycT = A.alloc("ycT", (3, S), BF16)
    transpose_to_T(k, yc, 3, ycT)
    outproj_partial(k, l, ycT, 3, 640, "c")
    A.release(m0)


OFF_RKV = 768
BLK = 256
PC_MU, PC_MUX, PC_W0, PC_A0, PC_KK, PC_KA, PC_RK, PC_LNG, PC_LNB, NPC = 0, 18, 20, 26, 32, 35, 38, 41, 44, 48


def host_rwkv_layout(inputs):
    out = {}
    pc = np.zeros((DEPTH, 128, NPC), np.float32)
    for l in range(DEPTH):
        mu = np.asarray(inputs["rk_mu"][l], np.float32)
        for d in range(2):
            for j in range(3):
                for p in range(3):
                    pc[l, :, PC_MU + d * 9 + j * 3 + p] = mu[d, j * 384 + p * 128: j * 384 + (p + 1) * 128]
            pc[l, 0:64, PC_MUX + d] = mu[d, 1152:1216]
            pc[l, 64:128, PC_MUX + d] = mu[d, 1216:1280]
            for p in range(3):
                pc[l, :, PC_W0 + d * 3 + p] = inputs["rk_w0"][l][d][p * 128:(p + 1) * 128]
                pc[l, :, PC_A0 + d * 3 + p] = inputs["rk_a0"][l][d][p * 128:(p + 1) * 128]
        for p in range(3):
            sl = slice(p * 128, (p + 1) * 128)
            pc[l, :, PC_KK + p] = inputs["rk_kk"][l][sl]
            pc[l, :, PC_KA + p] = inputs["rk_ka"][l][sl]
            pc[l, :, PC_RK + p] = np.asarray(inputs["rk_rk"][l]).reshape(384)[sl]
            pc[l, :, PC_LNG + p] = inputs["rk_ln_g"][l][sl]
            pc[l, :, PC_LNB + p] = inputs["rk_ln_b"][l][sl]
    out["c_pc"] = pc
    w_in = np.asarray(inputs["w_in"], np.float32)
    out["c_wx"] = np.ascontiguousarray(np.concatenate(
        [w_in[:, :, 1920:1984], w_in[:, :, 2048:2112], w_in[:, :, 1984:2048], w_in[:, :, 2112:2176], w_in[:, :, 2176:2304]], axis=2))
    return out


def rev_ap(ap, n):
    a = ap.ap
    return bass.AP(ap.tensor, ap.offset + (n - 1) * a[-1][0], [list(a[0]), [-a[-1][0], n]])


def psk(b, c0, c1):
    return [f"ps{b}q{q}" for q in range(c0 // 128, (c1 + 127) // 128)]


def rwkv_stage(k, l):
    P, A, ps, IN = k.P, k.A, k.ps, k.IN
    m0 = A.mark()
    k.mask4 = A.alloc("mask4", (512,)); k.rmask = A.alloc("rmask", (256,))
    P.dma("act", k.mask4.ap, IN["c_mask4"].ap(), writes=[k.mask4.k()], semkey="c5")
    P.dma("act", k.rmask.ap, IN["c_rmask"].ap(), writes=[k.rmask.k()], semkey="c7")
    ybT = A.alloc("ybT", (3, S), BF16)
    P.op("pool", lambda e: e.memset(ybT.ap, 0.0), writes=[ybT.k(i) for i in range(NT)])
    xsp = k.xspill.ap().rearrange("(i p) d -> p i d", p=128)
    for i in range(NT):
        P.dma("sp" if i % 2 == 0 else "act", xsp[:, i, :], k.xres.ap[:, i, :], reads=[k.xres.k(i)], writes=[("xsp", i)], semkey=("xso", i % 4))
    evs = []
    for kk_ in [q for q in P.keys if isinstance(q, tuple) and q[0] == k.xres.uid]:
        st = P.keys.pop(kk_)
        evs.extend(st["w"]); evs.extend(st["r"].values())
    AX = Arena.__new__(Arena)
    AX.P, AX.t, AX.n, AX.top, AX.gen, AX.live = P, A.t, k.xres.hi, k.xres.lo, 100000 + 1000 * l, []
    AX.pending = [(k.xres.lo, k.xres.hi, evs)]
    import os as _os
    LVL = int(_os.environ.get("RWKV_SETUP", "9"))
    pc = A.alloc("pc", (NPC + 4,))
    P.dma("sp", pc.ap[:, 0:NPC], IN["c_pc"].ap()[l], writes=[pc.k()], semkey="pc")
    PCO = NPC
    P.op("dve", lambda e: e.tensor_scalar(pc.ap[:, PCO:PCO + 3], pc.ap[:, PC_KA:PC_KA + 3], -1.0, 1.0, op0=ALU.mult, op1=ALU.add), reads=[pc.k()], writes=[pc.k()])
    wl = [A.alloc(f"wl{d}", (384,)) for d in range(2)]
    for d in range(2):
        P.dma("sp", wl[d].ap[0:64, :], IN["rk_w2"].ap()[l][d], writes=[wl[d].k()], semkey=("wl", d))
        P.dma("act", wl[d].ap[64:128, :], IN["rk_a2"].ap()[l][d], writes=[wl[d].k()], semkey=("wl2", d))
    g2b = A.alloc("g2b", (384,), BF16)
    P.dma("pool", g2b.ap, IN["rk_g2"].ap()[l], writes=[g2b.k()], semkey="g2b")
    siggd = A.alloc("siggd", (S,), BF16)
    wdad = [(A if int(_os.environ.get("WDAD_MAIN", "0")) else AX).alloc(f"wdad{d}", (S,)) for d in range(2)]
    mA = A.mark()
    wx = load_w_bf16(k, "wx", IN["c_wx"].ap()[l], 8, 384, "wx")
    zx = A.alloc("zx", (S + 2,)); tmp = A.alloc("tmpx", (S,))
    P.op("pool", lambda e: e.memset(zx.ap[:, 0:1], 0.0), writes=[zx.k()])
    P.op("pool", lambda e: e.memset(zx.ap[:, S + 1:S + 2], 0.0), writes=[zx.k()])
    n = 0
    for d in range(2 if LVL >= 2 else 0):
        for tg in range(4):
            b = n % 2; n += 1
            for c in range(8):
                P.op("pe", lambda e, c=c, d=d, tg=tg, b=b: e.matmul(ps[b][:, :], wx.ap[:, c, d * 128:(d + 1) * 128], k.hT.ap[:, c, tg * 512:(tg + 1) * 512], start=(c == 0), stop=(c == 7)),
                     reads=[wx.k(c)] + [k.hT.k(4 * tg + q) for q in range(4)], writes=[f"ps{b}"])
            P.op("act", lambda e, tg=tg, b=b: e.copy(zx.ap[:, 1 + tg * 512:1 + (tg + 1) * 512], ps[b][:, :]), reads=[f"ps{b}"], writes=[zx.k()])
        if LVL < 3:
            continue
        if d == 0:
            cur, prv = zx.ap[:, 1:S + 1], zx.ap[:, 0:S]
        else:
            cur, prv = rev_ap(zx.ap[:, 1:S + 1], S), rev_ap(zx.ap[:, 2:S + 2], S)
        P.op("dve", lambda e, cur=cur, prv=prv: e.tensor_tensor(tmp.ap, prv, cur, op=ALU.subtract), reads=[zx.k()], writes=[tmp.k()])
        P.op("dve", lambda e, cur=cur, d=d: e.scalar_tensor_tensor(wdad[d].ap, tmp.ap, pc.ap[:, PC_MUX + d:PC_MUX + d + 1], cur, op0=ALU.mult, op1=ALU.add),
             reads=[tmp.k(), pc.k(), zx.k()], writes=[wdad[d].k()])
        P.op("act", lambda e, d=d: e.activation(wdad[d].ap[0:64, :], wdad[d].ap[0:64, :], AF.Tanh), reads=[wdad[d].k()], writes=[wdad[d].k()])
    for tg in range(4 if LVL >= 4 else 0):
        b = n % 2; n += 1
        for c in range(8):
            P.op("pe", lambda e, c=c, tg=tg, b=b: e.matmul(ps[b][:, :], wx.ap[:, c, 256:384], k.hT.ap[:, c, tg * 512:(tg + 1) * 512], start=(c == 0), stop=(c == 7)),
                 reads=[wx.k(c)] + [k.hT.k(4 * tg + q) for q in range(4)], writes=[f"ps{b}"])
        P.op("act", lambda e, tg=tg, b=b: e.activation(siggd.ap[:, tg * 512:(tg + 1) * 512], ps[b][:, :], AF.Sigmoid), reads=[f"ps{b}"], writes=[siggd.k()])
    A.release(mA)
    NB = ["XR", "XK", "XV", "T1", "LW", "AA", "KK", "SQ", "CUM"]
    NB16 = ["XRb", "XKb", "KKb", "AAb", "XVb", "BH", "KH"]
    SB = []
    for s_ in range(2):
        sb = {nm: A.alloc(f"{nm}{s_}", (BLK,)) for nm in NB}
        sb.update({nm: A.alloc(f"{nm}{s_}", (BLK,), BF16) for nm in NB16})
        sb["PCc"] = A.alloc(f"PCc{s_}", (BLK // 128,))
        sb["AR"] = A.alloc(f"AR{s_}", (BLK // 128, 256), BF16)
        sb["FT"] = A.alloc(f"FT{s_}", (512,))
        sb["AM"] = [A.alloc(f"AM{s_}{x}", (512,), BF16) for x in range(2)]
        sb["M"] = [[A.alloc(f"M{s_}{x}{q}", (128,), BF16) for q in range(2)] for x in range(2)]
        sb["MT"] = [[A.alloc(f"MT{s_}{x}{q}", (128,), BF16) for q in range(2)] for x in range(2)]
        sb["B"] = [[A.alloc(f"Bq{s_}{x}{q}", (384,), BF16) for q in range(2)] for x in range(2)]
        sb["Q"] = [A.alloc(f"Q{s_}{x}", (128,)) for x in range(2)]
        sb["Q16"] = [A.alloc(f"Qb{s_}{x}", (128,), BF16) for x in range(2)]
        sb["RHS"] = A.alloc(f"RHS{s_}", (128,), BF16)
        sb["SAz"] = [A.alloc(f"SAz{s_}{x}", (128,), BF16) for x in range(2)]
        sb["Vz"] = [A.alloc(f"Vz{s_}{x}", (128,), BF16) for x in range(2)]
        sb["BHt"] = A.alloc(f"BHt{s_}", (128,), BF16); sb["KHt"] = A.alloc(f"KHt{s_}", (128,), BF16)
        sb["T"] = A.alloc(f"Tst{s_}", (128,)); sb["T16"] = A.alloc(f"Tsb{s_}", (128,), BF16)
        sb["pb"] = 4 * s_
        SB.append(sb)
    import os as _os
    for p in range(int(_os.environ.get('RWKV_PAIRS', '3'))):
        mX = AX.mark()
        wp = AX.alloc("wp", (8, 3, 128), BF16)
        wv_ = IN["w_in"].ap()[l].rearrange("(c q) n -> q c n", q=128)
        for j in range(3):
            c0 = OFF_RKV + j * 384 + p * 128
            P.dma("pool", wp.ap[:, :, j, :], wv_[:, :, c0:c0 + 128], writes=[wp.k(j)], semkey=("wp", j))
        zp = AX.alloc("zp", (3, S + 2))
        yacc = AX.alloc("yacc", (S,)); bonacc = AX.alloc("bonacc", (S,))
        P.op("pool", lambda e: e.memset(yacc.ap, 0.0), writes=[yacc.k(c) for c in range(NT)])
        P.op("pool", lambda e: e.memset(bonacc.ap, 0.0), writes=[bonacc.k(c) for c in range(NT)])
        for j in range(3):
            P.op("pool", lambda e, j=j: e.memset(zp.ap[:, j, 0:1], 0.0), writes=[zp.k(j)])
            P.op("pool", lambda e, j=j: e.memset(zp.ap[:, j, S + 1:S + 2], 0.0), writes=[zp.k(j)])
            for tg in range(4):
                b = n % 2; n += 1
                for c in range(8):
                    P.op("pe", lambda e, c=c, j=j, tg=tg, b=b: e.matmul(ps[b][:, :], wp.ap[:, c, j, :], k.hT.ap[:, c, tg * 512:(tg + 1) * 512], start=(c == 0), stop=(c == 7)),
                         reads=[wp.k(j)] + [k.hT.k(4 * tg + q) for q in range(4)], writes=[f"ps{b}"] + psk(b, 0, 512))
                P.op("act", lambda e, j=j, tg=tg, b=b: e.copy(zp.ap[:, j, 1 + tg * 512:1 + (tg + 1) * 512], ps[b][:, :]), reads=[f"ps{b}"] + psk(b, 0, 512), writes=[zp.k(j)])
        gens = [rwkv_stream(k, l, p, d, SB[d], pc, wl[d], wdad[d], zp, yacc, bonacc, PCO) for d in range(int(_os.environ.get("RWKV_NDIR", "2")))]
        while gens:
            for g in list(gens):
                try:
                    next(g)
                except StopIteration:
                    gens.remove(g)
        T1 = SB[0]["FT"]; T2 = SB[1]["FT"]
        for tg in range(4 if int(_os.environ.get("RWKV_FIN", "1")) else 0):
            cs = slice(tg * 512, (tg + 1) * 512)
            yk = [yacc.k(c) for c in range(4 * tg, 4 * tg + 4)]
            P.op("pe", lambda e, cs=cs: e.matmul(ps[0][:, :], k.onesblk64.ap, yacc.ap[:, cs], start=True, stop=True), reads=[k.onesblk64.k()] + yk, writes=["ps0"] + psk(0, 0, 512))
            P.op("dve", lambda e, cs=cs: e.tensor_tensor(yacc.ap[:, cs], yacc.ap[:, cs], ps[0][:, :], op=ALU.subtract), reads=["ps0"] + psk(0, 0, 512) + yk, writes=yk)
            P.op("act", lambda e, cs=cs: e.activation(T1.ap, yacc.ap[:, cs], AF.Square), reads=yk, writes=[T1.k()])
            P.op("pe", lambda e: e.matmul(ps[1][:, :], k.onesblk64.ap, T1.ap, start=True, stop=True), reads=[k.onesblk64.k(), T1.k()], writes=["ps1"] + psk(1, 0, 512))
            P.op("act", lambda e: e.activation(T2.ap, ps[1][:, :], AF.Sqrt, bias=k.epsgn.ap[:, 0:1]), reads=["ps1", k.epsgn.k()] + psk(1, 0, 512), writes=[T2.k()])
            P.op("dve", lambda e: e.reciprocal(T2.ap, T2.ap), reads=[T2.k()], writes=[T2.k()])
            P.op("dve", lambda e, cs=cs: e.tensor_tensor(yacc.ap[:, cs], yacc.ap[:, cs], T2.ap, op=ALU.mult), reads=yk + [T2.k()], writes=yk)
            P.op("dve", lambda e, cs=cs, p=p: e.tensor_scalar(yacc.ap[:, cs], yacc.ap[:, cs], pc.ap[:, PC_LNG + p:PC_LNG + p + 1], pc.ap[:, PC_LNB + p:PC_LNB + p + 1], op0=ALU.mult, op1=ALU.add),
                 reads=yk + [pc.k()], writes=yk)
            P.op("dve", lambda e, cs=cs: e.tensor_tensor(yacc.ap[:, cs], yacc.ap[:, cs], bonacc.ap[:, cs], op=ALU.add), reads=yk + [bonacc.k(c) for c in range(4 * tg, 4 * tg + 4)], writes=yk)
            P.op("pe", lambda e, cs=cs, p=p: e.matmul(ps[2][:, :], g2b.ap[:, p * 128:(p + 1) * 128], siggd.ap[:, cs], start=True, stop=True), reads=[g2b.k(), siggd.k()], writes=["ps2"] + psk(2, 0, 512))
            P.op("dve", lambda e, cs=cs, p=p: e.tensor_tensor(ybT.ap[:, p, cs], yacc.ap[:, cs], ps[2][:, :], op=ALU.mult), reads=yk + ["ps2"] + psk(2, 0, 512), writes=[ybT.k(c) for c in range(4 * tg, 4 * tg + 4)])
        AX.release(mX)
    AX.release((k.xres.lo, 0))
    evs = []
    for (_, _, e_) in AX.pending:
        evs.extend(e_)
    P.inherit[k.xres.uid] = evs
    for i in range(NT):
        P.dma("sp" if i % 2 == 0 else "act", k.xres.ap[:, i, :], xsp[:, i, :], reads=[("xsp", i)], writes=[k.xres.k(i)], semkey=("xsi", i % 4))
    if k.dbg and "yb" in k.dbg:
        t = k.nc.dram_tensor("dbg_yb", [384, S], BF16, kind="ExternalOutput")
        k.final_events.append(P.dma("sp", t.ap().rearrange("(c p) s -> p c s", p=128), ybT.ap, reads=[ybT.k(i) for i in range(NT)], semkey="dbg"))
    if LVL >= 5:
        outproj_partial(k, l, ybT, 3, 256, "b")
    A.release(m0)


def rwkv_stream(k, l, p, d, sb, pc, wl, wdad, zp, yacc, bonacc, PCO):
    P, ps = k.P, k.ps
    pb = sb["pb"]
    B0, B1, B2, B3 = pb, pb + 1, pb + 2, pb + 3
    XR, XK, XV, T1, LW, AA, KK, SQ, CUM, BH, KH, PCc = (sb[n_] for n_ in ["XR", "XK", "XV", "T1", "LW", "AA", "KK", "SQ", "CUM", "BH", "KH", "PCc"])
    XRb, XKb, KKb, AAb, XVb = (sb[n_] for n_ in ["XRb", "XKb", "KKb", "AAb", "XVb"])
    T = sb["T"]; T16 = sb["T16"]
    P.op("pool", lambda e: e.memset(T16.ap, 0.0), writes=[T16.k()])
    col = lambda c: pc.ap[:, c:c + 1]
    P.op("pool", lambda e: e.memset(T.ap, 0.0), writes=[T.k()])
    for x in range(2):
        P.op("pool", lambda e, x=x: e.memset(sb["SAz"][x].ap, 0.0), writes=[sb["SAz"][x].k()])
        P.op("pool", lambda e, x=x: e.memset(sb["Vz"][x].ap, 0.0), writes=[sb["Vz"][x].k()])
    import os as _os
    NBLK = int(_os.environ.get("RWKV_NBLK", str(S // BLK))); PH = int(_os.environ.get("RWKV_PHASE", "9"))
    def _blk(bi):
        t0 = bi * BLK
        if d == 0:
            cur = lambda j: zp.ap[:, j, 1 + t0:1 + t0 + BLK]
            prv = lambda j: zp.ap[:, j, t0:t0 + BLK]
            nat = lambda buf: buf.ap[:, t0:t0 + BLK]
            nchunks = list(range(t0 // 128, (t0 + BLK) // 128))
        else:
            a_ = S - t0 - BLK
            cur = lambda j: rev_ap(zp.ap[:, j, 1 + a_:1 + a_ + BLK], BLK)
            prv = lambda j: rev_ap(zp.ap[:, j, 2 + a_:2 + a_ + BLK], BLK)
            nat = lambda buf: rev_ap(buf.ap[:, a_:a_ + BLK], BLK)
            nchunks = list(range(a_ // 128, (a_ + BLK) // 128))
        scols = slice(t0, t0 + BLK)
        P0 = int(_os.environ.get("RWKV_P0", "9"))
        for j, X in enumerate((XR, XK, XV)):
            if P0 < 2:
                break
            P.op("dve", lambda e, j=j, prv=prv, cur=cur: e.tensor_tensor(T1.ap, prv(j), cur(j), op=ALU.subtract), reads=[zp.k(j)], writes=[T1.k()])
            P.op("dve", lambda e, j=j, X=X, cur=cur: e.scalar_tensor_tensor(X.ap, T1.ap, col(PC_MU + d * 9 + j * 3 + p), cur(j), op0=ALU.mult, op1=ALU.add),
                 reads=[T1.k(), zp.k(j), pc.k()], writes=[X.k()])
        if P0 >= 3:
            P.op("pe", lambda e, scols=scols: e.matmul(ps[B3][:, 0:BLK], wl.ap[0:64, p * 128:(p + 1) * 128], wdad.ap[0:64, scols], start=True, stop=True),
                 reads=[wl.k(), wdad.k()], writes=psk(B3, 0, BLK), serial=True)
        if P0 >= 4:
            P.op("pe", lambda e, scols=scols: e.matmul(ps[B3][:, 256:256 + BLK], wl.ap[64:128, p * 128:(p + 1) * 128], wdad.ap[64:128, scols], start=True, stop=True),
                 reads=[wl.k(), wdad.k()], writes=psk(B3, 256, 256 + BLK), serial=True)
        if P0 >= 5:
            P.op("act", lambda e: e.activation(LW.ap, ps[B3][:, 0:BLK], AF.Sigmoid, bias=col(PC_W0 + d * 3 + p)), reads=psk(B3, 0, BLK) + [pc.k()], writes=[LW.k()])
            P.op("act", lambda e: e.activation(AA.ap, ps[B3][:, 256:256 + BLK], AF.Sigmoid, bias=col(PC_A0 + d * 3 + p)), reads=psk(B3, 256, 256 + BLK) + [pc.k()], writes=[AA.k()])
        if P0 >= 6:
            P.op("pool", lambda e: e.tensor_scalar(LW.ap, LW.ap, -0.6065306597126334, None, op0=ALU.mult), reads=[LW.k()], writes=[LW.k()])
        yield
        if PH <= 1:
            return
        P.op("act", lambda e: e.activation(KK.ap, XK.ap, AF.Copy, scale=col(PC_KK + p)), reads=[XK.k(), pc.k()], writes=[KK.k()])
        P.op("act", lambda e: e.activation(SQ.ap, KK.ap, AF.Square), reads=[KK.k()], writes=[SQ.k()])
        P.op("pe", lambda e: e.matmul(ps[B2][:, 0:BLK], k.onesblk.ap, SQ.ap, start=True, stop=True), reads=[k.onesblk.k(), SQ.k()], writes=psk(B2, 0, BLK))
        P.op("act", lambda e: e.activation(SQ.ap, ps[B2][:, 0:BLK], AF.Sqrt), reads=psk(B2, 0, BLK), writes=[SQ.k()])
        P.op("dve", lambda e: e.tensor_scalar(SQ.ap, SQ.ap, 1e-12, None, op0=ALU.max), reads=[SQ.k()], writes=[SQ.k()])
        P.op("dve", lambda e: e.reciprocal(SQ.ap, SQ.ap), reads=[SQ.k()], writes=[SQ.k()])
        P.op("dve", lambda e: e.tensor_tensor(KK.ap, KK.ap, SQ.ap, op=ALU.mult), reads=[KK.k(), SQ.k()], writes=[KK.k()])
        P.op("pool", lambda e: e.tensor_scalar(T1.ap, AA.ap, col(PC_KA + p), col(PCO + p), op0=ALU.mult, op1=ALU.add), reads=[AA.k(), pc.k()], writes=[T1.k()])
        P.op("pool", lambda e: e.tensor_tensor(XK.ap, XK.ap, T1.ap, op=ALU.mult), reads=[XK.k(), T1.k()], writes=[XK.k()])
        P.op("dve", lambda e: e.scalar_tensor_tensor(T1.ap, XR.ap, col(PC_RK + p), XK.ap, op0=ALU.mult, op1=ALU.mult), reads=[XR.k(), XK.k(), pc.k()], writes=[T1.k()])
        P.op("pe", lambda e: e.matmul(ps[B2][:, 256:256 + BLK], k.onesblk.ap, T1.ap, start=True, stop=True), reads=[k.onesblk.k(), T1.k()], writes=psk(B2, 256, 256 + BLK))
        P.op("dve", lambda e: e.tensor_tensor(T1.ap, ps[B2][:, 256:256 + BLK], XV.ap, op=ALU.mult), reads=psk(B2, 256, 256 + BLK) + [XV.k()], writes=[T1.k()])
        bk = [bonacc.k(c) for c in nchunks]
        P.op("dve", lambda e: e.tensor_tensor(nat(bonacc), nat(bonacc), T1.ap, op=ALU.add), reads=bk + [T1.k()], writes=bk)
        P.op("pool", lambda e: e.tensor_tensor(AA.ap, AA.ap, KK.ap, op=ALU.mult), reads=[AA.k(), KK.k()], writes=[AA.k()])
        yield
        if PH <= 2:
            return
        P.op("dve", lambda e: e.tensor_tensor_scan(CUM.ap, k.rmask.ap[:, 0:BLK], LW.ap, 0.0, op0=ALU.mult, op1=ALU.add), reads=[k.rmask.k(), LW.k()], writes=[CUM.k()])
        P.op("pool", lambda e: e.tensor_tensor(LW.ap, CUM.ap, LW.ap, op=ALU.subtract), reads=[CUM.k(), LW.k()], writes=[LW.k()])
        P.op("act", lambda e: e.activation(T1.ap, CUM.ap, AF.Exp), reads=[CUM.k()], writes=[T1.k()])
        P.op("dve", lambda e: e.tensor_tensor(XR.ap, XR.ap, T1.ap, op=ALU.mult), reads=[XR.k(), T1.k()], writes=[XR.k()])
        P.op("act", lambda e: e.activation(SQ.ap, CUM.ap, AF.Exp, scale=-1.0), reads=[CUM.k()], writes=[SQ.k()])
        P.op("dve", lambda e: e.tensor_tensor(AA.ap, AA.ap, SQ.ap, op=ALU.mult), reads=[AA.k(), SQ.k()], writes=[AA.k()])
        P.op("dve", lambda e: e.tensor_tensor(XK.ap, XK.ap, SQ.ap, op=ALU.mult), reads=[XK.k(), SQ.k()], writes=[XK.k()])
        P.op("act", lambda e: e.activation(T1.ap, LW.ap, AF.Exp), reads=[LW.k()], writes=[T1.k()])
        P.op("dve", lambda e: e.scalar_tensor_tensor(KK.ap, KK.ap, -1.0, T1.ap, op0=ALU.mult, op1=ALU.mult), reads=[KK.k(), T1.k()], writes=[KK.k()])
        AR = sb["AR"]
        for src_, dst_ in ((XK, XKb), (AA, AAb), (XV, XVb)):
            P.op("act", lambda e, src_=src_, dst_=dst_: e.copy(dst_.ap, src_.ap), reads=[src_.k()], writes=[dst_.k()])
        P.op("act", lambda e: e.copy(AR.ap[:, :, 0:128], KK.ap.rearrange("p (c t) -> p c t", t=128)), reads=[KK.k()], writes=[AR.k()])
        P.op("act", lambda e: e.copy(AR.ap[:, :, 128:256], XR.ap.rearrange("p (c t) -> p c t", t=128)), reads=[XR.k()], writes=[AR.k()])
        P.op("act", lambda e: e.activation(PCc.ap, CUM.ap.rearrange("p (c t) -> p c t", t=128)[:, :, 127], AF.Exp), reads=[CUM.k()], writes=[PCc.k()])
        for ch in range(BLK // 128):
            cs = slice(ch * 128, (ch + 1) * 128)
            P.op("act", lambda e, cs=cs, ch=ch: e.activation(BH.ap[:, cs], AA.ap[:, cs], AF.Copy, scale=PCc.ap[:, ch:ch + 1]), reads=[AA.k(), PCc.k()], writes=[BH.k()])
            P.op("act", lambda e, cs=cs, ch=ch: e.activation(KH.ap[:, cs], XK.ap[:, cs], AF.Copy, scale=PCc.ap[:, ch:ch + 1]), reads=[XK.k(), PCc.k()], writes=[KH.k()])
        yield
        if PH <= 3:
            return
        def _chunk(ch):
            cs = slice(ch * 128, (ch + 1) * 128)
            AM, Mb, MTb, Q, RHS, SAz, Vz, BHt, KHt = sb["AM"], sb["M"], sb["MT"], sb["Q"], sb["RHS"], sb["SAz"], sb["Vz"], sb["BHt"], sb["KHt"]
            for x in range(2):
                hs = slice(64 * x, 64 * x + 64)
                bx = B0 + x
                for q, lh in enumerate((AAb, XKb)):
                    P.op("pe", lambda e, lh=lh, q=q, hs=hs, bx=bx: e.matmul(ps[bx][:, q * 256:(q + 1) * 256], lh.ap[hs, cs], sb["AR"].ap[hs, ch, :], start=True, stop=True),
                         reads=[lh.k(), sb["AR"].k()], writes=psk(bx, q * 256, (q + 1) * 256))
                P.op("pe", lambda e, hs=hs, x=x: e.matmul(ps[B2][:, x * 128:(x + 1) * 128], sb["AR"].ap[hs, ch, 0:128], AAb.ap[hs, cs], start=True, stop=True),
                     reads=[sb["AR"].k(), AAb.k()], writes=psk(B2, x * 128, (x + 1) * 128), serial=True)
                P.op("dve", lambda e, x=x, bx=bx: e.tensor_tensor(AM[x].ap, ps[bx][:, :], k.mask4.ap, op=ALU.mult), reads=psk(bx, 0, 512) + [k.mask4.k()], writes=[AM[x].k()])
                P.op("dve", lambda e, x=x: e.tensor_tensor(MTb[x][0].ap, ps[B2][:, x * 128:(x + 1) * 128], k.trils.ap, op=ALU.mult), reads=psk(B2, x * 128, (x + 1) * 128) + [k.trils.k()], writes=[MTb[x][0].k()])
            yield
            if PH <= 4:
                return
            pbt = ps[B3][:, 0:192].bitcast(BF16)
            for q, src in enumerate((XVb, BH, KH)):
                P.op("pe", lambda e, q=q, src=src: e.transpose(pbt[:, q * 128:(q + 1) * 128], src.ap[:, cs], k.ident16.ap), reads=[src.k(), k.ident16.k()], writes=psk(B3, 0, 192))
            P.op("act", lambda e: e.copy(Vz[0].ap[:, 0:64], pbt[:, 0:64]), reads=psk(B3, 0, 192), writes=[Vz[0].k()])
            P.op("act", lambda e: e.copy(Vz[1].ap[:, 64:128], pbt[:, 64:128]), reads=psk(B3, 0, 192), writes=[Vz[1].k()])
            P.op("dve", lambda e: e.tensor_copy(BHt.ap, pbt[:, 128:256]), reads=psk(B3, 0, 192), writes=[BHt.k()])
            P.op("dve", lambda e: e.tensor_copy(KHt.ap, pbt[:, 256:384]), reads=psk(B3, 0, 192), writes=[KHt.k()])
            Bq = sb["B"]
            for x in range(2):
                bx = B0 + x
                P.op("pe", lambda e, bx=bx, x=x: e.matmul(ps[bx][:, 0:128], AM[x].ap[:, 0:128], MTb[x][0].ap, start=True, stop=True), reads=[AM[x].k(), MTb[x][0].k()], writes=psk(bx, 0, 128))
                P.op("pe", lambda e, bx=bx, x=x: e.matmul(ps[bx][:, 128:256], MTb[x][0].ap, AM[x].ap[:, 0:128], start=True, stop=True), reads=[AM[x].k(), MTb[x][0].k()], writes=psk(bx, 128, 256))
                P.op("act", lambda e, bx=bx, x=x: e.copy(Bq[x][1].ap[:, 0:256], ps[bx][:, 0:256]), reads=psk(bx, 0, 256), writes=[Bq[x][1].k()])
                P.op("dve", lambda e, x=x: e.tensor_tensor(Bq[x][1].ap[:, 256:384], AM[x].ap[:, 0:128], k.ident.ap, op=ALU.add), reads=[AM[x].k(), k.ident.k()], writes=[Bq[x][1].k()])
            yield
            for lev in range(1, 7):
                cur, nxt = lev % 2, 1 - lev % 2
                for x in range(2):
                    bx = B0 + x
                    Bc, Bn = Bq[x][cur], Bq[x][nxt]
                    if lev < 6:
                        P.op("pe", lambda e, bx=bx, Bc=Bc: e.matmul(ps[bx][:, 0:128], Bc.ap[:, 128:256], Bc.ap[:, 0:128], start=True, stop=True), reads=[Bc.k()], writes=psk(bx, 0, 128))
                        P.op("pe", lambda e, bx=bx, Bc=Bc: e.matmul(ps[bx][:, 128:384], Bc.ap[:, 0:128], Bc.ap[:, 128:384], start=True, stop=True), reads=[Bc.k()], writes=psk(bx, 128, 384))
                        P.op("act", lambda e, bx=bx, Bn=Bn: e.copy(Bn.ap[:, 0:256], ps[bx][:, 0:256]), reads=psk(bx, 0, 384), writes=[Bn.k()])
                        P.op("dve", lambda e, bx=bx, Bc=Bc, Bn=Bn: e.tensor_tensor(Bn.ap[:, 256:384], Bc.ap[:, 256:384], ps[bx][:, 256:384], op=ALU.add), reads=psk(bx, 0, 384) + [Bc.k()], writes=[Bn.k()])
                    else:
                        P.op("pe", lambda e, bx=bx, Bc=Bc: e.matmul(ps[bx][:, 256:384], Bc.ap[:, 0:128], Bc.ap[:, 256:384], start=True, stop=True), reads=[Bc.k()], writes=psk(bx, 256, 384))
                        P.op("dve", lambda e, bx=bx, Bc=Bc, x=x: e.tensor_tensor(sb["Q16"][x].ap, Bc.ap[:, 256:384], ps[bx][:, 256:384], op=ALU.add), reads=psk(bx, 0, 384) + [Bc.k()], writes=[sb["Q16"][x].k()])
                yield
            for x in range(2):
                hs = slice(64 * x, 64 * x + 64)
                P.op("pe", lambda e, hs=hs: e.matmul(ps[B2][:, 256 + hs.start:256 + hs.stop], sb["AR"].ap[hs, ch, 0:128], T16.ap[hs, hs], start=True, stop=False), reads=[sb["AR"].k(), T16.k()], writes=psk(B2, 256, 384), serial=True)
                P.op("pe", lambda e, hs=hs, x=x: e.matmul(ps[B2][:, 256 + hs.start:256 + hs.stop], AM[x].ap[:, 256:384], Vz[x].ap[:, hs], start=False, stop=True), reads=[AM[x].k(), Vz[x].k()], writes=psk(B2, 256, 384))
            P.op("act", lambda e: e.copy(RHS.ap, ps[B2][:, 256:384]), reads=psk(B2, 256, 384), writes=[RHS.k()])
            for x in range(2):
                hs = slice(64 * x, 64 * x + 64)
                P.op("pe", lambda e, hs=hs, x=x: e.matmul(ps[B2][:, 384 + hs.start:384 + hs.stop], sb["Q16"][x].ap, RHS.ap[:, hs], start=True, stop=True), reads=[sb["Q16"][x].k(), RHS.k()], writes=psk(B2, 384, 512))
                P.op("act" if x == 0 else "dve", (lambda e, hs=hs, x=x: e.copy(SAz[x].ap[:, hs], ps[B2][:, 384 + hs.start:384 + hs.stop])) if x == 0 else
                     (lambda e, hs=hs, x=x: e.tensor_copy(SAz[x].ap[:, hs], ps[B2][:, 384 + hs.start:384 + hs.stop])), reads=psk(B2, 384, 512), writes=[SAz[x].k()])
            yield
            if PH <= 6:
                return
            ops_ = []
            for x in range(2):
                hs = slice(64 * x, 64 * x + 64)
                ops_.append((T16.ap[hs, :], sb["AR"].ap[hs, ch, 128:256], [T16.k(), sb["AR"].k()]))
                ops_.append((SAz[x].ap, AM[x].ap[:, 128:256], [SAz[x].k(), AM[x].k()]))
                ops_.append((Vz[x].ap, AM[x].ap[:, 384:512], [Vz[x].k(), AM[x].k()]))
            for q, (lh, rh, rd) in enumerate(ops_):
                P.op("pe", lambda e, lh=lh, rh=rh, q=q: e.matmul(ps[B3][:, 384:512], lh, rh, start=(q == 0), stop=(q == len(ops_) - 1)), reads=rd, writes=psk(B3, 384, 512), serial=(q % 3 == 0))
            cn = nchunks[ch] if d == 0 else nchunks[len(nchunks) - 1 - ch]
            if d == 0:
                ydst = yacc.ap[:, cn * 128:(cn + 1) * 128]
            else:
                ydst = rev_ap(yacc.ap[:, cn * 128:(cn + 1) * 128], 128)
            P.op("dve", lambda e, ydst=ydst: e.tensor_tensor(ydst, ydst, ps[B3][:, 384:512], op=ALU.add), reads=psk(B3, 384, 512) + [yacc.k(cn)], writes=[yacc.k(cn)])
            for x in range(2):
                hs = slice(64 * x, 64 * x + 64)
                P.op("pe", lambda e, hs=hs, x=x: e.matmul(ps[B2][:, hs], BHt.ap, SAz[x].ap[:, hs], start=True, stop=False), reads=[BHt.k(), SAz[x].k()], writes=psk(B2, 0, 128))
                P.op("pe", lambda e, hs=hs, x=x: e.matmul(ps[B2][:, hs], KHt.ap, Vz[x].ap[:, hs], start=False, stop=True), reads=[KHt.k(), Vz[x].k()], writes=psk(B2, 0, 128))
            for x in range(2):
                hs = slice(64 * x, 64 * x + 64)
                P.op("dve", lambda e, hs=hs, ch=ch: e.scalar_tensor_tensor(T.ap[hs, hs], T.ap[hs, hs], PCc.ap[hs, ch:ch + 1], ps[B2][hs, hs], op0=ALU.mult, op1=ALU.add),
                     reads=[T.k(), PCc.k()] + psk(B2, 0, 128), writes=[T.k()])
            P.op("act", lambda e: e.copy(T16.ap, T.ap), reads=[T.k()], writes=[T16.k()])
            yield
            if PH <= 7:
                return
        for ch in range(BLK // 128):
            yield from _chunk(ch)

    for bi in range(NBLK):
        yield from _blk(bi)


FB = 256
NFB = DFF // FB
NFC = DFF // 128
W2G = 2


def moe_stage(k, l):
    P, A, ps, IN = k.P, k.A, k.ps, k.IN
    m0 = A.mark()
    k.ohb = A.alloc("ohb", (2048,))
    P.dma("act", k.ohb.ap[0:16, :], IN["c_ohb"].ap(), writes=[k.ohb.k()], semkey="c4")
    x2b = A.alloc("x2b", (NT, D), BF16)
    aff = A.alloc("aff", (NT, NE))
    pm = A.alloc("pm", (NT, NE))
    pmT = A.alloc("pmT", (S,))
    m1 = A.mark()
    gb = A.alloc("gb2", (D,)); junk = A.alloc("junk2", (D,)); ss = A.alloc("ss2", (NT,)); rstd = A.alloc("rstd2", (NT,))
    x2f = [A.alloc(f"x2f{i}", (D,)) for i in range(2)]
    x2T = [A.alloc(f"x2T{i}", (8, 128)) for i in range(2)]
    rsb = A.alloc("rsb", (8, NE)); sm = A.alloc("smx", (NT, 4))
    affT = A.alloc("affT", (S,))
    P.dma("sp", gb.ap, IN["ln2_g"].ap()[l].partition_broadcast(128), writes=[gb.k()], semkey="gb")
    P.dma("act", rsb.ap, IN["router"].ap()[l].rearrange("(c p) e -> p c e", p=128), writes=[rsb.k()], semkey="rsb")
    P.op("pool", lambda e: e.memset(ss.ap, 0.0), writes=[ss.k(i) for i in range(NT)])
    P.op("pool", lambda e: e.memset(sm.ap, 0.0), writes=[sm.k()])
    for i in range(NT):
        P.op("act", lambda e, i=i: e.activation(junk.ap, k.xres.ap[:, i, :], AF.Square, accum_out=ss.ap[:, i:i + 1]),
             reads=[k.xres.k(i)], writes=[junk.k(), ss.k(i)])
    P.op("act", lambda e: e.activation(rstd.ap, ss.ap, AF.Sqrt, scale=1.0 / D, bias=k.eps6.ap[:, 0:1]),
         reads=[ss.k(i) for i in range(NT)] + [k.eps6.k()], writes=[rstd.k()])
    P.op("dve", lambda e: e.reciprocal(rstd.ap, rstd.ap), reads=[rstd.k()], writes=[rstd.k()])
    for i in range(NT):
        xf = x2f[i % 2]; xt = x2T[i % 2]
        P.op("dve", lambda e, i=i, xf=xf: e.scalar_tensor_tensor(xf.ap, k.xres.ap[:, i, :], rstd.ap[:, i:i + 1], gb.ap, op0=ALU.mult, op1=ALU.mult),
             reads=[k.xres.k(i), rstd.k(), gb.k()], writes=[xf.k()])
        P.op("act", lambda e, i=i, xf=xf: e.copy(x2b.ap[:, i, :], xf.ap), reads=[xf.k()], writes=[x2b.k(i)])
        for hb_ in range(2):
            b = 2 * (i % 2) + hb_
            for c in range(4):
                cc = hb_ * 4 + c
                P.op("pe", lambda e, b=b, c=c, cc=cc, xf=xf: e.transpose(ps[b][:, c * 128:(c + 1) * 128], xf.ap[:, cc * 128:(cc + 1) * 128], k.ident.ap),
                     reads=[xf.k(), k.ident.k()], writes=[f"ps{b}"])
            P.op("act" if hb_ == 0 else "dve",
                 (lambda e, b=b, hb_=hb_, xt=xt: e.copy(xt.ap[:, hb_ * 4:hb_ * 4 + 4, :], ps[b][:, :].rearrange("p (c t) -> p c t", c=4))) if hb_ == 0 else
                 (lambda e, b=b, hb_=hb_, xt=xt: e.tensor_copy(xt.ap[:, hb_ * 4:hb_ * 4 + 4, :], ps[b][:, :].rearrange("p (c t) -> p c t", c=4))),
                 reads=[f"ps{b}"], writes=[xt.k(hb_)])
        lb = 4 + i % 2
        for c in range(8):
            P.op("pe", lambda e, c=c, lb=lb, xt=xt: e.matmul(ps[lb][:, 0:NE], xt.ap[:, c, :], rsb.ap[:, c, :], start=(c == 0), stop=(c == 7)),
                 reads=[xt.k(0), xt.k(1), rsb.k()], writes=[f"ps{lb}"])
        P.op("dve", lambda e, i=i, lb=lb: e.tensor_reduce(sm.ap[:, i, 0:1], ps[lb][:, 0:NE], axis=AX.X, op=ALU.max), reads=[f"ps{lb}"], writes=[sm.k()])
        P.op("dve", lambda e, i=i: e.tensor_scalar(sm.ap[:, i, 0:1], sm.ap[:, i, 0:1], -1.0, None, op0=ALU.mult), reads=[sm.k()], writes=[sm.k()])
        P.op("act", lambda e, i=i, lb=lb: e.activation(aff.ap[:, i, :], ps[lb][:, 0:NE], AF.Exp, bias=sm.ap[:, i, 0:1], accum_out=sm.ap[:, i, 1:2]),
             reads=[f"ps{lb}", sm.k()], writes=[aff.k(), sm.k()])
        P.op("dve", lambda e, i=i: e.reciprocal(sm.ap[:, i, 2:3], sm.ap[:, i, 1:2]), reads=[sm.k()], writes=[sm.k()])
        P.op("dve", lambda e, i=i: e.tensor_scalar(aff.ap[:, i, :], aff.ap[:, i, :], sm.ap[:, i, 2:3], None, op0=ALU.mult), reads=[aff.k(), sm.k()], writes=[aff.k()])
        tb = 6 + (i // 4) % 2
        P.op("pe", lambda e, i=i, tb=tb: e.transpose(ps[tb][0:NE, (i % 4) * 128:(i % 4 + 1) * 128], aff.ap[:, i, :], k.ident.ap), reads=[aff.k(), k.ident.k()], writes=[f"ps{tb}"])
        if i % 4 == 3:
            P.op("act", lambda e, i=i, tb=tb: e.copy(affT.ap[0:NE, (i - 3) * 128:(i + 1) * 128], ps[tb][0:NE, :]), reads=[f"ps{tb}"], writes=[affT.k()])
    bs = A.alloc("bs", (8,)); bjunk = A.alloc("bjunk", (S,))
    LO, HI, MID, CNT, GE, D1 = (bs.ap[0:NE, j:j + 1] for j in range(6))
    P.op("pool", lambda e: e.memset(bs.ap, 0.0), writes=[bs.k()])
    P.op("pool", lambda e: e.memset(bs.ap[:, 1:2], 1.0), writes=[bs.k()])
    for it in range(30):
        P.op("dve", lambda e: e.tensor_tensor(MID, LO, HI, op=ALU.add), reads=[bs.k()], writes=[bs.k()])
        P.op("dve", lambda e: e.tensor_scalar(MID, MID, 0.5, None, op0=ALU.mult), reads=[bs.k()], writes=[bs.k()])
        P.op("dve", lambda e: e.tensor_scalar(bjunk.ap[0:NE, :], affT.ap[0:NE, :], MID, None, op0=ALU.is_gt, op1=ALU.add, accum_out=CNT),
             reads=[bs.k(), affT.k()], writes=[bs.k(), bjunk.k()])
        P.op("dve", lambda e: e.tensor_scalar(GE, CNT, float(CAP) - 0.5, None, op0=ALU.is_gt), reads=[bs.k()], writes=[bs.k()])
        P.op("dve", lambda e: e.tensor_tensor(D1, MID, LO, op=ALU.subtract), reads=[bs.k()], writes=[bs.k()])
        P.op("dve", lambda e: e.scalar_tensor_tensor(LO, D1, GE, LO, op0=ALU.mult, op1=ALU.add), reads=[bs.k()], writes=[bs.k()])
        P.op("dve", lambda e: e.tensor_tensor(D1, HI, MID, op=ALU.subtract), reads=[bs.k()], writes=[bs.k()])
        P.op("dve", lambda e: e.scalar_tensor_tensor(HI, D1, GE, MID, op0=ALU.mult, op1=ALU.add), reads=[bs.k()], writes=[bs.k()])
    dgt = A.alloc("dgt", (NE,)); thrb = A.alloc("thrb", (NE,))
    P.op("dve", lambda e: e.tensor_scalar(dgt.ap[0:NE, :], k.ident.ap[0:NE, 0:NE], LO, None, op0=ALU.mult), reads=[bs.k(), k.ident.k()], writes=[dgt.k()])
    P.op("pe", lambda e: e.matmul(ps[0][:, 0:NE], k.ones32.ap[0:NE, :], dgt.ap[0:NE, :], start=True, stop=True), reads=[k.ones32.k(), dgt.k()], writes=["ps0"])
    P.op("act", lambda e: e.copy(thrb.ap, ps[0][:, 0:NE]), reads=["ps0"], writes=[thrb.k()])
    mk16 = A.alloc("mk16", (NT, NE), BF16); mk32 = A.alloc("mk32", (NT, NE)); base = A.alloc("basec", (NE,))
    P.op("pool", lambda e: e.memset(base.ap, 0.0), writes=[base.k()])
    for i in range(NT):
        P.op("dve", lambda e, i=i: e.tensor_tensor(mk32.ap[:, i, :], aff.ap[:, i, :], thrb.ap, op=ALU.is_gt), reads=[aff.k(), thrb.k()], writes=[mk32.k(i)])
        P.op("act", lambda e, i=i: e.copy(mk16.ap[:, i, :], mk32.ap[:, i, :]), reads=[mk32.k(i)], writes=[mk16.k(i)])
        b = i % 2
        P.op("pe", lambda e, i=i, b=b: e.matmul(ps[b][:, 0:NE], k.trius16.ap, mk16.ap[:, i, :], start=True, stop=True), reads=[k.trius16.k(), mk16.k(i)], writes=[f"ps{b}"])
        P.op("pe", lambda e, i=i, b=b: e.matmul(ps[b][:, 128:128 + NE], k.ones16.ap, mk16.ap[:, i, :], start=True, stop=True), reads=[k.ones16.k(), mk16.k(i)], writes=[f"ps{b}"])
        P.op("dve", lambda e, i=i, b=b: e.scalar_tensor_tensor(pm.ap[:, i, :], ps[b][:, 0:NE], 1.0, base.ap, op0=ALU.add, op1=ALU.add), reads=[f"ps{b}", base.k()], writes=[pm.k(i)])
        P.op("dve", lambda e, i=i: e.tensor_tensor(pm.ap[:, i, :], pm.ap[:, i, :], mk32.ap[:, i, :], op=ALU.mult), reads=[pm.k(i), mk32.k(i)], writes=[pm.k(i)])
        P.op("dve", lambda e, i=i: e.tensor_scalar(pm.ap[:, i, :], pm.ap[:, i, :], -1.0, None, op0=ALU.add), reads=[pm.k(i)], writes=[pm.k(i)])
        P.op("dve", lambda e, b=b: e.tensor_tensor(base.ap, base.ap, ps[b][:, 128:128 + NE], op=ALU.add), reads=[f"ps{b}", base.k()], writes=[base.k()])
        tb = 6 + (i // 4) % 2
        P.op("pe", lambda e, i=i, tb=tb: e.transpose(ps[tb][0:NE, (i % 4) * 128:(i % 4 + 1) * 128], pm.ap[:, i, :], k.ident.ap), reads=[pm.k(i), k.ident.k()], writes=[f"ps{tb}"])
        if i % 4 == 3:
            P.op("act", lambda e, i=i, tb=tb: e.copy(pmT.ap[0:NE, (i - 3) * 128:(i + 1) * 128], ps[tb][0:NE, :]), reads=[f"ps{tb}"], writes=[pmT.k()])
    A.release(m1)
    SelE = A.alloc("SelE", (NT, CAP), BF16)
    SelT = [A.alloc(f"SelT{i}", (2, S), BF16) for i in range(2)]
    xgT = A.alloc("xgT", (8, CAP), BF16); actT = A.alloc("actT", (NFC, CAP), BF16)
    s1 = [A.alloc(f"s1_{i}", (CAP,)) for i in range(2)]
    ysb = A.alloc("ysb", (2, D), BF16)
    w1b = [A.alloc(f"w1b{i}", (8, FB), BF16) for i in range(2)]
    w3b = [A.alloc(f"w3b{i}", (8, FB), BF16) for i in range(2)]
    w2b = [A.alloc(f"w2b{i}", (W2G, D), BF16) for i in range(2)]
    pmT16 = A.alloc("pmT16", (S,), BF16); ohb16 = A.alloc("ohb16", (2048,), BF16)
    P.op("act", lambda e: e.copy(pmT16.ap[0:NE, :], pmT.ap[0:NE, :]), reads=[pmT.k()], writes=[pmT16.k()])
    P.op("act", lambda e: e.copy(ohb16.ap[0:NE, :], k.ohb.ap[0:NE, :]), reads=[k.ohb.k()], writes=[ohb16.k()])
    state = {"wn": 0}

    def sel_and_gather(ex):
        st_ = SelT[ex % 2]
        for i in range(NT):
            P.op("dve", lambda e, i=i, ex=ex: e.tensor_scalar(SelE.ap[:, i, :], k.iota256.ap, pm.ap[:, i, ex:ex + 1], None, op0=ALU.is_equal),
                 reads=[k.iota256.k(), pm.k(i)], writes=[SelE.k(i)])
        for st in range(2):
            for tg in range(4):
                P.op("pe", lambda e, ex=ex, tg=tg: e.matmul(ps[6][:, :], ohb16.ap[0:NE, ex * 128:(ex + 1) * 128], pmT16.ap[0:NE, tg * 512:(tg + 1) * 512], start=True, stop=True),
                     reads=[ohb16.k(), pmT16.k()], writes=["ps6"])
                P.op("dve", lambda e, st=st, tg=tg, st_=st_: e.tensor_scalar(st_.ap[:, st, tg * 512:(tg + 1) * 512], ps[6][:, :], k.slotidx.ap[:, st:st + 1], None, op0=ALU.is_equal),
                     reads=["ps6", k.slotidx.k()], writes=[st_.k(st, tg)])
        for c in range(8):
            b = 6 + c % 2
            for i in range(NT):
                P.op("pe", lambda e, c=c, i=i, b=b: e.matmul(ps[b][:, 0:CAP], x2b.ap[:, i, c * 128:(c + 1) * 128], SelE.ap[:, i, :], start=(i == 0), stop=(i == NT - 1)),
                     reads=[x2b.k(i), SelE.k(i)], writes=[f"ps{b}"])
            P.op("act", lambda e, c=c, b=b: e.copy(xgT.ap[:, c, :], ps[b][:, 0:CAP]), reads=[f"ps{b}"], writes=[xgT.k(c)])

    def ffn1(ex, fb):
        r = state["wn"] % 2; state["wn"] += 1
        w1v = IN["e_w1"].ap()[l][ex].rearrange("(c p) f -> p c f", p=128)
        w3v = IN["e_w3"].ap()[l][ex].rearrange("(c p) f -> p c f", p=128)
        w2v = IN["e_w2"].ap()[l][ex].rearrange("(g p) d -> p g d", p=128)
        P.dma("pool", w1b[r].ap, w1v[:, :, fb * FB:(fb + 1) * FB], writes=[w1b[r].k()], semkey=("w1", r))
        P.dma("pool", w3b[r].ap, w3v[:, :, fb * FB:(fb + 1) * FB], writes=[w3b[r].k()], semkey=("w3", r))
        P.dma("pool", w2b[r].ap, w2v[:, fb * W2G:(fb + 1) * W2G, :], writes=[w2b[r].k()], semkey=("w2", r))
        for q in range(FB // 128):
            fc = fb * (FB // 128) + q
            b = fc % 2
            for c in range(8):
                P.op("pe", lambda e, c=c, q=q, b=b, r=r: e.matmul(ps[b][:, 0:CAP], w1b[r].ap[:, c, q * 128:(q + 1) * 128], xgT.ap[:, c, :], start=(c == 0), stop=(c == 7)),
                     reads=[w1b[r].k(), xgT.k(c)], writes=[f"ps{b}"])
            for c in range(8):
                P.op("pe", lambda e, c=c, q=q, b=b, r=r: e.matmul(ps[b][:, CAP:2 * CAP], w3b[r].ap[:, c, q * 128:(q + 1) * 128], xgT.ap[:, c, :], start=(c == 0), stop=(c == 7)),
                     reads=[w3b[r].k(), xgT.k(c)], writes=[f"ps{b}"])
            P.op("act", lambda e, b=b: e.activation(s1[b].ap, ps[b][:, 0:CAP], AF.Silu), reads=[f"ps{b}"], writes=[s1[b].k()])
            P.op("dve", lambda e, b=b, fc=fc: e.tensor_tensor(actT.ap[:, fc, :], s1[b].ap, ps[b][:, CAP:2 * CAP], op=ALU.mult), reads=[f"ps{b}", s1[b].k()], writes=[actT.k(fc)])
        return r

    def ffn2(fb, r):
        for q in range(W2G):
            fc = fb * W2G + q
            for st in range(2):
                for half in range(2):
                    yb_ = 2 + st * 2 + half
                    P.op("pe", lambda e, fc=fc, q=q, st=st, half=half, yb_=yb_, r=r: e.matmul(ps[yb_][:, :], actT.ap[:, fc, st * 128:(st + 1) * 128], w2b[r].ap[:, q, half * 512:(half + 1) * 512],
                                                                                       start=(fc == 0), stop=(fc == NFC - 1)),
                         reads=[actT.k(fc), w2b[r].k()], writes=[f"ps{yb_}"])

    def scatter(ex):
        st_ = SelT[ex % 2]
        for i in range(NT):
            for half in range(2):
                b = 6 + (2 * i + half) % 2
                for st in range(2):
                    P.op("pe", lambda e, i=i, half=half, st=st, b=b, st_=st_: e.matmul(ps[b][:, :], st_.ap[:, st, i * 128:(i + 1) * 128], ysb.ap[:, st, half * 512:(half + 1) * 512], start=(st == 0), stop=(st == 1)),
                         reads=[st_.k(st, i // 4), ysb.k(st, half)], writes=[f"ps{b}"])
                P.op("dve", lambda e, i=i, half=half, b=b, ex=ex: e.scalar_tensor_tensor(k.xres.ap[:, i, half * 512:(half + 1) * 512], ps[b][:, :], aff.ap[:, i, ex:ex + 1],
                                                                                     k.xres.ap[:, i, half * 512:(half + 1) * 512], op0=ALU.mult, op1=ALU.add),
                     reads=[f"ps{b}", aff.k(), k.xres.k(i)], writes=[k.xres.k(i)])

    sel_and_gather(0)
    for ex in range(NE):
        rprev = ffn1(ex, 0)
        for fb in range(1, NFB):
            rcur = ffn1(ex, fb)
            ffn2(fb - 1, rprev)
            rprev = rcur
        ffn2(NFB - 1, rprev)
        for st in range(2):
            for half in range(2):
                yb_ = 2 + st * 2 + half
                P.op("act", lambda e, st=st, half=half, yb_=yb_: e.copy(ysb.ap[:, st, half * 512:(half + 1) * 512], ps[yb_][:, :]), reads=[f"ps{yb_}"], writes=[ysb.k(st, half)])
        if ex + 1 < NE:
            sel_and_gather(ex + 1)
        scatter(ex)
    A.release(m0)


_INPUT_NAMES = ["rel_bias", "ln1_g", "w_in", "w_out", "rk_mu", "rk_w0", "rk_w2", "rk_a0", "rk_a2", "rk_kk", "rk_ka",
                "rk_rk", "rk_g2", "rk_ln_g", "rk_ln_b", "ml_conv_w", "ml_conv_b", "ml_ib", "ml_fb", "ml_ln_g",
                "ln2_g", "router", "e_w1", "e_w3", "e_w2", "final_g"]


def make_in_maps(inputs, cores, names=None):
    consts = host_consts()
    shared = {n: np.ascontiguousarray(np.asarray(inputs[n], dtype=np.float32)) for n in _INPUT_NAMES if names is None or n in names}
    shared.update({n: v for n, v in consts.items() if names is None or n in names})
    if names is None or "c_pc" in names or "c_wx" in names:
        shared.update(host_rwkv_layout(inputs))
    x = np.asarray(inputs["x"], dtype=np.float32)
    maps = []
    for c in cores:
        m = dict(shared)
        m["x"] = np.ascontiguousarray(x[c])
        maps.append(m)
    return maps


def kernel(**inputs):
    nc, k = build()
    in_maps = make_in_maps(inputs, list(range(8)), set(k.IN.keys()))
    res = run_bass_kernel_spmd(nc, in_maps, core_ids=list(range(8)))
    return np.stack([np.asarray(r["out"], dtype=np.float32) for r in res.results], axis=0)
```
